# Optimizing a Trainium2 kernel written in Bass

```python
import jax, jax.numpy as jnp
from jax import lax
import numpy as np

D_MODEL = 1024
BATCH = 4
SEQ = 8192
DEPTH = 2

CTX_LEN = 256
GRID_W = 64
D_MIX = D_MODEL
DN_HEADS = 4
DN_HEAD_DIM = 128
DN_WIDTH = DN_HEADS * DN_HEAD_DIM
DN_CHUNK = 64
CONV_W = 5
GLA_HEADS = 4
GLA_DK = 64
GLA_DV = 128
GLA_K_WIDTH = GLA_HEADS * GLA_DK
GLA_V_WIDTH = GLA_HEADS * GLA_DV
GLA_GATE_RANK = 16
GLA_GATE_NORMALIZER = 16.0
GLA_CHUNK = 16
D_FF = 2816
N_EXPERTS = 8
TOP_K = 2
D_EXPERT = 3584
N_MOD = 6
NORM_EPS = 1e-6
IN_SIZES = (3 * DN_WIDTH, DN_WIDTH, 2 * DN_HEADS, 2 * DN_HEADS, GLA_K_WIDTH, GLA_K_WIDTH, GLA_V_WIDTH, GLA_V_WIDTH, 2 * GLA_GATE_RANK)
IN_COLS = 4 * DN_WIDTH + 4 * DN_HEADS + 2 * GLA_K_WIDTH + 2 * GLA_V_WIDTH + 2 * GLA_GATE_RANK

kernel_name = 'hybrid_deltanet_gla_prefix_moe_dit'


def rms_norm(x, gain):
    xf = x.astype(jnp.float32)
    y = xf * lax.rsqrt(jnp.mean(xf * xf, axis=-1, keepdims=True) + NORM_EPS)
    return (y * gain.astype(jnp.float32)).astype(x.dtype)


def l2_norm(t):
    return t * lax.rsqrt(jnp.sum(t * t, axis=-1, keepdims=True) + NORM_EPS)


def modulate(h, shift, scale):
    return h * (1 + scale) + shift


def centred_depthwise_conv(t, w):
    ch = t.shape[-1]
    return lax.conv_general_dilated(t, w[:, None, :].astype(t.dtype), window_strides=(1,),
                                    padding=[(CONV_W // 2, CONV_W // 2)],
                                    dimension_numbers=('NWC', 'WIO', 'NWC'), feature_group_count=ch)


def raster_to_columns(t, axis, rows):
    shp = t.shape
    t = t.reshape(shp[:axis] + (rows, GRID_W) + shp[axis + 1:])
    return jnp.swapaxes(t, axis, axis + 1).reshape(shp)


def columns_to_raster(t, axis, rows):
    shp = t.shape
    t = t.reshape(shp[:axis] + (GRID_W, rows) + shp[axis + 1:])
    return jnp.swapaxes(t, axis, axis + 1).reshape(shp)


def gated_delta_chunked(q, k, v, g, beta, s0):
    b_, h_, L, _ = q.shape
    dv = v.shape[-1]
    C = DN_CHUNK
    n = L // C
    q, k, v = (t.reshape(b_, h_, n, C, t.shape[-1]) for t in (q, k, v))
    g = jnp.cumsum(g.reshape(b_, h_, n, C), axis=-1)
    beta = beta.reshape(b_, h_, n, C, 1)
    causal = jnp.tril(jnp.ones((C, C), bool))
    strict = jnp.tril(jnp.ones((C, C), bool), -1)
    decay = jnp.exp(jnp.where(causal, g[..., :, None] - g[..., None, :], -jnp.inf))
    k_beta = k * beta
    lower = jnp.where(strict, jnp.einsum('bhncd,bhnmd->bhncm', k_beta, k) * decay, 0.0)
    rhs = jnp.concatenate([v * beta, k_beta * jnp.exp(g)[..., None]], axis=-1)
    uw = lax.linalg.triangular_solve(jnp.eye(C, dtype=q.dtype) + lower, rhs, left_side=True, lower=True)
    u, w = uw[..., :dv], uw[..., dv:]
    attn = jnp.einsum('bhncd,bhnmd->bhncm', q, k) * decay
    q_dec = q * jnp.exp(g)[..., None]
    k_dec = k * jnp.exp(g[..., -1:] - g)[..., None]
    g_last = jnp.exp(g[..., -1])

    def step(s, xs):
        u_c, w_c, a_c, q_c, k_c, gl_c = xs
        v_new = u_c - jnp.einsum('bhcd,bhde->bhce', w_c, s)
        o = jnp.einsum('bhcd,bhde->bhce', q_c, s) + jnp.einsum('bhcm,bhme->bhce', a_c, v_new)
        s = s * gl_c[..., None, None] + jnp.einsum('bhcd,bhce->bhde', k_c, v_new)
        return s, o

    xs = tuple(jnp.moveaxis(t, 2, 0) for t in (u, w, attn, q_dec, k_dec, g_last))
    s_fin, o = lax.scan(step, s0, xs)
    return jnp.moveaxis(o, 0, 2).reshape(b_, h_, L, dv), s_fin


def gla_chunked(q, k, v, log_a, s0):
    b_, h_, L, _ = q.shape
    dv = v.shape[-1]
    C = GLA_CHUNK
    n = L // C
    chunks = lambda t: jnp.moveaxis(t.reshape(b_, h_, n, C, t.shape[-1]), 2, 0)
    b_cum = jnp.cumsum(chunks(log_a), axis=3)
    causal = jnp.tril(jnp.ones((C, C), bool))[:, :, None]

    def step(s, xs):
        q_c, k_c, v_c, bc = xs
        rel = jnp.exp(jnp.where(causal, bc[:, :, :, None, :] - bc[:, :, None, :, :], -jnp.inf))
        attn = jnp.einsum('bhcd,bhmd,bhcmd->bhcm', q_c, k_c, rel)
        b_last = bc[:, :, -1:, :]
        o = jnp.einsum('bhcd,bhde->bhce', q_c * jnp.exp(bc), s) + jnp.einsum('bhcm,bhme->bhce', attn, v_c)
        s = s * jnp.exp(b_last[:, :, 0, :, None]) + jnp.einsum('bhcd,bhce->bhde', k_c * jnp.exp(b_last - bc), v_c)
        return s, o

    s_fin, o = lax.scan(step, s0, (chunks(q), chunks(k), chunks(v), b_cum))
    return jnp.moveaxis(o, 0, 2).reshape(b_, h_, L, dv), s_fin


def bidirectional_prefix(chunk_fn, ctx_shared, ctx_dir, lat_shared, lat_dir, s0):
    o_ctx, o_lat = None, None
    for d in range(2):
        flip = (lambda t: jnp.flip(t, axis=2)) if d == 1 else (lambda t: t)
        c_args = [flip(t) for t in ctx_shared + tuple(p[d] for p in ctx_dir)]
        l_args = [flip(t) for t in lat_shared + tuple(p[d] for p in lat_dir)]
        oc, s_ctx = chunk_fn(*c_args, s0)
        ol, _ = chunk_fn(*l_args, s_ctx)
        oc, ol = flip(oc), flip(ol)
        o_ctx = oc if o_ctx is None else o_ctx + oc
        o_lat = ol if o_lat is None else o_lat + ol
    return o_ctx, o_lat


def project_tokens(h, w_in, conv_qkv, dn_a_log, dn_dt_bias, gla_w_gate2, gla_b_gate):
    b_, L, _ = h.shape
    f32 = jnp.float32
    dn_qkv, dn_z, dn_beta, dn_a, gq, gk, gv, gz, g_lr = jnp.split(h @ w_in, np.cumsum(IN_SIZES)[:-1].tolist(), axis=-1)
    dn_qkv = jax.nn.silu(centred_depthwise_conv(dn_qkv, conv_qkv))
    dq, dk, dv = jnp.split(dn_qkv, 3, axis=-1)
    heads = lambda t, nh: t.reshape(b_, L, nh, -1).transpose(0, 2, 1, 3).astype(f32)
    dq = l2_norm(heads(dq, DN_HEADS)) * DN_HEAD_DIM ** -0.5
    dk = l2_norm(heads(dk, DN_HEADS))
    dv = heads(dv, DN_HEADS)
    beta = jax.nn.sigmoid(dn_beta.astype(f32)).reshape(b_, L, 2, DN_HEADS).transpose(2, 0, 3, 1)
    a = dn_a.astype(f32).reshape(b_, L, 2, DN_HEADS).transpose(2, 0, 3, 1)
    g = -jnp.exp(dn_a_log.astype(f32))[:, None, :, None] * jax.nn.softplus(a + dn_dt_bias.astype(f32)[:, None, :, None])
    gq = heads(gq, GLA_HEADS) * GLA_DK ** -0.5
    gk = heads(gk, GLA_HEADS)
    gv = heads(gv, GLA_HEADS)
    g_lr = g_lr.reshape(b_, L, 2, GLA_GATE_RANK)
    gate = jnp.einsum('blnr,nrk->nblk', g_lr, gla_w_gate2) + gla_b_gate[:, None, None, :]
    log_a = jax.nn.log_sigmoid(gate.astype(f32)) / GLA_GATE_NORMALIZER
    log_a = log_a.reshape(2, b_, L, GLA_HEADS, GLA_DK).transpose(0, 1, 3, 2, 4)
    return (dq, dk, dv), (g, beta), dn_z, (gq, gk, gv), (log_a,), gz


def gated_head_norm(o, z, gain):
    b_, nh, L, dv = o.shape
    o = rms_norm(o.transpose(0, 2, 1, 3), gain)
    return (o * jax.nn.silu(z.astype(jnp.float32)).reshape(b_, L, nh, dv)).reshape(b_, L, nh * dv).astype(z.dtype)


def swiglu(t, w_gu, w_down):
    gate, up = jnp.split(t @ w_gu, 2, axis=-1)
    return (jax.nn.silu(gate) * up) @ w_down


def moe_swiglu(h, router, w_gu, w_down):
    shp = h.shape
    t = h.reshape(-1, shp[-1])
    probs = jax.nn.softmax((t @ router).astype(jnp.float32), axis=-1)
    top_p, top_i = lax.top_k(probs, TOP_K)
    top_p = top_p / jnp.sum(top_p, axis=-1, keepdims=True)
    combine = jnp.einsum('tk,tke->te', top_p, jax.nn.one_hot(top_i, N_EXPERTS, dtype=jnp.float32)).astype(t.dtype)
    out = jnp.zeros_like(t)
    for e in range(N_EXPERTS):
        out = out + combine[:, e:e + 1] * swiglu(t, w_gu[e], w_down[e])
    return out.reshape(shp)


def channel_mixer(h, i, ffn_w_gu, ffn_w_down, moe_router, moe_w_gu, moe_w_down):
    if i % 2 == 0:
        return swiglu(h, ffn_w_gu[i // 2], ffn_w_down[i // 2])
    return moe_swiglu(h, moe_router[i // 2], moe_w_gu[i // 2], moe_w_down[i // 2])


def setup_inputs(seed: int = 0) -> dict:
    key = jax.random.key(seed)
    ks = jax.random.split(key, 24)
    f32 = jnp.float32
    n_dense = (DEPTH + 1) // 2
    n_moe = DEPTH // 2
    nrm = lambda k, shape, fan_in: jax.random.normal(k, shape, f32) * fan_in ** -0.5
    gain = lambda k, shape: 1.0 + 0.02 * jax.random.normal(k, shape, f32)
    dt = jnp.exp(jax.random.uniform(ks[9], (DEPTH, 2, DN_HEADS), f32, np.log(1e-3), np.log(1e-1)))
    return {
        'x': jax.random.normal(ks[0], (BATCH, SEQ, D_MODEL), f32),
        'c': jax.random.normal(ks[1], (BATCH, D_MODEL), f32),
        'ctx': jax.random.normal(ks[2], (BATCH, CTX_LEN, D_MODEL), f32),
        'c_ctx': jax.random.normal(ks[3], (D_MODEL,), f32),
        'w_ada': 0.5 * nrm(ks[4], (DEPTH, D_MODEL, N_MOD * D_MODEL), D_MODEL),
        'b_ada': 0.02 * jax.random.normal(ks[5], (DEPTH, N_MOD * D_MODEL), f32),
        'norm_mix': gain(ks[6], (DEPTH, D_MODEL)),
        'norm_ffn': gain(ks[7], (DEPTH, D_MODEL)),
        'w_in': nrm(ks[8], (DEPTH, D_MODEL, IN_COLS), D_MODEL),
        'conv_qkv': nrm(ks[10], (DEPTH, CONV_W, 3 * DN_WIDTH), CONV_W),
        'dn_a_log': jnp.log(jax.random.uniform(ks[11], (DEPTH, 2, DN_HEADS), f32, 1.0, 16.0)),
        'dn_dt_bias': dt + jnp.log(-jnp.expm1(-dt)),
        'dn_norm': gain(ks[12], (DEPTH, DN_HEAD_DIM)),
        'gla_w_gate2': nrm(ks[13], (DEPTH, 2, GLA_GATE_RANK, GLA_K_WIDTH), GLA_GATE_RANK),
        'gla_b_gate': 0.1 * jax.random.normal(ks[14], (DEPTH, 2, GLA_K_WIDTH), f32),
        'gla_norm': gain(ks[15], (DEPTH, GLA_DV)),
        'w_out': nrm(ks[16], (DEPTH, D_MIX, D_MODEL), D_MIX),
        'ffn_w_gu': nrm(ks[17], (n_dense, D_MODEL, 2 * D_FF), D_MODEL),
        'ffn_w_down': nrm(ks[18], (n_dense, D_FF, D_MODEL), D_FF),
        'moe_router': nrm(ks[19], (n_moe, D_MODEL, N_EXPERTS), D_MODEL),
        'moe_w_gu': nrm(ks[20], (n_moe, N_EXPERTS, D_MODEL, 2 * D_EXPERT), D_MODEL),
        'moe_w_down': nrm(ks[21], (n_moe, N_EXPERTS, D_EXPERT, D_MODEL), D_EXPERT),
        'final_norm': gain(ks[22], (D_MODEL,)),
    }


def reference(x, c, ctx, c_ctx, w_ada, b_ada, norm_mix, norm_ffn, w_in, conv_qkv, dn_a_log, dn_dt_bias, dn_norm,
              gla_w_gate2, gla_b_gate, gla_norm, w_out, ffn_w_gu, ffn_w_down, moe_router, moe_w_gu, moe_w_down,
              final_norm):
    b_ = x.shape[0]
    rows = x.shape[1] // GRID_W
    s0_dn = jnp.zeros((b_, DN_HEADS, DN_HEAD_DIM, DN_HEAD_DIM), jnp.float32)
    s0_gla = jnp.zeros((b_, GLA_HEADS, GLA_DK, GLA_DV), jnp.float32)
    for i in range(DEPTH):
        last = i == DEPTH - 1
        mod_l = jnp.split((jax.nn.silu(c) @ w_ada[i] + b_ada[i])[:, None, :], N_MOD, axis=-1)
        mod_c = jnp.split((jax.nn.silu(c_ctx) @ w_ada[i] + b_ada[i])[None, None, :], N_MOD, axis=-1)
        h_l = modulate(rms_norm(x, norm_mix[i]), mod_l[0], mod_l[1])
        h_c = modulate(rms_norm(ctx, norm_mix[i]), mod_c[0], mod_c[1])
        params = (w_in[i], conv_qkv[i], dn_a_log[i], dn_dt_bias[i], gla_w_gate2[i], gla_b_gate[i])
        dn_sh_c, dn_dir_c, dn_z_c, gla_sh_c, gla_dir_c, gla_z_c = project_tokens(h_c, *params)
        dn_sh_l, dn_dir_l, dn_z_l, gla_sh_l, gla_dir_l, gla_z_l = project_tokens(h_l, *params)
        o_dn_c, o_dn_l = bidirectional_prefix(gated_delta_chunked, dn_sh_c, dn_dir_c, dn_sh_l, dn_dir_l, s0_dn)
        gla_sh_l = tuple(raster_to_columns(t, 2, rows) for t in gla_sh_l)
        gla_dir_l = tuple(raster_to_columns(t, 3, rows) for t in gla_dir_l)
        o_gla_c, o_gla_l = bidirectional_prefix(gla_chunked, gla_sh_c, gla_dir_c, gla_sh_l, gla_dir_l, s0_gla)
        o_gla_l = columns_to_raster(o_gla_l, 2, rows)
        m_l = jnp.concatenate([gated_head_norm(o_dn_l, dn_z_l, dn_norm[i]),
                               gated_head_norm(o_gla_l, gla_z_l, gla_norm[i])], axis=-1)
        x = x + mod_l[2] * (m_l @ w_out[i])
        h_l = modulate(rms_norm(x, norm_ffn[i]), mod_l[3], mod_l[4])
        x = x + mod_l[5] * channel_mixer(h_l, i, ffn_w_gu, ffn_w_down, moe_router, moe_w_gu, moe_w_down)
        if not last:
            m_c = jnp.concatenate([gated_head_norm(o_dn_c, dn_z_c, dn_norm[i]),
                                   gated_head_norm(o_gla_c, gla_z_c, gla_norm[i])], axis=-1)
            ctx = ctx + mod_c[2] * (m_c @ w_out[i])
            h_c = modulate(rms_norm(ctx, norm_ffn[i]), mod_c[3], mod_c[4])
            ctx = ctx + mod_c[5] * channel_mixer(h_c, i, ffn_w_gu, ffn_w_down, moe_router, moe_w_gu, moe_w_down)
    return rms_norm(x, final_norm)
```

```python
import numpy as np
from contextlib import ExitStack
import concourse.bass as bass
import concourse.mybir as mybir
from concourse.bass_utils import run_bass_kernel_spmd

F32 = mybir.dt.float32
BF16 = mybir.dt.bfloat16
AF = mybir.ActivationFunctionType
ALU = mybir.AluOpType
AX = mybir.AxisListType

D = 1024
LC = 256
LL = 8192
LS = LC + LL
NT = LS // 128
IN_COLS = 3632
NTM = IN_COLS - 1536
EPS = 1e-6
D_FF = 2816
D_EXP = 3584
NE = 8
NOWN = 4096


class Dep:
    __slots__ = ("w", "r")

    def __init__(self):
        self.w = None
        self.r = {}


class Tile:
    excl = False

    def __init__(self, t):
        self.t = t
        self.dep = Dep()

    def __getitem__(self, idx):
        return self.t[idx]


class K:
    RING = 64

    def __init__(self, nc, es):
        self.nc = nc
        self.es = es
        self.E = {"pe": nc.tensor, "dve": nc.vector, "act": nc.scalar, "pool": nc.gpsimd, "sp": nc.sync}
        self.sems = {}
        for e in self.E:
            self.sems[e] = es.enter_context(nc.semaphore("sem_" + e))
        for i in range(self.RING):
            self.sems[("d", i)] = es.enter_context(nc.semaphore("dsem%d" % i))
        self.cnt = {e: 0 for e in self.E}
        self.seen = {e: {} for e in self.E}
        self.dn = 0
        self.dtar = [0] * self.RING
        self.nt = 0

    def sb(self, shape, dt, name=None):
        self.nt += 1
        return Tile(self.es.enter_context(self.nc.sbuf_tensor("%s_%d" % (name or "t", self.nt), list(shape), dt)))

    def psum(self, shape, dt, name=None):
        self.nt += 1
        t = Tile(self.es.enter_context(self.nc.psum_tensor(name or ("p%d" % self.nt), list(shape), dt)))
        t.excl = True
        return t

    def _wait(self, eng, tok):
        if tok is None:
            return
        key, val = tok
        if key == eng and eng == "pe":
            return
        if self.seen[eng].get(key, 0) >= val:
            return
        self.E[eng].wait_ge(self.sems[key], val)
        self.seen[eng][key] = val

    def _pre(self, eng, reads, writes):
        for d in reads:
            self._wait(eng, d.dep.w)
        for d in writes:
            self._wait(eng, d.dep.w)
            for tok in d.dep.r.values():
                self._wait(eng, tok)

    def _post(self, tok, reads, writes):
        for d in reads:
            d.dep.r[tok[0]] = tok
        for d in writes:
            d.dep.w = tok
            d.dep.r = {}

    def op(self, eng, fn, reads=(), writes=()):
        ex = [d for d in reads if getattr(d, "excl", False)]
        if ex:
            writes = list(writes) + ex
            reads = [d for d in reads if not getattr(d, "excl", False)]
        self._pre(eng, reads, writes)
        ins = fn(self.E[eng])
        ins.then_inc(self.sems[eng], 1)
        self.cnt[eng] += 1
        self._post((eng, self.cnt[eng]), reads, writes)

    def dma(self, q, out, in_, reads=(), writes=(), **kw):
        self._pre(q, reads, writes)
        slot = self.dn % self.RING
        self.dn += 1
        key = ("d", slot)
        if self.dtar[slot] > 0:
            self._wait(q, (key, self.dtar[slot]))
        self.dtar[slot] += 16
        self.E[q].dma_start(out=out, in_=in_, **kw).then_inc(self.sems[key], 16)
        self._post((key, self.dtar[slot]), reads, writes)

    def barrier(self):
        for e in self.E:
            for e2 in self.E:
                if e2 != e and self.cnt[e2] > 0:
                    self._wait(e, (e2, self.cnt[e2]))
            for s in range(self.RING):
                if self.dtar[s] > 0:
                    self._wait(e, (("d", s), self.dtar[s]))

    def scope(self):
        outer = self.es
        k = self

        class _S:
            def __enter__(s):
                k.es = ExitStack()
                return s

            def __exit__(s, *a):
                k.barrier()
                k.es.close()
                k.es = outer
                return False
        return _S()

    def finish(self, deps):
        for d in deps:
            self._wait("sp", d.dep.w)


class DR:
    def __init__(self):
        self.dep = Dep()


class Prog:
    def __init__(self, debug=(), nlayers=2, phases=("p0", "p1", "p2", "p3", "p4"), ntiles=NT):
        self.debug = tuple(debug)
        self.nlayers = nlayers
        self.phases = phases
        self.ntiles = ntiles
        self.p2_stop = 9
        nc = self.nc = bass.Bass("TRN2", target_bir_lowering=False)
        self.I = {}
        self.S = {}
        self.es = ExitStack()
        self.k = K(nc, self.es)

    def inp(self, name, shape):
        self.I[name] = self.nc.dram_tensor(name, list(shape), F32, kind="ExternalInput").ap()
        return self.I[name]

    def scr(self, name, shape, dt=F32):
        kind = "ExternalOutput" if name in self.debug else "Internal"
        self.S[name] = self.nc.dram_tensor(name, list(shape), dt, kind=kind).ap()
        return self.S[name]

    def declare(self):
        inp = self.inp
        inp("xs", [LS, D]); inp("cvec", [2, D]); inp("w_ada", [2, D, 6 * D]); inp("b_ada", [2, 6 * D])
        inp("norm_mix", [2, D]); inp("norm_ffn", [2, D]); inp("w_in", [2, D, IN_COLS]); inp("conv", [2, 5, 1536])
        inp("dn_a_log", [2, 8]); inp("dn_dt_bias", [2, 8]); inp("dn_norm", [2, 128])
        inp("gla_w_gate2", [2, 2, 16, 256]); inp("gla_b_gate", [2, 2, 256]); inp("gla_norm", [2, 128])
        if "p4" in self.phases:
            inp("w_out", [2, D, D]); inp("ffn_w_gu", [D, 2 * D_FF]); inp("ffn_w_down", [D_FF, D])
            if self.nlayers > 1:
                inp("moe_router", [D, NE]); inp("moe_w_gu", [NE, D, 2 * D_EXP]); inp("moe_w_down", [NE, D_EXP, D])
        inp("final_norm", [D])
        self.out = self.nc.dram_tensor("out", [NOWN, D], F32, kind="ExternalOutput").ap()
        self.out_dep = DR()
        self.scr("tm", [LS, NTM]); self.scr("qkvT", [1536, LS + 8])
        self.scr("o_dn", [2, LS, 512]); self.scr("o_gla", [2, LS, 512]); self.scr("x1", [LS, D])
        self.x1_dep = [DR() for _ in range(NT)]
        self.odn_dep = [[DR() for _ in range(NT)] for _ in range(2)]
        self.ogla_dep = [[DR() for _ in range(NT)] for _ in range(2)]
        self.tm_dep = [DR() for _ in range(NT)]
        self.qkv_dep = [DR() for _ in range(NT)]
        self.qkv_pad_dep = DR()

    def dbg(self, name, tile, shape, dt=F32):
        if name not in self.debug or name in self.S:
            return
        if len(shape) == 2 and len(tile.t.shape) == 4:
            ap = self.nc.dram_tensor(name, list(shape), dt, kind="ExternalOutput").ap()
            self.S[name] = ap
            self.k.dma("sp", ap, tile[:].rearrange("p a b c -> p (a b c)"), reads=[tile], writes=[DR()])
            return
        ap = self.nc.dram_tensor(name, list(shape), dt, kind="ExternalOutput").ap()
        self.S[name] = ap
        src_ap = tile[:]
        if len(shape) == 2 and len(tile.t.shape) == 3:
            src_ap = tile[:].rearrange("p a b -> p (a b)")
        self.k.dma("sp", ap, src_ap, reads=[tile], writes=[DR()])

    def consts(self):
        k = self.k
        self.ident_f = k.sb([128, 128], F32, "ident_f")
        self.ident_b = k.sb([128, 128], BF16, "ident_b")
        self.ones_b = k.sb([128, 128], BF16, "ones_b")
        self.ones_f = k.sb([128, 128], F32, "ones_f")
        self.zeros_f = k.sb([128, 512], F32, "zeros_f")
        idf, idb = self.ident_f, self.ident_b
        k.op("pool", lambda e: e.memset(idf[:], 1.0), writes=[idf])
        k.op("pool", lambda e: e.affine_select(out=idf[:], in_=idf[:], pattern=[[1, 128]], compare_op=ALU.is_equal,
                                                fill=0.0, base=0, channel_multiplier=-1), reads=[idf], writes=[idf])
        k.op("pool", lambda e: e.tensor_copy(out=idb[:], in_=idf[:]), reads=[idf], writes=[idb])
        k.op("pool", lambda e: e.memset(self.ones_b[:], 1.0), writes=[self.ones_b])
        k.op("pool", lambda e: e.memset(self.ones_f[:], 1.0), writes=[self.ones_f])
        k.op("pool", lambda e: e.memset(self.zeros_f[:], 0.0), writes=[self.zeros_f])
        self.ps = [k.psum([128, 512], F32, "ps%d" % i) for i in range(8)]
        self.psi = 0

    def bank(self):
        b = self.ps[self.psi % 8]
        self.psi += 1
        return b

    def rows_to_cols(self, rows, R, n, dst):
        k = self.k
        b = self.bank()
        for c in range(n):
            k.op("pe", lambda e, c=c: e.transpose(out=b[:, c * R:(c + 1) * R], in_=rows[0:R, c * 128:(c + 1) * 128],
                                                  identity=self.ident_f[0:R, 0:R]), reads=[rows, self.ident_f], writes=[b])
        k.op("dve", lambda e: e.tensor_copy(out=dst[:].rearrange("p n r -> p (n r)"), in_=b[:, 0:n * R]), reads=[b], writes=[dst])

    def p0_alloc(self):
        k = self.k
        self.crow = k.sb([2, D], F32, "crow")
        self.cT = k.sb([128, 8, 2], F32, "cT")
        self.wada = [k.sb([128, 8, 512], BF16, "wada%d" % i) for i in range(2)]
        self.modrow = k.sb([2, 6 * D], F32, "modrow")
        self.brow = k.sb([2, 6 * D], F32, "brow")
        self.nrow = k.sb([2, D], F32, "nrow")
        self.fnrow = k.sb([1, D], F32, "fnrow")

    def p0(self, l):
        k, I = self.k, self.I
        crow, cT, scT, modrow, brow, modT = self.crow, self.cT, self.scT, self.modrow, self.brow, self.modT
        if l == 0:
            k.dma("sp", crow[:], I["cvec"], writes=[crow])
            self.rows_to_cols(crow, 2, 8, cT)
            k.op("act", lambda e: e.activation(out=scT[:], in_=cT[:], func=AF.Silu), reads=[cT], writes=[scT])
            k.dma("sp", self.fnrow[:], I["final_norm"].rearrange("(o n) -> o n", o=1), writes=[self.fnrow])
            self.rows_to_cols(self.fnrow, 1, 8, self.fnT)
        for r in range(2):
            k.dma("sp", brow[r:r + 1, :], I["b_ada"][l:l + 1, :], writes=[brow])
        k.dma("sp", self.nrow[0:1, :], I["norm_mix"][l:l + 1, :], writes=[self.nrow])
        k.dma("sp", self.nrow[1:2, :], I["norm_ffn"][l:l + 1, :], writes=[self.nrow])
        self.rows_to_cols(self.nrow, 2, 8, self.nT)
        wsrc = I["w_ada"][l].rearrange("(k p) n -> p k n", p=128)
        for g in range(12):
            wt = self.wada[g % 2]
            k.dma("pool", wt[:], wsrc[:, :, g * 512:(g + 1) * 512], writes=[wt], max_dma_last_dim=2048)
            b = self.bank()
            for kk in range(8):
                k.op("pe", lambda e, kk=kk: e.matmul(b[0:2, :], lhsT=scT[:, kk, :], rhs=wt[:, kk, :], start=(kk == 0), stop=(kk == 7)),
                     reads=[scT, wt], writes=[b])
            k.op("dve", lambda e, g=g: e.tensor_tensor(out=modrow[:, g * 512:(g + 1) * 512], in0=b[0:2, :], in1=brow[:, g * 512:(g + 1) * 512], op=ALU.add),
                 reads=[b, brow], writes=[modrow])
        self.rows_to_cols(modrow, 2, 48, modT)
        for r in range(2):
            k.op("dve", lambda e, r=r: e.scalar_tensor_tensor(out=self.g1T[:, :, r], in0=modT[:, 8:16, r], scalar=1.0, in1=self.nT[:, :, 0], op0=ALU.add, op1=ALU.mult),
                 reads=[modT, self.nT], writes=[self.g1T])
            k.op("dve", lambda e, r=r: e.scalar_tensor_tensor(out=self.g2T[:, :, r], in0=modT[:, 32:40, r], scalar=1.0, in1=self.nT[:, :, 1], op0=ALU.add, op1=ALU.mult),
                 reads=[modT, self.nT], writes=[self.g2T])

    def p1_alloc(self):
        k = self.k
        self.w_fm = k.sb([128, 8, 1536], BF16, "w_fm")
        self.w_tm = k.sb([128, 8, NTM], BF16, "w_tm")
        self.xt = [k.sb([128, D], F32, "xt%d" % i) for i in range(2)]
        self.junk = k.sb([128, D], BF16, "junk")
        self.ss = [k.sb([128, 2], F32, "ss%d" % i) for i in range(2)]
        self.xn = [k.sb([128, D], BF16, "xn%d" % i) for i in range(2)]
        self.hT = [k.sb([128, 8, 512], BF16, "hT%d" % i) for i in range(2)]
        self.tmo = [k.sb([128, NTM], F32, "tmo%d" % i) for i in range(2)]
        self.qo = [k.sb([128, 512], F32, "qo%d" % i) for i in range(3)]

    def p1(self, l, xsrc, xsrc_dep):
        k, I, S = self.k, self.I, self.S
        wsrc = I["w_in"][l].rearrange("(k p) n -> p k n", p=128)
        for kk in range(8):
            k.dma("pool", self.w_fm[:, kk, :], wsrc[:, kk, 0:1536], writes=[self.w_fm], max_dma_last_dim=2048)
            k.dma("pool", self.w_tm[:, kk, :], wsrc[:, kk, 1536:IN_COLS], writes=[self.w_tm], max_dma_last_dim=2048)
        if l == 0:
            for (c0, n) in ((0, 2), (258, 4), (LS + 6, 2)):
                for ch in range(12):
                    k.dma("sp", S["qkvT"][ch * 128:(ch + 1) * 128, c0:c0 + n], self.zeros_f[:, 0:n], reads=[self.zeros_f], writes=[self.qkv_pad_dep])
        blocks = [(0, 2)] + [(2 + 4 * i, 4) for i in range(16)]
        evi = 0
        for bi, (t0, nt) in enumerate(blocks):
            if t0 >= self.ntiles:
                break
            hT = self.hT[bi % 2]
            T = nt * 128
            r = 1 if t0 < 2 else 0
            for ti in range(nt):
                i = t0 + ti
                x_t, ss, xn = self.xt[i % 2], self.ss[i % 2], self.xn[i % 2]
                k.dma("sp", x_t[:], xsrc[i * 128:(i + 1) * 128, :], reads=[xsrc_dep[i]], writes=[x_t])
                k.op("act", lambda e: e.activation(out=self.junk[:], in_=x_t[:], func=AF.Square, accum_out=ss[:, 0:1]), reads=[x_t], writes=[self.junk, ss])
                k.op("act", lambda e: e.activation(out=ss[:, 1:2], in_=ss[:, 0:1], func=AF.Sqrt, scale=1.0 / D, bias=self.eps_t[:, 0:1]), reads=[ss, self.eps_t], writes=[ss])
                k.op("dve", lambda e: e.reciprocal(out=ss[:, 1:2], in_=ss[:, 1:2]), reads=[ss], writes=[ss])
                k.op("dve", lambda e: e.tensor_scalar(out=xn[:], in0=x_t[:], scalar1=ss[:, 1:2], scalar2=None, op0=ALU.mult), reads=[x_t, ss], writes=[xn])
                b = self.bank()
                bb = b.t[:, :].bitcast(BF16)
                for kk in range(8):
                    k.op("pe", lambda e, kk=kk: e.transpose(out=bb[:, kk * 128:(kk + 1) * 128], in_=xn[:, kk * 128:(kk + 1) * 128], identity=self.ident_b[:]),
                         reads=[xn, self.ident_b], writes=[b])
                for kk in range(8):
                    if kk % 2 == 0:
                        k.op("act", lambda e, kk=kk: e.activation(out=hT[:, kk, ti * 128:(ti + 1) * 128], in_=bb[:, kk * 128:(kk + 1) * 128], func=AF.Identity,
                                                                  scale=self.g1T[:, kk, r:r + 1], bias=self.modT[:, kk, r:r + 1]),
                             reads=[b, self.g1T, self.modT], writes=[hT])
                    else:
                        k.op("dve", lambda e, kk=kk: e.tensor_scalar(out=hT[:, kk, ti * 128:(ti + 1) * 128], in0=bb[:, kk * 128:(kk + 1) * 128],
                                                                     scalar1=self.g1T[:, kk, r:r + 1], scalar2=self.modT[:, kk, r:r + 1], op0=ALU.mult, op1=ALU.add),
                             reads=[b, self.g1T, self.modT], writes=[hT])
                tmo = self.tmo[i % 2]
                for cg in range(5):
                    n = min(512, NTM - cg * 512)
                    b2 = self.bank()
                    for kk in range(8):
                        k.op("pe", lambda e, kk=kk: e.matmul(b2[:, 0:n], lhsT=hT[:, kk, ti * 128:(ti + 1) * 128], rhs=self.w_tm[:, kk, cg * 512:cg * 512 + n],
                                                             start=(kk == 0), stop=(kk == 7)), reads=[hT, self.w_tm], writes=[b2])
                    evi += 1
                    if evi % 2:
                        k.op("act", lambda e: e.copy(out=tmo[:, cg * 512:cg * 512 + n], in_=b2[:, 0:n]), reads=[b2], writes=[tmo])
                    else:
                        k.op("dve", lambda e: e.tensor_copy(out=tmo[:, cg * 512:cg * 512 + n], in_=b2[:, 0:n]), reads=[b2], writes=[tmo])
                k.dma("pool", S["tm"][i * 128:(i + 1) * 128, :], tmo[:], reads=[tmo], writes=[self.tm_dep[i]])
            col0 = t0 * 128 + (2 if t0 < 2 else 6)
            for ch in range(12):
                b3 = self.bank()
                for kk in range(8):
                    k.op("pe", lambda e, kk=kk: e.matmul(b3[:, 0:T], lhsT=self.w_fm[:, kk, ch * 128:(ch + 1) * 128], rhs=hT[:, kk, 0:T], start=(kk == 0), stop=(kk == 7)),
                         reads=[hT, self.w_fm], writes=[b3])
                qo = self.qo[ch % 3]
                evi += 1
                if evi % 2:
                    k.op("act", lambda e: e.copy(out=qo[:, 0:T], in_=b3[:, 0:T]), reads=[b3], writes=[qo])
                else:
                    k.op("dve", lambda e: e.tensor_copy(out=qo[:, 0:T], in_=b3[:, 0:T]), reads=[b3], writes=[qo])
                k.dma("pool", S["qkvT"][ch * 128:(ch + 1) * 128, col0:col0 + T], qo[:, 0:T], reads=[qo], writes=[self.qkv_dep[t0 + j] for j in range(nt)])


    def dir_masks(self, d):
        k = self.k
        mI = k.sb([128, 4, 128], BF16, "maskI")
        mS = k.sb([128, 4, 128], BF16, "maskS")
        tri = k.sb([128, 128], F32, "tri")
        sgn = 1 if d == 0 else -1
        for (m, cmp) in ((mI, ALU.is_ge), (mS, ALU.is_gt)):
            k.op("pool", lambda e: e.memset(m[:], 1.0), writes=[m])
            k.op("pool", lambda e: e.affine_select(out=m[:], in_=m[:], pattern=[[0, 4], [sgn, 128]], compare_op=cmp, fill=0.0,
                                                    base=0, channel_multiplier=-sgn), reads=[m], writes=[m])
        k.op("pool", lambda e: e.tensor_copy(out=tri[:], in_=mI[:, 0, :]), reads=[mI], writes=[tri])
        return mI, mS, tri

    def bcast_row(self, src_row_ap, n, dst):
        self.k.dma("sp", dst[:, 0:n], src_row_ap.broadcast_to([128, n]), writes=[dst])

    @staticmethod
    def interleave(gens):
        gens = list(gens)
        while gens:
            nxt = []
            for g in gens:
                try:
                    next(g)
                    nxt.append(g)
                except StopIteration:
                    pass
            gens = nxt

    def p2(self, l):
        k, I, S = self.k, self.I, self.S
        sb = k.sb
        flat = lambda t: t[:].rearrange("p a b -> p (a b)")
        convrow = sb([5, 1536], F32, "convrow")
        cw = sb([128, 12, 5], F32, "cw")
        k.dma("sp", convrow[:], I["conv"][l], writes=[convrow])
        self.rows_to_cols(convrow, 5, 12, cw)
        dtb = sb([128, 8], F32, "dtb")
        nA = sb([128, 8], F32, "nA")
        self.bcast_row(I["dn_dt_bias"][l:l + 1, :], 8, dtb)
        self.bcast_row(I["dn_a_log"][l:l + 1, :], 8, nA)
        k.op("act", lambda e: e.activation(out=nA[:], in_=nA[:], func=AF.Exp), reads=[nA], writes=[nA])
        k.op("dve", lambda e: e.tensor_scalar(out=nA[:], in0=nA[:], scalar1=-1.0, scalar2=None, op0=ALU.mult), reads=[nA], writes=[nA])
        ident4b = sb([128, 4, 128], BF16, "ident4b")
        for h in range(4):
            k.op("pool", lambda e: e.tensor_copy(out=ident4b[:, h, :], in_=self.ident_f[:]), reads=[self.ident_f], writes=[ident4b])
        qeps = sb([128, 1], F32, "qeps")
        k.op("pool", lambda e: e.memset(qeps[:], 128.0 * EPS), writes=[qeps])
        bd32 = sb([128, 4, 128], BF16, "bd32"); m1 = sb([128, 4, 128], BF16, "m1"); m2 = sb([128, 4, 128], BF16, "m2")
        for (bs_, nb_, dst) in ((32, 4, bd32), (64, 2, m1)):
            E = sb([4, 128], F32, "Eblk%d" % bs_)
            k.op("pool", lambda e: e.memset(E[:], 1.0), writes=[E])
            k.op("pool", lambda e: e.affine_select(out=E[:], in_=E[:], pattern=[[1, 128]], compare_op=ALU.is_ge, fill=0.0, base=0, channel_multiplier=-bs_), reads=[E], writes=[E])
            k.op("pool", lambda e: e.affine_select(out=E[:], in_=E[:], pattern=[[-1, 128]], compare_op=ALU.is_ge, fill=0.0, base=bs_ - 1, channel_multiplier=bs_), reads=[E], writes=[E])
            bm = self.bank()
            k.op("pe", lambda e: e.matmul(bm[:, 0:128], lhsT=E[0:nb_, :], rhs=E[0:nb_, :], start=True, stop=True), reads=[E], writes=[bm])
            for h in range(4):
                k.op("dve", lambda e: e.tensor_copy(out=dst[:, h, :], in_=bm[:, 0:128]), reads=[bm], writes=[dst])
        k.op("dve", lambda e: e.tensor_scalar(out=m2[:], in0=m1[:], scalar1=-1.0, scalar2=1.0, op0=ALU.mult, op1=ALU.add), reads=[m1], writes=[m2])
        k.op("dve", lambda e: e.tensor_tensor(out=m1[:], in0=m1[:], in1=bd32[:], op=ALU.subtract), reads=[m1, bd32], writes=[m1])
        raw = sb([128, 4, 516], F32, "raw")
        acc = [sb([128, 512], F32, "acc%d" % i) for i in range(2)]
        sil = [sb([128, 512], F32, "sil%d" % i) for i in range(2)]
        sq = [sb([128, 512], BF16, "sq0")] * 2
        rn = [sb([128, 512], F32, "rn0")] * 2
        KQT = [sb([128, 4, 2, 512], BF16, "KQT%d" % i) for i in range(2)]
        VT = [sb([128, 4, 512], BF16, "VT%d" % i) for i in range(2)]
        KVtm = [[sb([128, 8, 128], BF16, "KVtm%d_%d" % (j, i)) for i in range(4)] for j in range(2)]
        def mk(n, shape, dt, name):
            return [sb(shape, dt, "%s%d" % (name, i)) for i in range(n)]
        sc = mk(2, [128, 16], F32, "sc")
        Gd = mk(2, [128, 4, 128], F32, "Gd")
        Dm = mk(2, [128, 4, 128], F32, "Dm")
        Eg = mk(2, [128, 4, 128], F32, "Eg")
        A = mk(2, [128, 4, 128], BF16, "A"); AT = mk(2, [128, 4, 128], BF16, "AT")
        Abd = mk(2, [128, 4, 128], BF16, "Abd"); ATbd = mk(2, [128, 4, 128], BF16, "ATbd")
        Xa = mk(2, [128, 4, 128], BF16, "Xa"); XTa = mk(2, [128, 4, 128], BF16, "XTa")
        YT = mk(2, [128, 4, 128], BF16, "YT")
        R0 = mk(2, [128, 4, 128], BF16, "R0"); R1 = mk(2, [128, 4, 128], BF16, "R1")
        B1T = mk(2, [128, 4, 128], BF16, "B1T"); B2T = mk(2, [128, 4, 128], BF16, "B2T")
        Keg = mk(2, [128, 4, 128], BF16, "Keg")
        def mk2(shape, dt, name):
            return [[sb(shape, dt, "%s%d_%d" % (name, j, i)) for i in range(4)] for j in range(2)]
        sm = mk2([128, 32], F32, "sm")
        attnT = mk2([128, 4, 128], BF16, "attnT")
        up = mk2([128, 4, 128], F32, "up")
        wT = mk2([128, 4, 128], BF16, "wT")
        kdec = mk2([128, 4, 128], BF16, "kdec")
        qdecT = mk2([128, 4, 128], BF16, "qdecT")
        vnew = [sb([128, 4, 128], BF16, "vnew%d" % i) for i in range(2)]
        osb = [sb([128, 4, 128], F32, "osb%d" % i) for i in range(2)]
        Sf = sb([128, 4, 128], F32, "Sf")
        Sb = sb([128, 4, 128], BF16, "Sb")

        def stage_a(bk, bp, t0, nt):
            T = nt * 128
            col0 = t0 * 128 + (2 if t0 < 2 else 6)
            kq, vt = KQT[bp], VT[bp]
            for cg in range(3):
                k.dma("sp", raw[:, :, 0:T + 4], S["qkvT"][cg * 512:(cg + 1) * 512, col0 - 2:col0 + T + 2].rearrange("(c p) t -> p c t", p=128),
                      reads=[self.qkv_dep[i] for i in range(max(t0 - 1, 0), min(t0 + nt + 1, NT))] + [self.qkv_pad_dep], writes=[raw])
                for c4 in range(4):
                    ch = cg * 4 + c4
                    a_, s_, q_, r_ = acc[ch % 2], sil[ch % 2], sq[ch % 2], rn[ch % 2]
                    k.op("dve", lambda e: e.tensor_scalar(out=a_[:, 0:T], in0=raw[:, c4, 0:T], scalar1=cw[:, ch, 0:1], scalar2=None, op0=ALU.mult),
                         reads=[raw, cw], writes=[a_])
                    for j in range(1, 5):
                        k.op("dve", lambda e: e.scalar_tensor_tensor(out=a_[:, 0:T], in0=raw[:, c4, j:j + T], scalar=cw[:, ch, j:j + 1], in1=a_[:, 0:T],
                                                                     op0=ALU.mult, op1=ALU.add), reads=[raw, cw, a_], writes=[a_])
                    yield
                    if ch >= 8:
                        k.op("act", lambda e: e.activation(out=vt[:, ch - 8, 0:T], in_=a_[:, 0:T], func=AF.Silu), reads=[a_], writes=[vt])
                        continue
                    k.op("act", lambda e: e.activation(out=s_[:, 0:T], in_=a_[:, 0:T], func=AF.Silu), reads=[a_], writes=[s_])
                    k.op("act", lambda e: e.activation(out=q_[:, 0:T], in_=s_[:, 0:T], func=AF.Square), reads=[s_], writes=[q_])
                    yield
                    b = bk()
                    k.op("pe", lambda e: e.matmul(b[:, 0:T], lhsT=self.ones_b[:], rhs=q_[:, 0:T], start=True, stop=True), reads=[self.ones_b, q_], writes=[b])
                    if ch < 4:
                        k.op("act", lambda e: e.activation(out=r_[:, 0:T], in_=b[:, 0:T], func=AF.Sqrt, scale=128.0, bias=qeps[:, 0:1]), reads=[b, qeps], writes=[r_])
                    else:
                        k.op("act", lambda e: e.activation(out=r_[:, 0:T], in_=b[:, 0:T], func=AF.Sqrt, scale=1.0, bias=self.eps_t[:, 0:1]), reads=[b, self.eps_t], writes=[r_])
                    yield
                    k.op("dve", lambda e: e.reciprocal(out=r_[:, 0:T], in_=r_[:, 0:T]), reads=[r_], writes=[r_])
                    hh, which = (ch, 1) if ch < 4 else (ch - 4, 0)
                    k.op("dve", lambda e: e.tensor_tensor(out=kq[:, hh, which, 0:T], in0=s_[:, 0:T], in1=r_[:, 0:T], op=ALU.mult), reads=[s_, r_], writes=[kq])
                    yield
            for ti in range(nt):
                b = bk()
                bb = b.t[:, :].bitcast(BF16)
                for h in range(4):
                    k.op("pe", lambda e: e.transpose(out=bb[:, h * 128:(h + 1) * 128], in_=kq[:, h, 0, ti * 128:(ti + 1) * 128], identity=self.ident_b[:]),
                         reads=[kq, self.ident_b], writes=[b])
                    k.op("pe", lambda e: e.transpose(out=bb[:, (4 + h) * 128:(5 + h) * 128], in_=vt[:, h, ti * 128:(ti + 1) * 128], identity=self.ident_b[:]),
                         reads=[vt, self.ident_b], writes=[b])
                yield
                k.op("act", lambda e: e.copy(out=KVtm[bp][ti][:].rearrange("p a b -> p (a b)"), in_=bb[:, :]), reads=[b], writes=[KVtm[bp][ti]])
                yield

        def stage_b(bk, d, mI, mS, tri, bp, q, i, ti):
            kq, kv = KQT[bp], KVtm[bp][ti]
            tsl = slice(ti * 128, (ti + 1) * 128)
            sc_, sm_ = sc[q], sm[bp][ti]
            k.dma("sp", sc_[:], S["tm"][i * 128:(i + 1) * 128, 512:528], reads=[self.tm_dep[i]], writes=[sc_])
            k.op("act", lambda e: e.activation(out=sm_[:, 0:4], in_=sc_[:, 4 * d:4 * d + 4], func=AF.Exp, scale=-1.0), reads=[sc_], writes=[sm_])
            yield
            k.op("dve", lambda e: e.tensor_scalar(out=sm_[:, 0:4], in0=sm_[:, 0:4], scalar1=1.0, scalar2=None, op0=ALU.add), reads=[sm_], writes=[sm_])
            k.op("dve", lambda e: e.reciprocal(out=sm_[:, 0:4], in_=sm_[:, 0:4]), reads=[sm_], writes=[sm_])
            k.op("dve", lambda e: e.tensor_tensor(out=sm_[:, 4:8], in0=sc_[:, 8 + 4 * d:12 + 4 * d], in1=dtb[:, 4 * d:4 * d + 4], op=ALU.add), reads=[sc_, dtb], writes=[sm_])
            yield
            k.op("act", lambda e: e.activation(out=sm_[:, 4:8], in_=sm_[:, 4:8], func=AF.Exp), reads=[sm_], writes=[sm_])
            k.op("act", lambda e: e.activation(out=sm_[:, 4:8], in_=sm_[:, 4:8], func=AF.Ln, bias=1.0), reads=[sm_], writes=[sm_])
            yield
            k.op("dve", lambda e: e.tensor_tensor(out=sm_[:, 4:8], in0=sm_[:, 4:8], in1=nA[:, 4 * d:4 * d + 4], op=ALU.mult), reads=[sm_, nA], writes=[sm_])
            yield
            bs = bk()
            k.op("pe", lambda e: e.matmul(bs[:, 0:4], lhsT=tri[:], rhs=sm_[:, 4:8], start=True, stop=True), reads=[tri, sm_], writes=[bs])
            k.op("pe", lambda e: e.matmul(bs[:, 4:8], lhsT=self.ones_f[:], rhs=sm_[:, 4:8], start=True, stop=True), reads=[self.ones_f, sm_], writes=[bs])
            G = Gd[q]
            k.op("pool", lambda e: e.tensor_tensor(out=G[:], in0=mI[:], in1=sm_[:, 4:8].unsqueeze(2).to_broadcast([128, 4, 128]), op=ALU.mult),
                 reads=[mI, sm_], writes=[G])
            yield
            k.op("dve", lambda e: e.tensor_copy(out=sm_[:, 8:16], in_=bs[:, 0:8]), reads=[bs], writes=[sm_])
            bg = bk()
            k.op("pe", lambda e: e.matmul(bg[:, :], lhsT=self.ones_f[:], rhs=flat(G), start=True, stop=True), reads=[self.ones_f, G], writes=[bg])
            yield
            k.op("act", lambda e: e.activation(out=sm_[:, 16:20], in_=sm_[:, 8:12], func=AF.Exp), reads=[sm_], writes=[sm_])
            k.op("dve", lambda e: e.tensor_tensor(out=sm_[:, 20:24], in0=sm_[:, 12:16], in1=sm_[:, 8:12], op=ALU.subtract), reads=[sm_], writes=[sm_])
            bg3 = bg[:, :].rearrange("p (a b) -> p a b", a=4)
            for h in range(4):
                k.op("dve", lambda e: e.tensor_scalar(out=Dm[q][:, h, :], in0=bg3[:, h, :], scalar1=sm_[:, 8 + h:9 + h], scalar2=0.0, op0=ALU.subtract, op1=ALU.min),
                     reads=[bg, sm_], writes=[Dm[q]])
            yield
            k.op("act", lambda e: e.activation(out=sm_[:, 20:24], in_=sm_[:, 20:24], func=AF.Exp), reads=[sm_], writes=[sm_])
            k.op("act", lambda e: e.activation(out=sm_[:, 24:28], in_=sm_[:, 12:16], func=AF.Exp), reads=[sm_], writes=[sm_])
            k.op("act", lambda e: e.activation(out=flat(Eg[q]), in_=bg[:, :], func=AF.Exp), reads=[bg], writes=[Eg[q]])
            k.op("act", lambda e: e.activation(out=Dm[q][:], in_=Dm[q][:], func=AF.Exp), reads=[Dm[q]], writes=[Dm[q]])
            yield
            k.op("dve", lambda e: e.tensor_tensor(out=sm_[:, 20:24], in0=sm_[:, 20:24], in1=sm_[:, 0:4], op=ALU.mult), reads=[sm_], writes=[sm_])
            k.op("pool", lambda e: e.tensor_tensor(out=qdecT[bp][ti][:], in0=kq[:, :, 1, tsl], in1=Eg[q][:], op=ALU.mult), reads=[kq, Eg[q]], writes=[qdecT[bp][ti]])
            bet = sm_[:, 0:4].unsqueeze(2).to_broadcast([128, 4, 128])
            DmI, DmS = Gd[q], Eg[q]
            k.op("pool", lambda e: e.tensor_tensor(out=DmI[:], in0=Dm[q][:], in1=mI[:], op=ALU.mult), reads=[Dm[q], mI], writes=[DmI])
            yield
            k.op("pool", lambda e: e.tensor_tensor(out=DmS[:], in0=Dm[q][:], in1=mS[:], op=ALU.mult), reads=[Dm[q], mS], writes=[DmS])
            k.op("pool", lambda e: e.tensor_tensor(out=DmI[:], in0=DmI[:], in1=bet, op=ALU.mult), reads=[DmI, sm_], writes=[DmI])
            k.op("pool", lambda e: e.tensor_tensor(out=DmS[:], in0=DmS[:], in1=bet, op=ALU.mult), reads=[DmS, sm_], writes=[DmS])
            K3 = kv[:, 0:4, :]
            k.op("pool", lambda e: e.tensor_tensor(out=Keg[q][:], in0=K3, in1=sm_[:, 16:20].unsqueeze(2).to_broadcast([128, 4, 128]), op=ALU.mult), reads=[kv, sm_], writes=[Keg[q]])
            k.op("pool", lambda e: e.tensor_tensor(out=kdec[bp][ti][:], in0=K3, in1=sm_[:, 20:24].unsqueeze(2).to_broadcast([128, 4, 128]), op=ALU.mult), reads=[kv, sm_], writes=[kdec[bp][ti]])
            bkk, bkq = bk(), bk()
            for h in range(4):
                k.op("pe", lambda e: e.matmul(bkk[:, h * 128:(h + 1) * 128], lhsT=kq[:, h, 0, tsl], rhs=kq[:, h, 0, tsl], start=True, stop=True), reads=[kq], writes=[bkk])
                k.op("pe", lambda e: e.matmul(bkq[:, h * 128:(h + 1) * 128], lhsT=kq[:, h, 0, tsl], rhs=kq[:, h, 1, tsl], start=True, stop=True), reads=[kq], writes=[bkq])
            yield
            k.op("dve", lambda e: e.tensor_tensor(out=flat(A[q]), in0=bkk[:, :], in1=flat(DmS), op=ALU.mult), reads=[bkk, DmS], writes=[A[q]])
            k.op("dve", lambda e: e.tensor_tensor(out=flat(attnT[bp][ti]), in0=bkq[:, :], in1=flat(DmI), op=ALU.mult), reads=[bkq, DmI], writes=[attnT[bp][ti]])
            yield
            bt = bk()
            btb = bt.t[:, :].bitcast(BF16)
            for h in range(4):
                k.op("pe", lambda e: e.transpose(out=btb[:, h * 128:(h + 1) * 128], in_=A[q][:, h, :], identity=self.ident_b[:]), reads=[A[q], self.ident_b], writes=[bt])
            k.op("pool", lambda e: e.tensor_tensor(out=Abd[q][:], in0=A[q][:], in1=bd32[:], op=ALU.mult), reads=[A[q], bd32], writes=[Abd[q]])
            yield
            k.op("act", lambda e: e.copy(out=flat(AT[q]), in_=btb[:, 0:512]), reads=[bt], writes=[AT[q]])
            k.op("pool", lambda e: e.tensor_tensor(out=R0[q][:], in0=ident4b[:], in1=Abd[q][:], op=ALU.subtract), reads=[ident4b, Abd[q]], writes=[R0[q]])
            yield
            k.op("pool", lambda e: e.tensor_tensor(out=ATbd[q][:], in0=AT[q][:], in1=bd32[:], op=ALU.mult), reads=[AT[q], bd32], writes=[ATbd[q]])
            k.op("pool", lambda e: e.tensor_tensor(out=B1T[q][:], in0=AT[q][:], in1=m1[:], op=ALU.mult), reads=[AT[q], m1], writes=[B1T[q]])
            k.op("pool", lambda e: e.tensor_tensor(out=B2T[q][:], in0=AT[q][:], in1=m2[:], op=ALU.mult), reads=[AT[q], m2], writes=[B2T[q]])
            yield
            Xc, XTc, Rc = Abd[q], ATbd[q], R0[q]
            xbufs = [(Xa[q], XTa[q]), (A[q], AT[q])]
            rbufs = [R1[q], R0[q]]
            for lev in range(4):
                lastl = lev == 3
                Xn, XTn = xbufs[lev % 2]
                Rn = rbufs[lev % 2]
                b1 = bk()
                for h in range(4):
                    k.op("pe", lambda e: e.matmul(b1[:, h * 128:(h + 1) * 128], lhsT=Xc[:, h, :], rhs=XTc[:, h, :], start=True, stop=True), reads=[Xc, XTc], writes=[b1])
                if not lastl:
                    b2 = bk()
                    for h in range(4):
                        k.op("pe", lambda e: e.matmul(b2[:, h * 128:(h + 1) * 128], lhsT=XTc[:, h, :], rhs=Xc[:, h, :], start=True, stop=True), reads=[Xc, XTc], writes=[b2])
                yield
                k.op("dve", lambda e: e.tensor_tensor(out=flat(YT[q]), in0=b1[:, :], in1=flat(ident4b), op=ALU.add), reads=[b1, ident4b], writes=[YT[q]])
                if not lastl:
                    k.op("act", lambda e: e.copy(out=flat(XTn), in_=b1[:, :]), reads=[b1], writes=[XTn])
                    k.op("act", lambda e: e.copy(out=flat(Xn), in_=b2[:, :]), reads=[b2], writes=[Xn])
                yield
                b3 = bk()
                for h in range(4):
                    k.op("pe", lambda e: e.matmul(b3[:, h * 128:(h + 1) * 128], lhsT=YT[q][:, h, :], rhs=Rc[:, h, :], start=True, stop=True), reads=[YT[q], Rc], writes=[b3])
                yield
                k.op("dve", lambda e: e.tensor_copy(out=flat(Rn), in_=b3[:, :]), reads=[b3], writes=[Rn])
                yield
                Xc, XTc, Rc = Xn, XTn, Rn
            TTb, Pb = Xa[q], XTa[q]
            for mi, BT_ in enumerate((B1T[q], B2T[q])):
                Tn = rbufs[mi % 2]
                btt = bk()
                bttb = btt.t[:, :].bitcast(BF16)
                for h in range(4):
                    k.op("pe", lambda e: e.transpose(out=bttb[:, h * 128:(h + 1) * 128], in_=Rc[:, h, :], identity=self.ident_b[:]), reads=[Rc, self.ident_b], writes=[btt])
                bp_ = bk()
                for h in range(4):
                    k.op("pe", lambda e: e.matmul(bp_[:, h * 128:(h + 1) * 128], lhsT=BT_[:, h, :], rhs=Rc[:, h, :], start=True, stop=True), reads=[BT_, Rc], writes=[bp_])
                yield
                k.op("act", lambda e: e.copy(out=flat(TTb), in_=bttb[:, 0:512]), reads=[btt], writes=[TTb])
                k.op("dve", lambda e: e.tensor_copy(out=flat(Pb), in_=bp_[:, :]), reads=[bp_], writes=[Pb])
                yield
                bq = bk()
                for h in range(4):
                    k.op("pe", lambda e: e.matmul(bq[:, h * 128:(h + 1) * 128], lhsT=TTb[:, h, :], rhs=Pb[:, h, :], start=True, stop=True), reads=[TTb, Pb], writes=[bq])
                yield
                k.op("dve", lambda e: e.tensor_tensor(out=flat(Tn), in0=flat(Rc), in1=bq[:, :], op=ALU.subtract), reads=[Rc, bq], writes=[Tn])
                yield
                Rc = Tn
            T2T = Rc
            bu, bw = bk(), bk()
            for h in range(4):
                k.op("pe", lambda e: e.matmul(bu[:, h * 128:(h + 1) * 128], lhsT=T2T[:, h, :], rhs=kv[:, 4 + h, :], start=True, stop=True), reads=[T2T, kv], writes=[bu])
                k.op("pe", lambda e: e.matmul(bw[:, h * 128:(h + 1) * 128], lhsT=Keg[q][:, h, :], rhs=T2T[:, h, :], start=True, stop=True), reads=[T2T, Keg[q]], writes=[bw])
            yield
            k.op("act", lambda e: e.copy(out=flat(up[bp][ti]), in_=bu[:, :]), reads=[bu], writes=[up[bp][ti]])
            k.op("dve", lambda e: e.tensor_copy(out=flat(wT[bp][ti]), in_=bw[:, :]), reads=[bw], writes=[wT[bp][ti]])
            yield

        def scan(bk, d, bp, tiles):
            for n_, (i, ti) in enumerate(tiles):
                p = n_ % 2
                sm_ = sm[bp][ti]
                bws = bk()
                for h in range(4):
                    k.op("pe", lambda e: e.matmul(bws[:, h * 128:(h + 1) * 128], lhsT=wT[bp][ti][:, h, :], rhs=Sb[:, h, :], start=True, stop=True), reads=[wT[bp][ti], Sb], writes=[bws])
                yield
                k.op("dve", lambda e: e.tensor_tensor(out=flat(vnew[p]), in0=flat(up[bp][ti]), in1=bws[:, :], op=ALU.subtract), reads=[up[bp][ti], bws], writes=[vnew[p]])
                yield
                bo, bkv = bk(), bk()
                for h in range(4):
                    k.op("pe", lambda e: e.matmul(bkv[:, h * 128:(h + 1) * 128], lhsT=kdec[bp][ti][:, h, :], rhs=vnew[p][:, h, :], start=True, stop=True), reads=[kdec[bp][ti], vnew[p]], writes=[bkv])
                for h in range(4):
                    k.op("pe", lambda e: e.matmul(bo[:, h * 128:(h + 1) * 128], lhsT=qdecT[bp][ti][:, h, :], rhs=Sb[:, h, :], start=True, stop=False), reads=[qdecT[bp][ti], Sb], writes=[bo])
                    k.op("pe", lambda e: e.matmul(bo[:, h * 128:(h + 1) * 128], lhsT=attnT[bp][ti][:, h, :], rhs=vnew[p][:, h, :], start=False, stop=True), reads=[attnT[bp][ti], vnew[p]], writes=[bo])
                yield
                bkv3 = bkv[:, :].rearrange("p (a b) -> p a b", a=4)
                for h in range(4):
                    k.op("dve", lambda e: e.scalar_tensor_tensor(out=Sf[:, h, :], in0=Sf[:, h, :], scalar=sm_[:, 24 + h:25 + h], in1=bkv3[:, h, :], op0=ALU.mult, op1=ALU.add),
                         reads=[Sf, sm_, bkv], writes=[Sf])
                k.op("act", lambda e: e.copy(out=flat(osb[p]), in_=bo[:, :]), reads=[bo], writes=[osb[p]])
                yield
                k.op("act", lambda e: e.copy(out=Sb[:], in_=Sf[:]), reads=[Sf], writes=[Sb])
                k.dma("act", S["o_dn"][d, i * 128:(i + 1) * 128, :], flat(osb[p]), reads=[osb[p]], writes=[self.odn_dep[d][i]])
                yield

        def cyc(ids):
            st = [0]

            def f():
                b = self.ps[ids[st[0] % len(ids)]]
                st[0] += 1
                return b
            return f
        bk_b = [cyc([0, 1]), cyc([2, 3])]
        bk_a, bk_s = cyc([6]), cyc([4, 5])
        blocks_f = [(0, 2)] + [(2 + 4 * i, 4) for i in range(16)]
        blocks_f = [b_ for b_ in blocks_f if b_[0] < self.ntiles]
        gbi = 0
        for d in range(2):
            mI, mS, tri = self.dir_masks(d)
            k.op("pool", lambda e: e.memset(Sf[:], 0.0), writes=[Sf])
            k.op("pool", lambda e: e.memset(Sb[:], 0.0), writes=[Sb])
            blocks = blocks_f if d == 0 else [blocks_f[0]] + blocks_f[:0:-1]
            nb = len(blocks)
            self.interleave([stage_a(bk_a, gbi % 2, *blocks[0])])
            prev = None
            for bi, (t0, nt) in enumerate(blocks):
                bp = (gbi + bi) % 2
                tis = list(range(nt)) if d == 0 else list(range(nt - 1, -1, -1))
                for pair in range(0, nt, 2):
                    gens = [stage_b(bk_b[q], d, mI, mS, tri, bp, q, t0 + ti, ti) for q, ti in enumerate(tis[pair:pair + 2])]
                    if pair == 0 and bi + 1 < nb:
                        gens.append(stage_a(bk_a, (gbi + bi + 1) % 2, *blocks[bi + 1]))
                    if prev is not None and (pair == 2 or nt == 2):
                        gens.append(scan(bk_s, d, *prev))
                    self.interleave(gens)
                prev = (bp, [(t0 + ti, ti) for ti in tis])
            self.interleave([scan(bk_s, d, *prev)])
            gbi += nb

    def p3(self, l):
        k, I, S = self.k, self.I, self.S
        sb = k.sb

        def two(shape, dt, name):
            return [sb(shape, dt, name + "%d" % i) for i in range(2)]
        qkv = two([128, 1024], F32, "gqkv")
        glr = two([128, 16], F32, "glr")
        lhsTg = two([64, 128], F32, "lhsTg")
        W2 = sb([64, 256], F32, "W2")
        ex = two([128, 256], F32, "gex")
        la = two([128, 256], F32, "la")
        bcs = two([128, 256], F32, "bcs")
        ebc = two([128, 256], F32, "ebc")
        enbc = two([128, 256], F32, "enbc")
        ekd = two([128, 256], F32, "ekd")
        qs = two([128, 256], BF16, "gqs")
        ks = two([128, 256], BF16, "gks")
        kd = two([128, 256], BF16, "gkd")
        vb = two([128, 4, 128], BF16, "gvb")
        qT = two([128, 4, 128], BF16, "gqTm")
        kT = two([128, 2, 128], BF16, "gkT")
        att = two([128, 4, 128], BF16, "gatt")
        osb = two([128, 512], F32, "gosb")
        etc_ = two([128, 4], F32, "getc")
        Sf = sb([128, 2, 128], F32, "gSf")
        Sb = sb([128, 2, 128], BF16, "gSb")
        for t_ in qT:
            k.op("pool", lambda e: e.memset(t_[:], 0.0), writes=[t_])
        for t_ in lhsTg:
            k.op("pool", lambda e: e.memset(t_[:], 0.0), writes=[t_])
            k.op("pool", lambda e: e.memset(t_[32:33, :], 1.0), writes=[t_])
        tm_lat = S["tm"][LC:LS, :].rearrange("(r w) n -> w r n", w=64)
        alltm = self.tm_dep
        for d in range(2):
            mI, mS, tri = self.dir_masks(d)
            k.op("pool", lambda e: e.memset(W2[:], 0.0), writes=[W2])
            k.dma("sp", W2[0:16, :], I["gla_w_gate2"][l, d], writes=[W2])
            k.dma("sp", W2[32:33, :], I["gla_b_gate"][l, d:d + 1, :], writes=[W2])
            k.op("pool", lambda e: e.memset(Sf[:], 0.0), writes=[Sf])
            k.op("pool", lambda e: e.memset(Sb[:], 0.0), writes=[Sb])
            order = [("c", 0), ("c", 1)] + [("g", j) for j in range(64)]
            if d == 1:
                order = [("c", 1), ("c", 0)] + [("g", j) for j in range(63, -1, -1)]
            if self.ntiles < NT:
                order = order[:self.ntiles]
            def cyc(ids):
                st = [0]

                def f():
                    b = self.ps[ids[st[0] % len(ids)]]
                    st[0] += 1
                    return b
                return f
            bk_pre, bk_post = cyc([0, 1, 2, 3]), cyc([4, 5])

            def pre(bk, p, kind, j):
                if kind == "c":
                    rows = S["tm"][j * 128:(j + 1) * 128, :]
                else:
                    rows = tm_lat[j]
                k.dma("sp", qkv[p][:], rows[:, 528:1552], reads=alltm, writes=[qkv[p]])
                k.dma("sp", glr[p][:], rows[:, 2064 + 16 * d:2080 + 16 * d], reads=alltm, writes=[glr[p]])
                b0 = bk()
                k.op("pe", lambda e: e.transpose(out=b0[0:16, 0:128], in_=glr[p][:, :], identity=self.ident_f[:]), reads=[glr[p], self.ident_f], writes=[b0])
                k.op("act", lambda e: e.copy(out=lhsTg[p][0:16, :], in_=b0[0:16, 0:128]), reads=[b0], writes=[lhsTg[p]])
                yield
                bgt = bk()
                k.op("pe", lambda e: e.matmul(bgt[:, 0:256], lhsT=lhsTg[p][:, :], rhs=W2[:, :], start=True, stop=True), reads=[lhsTg[p], W2], writes=[bgt])
                k.op("act", lambda e: e.activation(out=ex[p][:], in_=bgt[:, 0:256], func=AF.Exp, scale=-1.0), reads=[bgt], writes=[ex[p]])
                yield
                k.op("act", lambda e: e.activation(out=ex[p][:], in_=ex[p][:], func=AF.Ln, bias=1.0), reads=[ex[p]], writes=[ex[p]])
                yield
                k.op("dve", lambda e: e.tensor_scalar(out=la[p][:], in0=ex[p][:], scalar1=-1.0 / 16.0, scalar2=None, op0=ALU.mult), reads=[ex[p]], writes=[la[p]])
                yield
                bbc, bto = bk(), bk()
                k.op("pe", lambda e: e.matmul(bbc[:, 0:256], lhsT=tri[:], rhs=la[p][:], start=True, stop=True), reads=[tri, la[p]], writes=[bbc])
                k.op("pe", lambda e: e.matmul(bto[:, 0:256], lhsT=self.ones_f[:], rhs=la[p][:], start=True, stop=True), reads=[self.ones_f, la[p]], writes=[bto])
                for hp in range(2):
                    k.op("pe", lambda e: e.matmul(bto[:, 256 + 2 * hp:258 + 2 * hp], lhsT=la[p][:, hp * 128:(hp + 1) * 128], rhs=self.ones_f[:, 0:2], start=True, stop=True),
                         reads=[la[p], self.ones_f], writes=[bto])
                k.op("act", lambda e: e.activation(out=ebc[p][:], in_=bbc[:, 0:256], func=AF.Exp), reads=[bbc], writes=[ebc[p]])
                yield
                k.op("act", lambda e: e.activation(out=enbc[p][:], in_=bbc[:, 0:256], func=AF.Exp, scale=-1.0), reads=[bbc], writes=[enbc[p]])
                yield
                k.op("act", lambda e: e.copy(out=bcs[p][:], in_=bbc[:, 0:256]), reads=[bbc], writes=[bcs[p]])
                yield
                k.op("dve", lambda e: e.tensor_tensor(out=ekd[p][:], in0=bto[:, 0:256], in1=bcs[p][:], op=ALU.subtract), reads=[bto, bcs[p]], writes=[ekd[p]])
                yield
                k.op("act", lambda e: e.activation(out=ekd[p][:], in_=ekd[p][:], func=AF.Exp), reads=[ekd[p]], writes=[ekd[p]])
                yield
                k.op("act", lambda e: e.activation(out=etc_[p][:], in_=bto[:, 256:260], func=AF.Exp), reads=[bto], writes=[etc_[p]])
                yield
                k.op("dve", lambda e: e.scalar_tensor_tensor(out=qs[p][:], in0=qkv[p][:, 0:256], scalar=0.125, in1=ebc[p][:], op0=ALU.mult, op1=ALU.mult), reads=[qkv[p], ebc[p]], writes=[qs[p]])
                yield
                k.op("dve", lambda e: e.tensor_tensor(out=ks[p][:], in0=qkv[p][:, 256:512], in1=enbc[p][:], op=ALU.mult), reads=[qkv[p], enbc[p]], writes=[ks[p]])
                yield
                k.op("pool", lambda e: e.tensor_tensor(out=kd[p][:], in0=qkv[p][:, 256:512], in1=ekd[p][:], op=ALU.mult), reads=[qkv[p], ekd[p]], writes=[kd[p]])
                yield
                k.op("pool", lambda e: e.tensor_copy(out=vb[p][:].rearrange("p a b -> p (a b)"), in_=qkv[p][:, 512:1024]), reads=[qkv[p]], writes=[vb[p]])
                yield
                btr = bk()
                btrb = btr.t[:, :].bitcast(BF16)
                for hp in range(2):
                    k.op("pe", lambda e: e.transpose(out=btrb[:, hp * 128:(hp + 1) * 128], in_=qs[p][:, hp * 128:(hp + 1) * 128], identity=self.ident_b[:]), reads=[qs[p], self.ident_b], writes=[btr])
                    k.op("pe", lambda e: e.transpose(out=btrb[:, (2 + hp) * 128:(3 + hp) * 128], in_=ks[p][:, hp * 128:(hp + 1) * 128], identity=self.ident_b[:]), reads=[ks[p], self.ident_b], writes=[btr])
                for h in range(4):
                    hb, hp = h % 2, h // 2
                    k.op("act", lambda e: e.copy(out=qT[p][hb * 64:(hb + 1) * 64, h, :], in_=btrb[hb * 64:(hb + 1) * 64, hp * 128:(hp + 1) * 128]), reads=[btr], writes=[qT[p]])
                k.op("act", lambda e: e.copy(out=kT[p][:].rearrange("p a b -> p (a b)"), in_=btrb[:, 256:512]), reads=[btr], writes=[kT[p]])
                yield
                bat = bk()
                for h in range(4):
                    hb, hp = h % 2, h // 2
                    ps_ = slice(hb * 64, (hb + 1) * 64)
                    k.op("pe", lambda e: e.matmul(bat[:, h * 128:(h + 1) * 128], lhsT=kT[p][:, hp, :], rhs=qT[p][:, h, :], start=True, stop=True), reads=[kT[p], qT[p]], writes=[bat])
                k.op("dve", lambda e: e.tensor_tensor(out=att[p][:].rearrange("p a b -> p (a b)"), in0=bat[:, :], in1=mI[:].rearrange("p a b -> p (a b)"), op=ALU.mult), reads=[bat, mI], writes=[att[p]])
                yield
                yield

            def post(bk, p, kind, j):
                if kind == "c":
                    orows = S["o_gla"][d, j * 128:(j + 1) * 128, :]
                    odeps = [self.ogla_dep[d][j]]
                else:
                    orows = S["o_gla"][d, LC:LS, :].rearrange("(r w) n -> w r n", w=64)[j]
                    odeps = self.ogla_dep[d][2:]
                bo, bkv = bk(), bk()
                for h in range(4):
                    hb, hp = h % 2, h // 2
                    ps_ = slice(hb * 64, (hb + 1) * 64)
                    k.op("pe", lambda e: e.matmul(bo[:, h * 128:(h + 1) * 128], lhsT=qT[p][:, h, :], rhs=Sb[:, hp, :], start=True, stop=False), reads=[qT[p], Sb], writes=[bo])
                    k.op("pe", lambda e: e.matmul(bo[:, h * 128:(h + 1) * 128], lhsT=att[p][:, h, :], rhs=vb[p][:, h, :], start=False, stop=True), reads=[att[p], vb[p]], writes=[bo])
                for h in range(4):
                    hb, hp = h % 2, h // 2
                    k.op("pe", lambda e: e.matmul(bkv[:, h * 128:(h + 1) * 128], lhsT=kd[p][:, hp * 128:(hp + 1) * 128], rhs=vb[p][:, h, :], start=True, stop=True),
                         reads=[kd[p], vb[p]], writes=[bkv])
                k.op("act", lambda e: e.copy(out=osb[p][:], in_=bo[:, :]), reads=[bo], writes=[osb[p]])
                yield
                k.dma("act", orows, osb[p][:], reads=[osb[p]], writes=odeps)
                for h in range(4):
                    hb, hp = h % 2, h // 2
                    ps_ = slice(hb * 64, (hb + 1) * 64)
                    k.op("dve", lambda e: e.scalar_tensor_tensor(out=Sf[ps_, hp, :], in0=Sf[ps_, hp, :], scalar=etc_[p][ps_, 2 * hp:2 * hp + 1], in1=bkv[ps_, h * 128:(h + 1) * 128], op0=ALU.mult, op1=ALU.add),
                         reads=[Sf, etc_[p], bkv], writes=[Sf])
                k.op("act", lambda e: e.copy(out=Sb[:], in_=Sf[:]), reads=[Sf], writes=[Sb])
                yield


                yield

            self.interleave([pre(bk_pre, 0, *order[0])])
            for step, (kind, j) in enumerate(order):
                gens = [post(bk_post, step % 2, kind, j)]
                if step + 1 < len(order):
                    gens.append(pre(bk_pre, (step + 1) % 2, *order[step + 1]))
                self.interleave(gens)
    def p4(self, l):
        k, I, S = self.k, self.I, self.S
        sb = k.sb
        last = l == self.nlayers - 1
        moe = (l % 2 == 1)
        T = 1024 if moe else 512
        NH = T // 512
        gnb = sb([128, 8, 128], F32, "gnb")
        for h in range(8):
            src_ = I["dn_norm"] if h < 4 else I["gla_norm"]
            k.dma("sp", gnb[:, h, :], src_[l:l + 1, :].broadcast_to([128, 128]), writes=[gnb])
        xT = sb([128, 8, T], F32, "xT")
        mT = sb([128, 8, T], BF16, "mT")
        sq2 = mT
        h2T = sb([128, 8, T], BF16, "h2T")
        rs2 = sb([128, T], F32, "rs2")
        tmpf = [sb([128, T], F32, "tmpf%d" % i) for i in range(2)]
        F = D_EXP if moe else D_FF
        NFC = F // 128
        actT = sb([128, NFC, T], BF16, "actT")
        ot = [sb([128, 512], F32, "ot%d" % i) for i in range(4)]
        zt = sb([128, 1024], F32, "zt")
        xt = sb([128, D], F32, "xt4")
        xo = xt
        osum = sb([128, 8, 128], F32, "osum")
        ssh = sb([128, 16], F32, "ssh")
        mb = sb([128, D], BF16, "mb")
        wb = [sb([128, 8, 512], BF16, "wb%d" % i) for i in range(3)]
        wd = [sb([128, 4, 512], BF16, "wd%d" % i) for i in range(2)]
        sg = [ot[0], ot[1]]
        if moe:
            rt = sb([128, 8, NE], F32, "rt")
            k.dma("sp", rt[:], I["moe_router"].rearrange("(k p) n -> p k n", p=128), writes=[rt])
            lgT = sb([8, T], F32, "lgT")
            lg = sb([128, 8, NE], F32, "lg")
            l2 = sb([128, 8, NE], F32, "l2")
            mx = sb([128, 16], F32, "mx")
            cbT = sb([8, T], F32, "cbT")
            CBe = [zt, xt]
            selE = sb([8, NE, 128], F32, "selE")
            for e_ in range(NE):
                k.op("pool", lambda e: e.tensor_copy(out=selE[0:8, e_, :], in_=self.ident_f[0:8, e_:e_ + 1].to_broadcast([8, 128])), reads=[self.ident_f], writes=[selE])
        ntb = T // 128
        if last:
            blocks = [(2 + ntb * i, ntb) for i in range(NOWN // T)]
        else:
            blocks = [(0, 2)] + [(2 + ntb * i, ntb) for i in range(LL // T)]
        wi = [0]

        def wload(src_ap, ncols):
            t_ = wb[wi[0] % 3]
            wi[0] += 1
            k.dma("pool", t_[:, :, 0:ncols], src_ap, writes=[t_], max_dma_last_dim=2048)
            return t_
        wout_src = I["w_out"][l].rearrange("(k p) n -> p k n", p=128)
        for (t0, nt) in blocks:
            if t0 >= self.ntiles:
                break
            Tb = nt * 128
            halves = [(h0, min(512, Tb - h0)) for h0 in range(0, Tb, 512)]
            r = 1 if t0 < 2 else 0
            for ti in range(nt):
                i = t0 + ti
                rows = slice(i * 128, (i + 1) * 128)
                tsl = slice(ti * 128, (ti + 1) * 128)
                k.dma("sp", ot[0][:], S["o_dn"][0, rows, :], reads=[self.odn_dep[0][i]], writes=[ot[0]])
                k.dma("sp", ot[1][:], S["o_dn"][1, rows, :], reads=[self.odn_dep[1][i]], writes=[ot[1]])
                k.dma("sp", ot[2][:], S["o_gla"][0, rows, :], reads=[self.ogla_dep[0][i]], writes=[ot[2]])
                k.dma("sp", ot[3][:], S["o_gla"][1, rows, :], reads=[self.ogla_dep[1][i]], writes=[ot[3]])
                k.dma("sp", zt[:, 0:512], S["tm"][rows, 0:512], reads=[self.tm_dep[i]], writes=[zt])
                k.dma("sp", zt[:, 512:1024], S["tm"][rows, 1552:2064], reads=[self.tm_dep[i]], writes=[zt])
                k.dma("sp", xt[:], self.xcur[rows, :], reads=[self.xcur_dep[i]], writes=[xt])
                osf = osum[:].rearrange("p a b -> p (a b)")
                k.op("dve", lambda e: e.tensor_tensor(out=osf[:, 0:512], in0=ot[0][:], in1=ot[1][:], op=ALU.add), reads=[ot[0], ot[1]], writes=[osum])
                k.op("dve", lambda e: e.tensor_tensor(out=osf[:, 512:1024], in0=ot[2][:], in1=ot[3][:], op=ALU.add), reads=[ot[2], ot[3]], writes=[osum])
                for hh in range(2):
                    k.op("act", lambda e: e.activation(out=ot[2 * hh][:], in_=osf[:, hh * 512:(hh + 1) * 512], func=AF.Square), reads=[osum], writes=[ot[2 * hh]])
                    k.op("dve", lambda e: e.tensor_reduce(out=ssh[:, 4 * hh:4 * hh + 4], in_=ot[2 * hh][:].rearrange("p (a b) -> p a b", a=4), axis=AX.X, op=ALU.add), reads=[ot[2 * hh]], writes=[ssh])
                k.op("act", lambda e: e.activation(out=ssh[:, 8:16], in_=ssh[:, 0:8], func=AF.Sqrt, scale=1.0 / 128.0, bias=self.eps_t[:, 0:1]), reads=[ssh, self.eps_t], writes=[ssh])
                k.op("dve", lambda e: e.reciprocal(out=ssh[:, 8:16], in_=ssh[:, 8:16]), reads=[ssh], writes=[ssh])
                k.op("act", lambda e: e.activation(out=zt[:], in_=zt[:], func=AF.Silu), reads=[zt], writes=[zt])
                for h in range(8):
                    k.op("dve", lambda e: e.scalar_tensor_tensor(out=osum[:, h, :], in0=osum[:, h, :], scalar=ssh[:, 8 + h:9 + h], in1=gnb[:, h, :], op0=ALU.mult, op1=ALU.mult),
                         reads=[osum, ssh, gnb], writes=[osum])
                k.op("dve", lambda e: e.tensor_tensor(out=mb[:], in0=osf, in1=zt[:], op=ALU.mult), reads=[osum, zt], writes=[mb])
                b = self.bank()
                bb = b.t[:, :].bitcast(BF16)
                for kk in range(8):
                    k.op("pe", lambda e: e.transpose(out=bb[:, kk * 128:(kk + 1) * 128], in_=mb[:, kk * 128:(kk + 1) * 128], identity=self.ident_b[:]), reads=[mb, self.ident_b], writes=[b])
                k.op("act", lambda e: e.copy(out=mT[:, :, tsl], in_=bb[:, :].rearrange("p (a b) -> p a b", a=8)), reads=[b], writes=[mT])
                for half in range(2):
                    b2 = self.bank()
                    for q in range(4):
                        kk = half * 4 + q
                        k.op("pe", lambda e: e.transpose(out=b2[:, q * 128:(q + 1) * 128], in_=xt[:, kk * 128:(kk + 1) * 128], identity=self.ident_f[:]), reads=[xt, self.ident_f], writes=[b2])
                    k.op("dve", lambda e: e.tensor_copy(out=xT[:, half * 4:half * 4 + 4, tsl], in_=b2[:, :].rearrange("p (a b) -> p a b", a=4)), reads=[b2], writes=[xT])
            for og in range(2):
                wt = wload(wout_src[:, :, og * 512:(og + 1) * 512], 512)
                for q in range(4):
                    oc = og * 4 + q
                    for (h0, hw) in halves:
                        b = self.bank()
                        for kk in range(8):
                            k.op("pe", lambda e: e.matmul(b[:, 0:hw], lhsT=wt[:, kk, q * 128:(q + 1) * 128], rhs=mT[:, kk, h0:h0 + hw], start=(kk == 0), stop=(kk == 7)), reads=[wt, mT], writes=[b])
                        k.op("dve", lambda e: e.scalar_tensor_tensor(out=xT[:, oc, h0:h0 + hw], in0=b[:, 0:hw], scalar=self.modT[:, 16 + oc, r:r + 1], in1=xT[:, oc, h0:h0 + hw], op0=ALU.mult, op1=ALU.add),
                             reads=[b, self.modT, xT], writes=[xT])

            def rms_bcast(dst):
                for kk in range(8):
                    k.op("act", lambda e: e.activation(out=sq2[:, kk, 0:Tb], in_=xT[:, kk, 0:Tb], func=AF.Square), reads=[xT], writes=[sq2])
                for (h0, hw) in halves:
                    bs_ = self.bank()
                    for kk in range(8):
                        k.op("pe", lambda e: e.matmul(bs_[:, 0:hw], lhsT=self.ones_b[:], rhs=sq2[:, kk, h0:h0 + hw], start=(kk == 0), stop=(kk == 7)), reads=[self.ones_b, sq2], writes=[bs_])
                    k.op("act", lambda e: e.activation(out=dst[:, h0:h0 + hw], in_=bs_[:, 0:hw], func=AF.Sqrt, scale=1.0 / D, bias=self.eps_t[:, 0:1]), reads=[bs_, self.eps_t], writes=[dst])
                k.op("dve", lambda e: e.reciprocal(out=dst[:, 0:Tb], in_=dst[:, 0:Tb]), reads=[dst], writes=[dst])
            rms_bcast(rs2)
            if moe:
                bl = [self.bank() for _ in halves]
            for kk in range(8):
                tf = tmpf[kk % 2]
                k.op("dve", lambda e: e.tensor_tensor(out=tf[:, 0:Tb], in0=xT[:, kk, 0:Tb], in1=rs2[:, 0:Tb], op=ALU.mult), reads=[xT, rs2], writes=[tf])
                k.op("act", lambda e: e.activation(out=h2T[:, kk, 0:Tb], in_=tf[:, 0:Tb], func=AF.Identity, scale=self.g2T[:, kk, r:r + 1], bias=self.modT[:, 24 + kk, r:r + 1]),
                     reads=[tf, self.g2T, self.modT], writes=[h2T])
                if moe:
                    k.op("dve", lambda e: e.tensor_scalar(out=tf[:, 0:Tb], in0=tf[:, 0:Tb], scalar1=self.g2T[:, kk, r:r + 1], scalar2=self.modT[:, 24 + kk, r:r + 1], op0=ALU.mult, op1=ALU.add),
                         reads=[tf, self.g2T, self.modT], writes=[tf])
                    for hi, (h0, hw) in enumerate(halves):
                        k.op("pe", lambda e: e.matmul(bl[hi][0:8, 0:hw], lhsT=rt[:, kk, :], rhs=tf[:, h0:h0 + hw], start=(kk == 0), stop=(kk == 7)), reads=[rt, tf], writes=[bl[hi]])
            experts = [None]
            if moe:
                experts = list(range(NE))
                for hi, (h0, hw) in enumerate(halves):
                    k.op("act", lambda e: e.copy(out=lgT[:, h0:h0 + hw], in_=bl[hi][0:8, 0:hw]), reads=[bl[hi]], writes=[lgT])
                bl2 = self.bank()
                for ti in range(nt):
                    k.op("pe", lambda e: e.transpose(out=bl2[:, ti * 8:(ti + 1) * 8], in_=lgT[0:8, ti * 128:(ti + 1) * 128], identity=self.ident_f[0:8, 0:8]), reads=[lgT, self.ident_f], writes=[bl2])
                k.op("dve", lambda e: e.tensor_copy(out=lg[:].rearrange("p a b -> p (a b)"), in_=bl2[:, 0:64]), reads=[bl2], writes=[lg])
                bc3 = lambda ap: ap.unsqueeze(2).to_broadcast([128, 8, NE])
                k.op("dve", lambda e: e.tensor_reduce(out=mx[:, 0:8], in_=lg[:], axis=AX.X, op=ALU.max), reads=[lg], writes=[mx])
                k.op("pool", lambda e: e.tensor_tensor(out=l2[:], in0=lg[:], in1=bc3(mx[:, 0:8]), op=ALU.subtract), reads=[lg, mx], writes=[l2])
                k.op("dve", lambda e: e.tensor_scalar(out=lg[:], in0=l2[:], scalar1=0.0, scalar2=-1e30, op0=ALU.is_equal, op1=ALU.mult), reads=[l2], writes=[lg])
                k.op("dve", lambda e: e.tensor_tensor(out=lg[:], in0=lg[:], in1=l2[:], op=ALU.add), reads=[lg, l2], writes=[lg])
                k.op("dve", lambda e: e.tensor_reduce(out=mx[:, 8:16], in_=lg[:], axis=AX.X, op=ALU.max), reads=[lg], writes=[mx])
                k.op("pool", lambda e: e.tensor_tensor(out=lg[:], in0=l2[:], in1=bc3(mx[:, 8:16]), op=ALU.subtract), reads=[l2, mx], writes=[lg])
                k.op("dve", lambda e: e.tensor_scalar(out=lg[:], in0=lg[:], scalar1=0.0, scalar2=None, op0=ALU.is_ge), reads=[lg], writes=[lg])
                k.op("act", lambda e: e.activation(out=l2[:], in_=l2[:], func=AF.Exp), reads=[l2], writes=[l2])
                k.op("dve", lambda e: e.tensor_tensor(out=l2[:], in0=l2[:], in1=lg[:], op=ALU.mult), reads=[l2, lg], writes=[l2])
                k.op("dve", lambda e: e.tensor_reduce(out=mx[:, 0:8], in_=l2[:], axis=AX.X, op=ALU.add), reads=[l2], writes=[mx])
                k.op("dve", lambda e: e.reciprocal(out=mx[:, 0:8], in_=mx[:, 0:8]), reads=[mx], writes=[mx])
                k.op("pool", lambda e: e.tensor_tensor(out=l2[:], in0=l2[:], in1=bc3(mx[:, 0:8]), op=ALU.mult), reads=[l2, mx], writes=[l2])
                for hi, (h0, hw) in enumerate(halves):
                    bl3 = self.bank()
                    for t4 in range(hw // 128):
                        ti = h0 // 128 + t4
                        k.op("pe", lambda e: e.transpose(out=bl3[0:8, t4 * 128:(t4 + 1) * 128], in_=l2[:, ti, :], identity=self.ident_f[:]), reads=[l2, self.ident_f], writes=[bl3])
                    k.op("act", lambda e: e.copy(out=cbT[:, h0:h0 + hw], in_=bl3[0:8, 0:hw]), reads=[bl3], writes=[cbT])
            for ex_ in experts:
                if moe:
                    wgu_src = I["moe_w_gu"][ex_].rearrange("(k p) n -> p k n", p=128)
                    wdn_src = I["moe_w_down"][ex_].rearrange("(f p) n -> p f n", p=128)
                    cbe = CBe[ex_ % 2]
                    for (h0, hw) in halves:
                        bcb = self.bank()
                        k.op("pe", lambda e: e.matmul(bcb[:, 0:hw], lhsT=selE[0:8, ex_, :], rhs=cbT[0:8, h0:h0 + hw], start=True, stop=True), reads=[selE, cbT], writes=[bcb])
                        k.op("act", lambda e: e.copy(out=cbe[:, h0:h0 + hw], in_=bcb[:, 0:hw]), reads=[bcb], writes=[cbe])
                else:
                    wgu_src = I["ffn_w_gu"].rearrange("(k p) n -> p k n", p=128)
                    wdn_src = I["ffn_w_down"].rearrange("(f p) n -> p f n", p=128)
                si = 0
                for g0 in range(0, F, 512):
                    gw = min(512, F - g0)
                    wgt = wload(wgu_src[:, :, g0:g0 + gw], gw)
                    wut = wload(wgu_src[:, :, F + g0:F + g0 + gw], gw)
                    for c in range(gw // 128):
                        fc = g0 // 128 + c
                        for (h0, hw) in halves:
                            bg_, bu_ = self.bank(), self.bank()
                            for kk in range(8):
                                k.op("pe", lambda e: e.matmul(bg_[:, 0:hw], lhsT=wgt[:, kk, c * 128:(c + 1) * 128], rhs=h2T[:, kk, h0:h0 + hw], start=(kk == 0), stop=(kk == 7)), reads=[wgt, h2T], writes=[bg_])
                            for kk in range(8):
                                k.op("pe", lambda e: e.matmul(bu_[:, 0:hw], lhsT=wut[:, kk, c * 128:(c + 1) * 128], rhs=h2T[:, kk, h0:h0 + hw], start=(kk == 0), stop=(kk == 7)), reads=[wut, h2T], writes=[bu_])
                            s_ = sg[si % 2]
                            si += 1
                            k.op("act", lambda e: e.activation(out=s_[:, 0:hw], in_=bg_[:, 0:hw], func=AF.Silu), reads=[bg_], writes=[s_])
                            if moe:
                                k.op("dve", lambda e: e.tensor_tensor(out=s_[:, 0:hw], in0=s_[:, 0:hw], in1=cbe[:, h0:h0 + hw], op=ALU.mult), reads=[s_, cbe], writes=[s_])
                            k.op("dve", lambda e: e.tensor_tensor(out=actT[:, fc, h0:h0 + hw], in0=bu_[:, 0:hw], in1=s_[:, 0:hw], op=ALU.mult), reads=[bu_, s_], writes=[actT])
                nf = F // 128
                for ocg in range(2):
                    banks = [[self.bank() for _ in range(4)] for _ in halves]
                    for f0 in range(0, nf, 4):
                        wdt = wd[wi[0] % 2]
                        wi[0] += 1
                        n4 = min(4, nf - f0)
                        k.dma("pool", wdt[:, 0:n4, 0:512], wdn_src[:, f0:f0 + n4, ocg * 512:(ocg + 1) * 512], writes=[wdt], max_dma_last_dim=2048)
                        for hi, (h0, hw) in enumerate(halves):
                            for q in range(4):
                                for fi in range(n4):
                                    f = f0 + fi
                                    k.op("pe", lambda e: e.matmul(banks[hi][q][:, 0:hw], lhsT=wdt[:, fi, q * 128:(q + 1) * 128], rhs=actT[:, f, h0:h0 + hw], start=(f == 0), stop=(f == nf - 1)),
                                         reads=[wdt, actT], writes=[banks[hi][q]])
                    for hi, (h0, hw) in enumerate(halves):
                        for q in range(4):
                            oc = ocg * 4 + q
                            k.op("dve", lambda e: e.scalar_tensor_tensor(out=xT[:, oc, h0:h0 + hw], in0=banks[hi][q][:, 0:hw], scalar=self.modT[:, 40 + oc, r:r + 1], in1=xT[:, oc, h0:h0 + hw], op0=ALU.mult, op1=ALU.add),
                                 reads=[banks[hi][q], self.modT, xT], writes=[xT])
            if last:
                rms_bcast(rs2)
                for kk in range(8):
                    k.op("dve", lambda e: e.scalar_tensor_tensor(out=xT[:, kk, 0:Tb], in0=xT[:, kk, 0:Tb], scalar=self.fnT[:, kk, 0:1], in1=rs2[:, 0:Tb], op0=ALU.mult, op1=ALU.mult),
                         reads=[xT, self.fnT, rs2], writes=[xT])
            for ti in range(nt):
                i = t0 + ti
                tsl = slice(ti * 128, (ti + 1) * 128)
                for half in range(2):
                    b2 = self.bank()
                    for q in range(4):
                        kk = half * 4 + q
                        k.op("pe", lambda e: e.transpose(out=b2[:, q * 128:(q + 1) * 128], in_=xT[:, kk, tsl], identity=self.ident_f[:]), reads=[xT, self.ident_f], writes=[b2])
                    k.op("act", lambda e: e.copy(out=xo[:, half * 512:(half + 1) * 512], in_=b2[:, :]), reads=[b2], writes=[xo])
                if last:
                    k.dma("sp", self.out[(i - 2) * 128:(i - 1) * 128, :], xo[:], reads=[xo], writes=[self.out_dep])
                else:
                    k.dma("sp", S["x1"][i * 128:(i + 1) * 128, :], xo[:], reads=[xo], writes=[self.x1_dep[i]])

    def build(self):
        self.declare()
        k = self.k
        self.consts()
        self.eps_t = k.sb([128, 1], F32, "eps_t")
        k.op("pool", lambda e: e.memset(self.eps_t[:], EPS), writes=[self.eps_t])
        self.modT = k.sb([128, 48, 2], F32, "modT")
        self.nT = k.sb([128, 8, 2], F32, "nT")
        self.g1T = k.sb([128, 8, 2], F32, "g1T")
        self.g2T = k.sb([128, 8, 2], F32, "g2T")
        self.fnT = k.sb([128, 8, 1], F32, "fnT")
        self.scT = k.sb([128, 8, 2], BF16, "scT")
        self.xs_dep = [DR() for _ in range(NT)]
        self.xcur, self.xcur_dep = self.I["xs"], self.xs_dep
        for l in range(self.nlayers):
            if "p0" in self.phases:
                with k.scope():
                    self.p0_alloc()
                    self.p0(l)
            if "p1" in self.phases:
                with k.scope():
                    self.p1_alloc()
                    self.p1(l, self.xcur, self.xcur_dep)
            if "p2" in self.phases:
                with k.scope():
                    self.p2(l)
            if "p3" in self.phases:
                with k.scope():
                    self.p3(l)
            if "p4" in self.phases:
                with k.scope():
                    self.p4(l)
                self.xcur, self.xcur_dep = self.S["x1"], self.x1_dep
        k.barrier()
        self.es.close()
        return self.nc


def _core_inputs(inp, core):
    b, j = core // 2, core % 2
    f = (lambda a: a[::-1]) if j else (lambda a: a)
    d = {}
    d["xs"] = np.ascontiguousarray(np.concatenate([f(inp["ctx"][b]), f(inp["x"][b])], axis=0))
    d["cvec"] = np.ascontiguousarray(np.stack([inp["c"][b], inp["c_ctx"]], axis=0))
    w_in = inp["w_in"]
    if j:
        w_in = w_in.copy()
        for base, n in ((2048, 4), (2056, 4), (3600, 16)):
            a = w_in[:, :, base:base + n].copy()
            w_in[:, :, base:base + n] = w_in[:, :, base + n:base + 2 * n]
            w_in[:, :, base + n:base + 2 * n] = a
    d["w_in"] = np.ascontiguousarray(w_in)
    sw = (lambda a: a[:, ::-1]) if j else (lambda a: a)
    d["conv"] = np.ascontiguousarray(sw(inp["conv_qkv"]))
    d["dn_a_log"] = np.ascontiguousarray(sw(inp["dn_a_log"]).reshape(2, 8))
    d["dn_dt_bias"] = np.ascontiguousarray(sw(inp["dn_dt_bias"]).reshape(2, 8))
    d["gla_w_gate2"] = np.ascontiguousarray(sw(inp["gla_w_gate2"]))
    d["gla_b_gate"] = np.ascontiguousarray(sw(inp["gla_b_gate"]))
    for n in ("w_ada", "b_ada", "norm_mix", "norm_ffn", "dn_norm", "gla_norm", "w_out", "final_norm"):
        d[n] = np.ascontiguousarray(inp[n])
    d["ffn_w_gu"] = np.ascontiguousarray(inp["ffn_w_gu"][0])
    d["ffn_w_down"] = np.ascontiguousarray(inp["ffn_w_down"][0])
    d["moe_router"] = np.ascontiguousarray(inp["moe_router"][0])
    d["moe_w_gu"] = np.ascontiguousarray(inp["moe_w_gu"][0])
    d["moe_w_down"] = np.ascontiguousarray(inp["moe_w_down"][0])
    return d


def kernel(**inputs):
    inp = {k_: np.asarray(v, dtype=np.float32) for k_, v in inputs.items()}
    prog = Prog()
    nc = prog.build()
    in_maps = []
    for core in range(8):
        ci = _core_inputs(inp, core)
        in_maps.append({n: ci[n] for n in prog.I})
    res = run_bass_kernel_spmd(nc, in_maps, core_ids=list(range(8)))
    out = np.empty((4, LL, D), np.float32)
    for core in range(8):
        b, j = core // 2, core % 2
        y = np.asarray(res.results[core]["out"], dtype=np.float32)
        if j == 0:
            out[b, 0:NOWN] = y
        else:
            out[b, LL - NOWN:LL] = y[::-1]
    return out
```

```python
import numpy as np
from contextlib import ExitStack
import concourse.bass as bass
import concourse.mybir as mybir
from concourse.bass_utils import run_bass_kernel_spmd

F32 = mybir.dt.float32
BF16 = mybir.dt.bfloat16
AF = mybir.ActivationFunctionType
ALU = mybir.AluOpType
AX = mybir.AxisListType

D = 1024
LC = 256
LL = 8192
LS = LC + LL
NT = LS // 128
IN_COLS = 3632
NTM = IN_COLS - 1536
EPS = 1e-6
D_FF = 2816
D_EXP = 3584
NE = 8
NOWN = 4096


class Dep:
    __slots__ = ("w", "r")

    def __init__(self):
        self.w = None
        self.r = {}


class Tile:
    excl = False

    def __init__(self, t):
        self.t = t
        self.dep = Dep()

    def __getitem__(self, idx):
        return self.t[idx]


class K:
    RING = 64

    def __init__(self, nc, es):
        self.nc = nc
        self.es = es
        self.E = {"pe": nc.tensor, "dve": nc.vector, "act": nc.scalar, "pool": nc.gpsimd, "sp": nc.sync}
        self.sems = {}
        for e in self.E:
            self.sems[e] = es.enter_context(nc.semaphore("sem_" + e))
        for i in range(self.RING):
            self.sems[("d", i)] = es.enter_context(nc.semaphore("dsem%d" % i))
        self.cnt = {e: 0 for e in self.E}
        self.seen = {e: {} for e in self.E}
        self.dn = 0
        self.dtar = [0] * self.RING
        self.nt = 0

    def sb(self, shape, dt, name=None):
        self.nt += 1
        return Tile(self.es.enter_context(self.nc.sbuf_tensor("%s_%d" % (name or "t", self.nt), list(shape), dt)))

    def psum(self, shape, dt, name=None):
        self.nt += 1
        t = Tile(self.es.enter_context(self.nc.psum_tensor(name or ("p%d" % self.nt), list(shape), dt)))
        t.excl = True
        return t

    def _wait(self, eng, tok):
        if tok is None:
            return
        key, val = tok
        if key == eng and eng == "pe":
            return
        if self.seen[eng].get(key, 0) >= val:
            return
        self.E[eng].wait_ge(self.sems[key], val)
        self.seen[eng][key] = val

    def _pre(self, eng, reads, writes):
        for d in reads:
            self._wait(eng, d.dep.w)
        for d in writes:
            self._wait(eng, d.dep.w)
            for tok in d.dep.r.values():
                self._wait(eng, tok)

    def _post(self, tok, reads, writes):
        for d in reads:
            d.dep.r[tok[0]] = tok
        for d in writes:
            d.dep.w = tok
            d.dep.r = {}

    def op(self, eng, fn, reads=(), writes=()):
        ex = [d for d in reads if getattr(d, "excl", False)]
        if ex:
            writes = list(writes) + ex
            reads = [d for d in reads if not getattr(d, "excl", False)]
        self._pre(eng, reads, writes)
        ins = fn(self.E[eng])
        ins.then_inc(self.sems[eng], 1)
        self.cnt[eng] += 1
        self._post((eng, self.cnt[eng]), reads, writes)

    def dma(self, q, out, in_, reads=(), writes=(), **kw):
        self._pre(q, reads, writes)
        slot = self.dn % self.RING
        self.dn += 1
        key = ("d", slot)
        if self.dtar[slot] > 0:
            self._wait(q, (key, self.dtar[slot]))
        self.dtar[slot] += 16
        self.E[q].dma_start(out=out, in_=in_, **kw).then_inc(self.sems[key], 16)
        self._post((key, self.dtar[slot]), reads, writes)

    def barrier(self):
        for e in self.E:
            for e2 in self.E:
                if e2 != e and self.cnt[e2] > 0:
                    self._wait(e, (e2, self.cnt[e2]))
            for s in range(self.RING):
                if self.dtar[s] > 0:
                    self._wait(e, (("d", s), self.dtar[s]))

    def scope(self):
        outer = self.es
        k = self

        class _S:
            def __enter__(s):
                k.es = ExitStack()
                return s

            def __exit__(s, *a):
                k.barrier()
                k.es.close()
                k.es = outer
                return False
        return _S()

    def finish(self, deps):
        for d in deps:
            self._wait("sp", d.dep.w)


class DR:
    def __init__(self):
        self.dep = Dep()


class Prog:
    def __init__(self, debug=(), nlayers=2, phases=("p0", "p1", "p2", "p3", "p4"), ntiles=NT):
        self.debug = tuple(debug)
        self.nlayers = nlayers
        self.phases = phases
        self.ntiles = ntiles
        self.p2_stop = 9
        nc = self.nc = bass.Bass("TRN2", target_bir_lowering=False)
        self.I = {}
        self.S = {}
        self.es = ExitStack()
        self.k = K(nc, self.es)

    def inp(self, name, shape):
        self.I[name] = self.nc.dram_tensor(name, list(shape), F32, kind="ExternalInput").ap()
        return self.I[name]

    def scr(self, name, shape, dt=F32):
        kind = "ExternalOutput" if name in self.debug else "Internal"
        self.S[name] = self.nc.dram_tensor(name, list(shape), dt, kind=kind).ap()
        return self.S[name]

    def declare(self):
        inp = self.inp
        inp("xs", [LS, D]); inp("cvec", [2, D]); inp("w_ada", [2, D, 6 * D]); inp("b_ada", [2, 6 * D])
        inp("norm_mix", [2, D]); inp("norm_ffn", [2, D]); inp("w_in", [2, D, IN_COLS]); inp("conv", [2, 5, 1536])
        inp("dn_a_log", [2, 8]); inp("dn_dt_bias", [2, 8]); inp("dn_norm", [2, 128])
        inp("gla_w_gate2", [2, 2, 16, 256]); inp("gla_b_gate", [2, 2, 256]); inp("gla_norm", [2, 128])
        if "p4" in self.phases:
            inp("w_out", [2, D, D]); inp("ffn_w_gu", [D, 2 * D_FF]); inp("ffn_w_down", [D_FF, D])
            if self.nlayers > 1:
                inp("moe_router", [D, NE]); inp("moe_w_gu", [NE, D, 2 * D_EXP]); inp("moe_w_down", [NE, D_EXP, D])
        inp("final_norm", [D])
        self.out = self.nc.dram_tensor("out", [NOWN, D], F32, kind="ExternalOutput").ap()
        self.out_dep = DR()
        self.scr("tm", [LS, NTM]); self.scr("qkvT", [1536, LS + 8], BF16)
        self.scr("o_dn", [2, LS, 512]); self.scr("o_gla", [2, LS, 512]); self.scr("x1", [LS, D])
        self.x1_dep = [DR() for _ in range(NT)]
        self.odn_dep = [[DR() for _ in range(NT)] for _ in range(2)]
        self.ogla_dep = [[DR() for _ in range(NT)] for _ in range(2)]
        self.tm_dep = [DR() for _ in range(NT)]
        self.qkv_dep = [DR() for _ in range(NT)]
        self.qkv_pad_dep = DR()

    def dbg(self, name, tile, shape, dt=F32):
        if name not in self.debug or name in self.S:
            return
        if len(shape) == 2 and len(tile.t.shape) == 4:
            ap = self.nc.dram_tensor(name, list(shape), dt, kind="ExternalOutput").ap()
            self.S[name] = ap
            self.k.dma("sp", ap, tile[:].rearrange("p a b c -> p (a b c)"), reads=[tile], writes=[DR()])
            return
        ap = self.nc.dram_tensor(name, list(shape), dt, kind="ExternalOutput").ap()
        self.S[name] = ap
        src_ap = tile[:]
        if len(shape) == 2 and len(tile.t.shape) == 3:
            src_ap = tile[:].rearrange("p a b -> p (a b)")
        self.k.dma("sp", ap, src_ap, reads=[tile], writes=[DR()])

    def consts(self):
        k = self.k
        self.ident_f = k.sb([128, 128], F32, "ident_f")
        self.ident_b = k.sb([128, 128], BF16, "ident_b")
        self.ones_b = k.sb([128, 128], BF16, "ones_b")
        self.ones_f = k.sb([128, 128], F32, "ones_f")
        self.zeros_f = k.sb([128, 8], BF16, "zeros_b")
        idf, idb = self.ident_f, self.ident_b
        k.op("pool", lambda e: e.memset(idf[:], 1.0), writes=[idf])
        k.op("pool", lambda e: e.affine_select(out=idf[:], in_=idf[:], pattern=[[1, 128]], compare_op=ALU.is_equal,
                                                fill=0.0, base=0, channel_multiplier=-1), reads=[idf], writes=[idf])
        k.op("pool", lambda e: e.tensor_copy(out=idb[:], in_=idf[:]), reads=[idf], writes=[idb])
        k.op("pool", lambda e: e.memset(self.ones_b[:], 1.0), writes=[self.ones_b])
        k.op("pool", lambda e: e.memset(self.ones_f[:], 1.0), writes=[self.ones_f])
        k.op("pool", lambda e: e.memset(self.zeros_f[:], 0.0), writes=[self.zeros_f])
        self.ps = [k.psum([128, 512], F32, "ps%d" % i) for i in range(8)]
        self.psi = 0

    def bank(self):
        b = self.ps[self.psi % 8]
        self.psi += 1
        return b

    def rows_to_cols(self, rows, R, n, dst):
        k = self.k
        b = self.bank()
        for c in range(n):
            k.op("pe", lambda e, c=c: e.transpose(out=b[:, c * R:(c + 1) * R], in_=rows[0:R, c * 128:(c + 1) * 128],
                                                  identity=self.ident_f[0:R, 0:R]), reads=[rows, self.ident_f], writes=[b])
        k.op("dve", lambda e: e.tensor_copy(out=dst[:].rearrange("p n r -> p (n r)"), in_=b[:, 0:n * R]), reads=[b], writes=[dst])

    def p0_alloc(self):
        k = self.k
        self.crow = k.sb([2, D], F32, "crow")
        self.cT = k.sb([128, 8, 2], F32, "cT")
        self.wada = [k.sb([128, 8, 512], BF16, "wada%d" % i) for i in range(2)]
        self.modrow = k.sb([2, 6 * D], F32, "modrow")
        self.brow = k.sb([2, 6 * D], F32, "brow")
        self.nrow = k.sb([2, D], F32, "nrow")
        self.fnrow = k.sb([1, D], F32, "fnrow")

    def p0(self, l):
        k, I = self.k, self.I
        crow, cT, scT, modrow, brow, modT = self.crow, self.cT, self.scT, self.modrow, self.brow, self.modT
        if l == 0:
            k.dma("sp", crow[:], I["cvec"], writes=[crow])
            self.rows_to_cols(crow, 2, 8, cT)
            k.op("act", lambda e: e.activation(out=scT[:], in_=cT[:], func=AF.Silu), reads=[cT], writes=[scT])
            k.dma("sp", self.fnrow[:], I["final_norm"].rearrange("(o n) -> o n", o=1), writes=[self.fnrow])
            self.rows_to_cols(self.fnrow, 1, 8, self.fnT)
        for r in range(2):
            k.dma("sp", brow[r:r + 1, :], I["b_ada"][l:l + 1, :], writes=[brow])
        k.dma("sp", self.nrow[0:1, :], I["norm_mix"][l:l + 1, :], writes=[self.nrow])
        k.dma("sp", self.nrow[1:2, :], I["norm_ffn"][l:l + 1, :], writes=[self.nrow])
        self.rows_to_cols(self.nrow, 2, 8, self.nT)
        wsrc = I["w_ada"][l].rearrange("(k p) n -> p k n", p=128)
        for g in range(12):
            wt = self.wada[g % 2]
            k.dma("pool", wt[:], wsrc[:, :, g * 512:(g + 1) * 512], writes=[wt], max_dma_last_dim=2048)
            b = self.bank()
            for kk in range(8):
                k.op("pe", lambda e, kk=kk: e.matmul(b[0:2, :], lhsT=scT[:, kk, :], rhs=wt[:, kk, :], start=(kk == 0), stop=(kk == 7)),
                     reads=[scT, wt], writes=[b])
            k.op("dve", lambda e, g=g: e.tensor_tensor(out=modrow[:, g * 512:(g + 1) * 512], in0=b[0:2, :], in1=brow[:, g * 512:(g + 1) * 512], op=ALU.add),
                 reads=[b, brow], writes=[modrow])
        self.rows_to_cols(modrow, 2, 48, modT)
        for r in range(2):
            k.op("dve", lambda e, r=r: e.scalar_tensor_tensor(out=self.g1T[:, :, r], in0=modT[:, 8:16, r], scalar=1.0, in1=self.nT[:, :, 0], op0=ALU.add, op1=ALU.mult),
                 reads=[modT, self.nT], writes=[self.g1T])
            k.op("dve", lambda e, r=r: e.scalar_tensor_tensor(out=self.g2T[:, :, r], in0=modT[:, 32:40, r], scalar=1.0, in1=self.nT[:, :, 1], op0=ALU.add, op1=ALU.mult),
                 reads=[modT, self.nT], writes=[self.g2T])

    def p1_alloc(self):
        k = self.k
        self.w_fm = k.sb([128, 8, 1536], BF16, "w_fm")
        self.w_tm = k.sb([128, 8, NTM], BF16, "w_tm")
        self.xt = [k.sb([128, D], F32, "xt%d" % i) for i in range(2)]
        self.junk = k.sb([128, D], BF16, "junk")
        self.ss = [k.sb([128, 2], F32, "ss%d" % i) for i in range(2)]
        self.xn = [k.sb([128, D], BF16, "xn%d" % i) for i in range(2)]
        self.hT = [k.sb([128, 8, 512], BF16, "hT%d" % i) for i in range(2)]
        self.tmo = [k.sb([128, NTM], F32, "tmo%d" % i) for i in range(2)]
        self.qo = [k.sb([128, 512], BF16, "qo%d" % i) for i in range(3)]

    def p1(self, l, xsrc, xsrc_dep):
        k, I, S = self.k, self.I, self.S
        wsrc = I["w_in"][l].rearrange("(k p) n -> p k n", p=128)
        for kk in range(8):
            k.dma("pool", self.w_fm[:, kk, :], wsrc[:, kk, 0:1536], writes=[self.w_fm], max_dma_last_dim=2048)
            k.dma("pool", self.w_tm[:, kk, :], wsrc[:, kk, 1536:IN_COLS], writes=[self.w_tm], max_dma_last_dim=2048)
        if l == 0:
            for (c0, n) in ((0, 2), (258, 4), (LS + 6, 2)):
                for ch in range(12):
                    k.dma("sp", S["qkvT"][ch * 128:(ch + 1) * 128, c0:c0 + n], self.zeros_f[:, 0:n], reads=[self.zeros_f], writes=[self.qkv_pad_dep])
        blocks = [(0, 2)] + [(2 + 4 * i, 4) for i in range(16)]
        evi = 0
        for bi, (t0, nt) in enumerate(blocks):
            if t0 >= self.ntiles:
                break
            hT = self.hT[bi % 2]
            T = nt * 128
            r = 1 if t0 < 2 else 0
            for ti in range(nt):
                i = t0 + ti
                x_t, ss, xn = self.xt[i % 2], self.ss[i % 2], self.xn[i % 2]
                k.dma("sp", x_t[:], xsrc[i * 128:(i + 1) * 128, :], reads=[xsrc_dep[i]], writes=[x_t])
                k.op("act", lambda e: e.activation(out=self.junk[:], in_=x_t[:], func=AF.Square, accum_out=ss[:, 0:1]), reads=[x_t], writes=[self.junk, ss])
                k.op("act", lambda e: e.activation(out=ss[:, 1:2], in_=ss[:, 0:1], func=AF.Sqrt, scale=1.0 / D, bias=self.eps_t[:, 0:1]), reads=[ss, self.eps_t], writes=[ss])
                k.op("dve", lambda e: e.reciprocal(out=ss[:, 1:2], in_=ss[:, 1:2]), reads=[ss], writes=[ss])
                k.op("dve", lambda e: e.tensor_scalar(out=xn[:], in0=x_t[:], scalar1=ss[:, 1:2], scalar2=None, op0=ALU.mult), reads=[x_t, ss], writes=[xn])
                b = self.bank()
                bb = b.t[:, :].bitcast(BF16)
                for kk in range(8):
                    k.op("pe", lambda e, kk=kk: e.transpose(out=bb[:, kk * 128:(kk + 1) * 128], in_=xn[:, kk * 128:(kk + 1) * 128], identity=self.ident_b[:]),
                         reads=[xn, self.ident_b], writes=[b])
                for kk in range(8):
                    if kk % 2 == 0:
                        k.op("act", lambda e, kk=kk: e.activation(out=hT[:, kk, ti * 128:(ti + 1) * 128], in_=bb[:, kk * 128:(kk + 1) * 128], func=AF.Identity,
                                                                  scale=self.g1T[:, kk, r:r + 1], bias=self.modT[:, kk, r:r + 1]),
                             reads=[b, self.g1T, self.modT], writes=[hT])
                    else:
                        k.op("dve", lambda e, kk=kk: e.tensor_scalar(out=hT[:, kk, ti * 128:(ti + 1) * 128], in0=bb[:, kk * 128:(kk + 1) * 128],
                                                                     scalar1=self.g1T[:, kk, r:r + 1], scalar2=self.modT[:, kk, r:r + 1], op0=ALU.mult, op1=ALU.add),
                             reads=[b, self.g1T, self.modT], writes=[hT])
                tmo = self.tmo[i % 2]
                for cg in range(5):
                    n = min(512, NTM - cg * 512)
                    b2 = self.bank()
                    for kk in range(8):
                        k.op("pe", lambda e, kk=kk: e.matmul(b2[:, 0:n], lhsT=hT[:, kk, ti * 128:(ti + 1) * 128], rhs=self.w_tm[:, kk, cg * 512:cg * 512 + n],
                                                             start=(kk == 0), stop=(kk == 7)), reads=[hT, self.w_tm], writes=[b2])
                    evi += 1
                    if evi % 2:
                        k.op("act", lambda e: e.copy(out=tmo[:, cg * 512:cg * 512 + n], in_=b2[:, 0:n]), reads=[b2], writes=[tmo])
                    else:
                        k.op("dve", lambda e: e.tensor_copy(out=tmo[:, cg * 512:cg * 512 + n], in_=b2[:, 0:n]), reads=[b2], writes=[tmo])
                k.dma("pool", S["tm"][i * 128:(i + 1) * 128, :], tmo[:], reads=[tmo], writes=[self.tm_dep[i]])
            col0 = t0 * 128 + (2 if t0 < 2 else 6)
            for ch in range(12):
                b3 = self.bank()
                for kk in range(8):
                    k.op("pe", lambda e, kk=kk: e.matmul(b3[:, 0:T], lhsT=self.w_fm[:, kk, ch * 128:(ch + 1) * 128], rhs=hT[:, kk, 0:T], start=(kk == 0), stop=(kk == 7)),
                         reads=[hT, self.w_fm], writes=[b3])
                qo = self.qo[ch % 3]
                evi += 1
                if evi % 2:
                    k.op("act", lambda e: e.copy(out=qo[:, 0:T], in_=b3[:, 0:T]), reads=[b3], writes=[qo])
                else:
                    k.op("dve", lambda e: e.tensor_copy(out=qo[:, 0:T], in_=b3[:, 0:T]), reads=[b3], writes=[qo])
                k.dma("pool", S["qkvT"][ch * 128:(ch + 1) * 128, col0:col0 + T], qo[:, 0:T], reads=[qo], writes=[self.qkv_dep[t0 + j] for j in range(nt)])


    def dir_masks(self, d):
        k = self.k
        mI = k.sb([128, 4, 128], BF16, "maskI")
        mS = k.sb([128, 4, 128], BF16, "maskS")
        tri = k.sb([128, 128], F32, "tri")
        sgn = 1 if d == 0 else -1
        for (m, cmp) in ((mI, ALU.is_ge), (mS, ALU.is_gt)):
            k.op("pool", lambda e: e.memset(m[:], 1.0), writes=[m])
            k.op("pool", lambda e: e.affine_select(out=m[:], in_=m[:], pattern=[[0, 4], [sgn, 128]], compare_op=cmp, fill=0.0,
                                                    base=0, channel_multiplier=-sgn), reads=[m], writes=[m])
        k.op("pool", lambda e: e.tensor_copy(out=tri[:], in_=mI[:, 0, :]), reads=[mI], writes=[tri])
        return mI, mS, tri

    def bcast_row(self, src_row_ap, n, dst):
        self.k.dma("sp", dst[:, 0:n], src_row_ap.broadcast_to([128, n]), writes=[dst])

    @staticmethod
    def interleave(gens):
        gens = list(gens)
        while gens:
            nxt = []
            for g in gens:
                try:
                    next(g)
                    nxt.append(g)
                except StopIteration:
                    pass
            gens = nxt

    def p2(self, l):
        k, I, S = self.k, self.I, self.S
        sb = k.sb
        flat = lambda t: t[:].rearrange("p a b -> p (a b)")
        convrow = sb([5, 1536], F32, "convrow")
        cw = sb([128, 12, 5], F32, "cw")
        k.dma("sp", convrow[:], I["conv"][l], writes=[convrow])
        self.rows_to_cols(convrow, 5, 12, cw)
        dtb = sb([128, 8], F32, "dtb")
        nA = sb([128, 8], F32, "nA")
        self.bcast_row(I["dn_dt_bias"][l:l + 1, :], 8, dtb)
        self.bcast_row(I["dn_a_log"][l:l + 1, :], 8, nA)
        k.op("act", lambda e: e.activation(out=nA[:], in_=nA[:], func=AF.Exp), reads=[nA], writes=[nA])
        k.op("dve", lambda e: e.tensor_scalar(out=nA[:], in0=nA[:], scalar1=-1.0, scalar2=None, op0=ALU.mult), reads=[nA], writes=[nA])
        ident4b = sb([128, 4, 128], BF16, "ident4b")
        for h in range(4):
            k.op("pool", lambda e: e.tensor_copy(out=ident4b[:, h, :], in_=self.ident_f[:]), reads=[self.ident_f], writes=[ident4b])
        qeps = sb([128, 1], F32, "qeps")
        k.op("pool", lambda e: e.memset(qeps[:], 128.0 * EPS), writes=[qeps])
        bd32 = sb([128, 4, 128], BF16, "bd32"); m1 = sb([128, 4, 128], BF16, "m1"); m2 = sb([128, 4, 128], BF16, "m2")
        for (bs_, nb_, dst) in ((32, 4, bd32), (64, 2, m1)):
            E = sb([4, 128], F32, "Eblk%d" % bs_)
            k.op("pool", lambda e: e.memset(E[:], 1.0), writes=[E])
            k.op("pool", lambda e: e.affine_select(out=E[:], in_=E[:], pattern=[[1, 128]], compare_op=ALU.is_ge, fill=0.0, base=0, channel_multiplier=-bs_), reads=[E], writes=[E])
            k.op("pool", lambda e: e.affine_select(out=E[:], in_=E[:], pattern=[[-1, 128]], compare_op=ALU.is_ge, fill=0.0, base=bs_ - 1, channel_multiplier=bs_), reads=[E], writes=[E])
            bm = self.bank()
            k.op("pe", lambda e: e.matmul(bm[:, 0:128], lhsT=E[0:nb_, :], rhs=E[0:nb_, :], start=True, stop=True), reads=[E], writes=[bm])
            for h in range(4):
                k.op("dve", lambda e: e.tensor_copy(out=dst[:, h, :], in_=bm[:, 0:128]), reads=[bm], writes=[dst])
        k.op("dve", lambda e: e.tensor_scalar(out=m2[:], in0=m1[:], scalar1=-1.0, scalar2=1.0, op0=ALU.mult, op1=ALU.add), reads=[m1], writes=[m2])
        k.op("dve", lambda e: e.tensor_tensor(out=m1[:], in0=m1[:], in1=bd32[:], op=ALU.subtract), reads=[m1, bd32], writes=[m1])
        raw = sb([128, 4, 516], BF16, "raw")
        diag = [sb([128, 5, 128], BF16, "diag%d" % i) for i in range(2)]
        acc = [sb([128, 512], F32, "acc%d" % i) for i in range(2)]
        sil = [sb([128, 512], F32, "sil%d" % i) for i in range(2)]
        sq = [sb([128, 512], BF16, "sq0")] * 2
        rn = [sb([128, 512], F32, "rn0")] * 2
        KQT = [sb([128, 4, 2, 512], BF16, "KQT%d" % i) for i in range(2)]
        VT = [sb([128, 4, 512], BF16, "VT%d" % i) for i in range(2)]
        KVtm = [[sb([128, 8, 128], BF16, "KVtm%d_%d" % (j, i)) for i in range(4)] for j in range(2)]
        def mk(n, shape, dt, name):
            return [sb(shape, dt, "%s%d" % (name, i)) for i in range(n)]
        sc = mk(2, [128, 16], F32, "sc")
        Gd = mk(2, [128, 4, 128], F32, "Gd")
        Dm = mk(2, [128, 4, 128], F32, "Dm")
        Eg = mk(2, [128, 4, 128], F32, "Eg")
        A = mk(2, [128, 4, 128], BF16, "A"); AT = mk(2, [128, 4, 128], BF16, "AT")
        Abd = mk(2, [128, 4, 128], BF16, "Abd"); ATbd = mk(2, [128, 4, 128], BF16, "ATbd")
        Xa = mk(2, [128, 4, 128], BF16, "Xa"); XTa = mk(2, [128, 4, 128], BF16, "XTa")
        YT = mk(2, [128, 4, 128], BF16, "YT")
        R0 = mk(2, [128, 4, 128], BF16, "R0"); R1 = mk(2, [128, 4, 128], BF16, "R1")
        B1T = mk(2, [128, 4, 128], BF16, "B1T"); B2T = mk(2, [128, 4, 128], BF16, "B2T")
        Keg = mk(2, [128, 4, 128], BF16, "Keg")
        def mk2(shape, dt, name):
            return [[sb(shape, dt, "%s%d_%d" % (name, j, i)) for i in range(4)] for j in range(2)]
        sm = mk2([128, 32], F32, "sm")
        attnT = mk2([128, 4, 128], BF16, "attnT")
        up = mk2([128, 4, 128], F32, "up")
        wT = mk2([128, 4, 128], BF16, "wT")
        kdec = mk2([128, 4, 128], BF16, "kdec")
        qdecT = mk2([128, 4, 128], BF16, "qdecT")
        vnew = [sb([128, 4, 128], BF16, "vnew%d" % i) for i in range(2)]
        osb = [sb([128, 4, 128], F32, "osb%d" % i) for i in range(2)]
        Sf = sb([128, 4, 128], F32, "Sf")
        Sb = sb([128, 4, 128], BF16, "Sb")

        def stage_a(bk, bp, t0, nt):
            T = nt * 128
            col0 = t0 * 128 + (2 if t0 < 2 else 6)
            kq, vt = KQT[bp], VT[bp]
            for cg in range(3):
                k.dma("sp", raw[:, :, 0:T + 4], S["qkvT"][cg * 512:(cg + 1) * 512, col0 - 2:col0 + T + 2].rearrange("(c p) t -> p c t", p=128),
                      reads=[self.qkv_dep[i] for i in range(max(t0 - 1, 0), min(t0 + nt + 1, NT))] + [self.qkv_pad_dep], writes=[raw])
                for c4 in range(4):
                    ch = cg * 4 + c4
                    dg = diag[ch % 2]
                    t1, s_, q_, r_ = acc[ch % 2], sil[ch % 2], sq[0], rn[0]
                    for j in range(5):
                        k.op("dve", lambda e: e.tensor_scalar(out=dg[:, j, :], in0=self.ident_b[:], scalar1=cw[:, ch, j:j + 1], scalar2=None, op0=ALU.mult),
                             reads=[self.ident_b, cw], writes=[dg])
                    yield
                    bc_ = bk()
                    for j in range(5):
                        k.op("pe", lambda e: e.matmul(bc_[:, 0:T], lhsT=dg[:, j, :], rhs=raw[:, c4, j:j + T], start=(j == 0), stop=(j == 4)), reads=[dg, raw], writes=[bc_])
                    yield
                    k.op("act", lambda e: e.activation(out=t1[:, 0:T], in_=bc_[:, 0:T], func=AF.Exp, scale=-1.0), reads=[bc_], writes=[t1])
                    yield
                    k.op("act", lambda e: e.activation(out=t1[:, 0:T], in_=t1[:, 0:T], func=AF.Ln, bias=1.0), reads=[t1], writes=[t1])
                    yield
                    k.op("act", lambda e: e.activation(out=t1[:, 0:T], in_=t1[:, 0:T], func=AF.Exp, scale=-1.0), reads=[t1], writes=[t1])
                    yield
                    if ch >= 8:
                        k.op("dve", lambda e: e.tensor_tensor(out=vt[:, ch - 8, 0:T], in0=bc_[:, 0:T], in1=t1[:, 0:T], op=ALU.mult), reads=[bc_, t1], writes=[vt])
                        yield
                        continue
                    k.op("dve", lambda e: e.tensor_tensor(out=s_[:, 0:T], in0=bc_[:, 0:T], in1=t1[:, 0:T], op=ALU.mult), reads=[bc_, t1], writes=[s_])
                    yield
                    k.op("act", lambda e: e.activation(out=q_[:, 0:T], in_=s_[:, 0:T], func=AF.Square), reads=[s_], writes=[q_])
                    yield
                    b = bk()
                    k.op("pe", lambda e: e.matmul(b[:, 0:T], lhsT=self.ones_b[:], rhs=q_[:, 0:T], start=True, stop=True), reads=[self.ones_b, q_], writes=[b])
                    if ch < 4:
                        k.op("act", lambda e: e.activation(out=r_[:, 0:T], in_=b[:, 0:T], func=AF.Ln, scale=128.0, bias=qeps[:, 0:1]), reads=[b, qeps], writes=[r_])
                    else:
                        k.op("act", lambda e: e.activation(out=r_[:, 0:T], in_=b[:, 0:T], func=AF.Ln, scale=1.0, bias=self.eps_t[:, 0:1]), reads=[b, self.eps_t], writes=[r_])
                    yield
                    k.op("act", lambda e: e.activation(out=r_[:, 0:T], in_=r_[:, 0:T], func=AF.Exp, scale=-0.5), reads=[r_], writes=[r_])
                    yield
                    hh, which = (ch, 1) if ch < 4 else (ch - 4, 0)
                    k.op("dve", lambda e: e.tensor_tensor(out=kq[:, hh, which, 0:T], in0=s_[:, 0:T], in1=r_[:, 0:T], op=ALU.mult), reads=[s_, r_], writes=[kq])
                    yield
            for ti in range(nt):
                b = bk()
                bb = b.t[:, :].bitcast(BF16)
                for h in range(4):
                    k.op("pe", lambda e: e.transpose(out=bb[:, h * 128:(h + 1) * 128], in_=kq[:, h, 0, ti * 128:(ti + 1) * 128], identity=self.ident_b[:]),
                         reads=[kq, self.ident_b], writes=[b])
                    k.op("pe", lambda e: e.transpose(out=bb[:, (4 + h) * 128:(5 + h) * 128], in_=vt[:, h, ti * 128:(ti + 1) * 128], identity=self.ident_b[:]),
                         reads=[vt, self.ident_b], writes=[b])
                yield
                k.op("act", lambda e: e.copy(out=KVtm[bp][ti][:].rearrange("p a b -> p (a b)"), in_=bb[:, :]), reads=[b], writes=[KVtm[bp][ti]])
                yield

        def stage_b(bk, d, mI, mS, tri, bp, q, i, ti):
            kq, kv = KQT[bp], KVtm[bp][ti]
            tsl = slice(ti * 128, (ti + 1) * 128)
            sc_, sm_ = sc[q], sm[bp][ti]
            k.dma("sp", sc_[:], S["tm"][i * 128:(i + 1) * 128, 512:528], reads=[self.tm_dep[i]], writes=[sc_])
            k.op("act", lambda e: e.activation(out=sm_[:, 0:4], in_=sc_[:, 4 * d:4 * d + 4], func=AF.Exp, scale=-1.0), reads=[sc_], writes=[sm_])
            yield
            k.op("dve", lambda e: e.tensor_scalar(out=sm_[:, 0:4], in0=sm_[:, 0:4], scalar1=1.0, scalar2=None, op0=ALU.add), reads=[sm_], writes=[sm_])
            k.op("dve", lambda e: e.reciprocal(out=sm_[:, 0:4], in_=sm_[:, 0:4]), reads=[sm_], writes=[sm_])
            k.op("dve", lambda e: e.tensor_tensor(out=sm_[:, 4:8], in0=sc_[:, 8 + 4 * d:12 + 4 * d], in1=dtb[:, 4 * d:4 * d + 4], op=ALU.add), reads=[sc_, dtb], writes=[sm_])
            yield
            k.op("act", lambda e: e.activation(out=sm_[:, 4:8], in_=sm_[:, 4:8], func=AF.Exp), reads=[sm_], writes=[sm_])
            k.op("act", lambda e: e.activation(out=sm_[:, 4:8], in_=sm_[:, 4:8], func=AF.Ln, bias=1.0), reads=[sm_], writes=[sm_])
            yield
            k.op("dve", lambda e: e.tensor_tensor(out=sm_[:, 4:8], in0=sm_[:, 4:8], in1=nA[:, 4 * d:4 * d + 4], op=ALU.mult), reads=[sm_, nA], writes=[sm_])
            yield
            bs = bk()
            k.op("pe", lambda e: e.matmul(bs[:, 0:4], lhsT=tri[:], rhs=sm_[:, 4:8], start=True, stop=True), reads=[tri, sm_], writes=[bs])
            k.op("pe", lambda e: e.matmul(bs[:, 4:8], lhsT=self.ones_f[:], rhs=sm_[:, 4:8], start=True, stop=True), reads=[self.ones_f, sm_], writes=[bs])
            G = Gd[q]
            k.op("pool", lambda e: e.tensor_tensor(out=G[:], in0=mI[:], in1=sm_[:, 4:8].unsqueeze(2).to_broadcast([128, 4, 128]), op=ALU.mult),
                 reads=[mI, sm_], writes=[G])
            yield
            k.op("dve", lambda e: e.tensor_copy(out=sm_[:, 8:16], in_=bs[:, 0:8]), reads=[bs], writes=[sm_])
            bg = bk()
            k.op("pe", lambda e: e.matmul(bg[:, :], lhsT=self.ones_f[:], rhs=flat(G), start=True, stop=True), reads=[self.ones_f, G], writes=[bg])
            yield
            k.op("act", lambda e: e.activation(out=sm_[:, 16:20], in_=sm_[:, 8:12], func=AF.Exp), reads=[sm_], writes=[sm_])
            k.op("dve", lambda e: e.tensor_tensor(out=sm_[:, 20:24], in0=sm_[:, 12:16], in1=sm_[:, 8:12], op=ALU.subtract), reads=[sm_], writes=[sm_])
            bg3 = bg[:, :].rearrange("p (a b) -> p a b", a=4)
            for h in range(4):
                k.op("dve", lambda e: e.tensor_scalar(out=Dm[q][:, h, :], in0=bg3[:, h, :], scalar1=sm_[:, 8 + h:9 + h], scalar2=0.0, op0=ALU.subtract, op1=ALU.min),
                     reads=[bg, sm_], writes=[Dm[q]])
            yield
            k.op("act", lambda e: e.activation(out=sm_[:, 20:24], in_=sm_[:, 20:24], func=AF.Exp), reads=[sm_], writes=[sm_])
            k.op("act", lambda e: e.activation(out=sm_[:, 24:28], in_=sm_[:, 12:16], func=AF.Exp), reads=[sm_], writes=[sm_])
            k.op("act", lambda e: e.activation(out=flat(Eg[q]), in_=bg[:, :], func=AF.Exp), reads=[bg], writes=[Eg[q]])
            k.op("act", lambda e: e.activation(out=Dm[q][:], in_=Dm[q][:], func=AF.Exp), reads=[Dm[q]], writes=[Dm[q]])
            yield
            k.op("dve", lambda e: e.tensor_tensor(out=sm_[:, 20:24], in0=sm_[:, 20:24], in1=sm_[:, 0:4], op=ALU.mult), reads=[sm_], writes=[sm_])
            k.op("pool", lambda e: e.tensor_tensor(out=qdecT[bp][ti][:], in0=kq[:, :, 1, tsl], in1=Eg[q][:], op=ALU.mult), reads=[kq, Eg[q]], writes=[qdecT[bp][ti]])
            bet = sm_[:, 0:4].unsqueeze(2).to_broadcast([128, 4, 128])
            DmI, DmS = Gd[q], Eg[q]
            k.op("pool", lambda e: e.tensor_tensor(out=DmI[:], in0=Dm[q][:], in1=mI[:], op=ALU.mult), reads=[Dm[q], mI], writes=[DmI])
            yield
            k.op("pool", lambda e: e.tensor_tensor(out=DmS[:], in0=Dm[q][:], in1=mS[:], op=ALU.mult), reads=[Dm[q], mS], writes=[DmS])
            k.op("pool", lambda e: e.tensor_tensor(out=DmI[:], in0=DmI[:], in1=bet, op=ALU.mult), reads=[DmI, sm_], writes=[DmI])
            k.op("pool", lambda e: e.tensor_tensor(out=DmS[:], in0=DmS[:], in1=bet, op=ALU.mult), reads=[DmS, sm_], writes=[DmS])
            K3 = kv[:, 0:4, :]
            k.op("pool", lambda e: e.tensor_tensor(out=Keg[q][:], in0=K3, in1=sm_[:, 16:20].unsqueeze(2).to_broadcast([128, 4, 128]), op=ALU.mult), reads=[kv, sm_], writes=[Keg[q]])
            k.op("pool", lambda e: e.tensor_tensor(out=kdec[bp][ti][:], in0=K3, in1=sm_[:, 20:24].unsqueeze(2).to_broadcast([128, 4, 128]), op=ALU.mult), reads=[kv, sm_], writes=[kdec[bp][ti]])
            bkk, bkq = bk(), bk()
            for h in range(4):
                k.op("pe", lambda e: e.matmul(bkk[:, h * 128:(h + 1) * 128], lhsT=kq[:, h, 0, tsl], rhs=kq[:, h, 0, tsl], start=True, stop=True), reads=[kq], writes=[bkk])
                k.op("pe", lambda e: e.matmul(bkq[:, h * 128:(h + 1) * 128], lhsT=kq[:, h, 0, tsl], rhs=kq[:, h, 1, tsl], start=True, stop=True), reads=[kq], writes=[bkq])
            yield
            k.op("dve", lambda e: e.tensor_tensor(out=flat(A[q]), in0=bkk[:, :], in1=flat(DmS), op=ALU.mult), reads=[bkk, DmS], writes=[A[q]])
            k.op("dve", lambda e: e.tensor_tensor(out=flat(attnT[bp][ti]), in0=bkq[:, :], in1=flat(DmI), op=ALU.mult), reads=[bkq, DmI], writes=[attnT[bp][ti]])
            yield
            bt = bk()
            btb = bt.t[:, :].bitcast(BF16)
            for h in range(4):
                k.op("pe", lambda e: e.transpose(out=btb[:, h * 128:(h + 1) * 128], in_=A[q][:, h, :], identity=self.ident_b[:]), reads=[A[q], self.ident_b], writes=[bt])
            k.op("pool", lambda e: e.tensor_tensor(out=Abd[q][:], in0=A[q][:], in1=bd32[:], op=ALU.mult), reads=[A[q], bd32], writes=[Abd[q]])
            yield
            k.op("act", lambda e: e.copy(out=flat(AT[q]), in_=btb[:, 0:512]), reads=[bt], writes=[AT[q]])
            k.op("pool", lambda e: e.tensor_tensor(out=R0[q][:], in0=ident4b[:], in1=Abd[q][:], op=ALU.subtract), reads=[ident4b, Abd[q]], writes=[R0[q]])
            yield
            k.op("pool", lambda e: e.tensor_tensor(out=ATbd[q][:], in0=AT[q][:], in1=bd32[:], op=ALU.mult), reads=[AT[q], bd32], writes=[ATbd[q]])
            k.op("pool", lambda e: e.tensor_tensor(out=B1T[q][:], in0=AT[q][:], in1=m1[:], op=ALU.mult), reads=[AT[q], m1], writes=[B1T[q]])
            k.op("pool", lambda e: e.tensor_tensor(out=B2T[q][:], in0=AT[q][:], in1=m2[:], op=ALU.mult), reads=[AT[q], m2], writes=[B2T[q]])
            yield
            Xc, XTc, Rc = Abd[q], ATbd[q], R0[q]
            xbufs = [(Xa[q], XTa[q]), (A[q], AT[q])]
            rbufs = [R1[q], R0[q]]
            for lev in range(4):
                lastl = lev == 3
                Xn, XTn = xbufs[lev % 2]
                Rn = rbufs[lev % 2]
                b1 = bk()
                for h in range(4):
                    k.op("pe", lambda e: e.matmul(b1[:, h * 128:(h + 1) * 128], lhsT=Xc[:, h, :], rhs=XTc[:, h, :], start=True, stop=True), reads=[Xc, XTc], writes=[b1])
                if not lastl:
                    b2 = bk()
                    for h in range(4):
                        k.op("pe", lambda e: e.matmul(b2[:, h * 128:(h + 1) * 128], lhsT=XTc[:, h, :], rhs=Xc[:, h, :], start=True, stop=True), reads=[Xc, XTc], writes=[b2])
                yield
                k.op("dve", lambda e: e.tensor_tensor(out=flat(YT[q]), in0=b1[:, :], in1=flat(ident4b), op=ALU.add), reads=[b1, ident4b], writes=[YT[q]])
                if not lastl:
                    k.op("act", lambda e: e.copy(out=flat(XTn), in_=b1[:, :]), reads=[b1], writes=[XTn])
                    k.op("act", lambda e: e.copy(out=flat(Xn), in_=b2[:, :]), reads=[b2], writes=[Xn])
                yield
                b3 = bk()
                for h in range(4):
                    k.op("pe", lambda e: e.matmul(b3[:, h * 128:(h + 1) * 128], lhsT=YT[q][:, h, :], rhs=Rc[:, h, :], start=True, stop=True), reads=[YT[q], Rc], writes=[b3])
                yield
                k.op("dve", lambda e: e.tensor_copy(out=flat(Rn), in_=b3[:, :]), reads=[b3], writes=[Rn])
                yield
                Xc, XTc, Rc = Xn, XTn, Rn
            TTb, Pb = Xa[q], XTa[q]
            for mi, BT_ in enumerate((B1T[q], B2T[q])):
                Tn = rbufs[mi % 2]
                btt = bk()
                bttb = btt.t[:, :].bitcast(BF16)
                for h in range(4):
                    k.op("pe", lambda e: e.transpose(out=bttb[:, h * 128:(h + 1) * 128], in_=Rc[:, h, :], identity=self.ident_b[:]), reads=[Rc, self.ident_b], writes=[btt])
                bp_ = bk()
                for h in range(4):
                    k.op("pe", lambda e: e.matmul(bp_[:, h * 128:(h + 1) * 128], lhsT=BT_[:, h, :], rhs=Rc[:, h, :], start=True, stop=True), reads=[BT_, Rc], writes=[bp_])
                yield
                k.op("act", lambda e: e.copy(out=flat(TTb), in_=bttb[:, 0:512]), reads=[btt], writes=[TTb])
                k.op("dve", lambda e: e.tensor_copy(out=flat(Pb), in_=bp_[:, :]), reads=[bp_], writes=[Pb])
                yield
                bq = bk()
                for h in range(4):
                    k.op("pe", lambda e: e.matmul(bq[:, h * 128:(h + 1) * 128], lhsT=TTb[:, h, :], rhs=Pb[:, h, :], start=True, stop=True), reads=[TTb, Pb], writes=[bq])
                yield
                k.op("dve", lambda e: e.tensor_tensor(out=flat(Tn), in0=flat(Rc), in1=bq[:, :], op=ALU.subtract), reads=[Rc, bq], writes=[Tn])
                yield
                Rc = Tn
            T2T = Rc
            bu, bw = bk(), bk()
            for h in range(4):
                k.op("pe", lambda e: e.matmul(bu[:, h * 128:(h + 1) * 128], lhsT=T2T[:, h, :], rhs=kv[:, 4 + h, :], start=True, stop=True), reads=[T2T, kv], writes=[bu])
                k.op("pe", lambda e: e.matmul(bw[:, h * 128:(h + 1) * 128], lhsT=Keg[q][:, h, :], rhs=T2T[:, h, :], start=True, stop=True), reads=[T2T, Keg[q]], writes=[bw])
            yield
            k.op("act", lambda e: e.copy(out=flat(up[bp][ti]), in_=bu[:, :]), reads=[bu], writes=[up[bp][ti]])
            k.op("dve", lambda e: e.tensor_copy(out=flat(wT[bp][ti]), in_=bw[:, :]), reads=[bw], writes=[wT[bp][ti]])
            yield

        def scan(bk, d, bp, tiles):
            for n_, (i, ti) in enumerate(tiles):
                p = n_ % 2
                sm_ = sm[bp][ti]
                bws = bk()
                for h in range(4):
                    k.op("pe", lambda e: e.matmul(bws[:, h * 128:(h + 1) * 128], lhsT=wT[bp][ti][:, h, :], rhs=Sb[:, h, :], start=True, stop=True), reads=[wT[bp][ti], Sb], writes=[bws])
                yield
                k.op("dve", lambda e: e.tensor_tensor(out=flat(vnew[p]), in0=flat(up[bp][ti]), in1=bws[:, :], op=ALU.subtract), reads=[up[bp][ti], bws], writes=[vnew[p]])
                yield
                bo, bkv = bk(), bk()
                for h in range(4):
                    k.op("pe", lambda e: e.matmul(bkv[:, h * 128:(h + 1) * 128], lhsT=kdec[bp][ti][:, h, :], rhs=vnew[p][:, h, :], start=True, stop=True), reads=[kdec[bp][ti], vnew[p]], writes=[bkv])
                for h in range(4):
                    k.op("pe", lambda e: e.matmul(bo[:, h * 128:(h + 1) * 128], lhsT=qdecT[bp][ti][:, h, :], rhs=Sb[:, h, :], start=True, stop=False), reads=[qdecT[bp][ti], Sb], writes=[bo])
                    k.op("pe", lambda e: e.matmul(bo[:, h * 128:(h + 1) * 128], lhsT=attnT[bp][ti][:, h, :], rhs=vnew[p][:, h, :], start=False, stop=True), reads=[attnT[bp][ti], vnew[p]], writes=[bo])
                yield
                bkv3 = bkv[:, :].rearrange("p (a b) -> p a b", a=4)
                for h in range(4):
                    k.op("dve", lambda e: e.scalar_tensor_tensor(out=Sf[:, h, :], in0=Sf[:, h, :], scalar=sm_[:, 24 + h:25 + h], in1=bkv3[:, h, :], op0=ALU.mult, op1=ALU.add),
                         reads=[Sf, sm_, bkv], writes=[Sf])
                k.op("act", lambda e: e.copy(out=flat(osb[p]), in_=bo[:, :]), reads=[bo], writes=[osb[p]])
                yield
                k.op("act", lambda e: e.copy(out=Sb[:], in_=Sf[:]), reads=[Sf], writes=[Sb])
                k.dma("act", S["o_dn"][d, i * 128:(i + 1) * 128, :], flat(osb[p]), reads=[osb[p]], writes=[self.odn_dep[d][i]])
                yield

        def cyc(ids):
            st = [0]

            def f():
                b = self.ps[ids[st[0] % len(ids)]]
                st[0] += 1
                return b
            return f
        bk_b = [cyc([0, 1]), cyc([2, 3])]
        bk_a, bk_s = cyc([6, 7]), cyc([4, 5])
        blocks_f = [(0, 2)] + [(2 + 4 * i, 4) for i in range(16)]
        blocks_f = [b_ for b_ in blocks_f if b_[0] < self.ntiles]
        gbi = 0
        for d in range(2):
            mI, mS, tri = self.dir_masks(d)
            k.op("pool", lambda e: e.memset(Sf[:], 0.0), writes=[Sf])
            k.op("pool", lambda e: e.memset(Sb[:], 0.0), writes=[Sb])
            blocks = blocks_f if d == 0 else [blocks_f[0]] + blocks_f[:0:-1]
            nb = len(blocks)
            self.interleave([stage_a(bk_a, gbi % 2, *blocks[0])])
            prev = None
            for bi, (t0, nt) in enumerate(blocks):
                bp = (gbi + bi) % 2
                tis = list(range(nt)) if d == 0 else list(range(nt - 1, -1, -1))
                for pair in range(0, nt, 2):
                    gens = [stage_b(bk_b[q], d, mI, mS, tri, bp, q, t0 + ti, ti) for q, ti in enumerate(tis[pair:pair + 2])]
                    if pair == 0 and bi + 1 < nb:
                        gens.append(stage_a(bk_a, (gbi + bi + 1) % 2, *blocks[bi + 1]))
                    if prev is not None and (pair == 2 or nt == 2):
                        gens.append(scan(bk_s, d, *prev))
                    self.interleave(gens)
                prev = (bp, [(t0 + ti, ti) for ti in tis])
            self.interleave([scan(bk_s, d, *prev)])
            gbi += nb

    def p3(self, l):
        k, I, S = self.k, self.I, self.S
        sb = k.sb

        def two(shape, dt, name):
            return [sb(shape, dt, name + "%d" % i) for i in range(2)]
        qkv = two([128, 1024], F32, "gqkv")
        glr = two([128, 16], F32, "glr")
        lhsTg = two([64, 128], F32, "lhsTg")
        W2 = sb([64, 256], F32, "W2")
        ex = two([128, 256], F32, "gex")
        la = two([128, 256], F32, "la")
        bcs = two([128, 256], F32, "bcs")
        ebc = two([128, 256], F32, "ebc")
        enbc = two([128, 256], F32, "enbc")
        ekd = two([128, 256], F32, "ekd")
        qs = two([128, 256], BF16, "gqs")
        ks = two([128, 256], BF16, "gks")
        kd = two([128, 256], BF16, "gkd")
        vb = two([128, 4, 128], BF16, "gvb")
        qT = two([128, 4, 128], BF16, "gqTm")
        kT = two([128, 2, 128], BF16, "gkT")
        att = two([128, 4, 128], BF16, "gatt")
        osb = two([128, 512], F32, "gosb")
        etc_ = two([128, 4], F32, "getc")
        Sf = sb([128, 2, 128], F32, "gSf")
        Sb = sb([128, 2, 128], BF16, "gSb")
        for t_ in qT:
            k.op("pool", lambda e: e.memset(t_[:], 0.0), writes=[t_])
        for t_ in lhsTg:
            k.op("pool", lambda e: e.memset(t_[:], 0.0), writes=[t_])
            k.op("pool", lambda e: e.memset(t_[32:33, :], 1.0), writes=[t_])
        tm_lat = S["tm"][LC:LS, :].rearrange("(r w) n -> w r n", w=64)
        alltm = self.tm_dep
        for d in range(2):
            mI, mS, tri = self.dir_masks(d)
            k.op("pool", lambda e: e.memset(W2[:], 0.0), writes=[W2])
            k.dma("sp", W2[0:16, :], I["gla_w_gate2"][l, d], writes=[W2])
            k.dma("sp", W2[32:33, :], I["gla_b_gate"][l, d:d + 1, :], writes=[W2])
            k.op("pool", lambda e: e.memset(Sf[:], 0.0), writes=[Sf])
            k.op("pool", lambda e: e.memset(Sb[:], 0.0), writes=[Sb])
            order = [("c", 0), ("c", 1)] + [("g", j) for j in range(64)]
            if d == 1:
                order = [("c", 1), ("c", 0)] + [("g", j) for j in range(63, -1, -1)]
            if self.ntiles < NT:
                order = order[:self.ntiles]
            def cyc(ids):
                st = [0]

                def f():
                    b = self.ps[ids[st[0] % len(ids)]]
                    st[0] += 1
                    return b
                return f
            bk_pre, bk_post = cyc([0, 1, 2, 3]), cyc([4, 5])

            def pre(bk, p, kind, j):
                if kind == "c":
                    rows = S["tm"][j * 128:(j + 1) * 128, :]
                else:
                    rows = tm_lat[j]
                k.dma("sp", qkv[p][:], rows[:, 528:1552], reads=alltm, writes=[qkv[p]])
                k.dma("sp", glr[p][:], rows[:, 2064 + 16 * d:2080 + 16 * d], reads=alltm, writes=[glr[p]])
                b0 = bk()
                k.op("pe", lambda e: e.transpose(out=b0[0:16, 0:128], in_=glr[p][:, :], identity=self.ident_f[:]), reads=[glr[p], self.ident_f], writes=[b0])
                k.op("act", lambda e: e.copy(out=lhsTg[p][0:16, :], in_=b0[0:16, 0:128]), reads=[b0], writes=[lhsTg[p]])
                yield
                bgt = bk()
                k.op("pe", lambda e: e.matmul(bgt[:, 0:256], lhsT=lhsTg[p][:, :], rhs=W2[:, :], start=True, stop=True), reads=[lhsTg[p], W2], writes=[bgt])
                k.op("act", lambda e: e.activation(out=ex[p][:], in_=bgt[:, 0:256], func=AF.Exp, scale=-1.0), reads=[bgt], writes=[ex[p]])
                yield
                k.op("act", lambda e: e.activation(out=ex[p][:], in_=ex[p][:], func=AF.Ln, bias=1.0), reads=[ex[p]], writes=[ex[p]])
                yield
                k.op("dve", lambda e: e.tensor_scalar(out=la[p][:], in0=ex[p][:], scalar1=-1.0 / 16.0, scalar2=None, op0=ALU.mult), reads=[ex[p]], writes=[la[p]])
                yield
                bbc, bto = bk(), bk()
                k.op("pe", lambda e: e.matmul(bbc[:, 0:256], lhsT=tri[:], rhs=la[p][:], start=True, stop=True), reads=[tri, la[p]], writes=[bbc])
                k.op("pe", lambda e: e.matmul(bto[:, 0:256], lhsT=self.ones_f[:], rhs=la[p][:], start=True, stop=True), reads=[self.ones_f, la[p]], writes=[bto])
                for hp in range(2):
                    k.op("pe", lambda e: e.matmul(bto[:, 256 + 2 * hp:258 + 2 * hp], lhsT=la[p][:, hp * 128:(hp + 1) * 128], rhs=self.ones_f[:, 0:2], start=True, stop=True),
                         reads=[la[p], self.ones_f], writes=[bto])
                k.op("act", lambda e: e.activation(out=ebc[p][:], in_=bbc[:, 0:256], func=AF.Exp), reads=[bbc], writes=[ebc[p]])
                yield
                k.op("act", lambda e: e.activation(out=enbc[p][:], in_=bbc[:, 0:256], func=AF.Exp, scale=-1.0), reads=[bbc], writes=[enbc[p]])
                yield
                k.op("act", lambda e: e.copy(out=bcs[p][:], in_=bbc[:, 0:256]), reads=[bbc], writes=[bcs[p]])
                yield
                k.op("dve", lambda e: e.tensor_tensor(out=ekd[p][:], in0=bto[:, 0:256], in1=bcs[p][:], op=ALU.subtract), reads=[bto, bcs[p]], writes=[ekd[p]])
                yield
                k.op("act", lambda e: e.activation(out=ekd[p][:], in_=ekd[p][:], func=AF.Exp), reads=[ekd[p]], writes=[ekd[p]])
                yield
                k.op("act", lambda e: e.activation(out=etc_[p][:], in_=bto[:, 256:260], func=AF.Exp), reads=[bto], writes=[etc_[p]])
                yield
                k.op("dve", lambda e: e.scalar_tensor_tensor(out=qs[p][:], in0=qkv[p][:, 0:256], scalar=0.125, in1=ebc[p][:], op0=ALU.mult, op1=ALU.mult), reads=[qkv[p], ebc[p]], writes=[qs[p]])
                yield
                k.op("dve", lambda e: e.tensor_tensor(out=ks[p][:], in0=qkv[p][:, 256:512], in1=enbc[p][:], op=ALU.mult), reads=[qkv[p], enbc[p]], writes=[ks[p]])
                yield
                k.op("pool", lambda e: e.tensor_tensor(out=kd[p][:], in0=qkv[p][:, 256:512], in1=ekd[p][:], op=ALU.mult), reads=[qkv[p], ekd[p]], writes=[kd[p]])
                yield
                k.op("pool", lambda e: e.tensor_copy(out=vb[p][:].rearrange("p a b -> p (a b)"), in_=qkv[p][:, 512:1024]), reads=[qkv[p]], writes=[vb[p]])
                yield
                btr = bk()
                btrb = btr.t[:, :].bitcast(BF16)
                for hp in range(2):
                    k.op("pe", lambda e: e.transpose(out=btrb[:, hp * 128:(hp + 1) * 128], in_=qs[p][:, hp * 128:(hp + 1) * 128], identity=self.ident_b[:]), reads=[qs[p], self.ident_b], writes=[btr])
                    k.op("pe", lambda e: e.transpose(out=btrb[:, (2 + hp) * 128:(3 + hp) * 128], in_=ks[p][:, hp * 128:(hp + 1) * 128], identity=self.ident_b[:]), reads=[ks[p], self.ident_b], writes=[btr])
                for h in range(4):
                    hb, hp = h % 2, h // 2
                    k.op("act", lambda e: e.copy(out=qT[p][hb * 64:(hb + 1) * 64, h, :], in_=btrb[hb * 64:(hb + 1) * 64, hp * 128:(hp + 1) * 128]), reads=[btr], writes=[qT[p]])
                k.op("act", lambda e: e.copy(out=kT[p][:].rearrange("p a b -> p (a b)"), in_=btrb[:, 256:512]), reads=[btr], writes=[kT[p]])
                yield
                bat = bk()
                for h in range(4):
                    hb, hp = h % 2, h // 2
                    ps_ = slice(hb * 64, (hb + 1) * 64)
                    k.op("pe", lambda e: e.matmul(bat[:, h * 128:(h + 1) * 128], lhsT=kT[p][:, hp, :], rhs=qT[p][:, h, :], start=True, stop=True), reads=[kT[p], qT[p]], writes=[bat])
                k.op("dve", lambda e: e.tensor_tensor(out=att[p][:].rearrange("p a b -> p (a b)"), in0=bat[:, :], in1=mI[:].rearrange("p a b -> p (a b)"), op=ALU.mult), reads=[bat, mI], writes=[att[p]])
                yield
                yield

            def post(bk, p, kind, j):
                if kind == "c":
                    orows = S["o_gla"][d, j * 128:(j + 1) * 128, :]
                    odeps = [self.ogla_dep[d][j]]
                else:
                    orows = S["o_gla"][d, LC:LS, :].rearrange("(r w) n -> w r n", w=64)[j]
                    odeps = self.ogla_dep[d][2:]
                bo, bkv = bk(), bk()
                for h in range(4):
                    hb, hp = h % 2, h // 2
                    ps_ = slice(hb * 64, (hb + 1) * 64)
                    k.op("pe", lambda e: e.matmul(bo[:, h * 128:(h + 1) * 128], lhsT=qT[p][:, h, :], rhs=Sb[:, hp, :], start=True, stop=False), reads=[qT[p], Sb], writes=[bo])
                    k.op("pe", lambda e: e.matmul(bo[:, h * 128:(h + 1) * 128], lhsT=att[p][:, h, :], rhs=vb[p][:, h, :], start=False, stop=True), reads=[att[p], vb[p]], writes=[bo])
                for h in range(4):
                    hb, hp = h % 2, h // 2
                    k.op("pe", lambda e: e.matmul(bkv[:, h * 128:(h + 1) * 128], lhsT=kd[p][:, hp * 128:(hp + 1) * 128], rhs=vb[p][:, h, :], start=True, stop=True),
                         reads=[kd[p], vb[p]], writes=[bkv])
                k.op("act", lambda e: e.copy(out=osb[p][:], in_=bo[:, :]), reads=[bo], writes=[osb[p]])
                yield
                k.dma("act", orows, osb[p][:], reads=[osb[p]], writes=odeps)
                for h in range(4):
                    hb, hp = h % 2, h // 2
                    ps_ = slice(hb * 64, (hb + 1) * 64)
                    k.op("dve", lambda e: e.scalar_tensor_tensor(out=Sf[ps_, hp, :], in0=Sf[ps_, hp, :], scalar=etc_[p][ps_, 2 * hp:2 * hp + 1], in1=bkv[ps_, h * 128:(h + 1) * 128], op0=ALU.mult, op1=ALU.add),
                         reads=[Sf, etc_[p], bkv], writes=[Sf])
                k.op("act", lambda e: e.copy(out=Sb[:], in_=Sf[:]), reads=[Sf], writes=[Sb])
                yield


                yield

            self.interleave([pre(bk_pre, 0, *order[0])])
            for step, (kind, j) in enumerate(order):
                gens = [post(bk_post, step % 2, kind, j)]
                if step + 1 < len(order):
                    gens.append(pre(bk_pre, (step + 1) % 2, *order[step + 1]))
                self.interleave(gens)
    def p4(self, l):
        k, I, S = self.k, self.I, self.S
        sb = k.sb
        last = l == self.nlayers - 1
        moe = (l % 2 == 1)
        T = 1024
        NH = T // 512
        gnb = sb([128, 8, 128], F32, "gnb")
        for h in range(8):
            src_ = I["dn_norm"] if h < 4 else I["gla_norm"]
            k.dma("sp", gnb[:, h, :], src_[l:l + 1, :].broadcast_to([128, 128]), writes=[gnb])
        xT = sb([128, 8, T], F32, "xT")
        mT = sb([128, 8, T], BF16, "mT")
        sq2 = mT
        h2T = sb([128, 8, T], BF16, "h2T")
        rs2 = sb([128, T], F32, "rs2")
        tmpf = [sb([128, T], F32, "tmpf%d" % i) for i in range(2)]
        F = D_EXP if moe else D_FF
        NFC = F // 128
        actT = sb([128, NFC, T], BF16, "actT")
        ot = [sb([128, 512], F32, "ot%d" % i) for i in range(4)]
        zt = sb([128, 1024], F32, "zt")
        xt = sb([128, D], F32, "xt4")
        xo = xt
        osum = sb([128, 8, 128], F32, "osum")
        ssh = sb([128, 16], F32, "ssh")
        mb = sb([128, D], BF16, "mb")
        wb = [sb([128, 8, 512], BF16, "wb%d" % i) for i in range(3)]
        wd = [sb([128, 4, 512], BF16, "wd%d" % i) for i in range(2)]
        sg = [ot[0], ot[1]]
        if moe:
            rt = sb([128, 8, NE], F32, "rt")
            k.dma("sp", rt[:], I["moe_router"].rearrange("(k p) n -> p k n", p=128), writes=[rt])
            lgT = sb([8, T], F32, "lgT")
            lg = sb([128, 8, NE], F32, "lg")
            l2 = sb([128, 8, NE], F32, "l2")
            mx = sb([128, 16], F32, "mx")
            cbT = sb([8, T], F32, "cbT")
            CBe = [zt, xt]
            selE = sb([8, NE, 128], F32, "selE")
            for e_ in range(NE):
                k.op("pool", lambda e: e.tensor_copy(out=selE[0:8, e_, :], in_=self.ident_f[0:8, e_:e_ + 1].to_broadcast([8, 128])), reads=[self.ident_f], writes=[selE])
        ntb = T // 128
        if last:
            blocks = [(2 + ntb * i, ntb) for i in range(NOWN // T)]
        else:
            blocks = [(0, 2)] + [(2 + ntb * i, ntb) for i in range(LL // T)]
        wi = [0]

        def wload(src_ap, ncols):
            t_ = wb[wi[0] % 3]
            wi[0] += 1
            k.dma("pool", t_[:, :, 0:ncols], src_ap, writes=[t_], max_dma_last_dim=2048)
            return t_
        wout_src = I["w_out"][l].rearrange("(k p) n -> p k n", p=128)
        for (t0, nt) in blocks:
            if t0 >= self.ntiles:
                break
            Tb = nt * 128
            halves = [(h0, min(512, Tb - h0)) for h0 in range(0, Tb, 512)]
            r = 1 if t0 < 2 else 0
            for ti in range(nt):
                i = t0 + ti
                rows = slice(i * 128, (i + 1) * 128)
                tsl = slice(ti * 128, (ti + 1) * 128)
                k.dma("sp", ot[0][:], S["o_dn"][0, rows, :], reads=[self.odn_dep[0][i]], writes=[ot[0]])
                k.dma("sp", ot[1][:], S["o_dn"][1, rows, :], reads=[self.odn_dep[1][i]], writes=[ot[1]])
                k.dma("sp", ot[2][:], S["o_gla"][0, rows, :], reads=[self.ogla_dep[0][i]], writes=[ot[2]])
                k.dma("sp", ot[3][:], S["o_gla"][1, rows, :], reads=[self.ogla_dep[1][i]], writes=[ot[3]])
                k.dma("sp", zt[:, 0:512], S["tm"][rows, 0:512], reads=[self.tm_dep[i]], writes=[zt])
                k.dma("sp", zt[:, 512:1024], S["tm"][rows, 1552:2064], reads=[self.tm_dep[i]], writes=[zt])
                k.dma("sp", xt[:], self.xcur[rows, :], reads=[self.xcur_dep[i]], writes=[xt])
                osf = osum[:].rearrange("p a b -> p (a b)")
                k.op("dve", lambda e: e.tensor_tensor(out=osf[:, 0:512], in0=ot[0][:], in1=ot[1][:], op=ALU.add), reads=[ot[0], ot[1]], writes=[osum])
                k.op("dve", lambda e: e.tensor_tensor(out=osf[:, 512:1024], in0=ot[2][:], in1=ot[3][:], op=ALU.add), reads=[ot[2], ot[3]], writes=[osum])
                for hh in range(2):
                    k.op("act", lambda e: e.activation(out=ot[2 * hh][:], in_=osf[:, hh * 512:(hh + 1) * 512], func=AF.Square), reads=[osum], writes=[ot[2 * hh]])
                    k.op("dve", lambda e: e.tensor_reduce(out=ssh[:, 4 * hh:4 * hh + 4], in_=ot[2 * hh][:].rearrange("p (a b) -> p a b", a=4), axis=AX.X, op=ALU.add), reads=[ot[2 * hh]], writes=[ssh])
                k.op("act", lambda e: e.activation(out=ssh[:, 8:16], in_=ssh[:, 0:8], func=AF.Sqrt, scale=1.0 / 128.0, bias=self.eps_t[:, 0:1]), reads=[ssh, self.eps_t], writes=[ssh])
                k.op("dve", lambda e: e.reciprocal(out=ssh[:, 8:16], in_=ssh[:, 8:16]), reads=[ssh], writes=[ssh])
                k.op("act", lambda e: e.activation(out=zt[:], in_=zt[:], func=AF.Silu), reads=[zt], writes=[zt])
                for h in range(8):
                    k.op("dve", lambda e: e.scalar_tensor_tensor(out=osum[:, h, :], in0=osum[:, h, :], scalar=ssh[:, 8 + h:9 + h], in1=gnb[:, h, :], op0=ALU.mult, op1=ALU.mult),
                         reads=[osum, ssh, gnb], writes=[osum])
                k.op("dve", lambda e: e.tensor_tensor(out=mb[:], in0=osf, in1=zt[:], op=ALU.mult), reads=[osum, zt], writes=[mb])
                b = self.bank()
                bb = b.t[:, :].bitcast(BF16)
                for kk in range(8):
                    k.op("pe", lambda e: e.transpose(out=bb[:, kk * 128:(kk + 1) * 128], in_=mb[:, kk * 128:(kk + 1) * 128], identity=self.ident_b[:]), reads=[mb, self.ident_b], writes=[b])
                k.op("act", lambda e: e.copy(out=mT[:, :, tsl], in_=bb[:, :].rearrange("p (a b) -> p a b", a=8)), reads=[b], writes=[mT])
                for half in range(2):
                    b2 = self.bank()
                    for q in range(4):
                        kk = half * 4 + q
                        k.op("pe", lambda e: e.transpose(out=b2[:, q * 128:(q + 1) * 128], in_=xt[:, kk * 128:(kk + 1) * 128], identity=self.ident_f[:]), reads=[xt, self.ident_f], writes=[b2])
                    k.op("dve", lambda e: e.tensor_copy(out=xT[:, half * 4:half * 4 + 4, tsl], in_=b2[:, :].rearrange("p (a b) -> p a b", a=4)), reads=[b2], writes=[xT])
            for og in range(2):
                wt = wload(wout_src[:, :, og * 512:(og + 1) * 512], 512)
                for q in range(4):
                    oc = og * 4 + q
                    for (h0, hw) in halves:
                        b = self.bank()
                        for kk in range(8):
                            k.op("pe", lambda e: e.matmul(b[:, 0:hw], lhsT=wt[:, kk, q * 128:(q + 1) * 128], rhs=mT[:, kk, h0:h0 + hw], start=(kk == 0), stop=(kk == 7)), reads=[wt, mT], writes=[b])
                        k.op("dve", lambda e: e.scalar_tensor_tensor(out=xT[:, oc, h0:h0 + hw], in0=b[:, 0:hw], scalar=self.modT[:, 16 + oc, r:r + 1], in1=xT[:, oc, h0:h0 + hw], op0=ALU.mult, op1=ALU.add),
                             reads=[b, self.modT, xT], writes=[xT])

            def rms_bcast(dst):
                for kk in range(8):
                    k.op("act", lambda e: e.activation(out=sq2[:, kk, 0:Tb], in_=xT[:, kk, 0:Tb], func=AF.Square), reads=[xT], writes=[sq2])
                for (h0, hw) in halves:
                    bs_ = self.bank()
                    for kk in range(8):
                        k.op("pe", lambda e: e.matmul(bs_[:, 0:hw], lhsT=self.ones_b[:], rhs=sq2[:, kk, h0:h0 + hw], start=(kk == 0), stop=(kk == 7)), reads=[self.ones_b, sq2], writes=[bs_])
                    k.op("act", lambda e: e.activation(out=dst[:, h0:h0 + hw], in_=bs_[:, 0:hw], func=AF.Sqrt, scale=1.0 / D, bias=self.eps_t[:, 0:1]), reads=[bs_, self.eps_t], writes=[dst])
                k.op("dve", lambda e: e.reciprocal(out=dst[:, 0:Tb], in_=dst[:, 0:Tb]), reads=[dst], writes=[dst])
            rms_bcast(rs2)
            if moe:
                bl = [self.bank() for _ in halves]
            for kk in range(8):
                tf = tmpf[kk % 2]
                k.op("dve", lambda e: e.tensor_tensor(out=tf[:, 0:Tb], in0=xT[:, kk, 0:Tb], in1=rs2[:, 0:Tb], op=ALU.mult), reads=[xT, rs2], writes=[tf])
                k.op("act", lambda e: e.activation(out=h2T[:, kk, 0:Tb], in_=tf[:, 0:Tb], func=AF.Identity, scale=self.g2T[:, kk, r:r + 1], bias=self.modT[:, 24 + kk, r:r + 1]),
                     reads=[tf, self.g2T, self.modT], writes=[h2T])
                if moe:
                    k.op("dve", lambda e: e.tensor_scalar(out=tf[:, 0:Tb], in0=tf[:, 0:Tb], scalar1=self.g2T[:, kk, r:r + 1], scalar2=self.modT[:, 24 + kk, r:r + 1], op0=ALU.mult, op1=ALU.add),
                         reads=[tf, self.g2T, self.modT], writes=[tf])
                    for hi, (h0, hw) in enumerate(halves):
                        k.op("pe", lambda e: e.matmul(bl[hi][0:8, 0:hw], lhsT=rt[:, kk, :], rhs=tf[:, h0:h0 + hw], start=(kk == 0), stop=(kk == 7)), reads=[rt, tf], writes=[bl[hi]])
            experts = [None]
            if moe:
                experts = list(range(NE))
                for hi, (h0, hw) in enumerate(halves):
                    k.op("act", lambda e: e.copy(out=lgT[:, h0:h0 + hw], in_=bl[hi][0:8, 0:hw]), reads=[bl[hi]], writes=[lgT])
                bl2 = self.bank()
                for ti in range(nt):
                    k.op("pe", lambda e: e.transpose(out=bl2[:, ti * 8:(ti + 1) * 8], in_=lgT[0:8, ti * 128:(ti + 1) * 128], identity=self.ident_f[0:8, 0:8]), reads=[lgT, self.ident_f], writes=[bl2])
                k.op("dve", lambda e: e.tensor_copy(out=lg[:].rearrange("p a b -> p (a b)"), in_=bl2[:, 0:64]), reads=[bl2], writes=[lg])
                bc3 = lambda ap: ap.unsqueeze(2).to_broadcast([128, 8, NE])
                k.op("dve", lambda e: e.tensor_reduce(out=mx[:, 0:8], in_=lg[:], axis=AX.X, op=ALU.max), reads=[lg], writes=[mx])
                k.op("pool", lambda e: e.tensor_tensor(out=l2[:], in0=lg[:], in1=bc3(mx[:, 0:8]), op=ALU.subtract), reads=[lg, mx], writes=[l2])
                k.op("dve", lambda e: e.tensor_scalar(out=lg[:], in0=l2[:], scalar1=0.0, scalar2=-1e30, op0=ALU.is_equal, op1=ALU.mult), reads=[l2], writes=[lg])
                k.op("dve", lambda e: e.tensor_tensor(out=lg[:], in0=lg[:], in1=l2[:], op=ALU.add), reads=[lg, l2], writes=[lg])
                k.op("dve", lambda e: e.tensor_reduce(out=mx[:, 8:16], in_=lg[:], axis=AX.X, op=ALU.max), reads=[lg], writes=[mx])
                k.op("pool", lambda e: e.tensor_tensor(out=lg[:], in0=l2[:], in1=bc3(mx[:, 8:16]), op=ALU.subtract), reads=[l2, mx], writes=[lg])
                k.op("dve", lambda e: e.tensor_scalar(out=lg[:], in0=lg[:], scalar1=0.0, scalar2=None, op0=ALU.is_ge), reads=[lg], writes=[lg])
                k.op("act", lambda e: e.activation(out=l2[:], in_=l2[:], func=AF.Exp), reads=[l2], writes=[l2])
                k.op("dve", lambda e: e.tensor_tensor(out=l2[:], in0=l2[:], in1=lg[:], op=ALU.mult), reads=[l2, lg], writes=[l2])
                k.op("dve", lambda e: e.tensor_reduce(out=mx[:, 0:8], in_=l2[:], axis=AX.X, op=ALU.add), reads=[l2], writes=[mx])
                k.op("dve", lambda e: e.reciprocal(out=mx[:, 0:8], in_=mx[:, 0:8]), reads=[mx], writes=[mx])
                k.op("pool", lambda e: e.tensor_tensor(out=l2[:], in0=l2[:], in1=bc3(mx[:, 0:8]), op=ALU.mult), reads=[l2, mx], writes=[l2])
                for hi, (h0, hw) in enumerate(halves):
                    bl3 = self.bank()
                    for t4 in range(hw // 128):
                        ti = h0 // 128 + t4
                        k.op("pe", lambda e: e.transpose(out=bl3[0:8, t4 * 128:(t4 + 1) * 128], in_=l2[:, ti, :], identity=self.ident_f[:]), reads=[l2, self.ident_f], writes=[bl3])
                    k.op("act", lambda e: e.copy(out=cbT[:, h0:h0 + hw], in_=bl3[0:8, 0:hw]), reads=[bl3], writes=[cbT])
            for ex_ in experts:
                if moe:
                    wgu_src = I["moe_w_gu"][ex_].rearrange("(k p) n -> p k n", p=128)
                    wdn_src = I["moe_w_down"][ex_].rearrange("(f p) n -> p f n", p=128)
                    cbe = CBe[ex_ % 2]
                    for (h0, hw) in halves:
                        bcb = self.bank()
                        k.op("pe", lambda e: e.matmul(bcb[:, 0:hw], lhsT=selE[0:8, ex_, :], rhs=cbT[0:8, h0:h0 + hw], start=True, stop=True), reads=[selE, cbT], writes=[bcb])
                        k.op("act", lambda e: e.copy(out=cbe[:, h0:h0 + hw], in_=bcb[:, 0:hw]), reads=[bcb], writes=[cbe])
                else:
                    wgu_src = I["ffn_w_gu"].rearrange("(k p) n -> p k n", p=128)
                    wdn_src = I["ffn_w_down"].rearrange("(f p) n -> p f n", p=128)
                si = 0
                for g0 in range(0, F, 512):
                    gw = min(512, F - g0)
                    wgt = wload(wgu_src[:, :, g0:g0 + gw], gw)
                    wut = wload(wgu_src[:, :, F + g0:F + g0 + gw], gw)
                    for c in range(gw // 128):
                        fc = g0 // 128 + c
                        for (h0, hw) in halves:
                            bg_, bu_ = self.bank(), self.bank()
                            for kk in range(8):
                                k.op("pe", lambda e: e.matmul(bg_[:, 0:hw], lhsT=wgt[:, kk, c * 128:(c + 1) * 128], rhs=h2T[:, kk, h0:h0 + hw], start=(kk == 0), stop=(kk == 7)), reads=[wgt, h2T], writes=[bg_])
                            for kk in range(8):
                                k.op("pe", lambda e: e.matmul(bu_[:, 0:hw], lhsT=wut[:, kk, c * 128:(c + 1) * 128], rhs=h2T[:, kk, h0:h0 + hw], start=(kk == 0), stop=(kk == 7)), reads=[wut, h2T], writes=[bu_])
                            s_ = sg[si % 2]
                            si += 1
                            k.op("act", lambda e: e.activation(out=s_[:, 0:hw], in_=bg_[:, 0:hw], func=AF.Silu), reads=[bg_], writes=[s_])
                            if moe:
                                k.op("dve", lambda e: e.tensor_tensor(out=s_[:, 0:hw], in0=s_[:, 0:hw], in1=cbe[:, h0:h0 + hw], op=ALU.mult), reads=[s_, cbe], writes=[s_])
                            k.op("dve", lambda e: e.tensor_tensor(out=actT[:, fc, h0:h0 + hw], in0=bu_[:, 0:hw], in1=s_[:, 0:hw], op=ALU.mult), reads=[bu_, s_], writes=[actT])
                nf = F // 128
                for ocg in range(2):
                    banks = [[self.bank() for _ in range(4)] for _ in halves]
                    for f0 in range(0, nf, 4):
                        wdt = wd[wi[0] % 2]
                        wi[0] += 1
                        n4 = min(4, nf - f0)
                        k.dma("pool", wdt[:, 0:n4, 0:512], wdn_src[:, f0:f0 + n4, ocg * 512:(ocg + 1) * 512], writes=[wdt], max_dma_last_dim=2048)
                        for hi, (h0, hw) in enumerate(halves):
                            for q in range(4):
                                for fi in range(n4):
                                    f = f0 + fi
                                    k.op("pe", lambda e: e.matmul(banks[hi][q][:, 0:hw], lhsT=wdt[:, fi, q * 128:(q + 1) * 128], rhs=actT[:, f, h0:h0 + hw], start=(f == 0), stop=(f == nf - 1)),
                                         reads=[wdt, actT], writes=[banks[hi][q]])
                    for hi, (h0, hw) in enumerate(halves):
                        for q in range(4):
                            oc = ocg * 4 + q
                            k.op("dve", lambda e: e.scalar_tensor_tensor(out=xT[:, oc, h0:h0 + hw], in0=banks[hi][q][:, 0:hw], scalar=self.modT[:, 40 + oc, r:r + 1], in1=xT[:, oc, h0:h0 + hw], op0=ALU.mult, op1=ALU.add),
                                 reads=[banks[hi][q], self.modT, xT], writes=[xT])
            if last:
                rms_bcast(rs2)
                for kk in range(8):
                    k.op("dve", lambda e: e.scalar_tensor_tensor(out=xT[:, kk, 0:Tb], in0=xT[:, kk, 0:Tb], scalar=self.fnT[:, kk, 0:1], in1=rs2[:, 0:Tb], op0=ALU.mult, op1=ALU.mult),
                         reads=[xT, self.fnT, rs2], writes=[xT])
            for ti in range(nt):
                i = t0 + ti
                tsl = slice(ti * 128, (ti + 1) * 128)
                for half in range(2):
                    b2 = self.bank()
                    for q in range(4):
                        kk = half * 4 + q
                        k.op("pe", lambda e: e.transpose(out=b2[:, q * 128:(q + 1) * 128], in_=xT[:, kk, tsl], identity=self.ident_f[:]), reads=[xT, self.ident_f], writes=[b2])
                    k.op("act", lambda e: e.copy(out=xo[:, half * 512:(half + 1) * 512], in_=b2[:, :]), reads=[b2], writes=[xo])
                if last:
                    k.dma("sp", self.out[(i - 2) * 128:(i - 1) * 128, :], xo[:], reads=[xo], writes=[self.out_dep])
                else:
                    k.dma("sp", S["x1"][i * 128:(i + 1) * 128, :], xo[:], reads=[xo], writes=[self.x1_dep[i]])

    def build(self):
        self.declare()
        k = self.k
        self.consts()
        self.eps_t = k.sb([128, 1], F32, "eps_t")
        k.op("pool", lambda e: e.memset(self.eps_t[:], EPS), writes=[self.eps_t])
        self.modT = k.sb([128, 48, 2], F32, "modT")
        self.nT = k.sb([128, 8, 2], F32, "nT")
        self.g1T = k.sb([128, 8, 2], F32, "g1T")
        self.g2T = k.sb([128, 8, 2], F32, "g2T")
        self.fnT = k.sb([128, 8, 1], F32, "fnT")
        self.scT = k.sb([128, 8, 2], BF16, "scT")
        self.xs_dep = [DR() for _ in range(NT)]
        self.xcur, self.xcur_dep = self.I["xs"], self.xs_dep
        for l in range(self.nlayers):
            if "p0" in self.phases:
                with k.scope():
                    self.p0_alloc()
                    self.p0(l)
            if "p1" in self.phases:
                with k.scope():
                    self.p1_alloc()
                    self.p1(l, self.xcur, self.xcur_dep)
            if "p2" in self.phases:
                with k.scope():
                    self.p2(l)
            if "p3" in self.phases:
                with k.scope():
                    self.p3(l)
            if "p4" in self.phases:
                with k.scope():
                    self.p4(l)
                self.xcur, self.xcur_dep = self.S["x1"], self.x1_dep
        k.barrier()
        self.es.close()
        return self.nc


def _core_inputs(inp, core):
    b, j = core // 2, core % 2
    f = (lambda a: a[::-1]) if j else (lambda a: a)
    d = {}
    d["xs"] = np.ascontiguousarray(np.concatenate([f(inp["ctx"][b]), f(inp["x"][b])], axis=0))
    d["cvec"] = np.ascontiguousarray(np.stack([inp["c"][b], inp["c_ctx"]], axis=0))
    w_in = inp["w_in"]
    if j:
        w_in = w_in.copy()
        for base, n in ((2048, 4), (2056, 4), (3600, 16)):
            a = w_in[:, :, base:base + n].copy()
            w_in[:, :, base:base + n] = w_in[:, :, base + n:base + 2 * n]
            w_in[:, :, base + n:base + 2 * n] = a
    d["w_in"] = np.ascontiguousarray(w_in)
    sw = (lambda a: a[:, ::-1]) if j else (lambda a: a)
    d["conv"] = np.ascontiguousarray(sw(inp["conv_qkv"]))
    d["dn_a_log"] = np.ascontiguousarray(sw(inp["dn_a_log"]).reshape(2, 8))
    d["dn_dt_bias"] = np.ascontiguousarray(sw(inp["dn_dt_bias"]).reshape(2, 8))
    d["gla_w_gate2"] = np.ascontiguousarray(sw(inp["gla_w_gate2"]))
    d["gla_b_gate"] = np.ascontiguousarray(sw(inp["gla_b_gate"]))
    for n in ("w_ada", "b_ada", "norm_mix", "norm_ffn", "dn_norm", "gla_norm", "w_out", "final_norm"):
        d[n] = np.ascontiguousarray(inp[n])
    d["ffn_w_gu"] = np.ascontiguousarray(inp["ffn_w_gu"][0])
    d["ffn_w_down"] = np.ascontiguousarray(inp["ffn_w_down"][0])
    d["moe_router"] = np.ascontiguousarray(inp["moe_router"][0])
    d["moe_w_gu"] = np.ascontiguousarray(inp["moe_w_gu"][0])
    d["moe_w_down"] = np.ascontiguousarray(inp["moe_w_down"][0])
    return d


def kernel(**inputs):
    inp = {k_: np.asarray(v, dtype=np.float32) for k_, v in inputs.items()}
    prog = Prog()
    nc = prog.build()
    in_maps = []
    for core in range(8):
        ci = _core_inputs(inp, core)
        in_maps.append({n: ci[n] for n in prog.I})
    res = run_bass_kernel_spmd(nc, in_maps, core_ids=list(range(8)))
    out = np.empty((4, LL, D), np.float32)
    for core in range(8):
        b, j = core // 2, core % 2
        y = np.asarray(res.results[core]["out"], dtype=np.float32)
        if j == 0:
            out[b, 0:NOWN] = y
        else:
            out[b, LL - NOWN:LL] = y[::-1]
    return out
```

```python
import numpy as np
from contextlib import ExitStack
import concourse.bass as bass
import concourse.mybir as mybir
from concourse.bass_utils import run_bass_kernel_spmd

F32 = mybir.dt.float32
BF16 = mybir.dt.bfloat16
AF = mybir.ActivationFunctionType
ALU = mybir.AluOpType
AX = mybir.AxisListType

D = 1024
LC = 256
LL = 8192
LS = LC + LL
NT = LS // 128
IN_COLS = 3632
NTM = IN_COLS - 1536
EPS = 1e-6
D_FF = 2816
D_EXP = 3584
NE = 8
NOWN = 4096


class Dep:
    __slots__ = ("w", "r")

    def __init__(self):
        self.w = None
        self.r = {}


class Tile:
    excl = False

    def __init__(self, t):
        self.t = t
        self.dep = Dep()

    def __getitem__(self, idx):
        return self.t[idx]


class K:
    RING = 64

    def __init__(self, nc, es):
        self.nc = nc
        self.es = es
        self.E = {"pe": nc.tensor, "dve": nc.vector, "act": nc.scalar, "pool": nc.gpsimd, "sp": nc.sync}
        self.sems = {}
        for e in self.E:
            self.sems[e] = es.enter_context(nc.semaphore("sem_" + e))
        for i in range(self.RING):
            self.sems[("d", i)] = es.enter_context(nc.semaphore("dsem%d" % i))
        self.cnt = {e: 0 for e in self.E}
        self.seen = {e: {} for e in self.E}
        self.dn = 0
        self.dtar = [0] * self.RING
        self.nt = 0

    def sb(self, shape, dt, name=None):
        self.nt += 1
        return Tile(self.es.enter_context(self.nc.sbuf_tensor("%s_%d" % (name or "t", self.nt), list(shape), dt)))

    def psum(self, shape, dt, name=None):
        self.nt += 1
        t = Tile(self.es.enter_context(self.nc.psum_tensor(name or ("p%d" % self.nt), list(shape), dt)))
        t.excl = True
        return t

    def _wait(self, eng, tok):
        if tok is None:
            return
        key, val = tok
        if key == eng and eng == "pe":
            return
        if self.seen[eng].get(key, 0) >= val:
            return
        self.E[eng].wait_ge(self.sems[key], val)
        self.seen[eng][key] = val

    def _pre(self, eng, reads, writes):
        for d in reads:
            self._wait(eng, d.dep.w)
        for d in writes:
            self._wait(eng, d.dep.w)
            for tok in d.dep.r.values():
                self._wait(eng, tok)

    def _post(self, tok, reads, writes):
        for d in reads:
            d.dep.r[tok[0]] = tok
        for d in writes:
            d.dep.w = tok
            d.dep.r = {}

    def op(self, eng, fn, reads=(), writes=()):
        ex = [d for d in reads if getattr(d, "excl", False)]
        if ex:
            writes = list(writes) + ex
            reads = [d for d in reads if not getattr(d, "excl", False)]
        self._pre(eng, reads, writes)
        ins = fn(self.E[eng])
        ins.then_inc(self.sems[eng], 1)
        self.cnt[eng] += 1
        self._post((eng, self.cnt[eng]), reads, writes)

    def dma(self, q, out, in_, reads=(), writes=(), **kw):
        self._pre(q, reads, writes)
        slot = self.dn % self.RING
        self.dn += 1
        key = ("d", slot)
        if self.dtar[slot] > 0:
            self._wait(q, (key, self.dtar[slot]))
        self.dtar[slot] += 16
        self.E[q].dma_start(out=out, in_=in_, **kw).then_inc(self.sems[key], 16)
        self._post((key, self.dtar[slot]), reads, writes)

    def barrier(self):
        for e in self.E:
            for e2 in self.E:
                if e2 != e and self.cnt[e2] > 0:
                    self._wait(e, (e2, self.cnt[e2]))
            for s in range(self.RING):
                if self.dtar[s] > 0:
                    self._wait(e, (("d", s), self.dtar[s]))

    def scope(self):
        outer = self.es
        k = self

        class _S:
            def __enter__(s):
                k.es = ExitStack()
                return s

            def __exit__(s, *a):
                k.barrier()
                k.es.close()
                k.es = outer
                return False
        return _S()

    def finish(self, deps):
        for d in deps:
            self._wait("sp", d.dep.w)


class DR:
    def __init__(self):
        self.dep = Dep()


class Prog:
    def __init__(self, debug=(), nlayers=2, phases=("p0", "p1", "p2", "p3", "p4"), ntiles=NT):
        self.debug = tuple(debug)
        self.nlayers = nlayers
        self.phases = phases
        self.ntiles = ntiles
        self.p2_stop = 9
        nc = self.nc = bass.Bass("TRN2", target_bir_lowering=False)
        self.I = {}
        self.S = {}
        self.es = ExitStack()
        self.k = K(nc, self.es)

    def inp(self, name, shape):
        self.I[name] = self.nc.dram_tensor(name, list(shape), F32, kind="ExternalInput").ap()
        return self.I[name]

    def scr(self, name, shape, dt=F32):
        kind = "ExternalOutput" if name in self.debug else "Internal"
        self.S[name] = self.nc.dram_tensor(name, list(shape), dt, kind=kind).ap()
        return self.S[name]

    def declare(self):
        inp = self.inp
        inp("xs", [LS, D]); inp("cvec", [2, D]); inp("w_ada", [2, D, 6 * D]); inp("b_ada", [2, 6 * D])
        inp("norm_mix", [2, D]); inp("norm_ffn", [2, D]); inp("w_in", [2, D, IN_COLS]); inp("conv", [2, 5, 1536])
        inp("dn_a_log", [2, 8]); inp("dn_dt_bias", [2, 8]); inp("dn_norm", [2, 128])
        inp("gla_w_gate2", [2, 2, 16, 256]); inp("gla_b_gate", [2, 2, 256]); inp("gla_norm", [2, 128])
        if "p4" in self.phases:
            inp("w_out", [2, D, D]); inp("ffn_w_gu", [D, 2 * D_FF]); inp("ffn_w_down", [D_FF, D])
            if self.nlayers > 1:
                inp("moe_router", [D, NE]); inp("moe_w_gu", [NE, D, 2 * D_EXP]); inp("moe_w_down", [NE, D_EXP, D])
        inp("final_norm", [D])
        self.out = self.nc.dram_tensor("out", [NOWN, D], F32, kind="ExternalOutput").ap()
        self.out_dep = DR()
        self.scr("tm", [LS, NTM]); self.scr("qkvT", [1536, LS + 8], BF16)
        self.scr("o_dn", [2, LS, 512]); self.scr("o_gla", [2, LS, 512]); self.scr("x1", [LS, D])
        self.x1_dep = [DR() for _ in range(NT)]
        self.odn_dep = [[DR() for _ in range(NT)] for _ in range(2)]
        self.ogla_dep = [[DR() for _ in range(NT)] for _ in range(2)]
        self.tm_dep = [DR() for _ in range(NT)]
        self.qkv_dep = [DR() for _ in range(NT)]
        self.qkv_pad_dep = DR()

    def dbg(self, name, tile, shape, dt=F32):
        if name not in self.debug or name in self.S:
            return
        if len(shape) == 2 and len(tile.t.shape) == 4:
            ap = self.nc.dram_tensor(name, list(shape), dt, kind="ExternalOutput").ap()
            self.S[name] = ap
            self.k.dma("sp", ap, tile[:].rearrange("p a b c -> p (a b c)"), reads=[tile], writes=[DR()])
            return
        ap = self.nc.dram_tensor(name, list(shape), dt, kind="ExternalOutput").ap()
        self.S[name] = ap
        src_ap = tile[:]
        if len(shape) == 2 and len(tile.t.shape) == 3:
            src_ap = tile[:].rearrange("p a b -> p (a b)")
        self.k.dma("sp", ap, src_ap, reads=[tile], writes=[DR()])

    def consts(self):
        k = self.k
        self.ident_f = k.sb([128, 128], F32, "ident_f")
        self.ident_b = k.sb([128, 128], BF16, "ident_b")
        self.ones_b = k.sb([128, 128], BF16, "ones_b")
        self.ones_f = k.sb([128, 128], F32, "ones_f")
        self.zeros_f = k.sb([128, 8], BF16, "zeros_b")
        idf, idb = self.ident_f, self.ident_b
        k.op("pool", lambda e: e.memset(idf[:], 1.0), writes=[idf])
        k.op("pool", lambda e: e.affine_select(out=idf[:], in_=idf[:], pattern=[[1, 128]], compare_op=ALU.is_equal,
                                                fill=0.0, base=0, channel_multiplier=-1), reads=[idf], writes=[idf])
        k.op("pool", lambda e: e.tensor_copy(out=idb[:], in_=idf[:]), reads=[idf], writes=[idb])
        k.op("pool", lambda e: e.memset(self.ones_b[:], 1.0), writes=[self.ones_b])
        k.op("pool", lambda e: e.memset(self.ones_f[:], 1.0), writes=[self.ones_f])
        k.op("pool", lambda e: e.memset(self.zeros_f[:], 0.0), writes=[self.zeros_f])
        self.ps = [k.psum([128, 512], F32, "ps%d" % i) for i in range(8)]
        self.psi = 0

    def bank(self):
        b = self.ps[self.psi % 8]
        self.psi += 1
        return b

    def rows_to_cols(self, rows, R, n, dst):
        k = self.k
        b = self.bank()
        for c in range(n):
            k.op("pe", lambda e, c=c: e.transpose(out=b[:, c * R:(c + 1) * R], in_=rows[0:R, c * 128:(c + 1) * 128],
                                                  identity=self.ident_f[0:R, 0:R]), reads=[rows, self.ident_f], writes=[b])
        k.op("dve", lambda e: e.tensor_copy(out=dst[:].rearrange("p n r -> p (n r)"), in_=b[:, 0:n * R]), reads=[b], writes=[dst])

    def p0_alloc(self):
        k = self.k
        self.crow = k.sb([2, D], F32, "crow")
        self.cT = k.sb([128, 8, 2], F32, "cT")
        self.wada = [k.sb([128, 8, 512], BF16, "wada%d" % i) for i in range(2)]
        self.modrow = k.sb([2, 6 * D], F32, "modrow")
        self.brow = k.sb([2, 6 * D], F32, "brow")
        self.nrow = k.sb([2, D], F32, "nrow")
        self.fnrow = k.sb([1, D], F32, "fnrow")

    def p0(self, l):
        k, I = self.k, self.I
        crow, cT, scT, modrow, brow, modT = self.crow, self.cT, self.scT, self.modrow, self.brow, self.modT
        if l == 0:
            k.dma("sp", crow[:], I["cvec"], writes=[crow])
            self.rows_to_cols(crow, 2, 8, cT)
            k.op("act", lambda e: e.activation(out=scT[:], in_=cT[:], func=AF.Silu), reads=[cT], writes=[scT])
            k.dma("sp", self.fnrow[:], I["final_norm"].rearrange("(o n) -> o n", o=1), writes=[self.fnrow])
            self.rows_to_cols(self.fnrow, 1, 8, self.fnT)
        for r in range(2):
            k.dma("sp", brow[r:r + 1, :], I["b_ada"][l:l + 1, :], writes=[brow])
        k.dma("sp", self.nrow[0:1, :], I["norm_mix"][l:l + 1, :], writes=[self.nrow])
        k.dma("sp", self.nrow[1:2, :], I["norm_ffn"][l:l + 1, :], writes=[self.nrow])
        self.rows_to_cols(self.nrow, 2, 8, self.nT)
        wsrc = I["w_ada"][l].rearrange("(k p) n -> p k n", p=128)
        for g in range(12):
            wt = self.wada[g % 2]
            k.dma("pool", wt[:], wsrc[:, :, g * 512:(g + 1) * 512], writes=[wt], max_dma_last_dim=2048)
            b = self.bank()
            for kk in range(8):
                k.op("pe", lambda e, kk=kk: e.matmul(b[0:2, :], lhsT=scT[:, kk, :], rhs=wt[:, kk, :], start=(kk == 0), stop=(kk == 7)),
                     reads=[scT, wt], writes=[b])
            k.op("dve", lambda e, g=g: e.tensor_tensor(out=modrow[:, g * 512:(g + 1) * 512], in0=b[0:2, :], in1=brow[:, g * 512:(g + 1) * 512], op=ALU.add),
                 reads=[b, brow], writes=[modrow])
        self.rows_to_cols(modrow, 2, 48, modT)
        for r in range(2):
            k.op("dve", lambda e, r=r: e.scalar_tensor_tensor(out=self.g1T[:, :, r], in0=modT[:, 8:16, r], scalar=1.0, in1=self.nT[:, :, 0], op0=ALU.add, op1=ALU.mult),
                 reads=[modT, self.nT], writes=[self.g1T])
            k.op("dve", lambda e, r=r: e.scalar_tensor_tensor(out=self.g2T[:, :, r], in0=modT[:, 32:40, r], scalar=1.0, in1=self.nT[:, :, 1], op0=ALU.add, op1=ALU.mult),
                 reads=[modT, self.nT], writes=[self.g2T])

    def p1_alloc(self):
        k = self.k
        self.w_fm = k.sb([128, 8, 1536], BF16, "w_fm")
        self.w_tm = k.sb([128, 8, NTM], BF16, "w_tm")
        self.xt = [k.sb([128, D], F32, "xt%d" % i) for i in range(2)]
        self.junk = k.sb([128, D], BF16, "junk")
        self.ss = [k.sb([128, 2], F32, "ss%d" % i) for i in range(2)]
        self.xn = [k.sb([128, D], BF16, "xn%d" % i) for i in range(2)]
        self.hT = [k.sb([128, 8, 512], BF16, "hT%d" % i) for i in range(2)]
        self.tmo = [k.sb([128, NTM], F32, "tmo%d" % i) for i in range(2)]
        self.qo = [k.sb([128, 512], BF16, "qo%d" % i) for i in range(3)]

    def p1(self, l, xsrc, xsrc_dep):
        k, I, S = self.k, self.I, self.S
        wsrc = I["w_in"][l].rearrange("(k p) n -> p k n", p=128)
        for kk in range(8):
            k.dma("pool", self.w_fm[:, kk, :], wsrc[:, kk, 0:1536], writes=[self.w_fm], max_dma_last_dim=2048)
            k.dma("pool", self.w_tm[:, kk, :], wsrc[:, kk, 1536:IN_COLS], writes=[self.w_tm], max_dma_last_dim=2048)
        if l == 0:
            for (c0, n) in ((0, 2), (258, 4), (LS + 6, 2)):
                for ch in range(12):
                    k.dma("sp", S["qkvT"][ch * 128:(ch + 1) * 128, c0:c0 + n], self.zeros_f[:, 0:n], reads=[self.zeros_f], writes=[self.qkv_pad_dep])
        blocks = [(0, 2)] + [(2 + 4 * i, 4) for i in range(16)]
        blocks = [b_ for b_ in blocks if b_[0] < self.ntiles]

        def cyc(ids):
            st = [0]

            def f():
                b = self.ps[ids[st[0] % len(ids)]]
                st[0] += 1
                return b
            return f
        bk_p, bk_t, bk_f = cyc([0, 1]), cyc([2, 3, 4]), cyc([5, 6, 7])

        def prep(bk, hT, t0, nt):
            r = 1 if t0 < 2 else 0
            for ti in range(nt):
                i = t0 + ti
                x_t, ss, xn = self.xt[i % 2], self.ss[i % 2], self.xn[i % 2]
                k.dma("sp", x_t[:], xsrc[i * 128:(i + 1) * 128, :], reads=[xsrc_dep[i]], writes=[x_t])
                k.op("act", lambda e: e.activation(out=self.junk[:], in_=x_t[:], func=AF.Square, accum_out=ss[:, 0:1]), reads=[x_t], writes=[self.junk, ss])
                yield
                k.op("act", lambda e: e.activation(out=ss[:, 1:2], in_=ss[:, 0:1], func=AF.Sqrt, scale=1.0 / D, bias=self.eps_t[:, 0:1]), reads=[ss, self.eps_t], writes=[ss])
                yield
                k.op("dve", lambda e: e.reciprocal(out=ss[:, 1:2], in_=ss[:, 1:2]), reads=[ss], writes=[ss])
                k.op("dve", lambda e: e.tensor_scalar(out=xn[:], in0=x_t[:], scalar1=ss[:, 1:2], scalar2=None, op0=ALU.mult), reads=[x_t, ss], writes=[xn])
                yield
                b = bk()
                bb = b.t[:, :].bitcast(BF16)
                for kk in range(8):
                    k.op("pe", lambda e, kk=kk: e.transpose(out=bb[:, kk * 128:(kk + 1) * 128], in_=xn[:, kk * 128:(kk + 1) * 128], identity=self.ident_b[:]),
                         reads=[xn, self.ident_b], writes=[b])
                yield
                for kk in range(8):
                    if kk % 2 == 0:
                        k.op("act", lambda e, kk=kk: e.activation(out=hT[:, kk, ti * 128:(ti + 1) * 128], in_=bb[:, kk * 128:(kk + 1) * 128], func=AF.Identity,
                                                                  scale=self.g1T[:, kk, r:r + 1], bias=self.modT[:, kk, r:r + 1]),
                             reads=[b, self.g1T, self.modT], writes=[hT])
                    else:
                        k.op("dve", lambda e, kk=kk: e.tensor_scalar(out=hT[:, kk, ti * 128:(ti + 1) * 128], in0=bb[:, kk * 128:(kk + 1) * 128],
                                                                     scalar1=self.g1T[:, kk, r:r + 1], scalar2=self.modT[:, kk, r:r + 1], op0=ALU.mult, op1=ALU.add),
                             reads=[b, self.g1T, self.modT], writes=[hT])
                    if kk % 2:
                        yield

        def tmproj(bk, hT, t0, nt):
            ev = 0
            for ti in range(nt):
                i = t0 + ti
                tmo = self.tmo[i % 2]
                for cg in range(5):
                    n = min(512, NTM - cg * 512)
                    b2 = bk()
                    for kk in range(8):
                        k.op("pe", lambda e, kk=kk: e.matmul(b2[:, 0:n], lhsT=hT[:, kk, ti * 128:(ti + 1) * 128], rhs=self.w_tm[:, kk, cg * 512:cg * 512 + n],
                                                             start=(kk == 0), stop=(kk == 7)), reads=[hT, self.w_tm], writes=[b2])
                    yield
                    ev += 1
                    if ev % 2:
                        k.op("act", lambda e: e.copy(out=tmo[:, cg * 512:cg * 512 + n], in_=b2[:, 0:n]), reads=[b2], writes=[tmo])
                    else:
                        k.op("dve", lambda e: e.tensor_copy(out=tmo[:, cg * 512:cg * 512 + n], in_=b2[:, 0:n]), reads=[b2], writes=[tmo])
                k.dma("pool", S["tm"][i * 128:(i + 1) * 128, :], tmo[:], reads=[tmo], writes=[self.tm_dep[i]])
                yield

        def fmproj(bk, hT, t0, nt):
            T = nt * 128
            col0 = t0 * 128 + (2 if t0 < 2 else 6)
            for ch in range(12):
                b3 = bk()
                for kk in range(8):
                    k.op("pe", lambda e, kk=kk: e.matmul(b3[:, 0:T], lhsT=self.w_fm[:, kk, ch * 128:(ch + 1) * 128], rhs=hT[:, kk, 0:T], start=(kk == 0), stop=(kk == 7)),
                         reads=[hT, self.w_fm], writes=[b3])
                yield
                qo = self.qo[ch % 3]
                if ch % 2:
                    k.op("act", lambda e: e.copy(out=qo[:, 0:T], in_=b3[:, 0:T]), reads=[b3], writes=[qo])
                else:
                    k.op("dve", lambda e: e.tensor_copy(out=qo[:, 0:T], in_=b3[:, 0:T]), reads=[b3], writes=[qo])
                k.dma("pool", S["qkvT"][ch * 128:(ch + 1) * 128, col0:col0 + T], qo[:, 0:T], reads=[qo], writes=[self.qkv_dep[t0 + j] for j in range(nt)])
                yield

        self.interleave([prep(bk_p, self.hT[0], *blocks[0])])
        for bi, (t0, nt) in enumerate(blocks):
            hT = self.hT[bi % 2]
            gens = [tmproj(bk_t, hT, t0, nt), fmproj(bk_f, hT, t0, nt)]
            if bi + 1 < len(blocks):
                gens.append(prep(bk_p, self.hT[(bi + 1) % 2], *blocks[bi + 1]))
            self.interleave(gens)

    def dir_masks(self, d):
        k = self.k
        mI = k.sb([128, 4, 128], BF16, "maskI")
        mS = k.sb([128, 4, 128], BF16, "maskS")
        tri = k.sb([128, 128], F32, "tri")
        sgn = 1 if d == 0 else -1
        for (m, cmp) in ((mI, ALU.is_ge), (mS, ALU.is_gt)):
            k.op("pool", lambda e: e.memset(m[:], 1.0), writes=[m])
            k.op("pool", lambda e: e.affine_select(out=m[:], in_=m[:], pattern=[[0, 4], [sgn, 128]], compare_op=cmp, fill=0.0,
                                                    base=0, channel_multiplier=-sgn), reads=[m], writes=[m])
        k.op("pool", lambda e: e.tensor_copy(out=tri[:], in_=mI[:, 0, :]), reads=[mI], writes=[tri])
        return mI, mS, tri

    def bcast_row(self, src_row_ap, n, dst):
        self.k.dma("sp", dst[:, 0:n], src_row_ap.broadcast_to([128, n]), writes=[dst])

    @staticmethod
    def interleave(gens):
        gens = list(gens)
        while gens:
            nxt = []
            for g in gens:
                try:
                    next(g)
                    nxt.append(g)
                except StopIteration:
                    pass
            gens = nxt

    def p2(self, l):
        k, I, S = self.k, self.I, self.S
        sb = k.sb
        flat = lambda t: t[:].rearrange("p a b -> p (a b)")
        convrow = sb([5, 1536], F32, "convrow")
        cw = sb([128, 12, 5], F32, "cw")
        k.dma("sp", convrow[:], I["conv"][l], writes=[convrow])
        self.rows_to_cols(convrow, 5, 12, cw)
        dtb = sb([128, 8], F32, "dtb")
        nA = sb([128, 8], F32, "nA")
        self.bcast_row(I["dn_dt_bias"][l:l + 1, :], 8, dtb)
        self.bcast_row(I["dn_a_log"][l:l + 1, :], 8, nA)
        k.op("act", lambda e: e.activation(out=nA[:], in_=nA[:], func=AF.Exp), reads=[nA], writes=[nA])
        k.op("dve", lambda e: e.tensor_scalar(out=nA[:], in0=nA[:], scalar1=-1.0, scalar2=None, op0=ALU.mult), reads=[nA], writes=[nA])
        ident4b = sb([128, 4, 128], BF16, "ident4b")
        for h in range(4):
            k.op("pool", lambda e: e.tensor_copy(out=ident4b[:, h, :], in_=self.ident_f[:]), reads=[self.ident_f], writes=[ident4b])
        qeps = sb([128, 1], F32, "qeps")
        k.op("pool", lambda e: e.memset(qeps[:], 128.0 * EPS), writes=[qeps])
        bd32 = sb([128, 4, 128], BF16, "bd32"); m1 = sb([128, 4, 128], BF16, "m1"); m2 = sb([128, 4, 128], BF16, "m2")
        for (bs_, nb_, dst) in ((32, 4, bd32), (64, 2, m1)):
            E = sb([4, 128], F32, "Eblk%d" % bs_)
            k.op("pool", lambda e: e.memset(E[:], 1.0), writes=[E])
            k.op("pool", lambda e: e.affine_select(out=E[:], in_=E[:], pattern=[[1, 128]], compare_op=ALU.is_ge, fill=0.0, base=0, channel_multiplier=-bs_), reads=[E], writes=[E])
            k.op("pool", lambda e: e.affine_select(out=E[:], in_=E[:], pattern=[[-1, 128]], compare_op=ALU.is_ge, fill=0.0, base=bs_ - 1, channel_multiplier=bs_), reads=[E], writes=[E])
            bm = self.bank()
            k.op("pe", lambda e: e.matmul(bm[:, 0:128], lhsT=E[0:nb_, :], rhs=E[0:nb_, :], start=True, stop=True), reads=[E], writes=[bm])
            for h in range(4):
                k.op("dve", lambda e: e.tensor_copy(out=dst[:, h, :], in_=bm[:, 0:128]), reads=[bm], writes=[dst])
        k.op("dve", lambda e: e.tensor_scalar(out=m2[:], in0=m1[:], scalar1=-1.0, scalar2=1.0, op0=ALU.mult, op1=ALU.add), reads=[m1], writes=[m2])
        k.op("dve", lambda e: e.tensor_tensor(out=m1[:], in0=m1[:], in1=bd32[:], op=ALU.subtract), reads=[m1, bd32], writes=[m1])
        raw = sb([128, 4, 516], BF16, "raw")
        diag = [sb([128, 5, 128], BF16, "diag%d" % i) for i in range(2)]
        acc = [sb([128, 512], F32, "acc%d" % i) for i in range(2)]
        sil = [sb([128, 512], F32, "sil%d" % i) for i in range(2)]
        sq = [sb([128, 512], BF16, "sq0")] * 2
        rn = [sb([128, 512], F32, "rn0")] * 2
        KQT = [sb([128, 4, 2, 512], BF16, "KQT%d" % i) for i in range(2)]
        VT = [sb([128, 4, 512], BF16, "VT%d" % i) for i in range(2)]
        KVtm = [[sb([128, 8, 128], BF16, "KVtm%d_%d" % (j, i)) for i in range(4)] for j in range(2)]
        def mk(n, shape, dt, name):
            return [sb(shape, dt, "%s%d" % (name, i)) for i in range(n)]
        sc4 = sb([128, 4, 16], F32, "sc4")
        Gd = mk(2, [128, 4, 128], F32, "Gd")
        Dm = mk(2, [128, 4, 128], F32, "Dm")
        Eg = mk(2, [128, 4, 128], F32, "Eg")
        A = mk(2, [128, 4, 128], BF16, "A"); AT = mk(2, [128, 4, 128], BF16, "AT")
        Abd = mk(2, [128, 4, 128], BF16, "Abd"); ATbd = mk(2, [128, 4, 128], BF16, "ATbd")
        Xa = mk(2, [128, 4, 128], BF16, "Xa"); XTa = mk(2, [128, 4, 128], BF16, "XTa")
        YT = mk(2, [128, 4, 128], BF16, "YT")
        R0 = mk(2, [128, 4, 128], BF16, "R0"); R1 = mk(2, [128, 4, 128], BF16, "R1")
        B1T = mk(2, [128, 4, 128], BF16, "B1T"); B2T = mk(2, [128, 4, 128], BF16, "B2T")
        Keg = mk(2, [128, 4, 128], BF16, "Keg")
        def mk2(shape, dt, name):
            return [[sb(shape, dt, "%s%d_%d" % (name, j, i)) for i in range(4)] for j in range(2)]
        smb = [sb([128, 4, 32], F32, "smb%d" % j) for j in range(3)]
        attnT = mk2([128, 4, 128], BF16, "attnT")
        up = mk2([128, 4, 128], F32, "up")
        wT = mk2([128, 4, 128], BF16, "wT")
        kdec = mk2([128, 4, 128], BF16, "kdec")
        qdecT = mk2([128, 4, 128], BF16, "qdecT")
        vnew = [sb([128, 4, 128], BF16, "vnew%d" % i) for i in range(2)]
        osb = [sb([128, 4, 128], F32, "osb%d" % i) for i in range(2)]
        Sf = sb([128, 4, 128], F32, "Sf")
        Sb = sb([128, 4, 128], BF16, "Sb")

        def stage_a(bk, d, tri, si, bp, t0, nt):
            T = nt * 128
            col0 = t0 * 128 + (2 if t0 < 2 else 6)
            kq, vt = KQT[bp], VT[bp]
            smT = smb[si]
            v = lambda a, b_: smT[:, 0:nt, a:b_]
            bcs = lambda t_: t_[:, 4 * d:4 * d + 4].unsqueeze(1).to_broadcast([128, nt, 4])
            k.dma("sp", sc4[:, 0:nt, :], S["tm"][t0 * 128:(t0 + nt) * 128, 512:528].rearrange("(t p) n -> p t n", p=128),
                  reads=[self.tm_dep[i] for i in range(t0, t0 + nt)], writes=[sc4])
            k.op("act", lambda e: e.activation(out=v(0, 4), in_=sc4[:, 0:nt, 4 * d:4 * d + 4], func=AF.Exp, scale=-1.0), reads=[sc4], writes=[smT])
            k.op("pool", lambda e: e.tensor_tensor(out=v(4, 8), in0=sc4[:, 0:nt, 8 + 4 * d:12 + 4 * d], in1=bcs(dtb), op=ALU.add), reads=[sc4, dtb], writes=[smT])
            yield
            k.op("dve", lambda e: e.tensor_scalar(out=v(0, 4), in0=v(0, 4), scalar1=1.0, scalar2=None, op0=ALU.add), reads=[smT], writes=[smT])
            k.op("dve", lambda e: e.reciprocal(out=v(0, 4), in_=v(0, 4)), reads=[smT], writes=[smT])
            k.op("act", lambda e: e.activation(out=v(4, 8), in_=v(4, 8), func=AF.Exp), reads=[smT], writes=[smT])
            yield
            k.op("act", lambda e: e.activation(out=v(4, 8), in_=v(4, 8), func=AF.Ln, bias=1.0), reads=[smT], writes=[smT])
            yield
            k.op("pool", lambda e: e.tensor_tensor(out=v(4, 8), in0=v(4, 8), in1=bcs(nA), op=ALU.mult), reads=[smT, nA], writes=[smT])
            yield
            bs = bk()
            for ti in range(nt):
                k.op("pe", lambda e: e.matmul(bs[:, ti * 8:ti * 8 + 4], lhsT=tri[:], rhs=smT[:, ti, 4:8], start=True, stop=True), reads=[tri, smT], writes=[bs])
                k.op("pe", lambda e: e.matmul(bs[:, ti * 8 + 4:ti * 8 + 8], lhsT=self.ones_f[:], rhs=smT[:, ti, 4:8], start=True, stop=True), reads=[self.ones_f, smT], writes=[bs])
            yield
            k.op("dve", lambda e: e.tensor_copy(out=v(8, 16), in_=bs[:, 0:nt * 8].rearrange("p (t n) -> p t n", t=nt)), reads=[bs], writes=[smT])
            yield
            k.op("act", lambda e: e.activation(out=v(16, 20), in_=v(8, 12), func=AF.Exp), reads=[smT], writes=[smT])
            k.op("dve", lambda e: e.tensor_tensor(out=v(20, 24), in0=v(12, 16), in1=v(8, 12), op=ALU.subtract), reads=[smT], writes=[smT])
            yield
            k.op("act", lambda e: e.activation(out=v(20, 24), in_=v(20, 24), func=AF.Exp), reads=[smT], writes=[smT])
            k.op("act", lambda e: e.activation(out=v(24, 28), in_=v(12, 16), func=AF.Exp), reads=[smT], writes=[smT])
            yield
            k.op("dve", lambda e: e.tensor_tensor(out=v(20, 24), in0=v(20, 24), in1=v(0, 4), op=ALU.mult), reads=[smT], writes=[smT])
            yield
            for cg in range(3):
                k.dma("sp", raw[:, :, 0:T + 4], S["qkvT"][cg * 512:(cg + 1) * 512, col0 - 2:col0 + T + 2].rearrange("(c p) t -> p c t", p=128),
                      reads=[self.qkv_dep[i] for i in range(max(t0 - 1, 0), min(t0 + nt + 1, NT))] + [self.qkv_pad_dep], writes=[raw])
                for c4 in range(4):
                    ch = cg * 4 + c4
                    dg = diag[ch % 2]
                    t1, s_, q_, r_ = acc[ch % 2], sil[ch % 2], sq[0], rn[0]
                    for j in range(5):
                        k.op("dve", lambda e: e.tensor_scalar(out=dg[:, j, :], in0=self.ident_b[:], scalar1=cw[:, ch, j:j + 1], scalar2=None, op0=ALU.mult),
                             reads=[self.ident_b, cw], writes=[dg])
                    yield
                    bc_ = bk()
                    for j in range(5):
                        k.op("pe", lambda e: e.matmul(bc_[:, 0:T], lhsT=dg[:, j, :], rhs=raw[:, c4, j:j + T], start=(j == 0), stop=(j == 4)), reads=[dg, raw], writes=[bc_])
                    yield
                    k.op("act", lambda e: e.activation(out=t1[:, 0:T], in_=bc_[:, 0:T], func=AF.Exp, scale=-1.0), reads=[bc_], writes=[t1])
                    yield
                    k.op("act", lambda e: e.activation(out=t1[:, 0:T], in_=t1[:, 0:T], func=AF.Ln, bias=1.0), reads=[t1], writes=[t1])
                    yield
                    k.op("act", lambda e: e.activation(out=t1[:, 0:T], in_=t1[:, 0:T], func=AF.Exp, scale=-1.0), reads=[t1], writes=[t1])
                    yield
                    if ch >= 8:
                        k.op("dve", lambda e: e.tensor_tensor(out=vt[:, ch - 8, 0:T], in0=bc_[:, 0:T], in1=t1[:, 0:T], op=ALU.mult), reads=[bc_, t1], writes=[vt])
                        yield
                        continue
                    k.op("dve", lambda e: e.tensor_tensor(out=s_[:, 0:T], in0=bc_[:, 0:T], in1=t1[:, 0:T], op=ALU.mult), reads=[bc_, t1], writes=[s_])
                    yield
                    k.op("act", lambda e: e.activation(out=q_[:, 0:T], in_=s_[:, 0:T], func=AF.Square), reads=[s_], writes=[q_])
                    yield
                    b = bk()
                    k.op("pe", lambda e: e.matmul(b[:, 0:T], lhsT=self.ones_b[:], rhs=q_[:, 0:T], start=True, stop=True), reads=[self.ones_b, q_], writes=[b])
                    if ch < 4:
                        k.op("act", lambda e: e.activation(out=r_[:, 0:T], in_=b[:, 0:T], func=AF.Ln, scale=128.0, bias=qeps[:, 0:1]), reads=[b, qeps], writes=[r_])
                    else:
                        k.op("act", lambda e: e.activation(out=r_[:, 0:T], in_=b[:, 0:T], func=AF.Ln, scale=1.0, bias=self.eps_t[:, 0:1]), reads=[b, self.eps_t], writes=[r_])
                    yield
                    k.op("act", lambda e: e.activation(out=r_[:, 0:T], in_=r_[:, 0:T], func=AF.Exp, scale=-0.5), reads=[r_], writes=[r_])
                    yield
                    hh, which = (ch, 1) if ch < 4 else (ch - 4, 0)
                    k.op("dve", lambda e: e.tensor_tensor(out=kq[:, hh, which, 0:T], in0=s_[:, 0:T], in1=r_[:, 0:T], op=ALU.mult), reads=[s_, r_], writes=[kq])
                    yield
            for ti in range(nt):
                b = bk()
                bb = b.t[:, :].bitcast(BF16)
                for h in range(4):
                    k.op("pe", lambda e: e.transpose(out=bb[:, h * 128:(h + 1) * 128], in_=kq[:, h, 0, ti * 128:(ti + 1) * 128], identity=self.ident_b[:]),
                         reads=[kq, self.ident_b], writes=[b])
                    k.op("pe", lambda e: e.transpose(out=bb[:, (4 + h) * 128:(5 + h) * 128], in_=vt[:, h, ti * 128:(ti + 1) * 128], identity=self.ident_b[:]),
                         reads=[vt, self.ident_b], writes=[b])
                yield
                k.op("act", lambda e: e.copy(out=KVtm[bp][ti][:].rearrange("p a b -> p (a b)"), in_=bb[:, :]), reads=[b], writes=[KVtm[bp][ti]])
                yield

        def stage_b(bk, d, mI, mS, tri, si, bp, q, i, ti):
            kq, kv = KQT[bp], KVtm[bp][ti]
            tsl = slice(ti * 128, (ti + 1) * 128)
            smT = smb[si]
            sm_ = smT[:, ti, :]
            G = Gd[q]
            k.op("pool", lambda e: e.tensor_tensor(out=G[:], in0=mI[:], in1=sm_[:, 4:8].unsqueeze(2).to_broadcast([128, 4, 128]), op=ALU.mult),
                 reads=[mI, smT], writes=[G])
            yield
            bg = bk()
            k.op("pe", lambda e: e.matmul(bg[:, :], lhsT=self.ones_f[:], rhs=flat(G), start=True, stop=True), reads=[self.ones_f, G], writes=[bg])
            yield
            bg3 = bg[:, :].rearrange("p (a b) -> p a b", a=4)
            for h in range(4):
                k.op("dve", lambda e: e.tensor_scalar(out=Dm[q][:, h, :], in0=bg3[:, h, :], scalar1=sm_[:, 8 + h:9 + h], scalar2=0.0, op0=ALU.subtract, op1=ALU.min),
                     reads=[bg, smT], writes=[Dm[q]])
            yield
            k.op("act", lambda e: e.activation(out=flat(Eg[q]), in_=bg[:, :], func=AF.Exp), reads=[bg], writes=[Eg[q]])
            k.op("act", lambda e: e.activation(out=Dm[q][:], in_=Dm[q][:], func=AF.Exp), reads=[Dm[q]], writes=[Dm[q]])
            yield
            k.op("pool", lambda e: e.tensor_tensor(out=qdecT[bp][ti][:], in0=kq[:, :, 1, tsl], in1=Eg[q][:], op=ALU.mult), reads=[kq, Eg[q]], writes=[qdecT[bp][ti]])
            bet = sm_[:, 0:4].unsqueeze(2).to_broadcast([128, 4, 128])
            DmI, DmS = Gd[q], Eg[q]
            k.op("pool", lambda e: e.tensor_tensor(out=DmI[:], in0=Dm[q][:], in1=mI[:], op=ALU.mult), reads=[Dm[q], mI], writes=[DmI])
            yield
            k.op("pool", lambda e: e.tensor_tensor(out=DmS[:], in0=Dm[q][:], in1=mS[:], op=ALU.mult), reads=[Dm[q], mS], writes=[DmS])
            k.op("pool", lambda e: e.tensor_tensor(out=DmI[:], in0=DmI[:], in1=bet, op=ALU.mult), reads=[DmI, smT], writes=[DmI])
            k.op("pool", lambda e: e.tensor_tensor(out=DmS[:], in0=DmS[:], in1=bet, op=ALU.mult), reads=[DmS, smT], writes=[DmS])
            K3 = kv[:, 0:4, :]
            k.op("pool", lambda e: e.tensor_tensor(out=Keg[q][:], in0=K3, in1=sm_[:, 16:20].unsqueeze(2).to_broadcast([128, 4, 128]), op=ALU.mult), reads=[kv, smT], writes=[Keg[q]])
            k.op("pool", lambda e: e.tensor_tensor(out=kdec[bp][ti][:], in0=K3, in1=sm_[:, 20:24].unsqueeze(2).to_broadcast([128, 4, 128]), op=ALU.mult), reads=[kv, smT], writes=[kdec[bp][ti]])
            bkk, bkq = bk(), bk()
            for h in range(4):
                k.op("pe", lambda e: e.matmul(bkk[:, h * 128:(h + 1) * 128], lhsT=kq[:, h, 0, tsl], rhs=kq[:, h, 0, tsl], start=True, stop=True), reads=[kq], writes=[bkk])
                k.op("pe", lambda e: e.matmul(bkq[:, h * 128:(h + 1) * 128], lhsT=kq[:, h, 0, tsl], rhs=kq[:, h, 1, tsl], start=True, stop=True), reads=[kq], writes=[bkq])
            yield
            k.op("dve", lambda e: e.tensor_tensor(out=flat(A[q]), in0=bkk[:, :], in1=flat(DmS), op=ALU.mult), reads=[bkk, DmS], writes=[A[q]])
            k.op("dve", lambda e: e.tensor_tensor(out=flat(attnT[bp][ti]), in0=bkq[:, :], in1=flat(DmI), op=ALU.mult), reads=[bkq, DmI], writes=[attnT[bp][ti]])
            yield
            bt = bk()
            btb = bt.t[:, :].bitcast(BF16)
            for h in range(4):
                k.op("pe", lambda e: e.transpose(out=btb[:, h * 128:(h + 1) * 128], in_=A[q][:, h, :], identity=self.ident_b[:]), reads=[A[q], self.ident_b], writes=[bt])
            k.op("pool", lambda e: e.tensor_tensor(out=Abd[q][:], in0=A[q][:], in1=bd32[:], op=ALU.mult), reads=[A[q], bd32], writes=[Abd[q]])
            yield
            k.op("act", lambda e: e.copy(out=flat(AT[q]), in_=btb[:, 0:512]), reads=[bt], writes=[AT[q]])
            k.op("pool", lambda e: e.tensor_tensor(out=R0[q][:], in0=ident4b[:], in1=Abd[q][:], op=ALU.subtract), reads=[ident4b, Abd[q]], writes=[R0[q]])
            yield
            k.op("pool", lambda e: e.tensor_tensor(out=ATbd[q][:], in0=AT[q][:], in1=bd32[:], op=ALU.mult), reads=[AT[q], bd32], writes=[ATbd[q]])
            k.op("pool", lambda e: e.tensor_tensor(out=B1T[q][:], in0=AT[q][:], in1=m1[:], op=ALU.mult), reads=[AT[q], m1], writes=[B1T[q]])
            k.op("pool", lambda e: e.tensor_tensor(out=B2T[q][:], in0=AT[q][:], in1=m2[:], op=ALU.mult), reads=[AT[q], m2], writes=[B2T[q]])
            yield
            Xc, XTc, Rc = Abd[q], ATbd[q], R0[q]
            xbufs = [(Xa[q], XTa[q]), (A[q], AT[q])]
            rbufs = [R1[q], R0[q]]
            for lev in range(4):
                lastl = lev == 3
                Xn, XTn = xbufs[lev % 2]
                Rn = rbufs[lev % 2]
                b1 = bk()
                for h in range(4):
                    k.op("pe", lambda e: e.matmul(b1[:, h * 128:(h + 1) * 128], lhsT=Xc[:, h, :], rhs=XTc[:, h, :], start=True, stop=True), reads=[Xc, XTc], writes=[b1])
                if not lastl:
                    b2 = bk()
                    for h in range(4):
                        k.op("pe", lambda e: e.matmul(b2[:, h * 128:(h + 1) * 128], lhsT=XTc[:, h, :], rhs=Xc[:, h, :], start=True, stop=True), reads=[Xc, XTc], writes=[b2])
                yield
                k.op("dve", lambda e: e.tensor_tensor(out=flat(YT[q]), in0=b1[:, :], in1=flat(ident4b), op=ALU.add), reads=[b1, ident4b], writes=[YT[q]])
                if not lastl:
                    k.op("act", lambda e: e.copy(out=flat(XTn), in_=b1[:, :]), reads=[b1], writes=[XTn])
                    k.op("act", lambda e: e.copy(out=flat(Xn), in_=b2[:, :]), reads=[b2], writes=[Xn])
                yield
                b3 = bk()
                for h in range(4):
                    k.op("pe", lambda e: e.matmul(b3[:, h * 128:(h + 1) * 128], lhsT=YT[q][:, h, :], rhs=Rc[:, h, :], start=True, stop=True), reads=[YT[q], Rc], writes=[b3])
                yield
                k.op("dve", lambda e: e.tensor_copy(out=flat(Rn), in_=b3[:, :]), reads=[b3], writes=[Rn])
                yield
                Xc, XTc, Rc = Xn, XTn, Rn
            TTb, Pb = Xa[q], XTa[q]
            for mi, BT_ in enumerate((B1T[q], B2T[q])):
                Tn = rbufs[mi % 2]
                btt = bk()
                bttb = btt.t[:, :].bitcast(BF16)
                for h in range(4):
                    k.op("pe", lambda e: e.transpose(out=bttb[:, h * 128:(h + 1) * 128], in_=Rc[:, h, :], identity=self.ident_b[:]), reads=[Rc, self.ident_b], writes=[btt])
                bp_ = bk()
                for h in range(4):
                    k.op("pe", lambda e: e.matmul(bp_[:, h * 128:(h + 1) * 128], lhsT=BT_[:, h, :], rhs=Rc[:, h, :], start=True, stop=True), reads=[BT_, Rc], writes=[bp_])
                yield
                k.op("act", lambda e: e.copy(out=flat(TTb), in_=bttb[:, 0:512]), reads=[btt], writes=[TTb])
                k.op("dve", lambda e: e.tensor_copy(out=flat(Pb), in_=bp_[:, :]), reads=[bp_], writes=[Pb])
                yield
                bq = bk()
                for h in range(4):
                    k.op("pe", lambda e: e.matmul(bq[:, h * 128:(h + 1) * 128], lhsT=TTb[:, h, :], rhs=Pb[:, h, :], start=True, stop=True), reads=[TTb, Pb], writes=[bq])
                yield
                k.op("dve", lambda e: e.tensor_tensor(out=flat(Tn), in0=flat(Rc), in1=bq[:, :], op=ALU.subtract), reads=[Rc, bq], writes=[Tn])
                yield
                Rc = Tn
            T2T = Rc
            bu, bw = bk(), bk()
            for h in range(4):
                k.op("pe", lambda e: e.matmul(bu[:, h * 128:(h + 1) * 128], lhsT=T2T[:, h, :], rhs=kv[:, 4 + h, :], start=True, stop=True), reads=[T2T, kv], writes=[bu])
                k.op("pe", lambda e: e.matmul(bw[:, h * 128:(h + 1) * 128], lhsT=Keg[q][:, h, :], rhs=T2T[:, h, :], start=True, stop=True), reads=[T2T, Keg[q]], writes=[bw])
            yield
            k.op("act", lambda e: e.copy(out=flat(up[bp][ti]), in_=bu[:, :]), reads=[bu], writes=[up[bp][ti]])
            k.op("dve", lambda e: e.tensor_copy(out=flat(wT[bp][ti]), in_=bw[:, :]), reads=[bw], writes=[wT[bp][ti]])
            yield

        def scan(bk, d, si, bp, tiles):
            for n_, (i, ti) in enumerate(tiles):
                p = n_ % 2
                smT = smb[si]
                sm_ = smT[:, ti, :]
                bws = bk()
                for h in range(4):
                    k.op("pe", lambda e: e.matmul(bws[:, h * 128:(h + 1) * 128], lhsT=wT[bp][ti][:, h, :], rhs=Sb[:, h, :], start=True, stop=True), reads=[wT[bp][ti], Sb], writes=[bws])
                yield
                k.op("dve", lambda e: e.tensor_tensor(out=flat(vnew[p]), in0=flat(up[bp][ti]), in1=bws[:, :], op=ALU.subtract), reads=[up[bp][ti], bws], writes=[vnew[p]])
                yield
                bo, bkv = bk(), bk()
                for h in range(4):
                    k.op("pe", lambda e: e.matmul(bkv[:, h * 128:(h + 1) * 128], lhsT=kdec[bp][ti][:, h, :], rhs=vnew[p][:, h, :], start=True, stop=True), reads=[kdec[bp][ti], vnew[p]], writes=[bkv])
                for h in range(4):
                    k.op("pe", lambda e: e.matmul(bo[:, h * 128:(h + 1) * 128], lhsT=qdecT[bp][ti][:, h, :], rhs=Sb[:, h, :], start=True, stop=False), reads=[qdecT[bp][ti], Sb], writes=[bo])
                    k.op("pe", lambda e: e.matmul(bo[:, h * 128:(h + 1) * 128], lhsT=attnT[bp][ti][:, h, :], rhs=vnew[p][:, h, :], start=False, stop=True), reads=[attnT[bp][ti], vnew[p]], writes=[bo])
                yield
                bkv3 = bkv[:, :].rearrange("p (a b) -> p a b", a=4)
                for h in range(4):
                    k.op("dve", lambda e: e.scalar_tensor_tensor(out=Sf[:, h, :], in0=Sf[:, h, :], scalar=sm_[:, 24 + h:25 + h], in1=bkv3[:, h, :], op0=ALU.mult, op1=ALU.add),
                         reads=[Sf, smT, bkv], writes=[Sf])
                k.op("act", lambda e: e.copy(out=flat(osb[p]), in_=bo[:, :]), reads=[bo], writes=[osb[p]])
                yield
                k.op("act", lambda e: e.copy(out=Sb[:], in_=Sf[:]), reads=[Sf], writes=[Sb])
                k.dma("act", S["o_dn"][d, i * 128:(i + 1) * 128, :], flat(osb[p]), reads=[osb[p]], writes=[self.odn_dep[d][i]])
                yield

        def cyc(ids):
            st = [0]

            def f():
                b = self.ps[ids[st[0] % len(ids)]]
                st[0] += 1
                return b
            return f
        bk_b = [cyc([0, 1]), cyc([2, 3])]
        bk_a, bk_s = cyc([6, 7]), cyc([4, 5])
        blocks_f = [(0, 2)] + [(2 + 4 * i, 4) for i in range(16)]
        blocks_f = [b_ for b_ in blocks_f if b_[0] < self.ntiles]
        gbi = 0
        for d in range(2):
            mI, mS, tri = self.dir_masks(d)
            k.op("pool", lambda e: e.memset(Sf[:], 0.0), writes=[Sf])
            k.op("pool", lambda e: e.memset(Sb[:], 0.0), writes=[Sb])
            blocks = blocks_f if d == 0 else [blocks_f[0]] + blocks_f[:0:-1]
            nb = len(blocks)
            self.interleave([stage_a(bk_a, d, tri, gbi % 3, gbi % 2, *blocks[0])])
            prev = None
            for bi, (t0, nt) in enumerate(blocks):
                bp = (gbi + bi) % 2
                tis = list(range(nt)) if d == 0 else list(range(nt - 1, -1, -1))
                for pair in range(0, nt, 2):
                    gens = [stage_b(bk_b[q], d, mI, mS, tri, (gbi + bi) % 3, bp, q, t0 + ti, ti) for q, ti in enumerate(tis[pair:pair + 2])]
                    if pair == 0 and bi + 1 < nb:
                        gens.append(stage_a(bk_a, d, tri, (gbi + bi + 1) % 3, (gbi + bi + 1) % 2, *blocks[bi + 1]))
                    if prev is not None and (pair == 2 or nt == 2):
                        gens.append(scan(bk_s, d, *prev))
                    self.interleave(gens)
                prev = ((gbi + bi) % 3, bp, [(t0 + ti, ti) for ti in tis])
            self.interleave([scan(bk_s, d, *prev)])
            gbi += nb

    def p3(self, l):
        k, I, S = self.k, self.I, self.S
        sb = k.sb

        def two(shape, dt, name):
            return [sb(shape, dt, name + "%d" % i) for i in range(2)]
        qkv = two([128, 1024], F32, "gqkv")
        glr = two([128, 16], F32, "glr")
        lhsTg = two([64, 128], F32, "lhsTg")
        W2 = sb([64, 256], F32, "W2")
        ex = two([128, 256], F32, "gex")
        la = two([128, 256], F32, "la")
        bcs = two([128, 256], F32, "bcs")
        ebc = two([128, 256], F32, "ebc")
        enbc = two([128, 256], F32, "enbc")
        ekd = two([128, 256], F32, "ekd")
        qs = two([128, 256], BF16, "gqs")
        ks = two([128, 256], BF16, "gks")
        kd = two([128, 256], BF16, "gkd")
        vb = two([128, 4, 128], BF16, "gvb")
        qT = two([128, 4, 128], BF16, "gqTm")
        kT = two([128, 2, 128], BF16, "gkT")
        att = two([128, 4, 128], BF16, "gatt")
        osb = two([128, 512], F32, "gosb")
        etc_ = two([128, 4], F32, "getc")
        Sf = sb([128, 2, 128], F32, "gSf")
        Sb = sb([128, 2, 128], BF16, "gSb")
        for t_ in qT:
            k.op("pool", lambda e: e.memset(t_[:], 0.0), writes=[t_])
        for t_ in lhsTg:
            k.op("pool", lambda e: e.memset(t_[:], 0.0), writes=[t_])
            k.op("pool", lambda e: e.memset(t_[32:33, :], 1.0), writes=[t_])
        tm_lat = S["tm"][LC:LS, :].rearrange("(r w) n -> w r n", w=64)
        alltm = self.tm_dep
        for d in range(2):
            mI, mS, tri = self.dir_masks(d)
            k.op("pool", lambda e: e.memset(W2[:], 0.0), writes=[W2])
            k.dma("sp", W2[0:16, :], I["gla_w_gate2"][l, d], writes=[W2])
            k.dma("sp", W2[32:33, :], I["gla_b_gate"][l, d:d + 1, :], writes=[W2])
            k.op("pool", lambda e: e.memset(Sf[:], 0.0), writes=[Sf])
            k.op("pool", lambda e: e.memset(Sb[:], 0.0), writes=[Sb])
            order = [("c", 0), ("c", 1)] + [("g", j) for j in range(64)]
            if d == 1:
                order = [("c", 1), ("c", 0)] + [("g", j) for j in range(63, -1, -1)]
            if self.ntiles < NT:
                order = order[:self.ntiles]
            def cyc(ids):
                st = [0]

                def f():
                    b = self.ps[ids[st[0] % len(ids)]]
                    st[0] += 1
                    return b
                return f
            bk_pre, bk_post = cyc([0, 1, 2, 3]), cyc([4, 5])

            def pre(bk, p, kind, j):
                if kind == "c":
                    rows = S["tm"][j * 128:(j + 1) * 128, :]
                else:
                    rows = tm_lat[j]
                k.dma("sp", qkv[p][:], rows[:, 528:1552], reads=alltm, writes=[qkv[p]])
                k.dma("sp", glr[p][:], rows[:, 2064 + 16 * d:2080 + 16 * d], reads=alltm, writes=[glr[p]])
                b0 = bk()
                k.op("pe", lambda e: e.transpose(out=b0[0:16, 0:128], in_=glr[p][:, :], identity=self.ident_f[:]), reads=[glr[p], self.ident_f], writes=[b0])
                k.op("act", lambda e: e.copy(out=lhsTg[p][0:16, :], in_=b0[0:16, 0:128]), reads=[b0], writes=[lhsTg[p]])
                yield
                bgt = bk()
                k.op("pe", lambda e: e.matmul(bgt[:, 0:256], lhsT=lhsTg[p][:, :], rhs=W2[:, :], start=True, stop=True), reads=[lhsTg[p], W2], writes=[bgt])
                k.op("act", lambda e: e.activation(out=ex[p][:], in_=bgt[:, 0:256], func=AF.Exp, scale=-1.0), reads=[bgt], writes=[ex[p]])
                yield
                k.op("act", lambda e: e.activation(out=ex[p][:], in_=ex[p][:], func=AF.Ln, bias=1.0), reads=[ex[p]], writes=[ex[p]])
                yield
                k.op("dve", lambda e: e.tensor_scalar(out=la[p][:], in0=ex[p][:], scalar1=-1.0 / 16.0, scalar2=None, op0=ALU.mult), reads=[ex[p]], writes=[la[p]])
                yield
                bbc, bto = bk(), bk()
                k.op("pe", lambda e: e.matmul(bbc[:, 0:256], lhsT=tri[:], rhs=la[p][:], start=True, stop=True), reads=[tri, la[p]], writes=[bbc])
                k.op("pe", lambda e: e.matmul(bto[:, 0:256], lhsT=self.ones_f[:], rhs=la[p][:], start=True, stop=True), reads=[self.ones_f, la[p]], writes=[bto])
                for hp in range(2):
                    k.op("pe", lambda e: e.matmul(bto[:, 256 + 2 * hp:258 + 2 * hp], lhsT=la[p][:, hp * 128:(hp + 1) * 128], rhs=self.ones_f[:, 0:2], start=True, stop=True),
                         reads=[la[p], self.ones_f], writes=[bto])
                k.op("act", lambda e: e.activation(out=ebc[p][:], in_=bbc[:, 0:256], func=AF.Exp), reads=[bbc], writes=[ebc[p]])
                yield
                k.op("act", lambda e: e.activation(out=enbc[p][:], in_=bbc[:, 0:256], func=AF.Exp, scale=-1.0), reads=[bbc], writes=[enbc[p]])
                yield
                k.op("act", lambda e: e.copy(out=bcs[p][:], in_=bbc[:, 0:256]), reads=[bbc], writes=[bcs[p]])
                yield
                k.op("dve", lambda e: e.tensor_tensor(out=ekd[p][:], in0=bto[:, 0:256], in1=bcs[p][:], op=ALU.subtract), reads=[bto, bcs[p]], writes=[ekd[p]])
                yield
                k.op("act", lambda e: e.activation(out=ekd[p][:], in_=ekd[p][:], func=AF.Exp), reads=[ekd[p]], writes=[ekd[p]])
                yield
                k.op("act", lambda e: e.activation(out=etc_[p][:], in_=bto[:, 256:260], func=AF.Exp), reads=[bto], writes=[etc_[p]])
                yield
                k.op("dve", lambda e: e.scalar_tensor_tensor(out=qs[p][:], in0=qkv[p][:, 0:256], scalar=0.125, in1=ebc[p][:], op0=ALU.mult, op1=ALU.mult), reads=[qkv[p], ebc[p]], writes=[qs[p]])
                yield
                k.op("dve", lambda e: e.tensor_tensor(out=ks[p][:], in0=qkv[p][:, 256:512], in1=enbc[p][:], op=ALU.mult), reads=[qkv[p], enbc[p]], writes=[ks[p]])
                yield
                k.op("pool", lambda e: e.tensor_tensor(out=kd[p][:], in0=qkv[p][:, 256:512], in1=ekd[p][:], op=ALU.mult), reads=[qkv[p], ekd[p]], writes=[kd[p]])
                yield
                k.op("pool", lambda e: e.tensor_copy(out=vb[p][:].rearrange("p a b -> p (a b)"), in_=qkv[p][:, 512:1024]), reads=[qkv[p]], writes=[vb[p]])
                yield
                btr = bk()
                btrb = btr.t[:, :].bitcast(BF16)
                for hp in range(2):
                    k.op("pe", lambda e: e.transpose(out=btrb[:, hp * 128:(hp + 1) * 128], in_=qs[p][:, hp * 128:(hp + 1) * 128], identity=self.ident_b[:]), reads=[qs[p], self.ident_b], writes=[btr])
                    k.op("pe", lambda e: e.transpose(out=btrb[:, (2 + hp) * 128:(3 + hp) * 128], in_=ks[p][:, hp * 128:(hp + 1) * 128], identity=self.ident_b[:]), reads=[ks[p], self.ident_b], writes=[btr])
                for h in range(4):
                    hb, hp = h % 2, h // 2
                    k.op("act", lambda e: e.copy(out=qT[p][hb * 64:(hb + 1) * 64, h, :], in_=btrb[hb * 64:(hb + 1) * 64, hp * 128:(hp + 1) * 128]), reads=[btr], writes=[qT[p]])
                k.op("act", lambda e: e.copy(out=kT[p][:].rearrange("p a b -> p (a b)"), in_=btrb[:, 256:512]), reads=[btr], writes=[kT[p]])
                yield
                bat = bk()
                for h in range(4):
                    hb, hp = h % 2, h // 2
                    ps_ = slice(hb * 64, (hb + 1) * 64)
                    k.op("pe", lambda e: e.matmul(bat[:, h * 128:(h + 1) * 128], lhsT=kT[p][:, hp, :], rhs=qT[p][:, h, :], start=True, stop=True), reads=[kT[p], qT[p]], writes=[bat])
                k.op("dve", lambda e: e.tensor_tensor(out=att[p][:].rearrange("p a b -> p (a b)"), in0=bat[:, :], in1=mI[:].rearrange("p a b -> p (a b)"), op=ALU.mult), reads=[bat, mI], writes=[att[p]])
                yield
                yield

            def post(bk, p, kind, j):
                if kind == "c":
                    orows = S["o_gla"][d, j * 128:(j + 1) * 128, :]
                    odeps = [self.ogla_dep[d][j]]
                else:
                    orows = S["o_gla"][d, LC:LS, :].rearrange("(r w) n -> w r n", w=64)[j]
                    odeps = self.ogla_dep[d][2:]
                bo, bkv = bk(), bk()
                for h in range(4):
                    hb, hp = h % 2, h // 2
                    ps_ = slice(hb * 64, (hb + 1) * 64)
                    k.op("pe", lambda e: e.matmul(bo[:, h * 128:(h + 1) * 128], lhsT=qT[p][:, h, :], rhs=Sb[:, hp, :], start=True, stop=False), reads=[qT[p], Sb], writes=[bo])
                    k.op("pe", lambda e: e.matmul(bo[:, h * 128:(h + 1) * 128], lhsT=att[p][:, h, :], rhs=vb[p][:, h, :], start=False, stop=True), reads=[att[p], vb[p]], writes=[bo])
                for h in range(4):
                    hb, hp = h % 2, h // 2
                    k.op("pe", lambda e: e.matmul(bkv[:, h * 128:(h + 1) * 128], lhsT=kd[p][:, hp * 128:(hp + 1) * 128], rhs=vb[p][:, h, :], start=True, stop=True),
                         reads=[kd[p], vb[p]], writes=[bkv])
                k.op("act", lambda e: e.copy(out=osb[p][:], in_=bo[:, :]), reads=[bo], writes=[osb[p]])
                yield
                k.dma("act", orows, osb[p][:], reads=[osb[p]], writes=odeps)
                for h in range(4):
                    hb, hp = h % 2, h // 2
                    ps_ = slice(hb * 64, (hb + 1) * 64)
                    k.op("dve", lambda e: e.scalar_tensor_tensor(out=Sf[ps_, hp, :], in0=Sf[ps_, hp, :], scalar=etc_[p][ps_, 2 * hp:2 * hp + 1], in1=bkv[ps_, h * 128:(h + 1) * 128], op0=ALU.mult, op1=ALU.add),
                         reads=[Sf, etc_[p], bkv], writes=[Sf])
                k.op("act", lambda e: e.copy(out=Sb[:], in_=Sf[:]), reads=[Sf], writes=[Sb])
                yield


                yield

            self.interleave([pre(bk_pre, 0, *order[0])])
            for step, (kind, j) in enumerate(order):
                gens = [post(bk_post, step % 2, kind, j)]
                if step + 1 < len(order):
                    gens.append(pre(bk_pre, (step + 1) % 2, *order[step + 1]))
                self.interleave(gens)
    def p4(self, l):
        k, I, S = self.k, self.I, self.S
        sb = k.sb
        last = l == self.nlayers - 1
        moe = (l % 2 == 1)
        T = 1024
        NH = T // 512
        gnb = sb([128, 8, 128], F32, "gnb")
        for h in range(8):
            src_ = I["dn_norm"] if h < 4 else I["gla_norm"]
            k.dma("sp", gnb[:, h, :], src_[l:l + 1, :].broadcast_to([128, 128]), writes=[gnb])
        xT = sb([128, 8, T], F32, "xT")
        mT = sb([128, 8, T], BF16, "mT")
        sq2 = mT
        h2T = sb([128, 8, T], BF16, "h2T")
        rs2 = sb([128, T], F32, "rs2")
        tmpf = [sb([128, T], F32, "tmpf%d" % i) for i in range(2)]
        F = D_EXP if moe else D_FF
        NFC = F // 128
        actT = sb([128, NFC, T], BF16, "actT")
        ot = [sb([128, 512], F32, "ot%d" % i) for i in range(4)]
        zt = sb([128, 1024], F32, "zt")
        xt = sb([128, D], F32, "xt4")
        xo = xt
        osum = sb([128, 8, 128], F32, "osum")
        ssh = sb([128, 16], F32, "ssh")
        mb = sb([128, D], BF16, "mb")
        wb = [sb([128, 8, 512], BF16, "wb%d" % i) for i in range(3)]
        wd = [sb([128, 4, 512], BF16, "wd%d" % i) for i in range(2)]
        sg = [ot[0], ot[1]]
        if moe:
            rt = sb([128, 8, NE], F32, "rt")
            k.dma("sp", rt[:], I["moe_router"].rearrange("(k p) n -> p k n", p=128), writes=[rt])
            lgT = sb([8, T], F32, "lgT")
            lg = sb([128, 8, NE], F32, "lg")
            l2 = sb([128, 8, NE], F32, "l2")
            mx = sb([128, 16], F32, "mx")
            cbT = sb([8, T], F32, "cbT")
            CBe = [zt, xt]
            selE = sb([8, NE, 128], F32, "selE")
            for e_ in range(NE):
                k.op("pool", lambda e: e.tensor_copy(out=selE[0:8, e_, :], in_=self.ident_f[0:8, e_:e_ + 1].to_broadcast([8, 128])), reads=[self.ident_f], writes=[selE])
        ntb = T // 128
        if last:
            blocks = [(2 + ntb * i, ntb) for i in range(NOWN // T)]
        else:
            blocks = [(0, 2)] + [(2 + ntb * i, ntb) for i in range(LL // T)]
        wi = [0]

        def wload(src_ap, ncols):
            t_ = wb[wi[0] % 3]
            wi[0] += 1
            k.dma("pool", t_[:, :, 0:ncols], src_ap, writes=[t_], max_dma_last_dim=2048)
            return t_
        wout_src = I["w_out"][l].rearrange("(k p) n -> p k n", p=128)
        for (t0, nt) in blocks:
            if t0 >= self.ntiles:
                break
            Tb = nt * 128
            halves = [(h0, min(512, Tb - h0)) for h0 in range(0, Tb, 512)]
            r = 1 if t0 < 2 else 0
            for ti in range(nt):
                i = t0 + ti
                rows = slice(i * 128, (i + 1) * 128)
                tsl = slice(ti * 128, (ti + 1) * 128)
                k.dma("sp", ot[0][:], S["o_dn"][0, rows, :], reads=[self.odn_dep[0][i]], writes=[ot[0]])
                k.dma("sp", ot[1][:], S["o_dn"][1, rows, :], reads=[self.odn_dep[1][i]], writes=[ot[1]])
                k.dma("sp", ot[2][:], S["o_gla"][0, rows, :], reads=[self.ogla_dep[0][i]], writes=[ot[2]])
                k.dma("sp", ot[3][:], S["o_gla"][1, rows, :], reads=[self.ogla_dep[1][i]], writes=[ot[3]])
                k.dma("sp", zt[:, 0:512], S["tm"][rows, 0:512], reads=[self.tm_dep[i]], writes=[zt])
                k.dma("sp", zt[:, 512:1024], S["tm"][rows, 1552:2064], reads=[self.tm_dep[i]], writes=[zt])
                k.dma("sp", xt[:], self.xcur[rows, :], reads=[self.xcur_dep[i]], writes=[xt])
                osf = osum[:].rearrange("p a b -> p (a b)")
                k.op("dve", lambda e: e.tensor_tensor(out=osf[:, 0:512], in0=ot[0][:], in1=ot[1][:], op=ALU.add), reads=[ot[0], ot[1]], writes=[osum])
                k.op("dve", lambda e: e.tensor_tensor(out=osf[:, 512:1024], in0=ot[2][:], in1=ot[3][:], op=ALU.add), reads=[ot[2], ot[3]], writes=[osum])
                for hh in range(2):
                    k.op("act", lambda e: e.activation(out=ot[2 * hh][:], in_=osf[:, hh * 512:(hh + 1) * 512], func=AF.Square), reads=[osum], writes=[ot[2 * hh]])
                    k.op("dve", lambda e: e.tensor_reduce(out=ssh[:, 4 * hh:4 * hh + 4], in_=ot[2 * hh][:].rearrange("p (a b) -> p a b", a=4), axis=AX.X, op=ALU.add), reads=[ot[2 * hh]], writes=[ssh])
                k.op("act", lambda e: e.activation(out=ssh[:, 8:16], in_=ssh[:, 0:8], func=AF.Sqrt, scale=1.0 / 128.0, bias=self.eps_t[:, 0:1]), reads=[ssh, self.eps_t], writes=[ssh])
                k.op("dve", lambda e: e.reciprocal(out=ssh[:, 8:16], in_=ssh[:, 8:16]), reads=[ssh], writes=[ssh])
                k.op("act", lambda e: e.activation(out=zt[:], in_=zt[:], func=AF.Silu), reads=[zt], writes=[zt])
                for h in range(8):
                    k.op("dve", lambda e: e.scalar_tensor_tensor(out=osum[:, h, :], in0=osum[:, h, :], scalar=ssh[:, 8 + h:9 + h], in1=gnb[:, h, :], op0=ALU.mult, op1=ALU.mult),
                         reads=[osum, ssh, gnb], writes=[osum])
                k.op("dve", lambda e: e.tensor_tensor(out=mb[:], in0=osf, in1=zt[:], op=ALU.mult), reads=[osum, zt], writes=[mb])
                b = self.bank()
                bb = b.t[:, :].bitcast(BF16)
                for kk in range(8):
                    k.op("pe", lambda e: e.transpose(out=bb[:, kk * 128:(kk + 1) * 128], in_=mb[:, kk * 128:(kk + 1) * 128], identity=self.ident_b[:]), reads=[mb, self.ident_b], writes=[b])
                k.op("act", lambda e: e.copy(out=mT[:, :, tsl], in_=bb[:, :].rearrange("p (a b) -> p a b", a=8)), reads=[b], writes=[mT])
                for half in range(2):
                    b2 = self.bank()
                    for q in range(4):
                        kk = half * 4 + q
                        k.op("pe", lambda e: e.transpose(out=b2[:, q * 128:(q + 1) * 128], in_=xt[:, kk * 128:(kk + 1) * 128], identity=self.ident_f[:]), reads=[xt, self.ident_f], writes=[b2])
                    k.op("dve", lambda e: e.tensor_copy(out=xT[:, half * 4:half * 4 + 4, tsl], in_=b2[:, :].rearrange("p (a b) -> p a b", a=4)), reads=[b2], writes=[xT])
            for og in range(2):
                wt = wload(wout_src[:, :, og * 512:(og + 1) * 512], 512)
                for q in range(4):
                    oc = og * 4 + q
                    for (h0, hw) in halves:
                        b = self.bank()
                        for kk in range(8):
                            k.op("pe", lambda e: e.matmul(b[:, 0:hw], lhsT=wt[:, kk, q * 128:(q + 1) * 128], rhs=mT[:, kk, h0:h0 + hw], start=(kk == 0), stop=(kk == 7)), reads=[wt, mT], writes=[b])
                        k.op("dve", lambda e: e.scalar_tensor_tensor(out=xT[:, oc, h0:h0 + hw], in0=b[:, 0:hw], scalar=self.modT[:, 16 + oc, r:r + 1], in1=xT[:, oc, h0:h0 + hw], op0=ALU.mult, op1=ALU.add),
                             reads=[b, self.modT, xT], writes=[xT])

            def rms_bcast(dst):
                for kk in range(8):
                    k.op("act", lambda e: e.activation(out=sq2[:, kk, 0:Tb], in_=xT[:, kk, 0:Tb], func=AF.Square), reads=[xT], writes=[sq2])
                for (h0, hw) in halves:
                    bs_ = self.bank()
                    for kk in range(8):
                        k.op("pe", lambda e: e.matmul(bs_[:, 0:hw], lhsT=self.ones_b[:], rhs=sq2[:, kk, h0:h0 + hw], start=(kk == 0), stop=(kk == 7)), reads=[self.ones_b, sq2], writes=[bs_])
                    k.op("act", lambda e: e.activation(out=dst[:, h0:h0 + hw], in_=bs_[:, 0:hw], func=AF.Sqrt, scale=1.0 / D, bias=self.eps_t[:, 0:1]), reads=[bs_, self.eps_t], writes=[dst])
                k.op("dve", lambda e: e.reciprocal(out=dst[:, 0:Tb], in_=dst[:, 0:Tb]), reads=[dst], writes=[dst])
            rms_bcast(rs2)
            if moe:
                bl = [self.bank() for _ in halves]
            for kk in range(8):
                tf = tmpf[kk % 2]
                k.op("dve", lambda e: e.tensor_tensor(out=tf[:, 0:Tb], in0=xT[:, kk, 0:Tb], in1=rs2[:, 0:Tb], op=ALU.mult), reads=[xT, rs2], writes=[tf])
                k.op("act", lambda e: e.activation(out=h2T[:, kk, 0:Tb], in_=tf[:, 0:Tb], func=AF.Identity, scale=self.g2T[:, kk, r:r + 1], bias=self.modT[:, 24 + kk, r:r + 1]),
                     reads=[tf, self.g2T, self.modT], writes=[h2T])
                if moe:
                    k.op("dve", lambda e: e.tensor_scalar(out=tf[:, 0:Tb], in0=tf[:, 0:Tb], scalar1=self.g2T[:, kk, r:r + 1], scalar2=self.modT[:, 24 + kk, r:r + 1], op0=ALU.mult, op1=ALU.add),
                         reads=[tf, self.g2T, self.modT], writes=[tf])
                    for hi, (h0, hw) in enumerate(halves):
                        k.op("pe", lambda e: e.matmul(bl[hi][0:8, 0:hw], lhsT=rt[:, kk, :], rhs=tf[:, h0:h0 + hw], start=(kk == 0), stop=(kk == 7)), reads=[rt, tf], writes=[bl[hi]])
            experts = [None]
            if moe:
                experts = list(range(NE))
                for hi, (h0, hw) in enumerate(halves):
                    k.op("act", lambda e: e.copy(out=lgT[:, h0:h0 + hw], in_=bl[hi][0:8, 0:hw]), reads=[bl[hi]], writes=[lgT])
                bl2 = self.bank()
                for ti in range(nt):
                    k.op("pe", lambda e: e.transpose(out=bl2[:, ti * 8:(ti + 1) * 8], in_=lgT[0:8, ti * 128:(ti + 1) * 128], identity=self.ident_f[0:8, 0:8]), reads=[lgT, self.ident_f], writes=[bl2])
                k.op("dve", lambda e: e.tensor_copy(out=lg[:].rearrange("p a b -> p (a b)"), in_=bl2[:, 0:64]), reads=[bl2], writes=[lg])
                bc3 = lambda ap: ap.unsqueeze(2).to_broadcast([128, 8, NE])
                k.op("dve", lambda e: e.tensor_reduce(out=mx[:, 0:8], in_=lg[:], axis=AX.X, op=ALU.max), reads=[lg], writes=[mx])
                k.op("pool", lambda e: e.tensor_tensor(out=l2[:], in0=lg[:], in1=bc3(mx[:, 0:8]), op=ALU.subtract), reads=[lg, mx], writes=[l2])
                k.op("dve", lambda e: e.tensor_scalar(out=lg[:], in0=l2[:], scalar1=0.0, scalar2=-1e30, op0=ALU.is_equal, op1=ALU.mult), reads=[l2], writes=[lg])
                k.op("dve", lambda e: e.tensor_tensor(out=lg[:], in0=lg[:], in1=l2[:], op=ALU.add), reads=[lg, l2], writes=[lg])
                k.op("dve", lambda e: e.tensor_reduce(out=mx[:, 8:16], in_=lg[:], axis=AX.X, op=ALU.max), reads=[lg], writes=[mx])
                k.op("pool", lambda e: e.tensor_tensor(out=lg[:], in0=l2[:], in1=bc3(mx[:, 8:16]), op=ALU.subtract), reads=[l2, mx], writes=[lg])
                k.op("dve", lambda e: e.tensor_scalar(out=lg[:], in0=lg[:], scalar1=0.0, scalar2=None, op0=ALU.is_ge), reads=[lg], writes=[lg])
                k.op("act", lambda e: e.activation(out=l2[:], in_=l2[:], func=AF.Exp), reads=[l2], writes=[l2])
                k.op("dve", lambda e: e.tensor_tensor(out=l2[:], in0=l2[:], in1=lg[:], op=ALU.mult), reads=[l2, lg], writes=[l2])
                k.op("dve", lambda e: e.tensor_reduce(out=mx[:, 0:8], in_=l2[:], axis=AX.X, op=ALU.add), reads=[l2], writes=[mx])
                k.op("dve", lambda e: e.reciprocal(out=mx[:, 0:8], in_=mx[:, 0:8]), reads=[mx], writes=[mx])
                k.op("pool", lambda e: e.tensor_tensor(out=l2[:], in0=l2[:], in1=bc3(mx[:, 0:8]), op=ALU.mult), reads=[l2, mx], writes=[l2])
                for hi, (h0, hw) in enumerate(halves):
                    bl3 = self.bank()
                    for t4 in range(hw // 128):
                        ti = h0 // 128 + t4
                        k.op("pe", lambda e: e.transpose(out=bl3[0:8, t4 * 128:(t4 + 1) * 128], in_=l2[:, ti, :], identity=self.ident_f[:]), reads=[l2, self.ident_f], writes=[bl3])
                    k.op("act", lambda e: e.copy(out=cbT[:, h0:h0 + hw], in_=bl3[0:8, 0:hw]), reads=[bl3], writes=[cbT])
            for ex_ in experts:
                if moe:
                    wgu_src = I["moe_w_gu"][ex_].rearrange("(k p) n -> p k n", p=128)
                    wdn_src = I["moe_w_down"][ex_].rearrange("(f p) n -> p f n", p=128)
                    cbe = CBe[ex_ % 2]
                    for (h0, hw) in halves:
                        bcb = self.bank()
                        k.op("pe", lambda e: e.matmul(bcb[:, 0:hw], lhsT=selE[0:8, ex_, :], rhs=cbT[0:8, h0:h0 + hw], start=True, stop=True), reads=[selE, cbT], writes=[bcb])
                        k.op("act", lambda e: e.copy(out=cbe[:, h0:h0 + hw], in_=bcb[:, 0:hw]), reads=[bcb], writes=[cbe])
                else:
                    wgu_src = I["ffn_w_gu"].rearrange("(k p) n -> p k n", p=128)
                    wdn_src = I["ffn_w_down"].rearrange("(f p) n -> p f n", p=128)
                si = 0
                for g0 in range(0, F, 512):
                    gw = min(512, F - g0)
                    wgt = wload(wgu_src[:, :, g0:g0 + gw], gw)
                    wut = wload(wgu_src[:, :, F + g0:F + g0 + gw], gw)
                    for c in range(gw // 128):
                        fc = g0 // 128 + c
                        for (h0, hw) in halves:
                            bg_, bu_ = self.bank(), self.bank()
                            for kk in range(8):
                                k.op("pe", lambda e: e.matmul(bg_[:, 0:hw], lhsT=wgt[:, kk, c * 128:(c + 1) * 128], rhs=h2T[:, kk, h0:h0 + hw], start=(kk == 0), stop=(kk == 7)), reads=[wgt, h2T], writes=[bg_])
                            for kk in range(8):
                                k.op("pe", lambda e: e.matmul(bu_[:, 0:hw], lhsT=wut[:, kk, c * 128:(c + 1) * 128], rhs=h2T[:, kk, h0:h0 + hw], start=(kk == 0), stop=(kk == 7)), reads=[wut, h2T], writes=[bu_])
                            s_ = sg[si % 2]
                            si += 1
                            k.op("act", lambda e: e.activation(out=s_[:, 0:hw], in_=bg_[:, 0:hw], func=AF.Silu), reads=[bg_], writes=[s_])
                            if moe:
                                k.op("dve", lambda e: e.tensor_tensor(out=s_[:, 0:hw], in0=s_[:, 0:hw], in1=cbe[:, h0:h0 + hw], op=ALU.mult), reads=[s_, cbe], writes=[s_])
                            k.op("dve", lambda e: e.tensor_tensor(out=actT[:, fc, h0:h0 + hw], in0=bu_[:, 0:hw], in1=s_[:, 0:hw], op=ALU.mult), reads=[bu_, s_], writes=[actT])
                nf = F // 128
                for ocg in range(2):
                    banks = [[self.bank() for _ in range(4)] for _ in halves]
                    for f0 in range(0, nf, 4):
                        wdt = wd[wi[0] % 2]
                        wi[0] += 1
                        n4 = min(4, nf - f0)
                        k.dma("pool", wdt[:, 0:n4, 0:512], wdn_src[:, f0:f0 + n4, ocg * 512:(ocg + 1) * 512], writes=[wdt], max_dma_last_dim=2048)
                        for hi, (h0, hw) in enumerate(halves):
                            for q in range(4):
                                for fi in range(n4):
                                    f = f0 + fi
                                    k.op("pe", lambda e: e.matmul(banks[hi][q][:, 0:hw], lhsT=wdt[:, fi, q * 128:(q + 1) * 128], rhs=actT[:, f, h0:h0 + hw], start=(f == 0), stop=(f == nf - 1)),
                                         reads=[wdt, actT], writes=[banks[hi][q]])
                    for hi, (h0, hw) in enumerate(halves):
                        for q in range(4):
                            oc = ocg * 4 + q
                            k.op("dve", lambda e: e.scalar_tensor_tensor(out=xT[:, oc, h0:h0 + hw], in0=banks[hi][q][:, 0:hw], scalar=self.modT[:, 40 + oc, r:r + 1], in1=xT[:, oc, h0:h0 + hw], op0=ALU.mult, op1=ALU.add),
                                 reads=[banks[hi][q], self.modT, xT], writes=[xT])
            if last:
                rms_bcast(rs2)
                for kk in range(8):
                    k.op("dve", lambda e: e.scalar_tensor_tensor(out=xT[:, kk, 0:Tb], in0=xT[:, kk, 0:Tb], scalar=self.fnT[:, kk, 0:1], in1=rs2[:, 0:Tb], op0=ALU.mult, op1=ALU.mult),
                         reads=[xT, self.fnT, rs2], writes=[xT])
            for ti in range(nt):
                i = t0 + ti
                tsl = slice(ti * 128, (ti + 1) * 128)
                for half in range(2):
                    b2 = self.bank()
                    for q in range(4):
                        kk = half * 4 + q
                        k.op("pe", lambda e: e.transpose(out=b2[:, q * 128:(q + 1) * 128], in_=xT[:, kk, tsl], identity=self.ident_f[:]), reads=[xT, self.ident_f], writes=[b2])
                    k.op("act", lambda e: e.copy(out=xo[:, half * 512:(half + 1) * 512], in_=b2[:, :]), reads=[b2], writes=[xo])
                if last:
                    k.dma("sp", self.out[(i - 2) * 128:(i - 1) * 128, :], xo[:], reads=[xo], writes=[self.out_dep])
                else:
                    k.dma("sp", S["x1"][i * 128:(i + 1) * 128, :], xo[:], reads=[xo], writes=[self.x1_dep[i]])

    def build(self):
        self.declare()
        k = self.k
        self.consts()
        self.eps_t = k.sb([128, 1], F32, "eps_t")
        k.op("pool", lambda e: e.memset(self.eps_t[:], EPS), writes=[self.eps_t])
        self.modT = k.sb([128, 48, 2], F32, "modT")
        self.nT = k.sb([128, 8, 2], F32, "nT")
        self.g1T = k.sb([128, 8, 2], F32, "g1T")
        self.g2T = k.sb([128, 8, 2], F32, "g2T")
        self.fnT = k.sb([128, 8, 1], F32, "fnT")
        self.scT = k.sb([128, 8, 2], BF16, "scT")
        self.xs_dep = [DR() for _ in range(NT)]
        self.xcur, self.xcur_dep = self.I["xs"], self.xs_dep
        for l in range(self.nlayers):
            if "p0" in self.phases:
                with k.scope():
                    self.p0_alloc()
                    self.p0(l)
            if "p1" in self.phases:
                with k.scope():
                    self.p1_alloc()
                    self.p1(l, self.xcur, self.xcur_dep)
            if "p2" in self.phases:
                with k.scope():
                    self.p2(l)
            if "p3" in self.phases:
                with k.scope():
                    self.p3(l)
            if "p4" in self.phases:
                with k.scope():
                    self.p4(l)
                self.xcur, self.xcur_dep = self.S["x1"], self.x1_dep
        k.barrier()
        self.es.close()
        return self.nc


def _core_inputs(inp, core):
    b, j = core // 2, core % 2
    f = (lambda a: a[::-1]) if j else (lambda a: a)
    d = {}
    d["xs"] = np.ascontiguousarray(np.concatenate([f(inp["ctx"][b]), f(inp["x"][b])], axis=0))
    d["cvec"] = np.ascontiguousarray(np.stack([inp["c"][b], inp["c_ctx"]], axis=0))
    w_in = inp["w_in"]
    if j:
        w_in = w_in.copy()
        for base, n in ((2048, 4), (2056, 4), (3600, 16)):
            a = w_in[:, :, base:base + n].copy()
            w_in[:, :, base:base + n] = w_in[:, :, base + n:base + 2 * n]
            w_in[:, :, base + n:base + 2 * n] = a
    d["w_in"] = np.ascontiguousarray(w_in)
    sw = (lambda a: a[:, ::-1]) if j else (lambda a: a)
    d["conv"] = np.ascontiguousarray(sw(inp["conv_qkv"]))
    d["dn_a_log"] = np.ascontiguousarray(sw(inp["dn_a_log"]).reshape(2, 8))
    d["dn_dt_bias"] = np.ascontiguousarray(sw(inp["dn_dt_bias"]).reshape(2, 8))
    d["gla_w_gate2"] = np.ascontiguousarray(sw(inp["gla_w_gate2"]))
    d["gla_b_gate"] = np.ascontiguousarray(sw(inp["gla_b_gate"]))
    for n in ("w_ada", "b_ada", "norm_mix", "norm_ffn", "dn_norm", "gla_norm", "w_out", "final_norm"):
        d[n] = np.ascontiguousarray(inp[n])
    d["ffn_w_gu"] = np.ascontiguousarray(inp["ffn_w_gu"][0])
    d["ffn_w_down"] = np.ascontiguousarray(inp["ffn_w_down"][0])
    d["moe_router"] = np.ascontiguousarray(inp["moe_router"][0])
    d["moe_w_gu"] = np.ascontiguousarray(inp["moe_w_gu"][0])
    d["moe_w_down"] = np.ascontiguousarray(inp["moe_w_down"][0])
    return d


def kernel(**inputs):
    inp = {k_: np.asarray(v, dtype=np.float32) for k_, v in inputs.items()}
    prog = Prog()
    nc = prog.build()
    in_maps = []
    for core in range(8):
        ci = _core_inputs(inp, core)
        in_maps.append({n: ci[n] for n in prog.I})
    res = run_bass_kernel_spmd(nc, in_maps, core_ids=list(range(8)))
    out = np.empty((4, LL, D), np.float32)
    for core in range(8):
        b, j = core // 2, core % 2
        y = np.asarray(res.results[core]["out"], dtype=np.float32)
        if j == 0:
            out[b, 0:NOWN] = y
        else:
            out[b, LL - NOWN:LL] = y[::-1]
    return out
```

```python
import numpy as np
from contextlib import ExitStack
import concourse.bass as bass
import concourse.mybir as mybir
from concourse.bass_utils import run_bass_kernel_spmd

F32 = mybir.dt.float32
BF16 = mybir.dt.bfloat16
AF = mybir.ActivationFunctionType
ALU = mybir.AluOpType
AX = mybir.AxisListType

D = 1024
LC = 256
LL = 8192
LS = LC + LL
NT = LS // 128
IN_COLS = 3632
NTM = IN_COLS - 1536
EPS = 1e-6
D_FF = 2816
D_EXP = 3584
NE = 8
NOWN = 4096


class Dep:
    __slots__ = ("w", "r")

    def __init__(self):
        self.w = None
        self.r = {}


class Tile:
    excl = False

    def __init__(self, t):
        self.t = t
        self.dep = Dep()

    def __getitem__(self, idx):
        return self.t[idx]


class K:
    RING = 64

    def __init__(self, nc, es):
        self.nc = nc
        self.es = es
        self.E = {"pe": nc.tensor, "dve": nc.vector, "act": nc.scalar, "pool": nc.gpsimd, "sp": nc.sync}
        self.sems = {}
        for e in self.E:
            self.sems[e] = es.enter_context(nc.semaphore("sem_" + e))
        for i in range(self.RING):
            self.sems[("d", i)] = es.enter_context(nc.semaphore("dsem%d" % i))
        self.cnt = {e: 0 for e in self.E}
        self.seen = {e: {} for e in self.E}
        self.dn = 0
        self.dtar = [0] * self.RING
        self.nt = 0

    def sb(self, shape, dt, name=None):
        self.nt += 1
        return Tile(self.es.enter_context(self.nc.sbuf_tensor("%s_%d" % (name or "t", self.nt), list(shape), dt)))

    def psum(self, shape, dt, name=None):
        self.nt += 1
        t = Tile(self.es.enter_context(self.nc.psum_tensor(name or ("p%d" % self.nt), list(shape), dt)))
        t.excl = True
        return t

    def _wait(self, eng, tok):
        if tok is None:
            return
        key, val = tok
        if key == eng and eng == "pe":
            return
        if self.seen[eng].get(key, 0) >= val:
            return
        self.E[eng].wait_ge(self.sems[key], val)
        self.seen[eng][key] = val

    def _pre(self, eng, reads, writes):
        for d in reads:
            self._wait(eng, d.dep.w)
        for d in writes:
            self._wait(eng, d.dep.w)
            for tok in d.dep.r.values():
                self._wait(eng, tok)

    def _post(self, tok, reads, writes):
        for d in reads:
            d.dep.r[tok[0]] = tok
        for d in writes:
            d.dep.w = tok
            d.dep.r = {}

    def op(self, eng, fn, reads=(), writes=()):
        ex = [d for d in reads if getattr(d, "excl", False)]
        if ex:
            writes = list(writes) + ex
            reads = [d for d in reads if not getattr(d, "excl", False)]
        self._pre(eng, reads, writes)
        ins = fn(self.E[eng])
        ins.then_inc(self.sems[eng], 1)
        self.cnt[eng] += 1
        self._post((eng, self.cnt[eng]), reads, writes)

    def dma(self, q, out, in_, reads=(), writes=(), **kw):
        self._pre(q, reads, writes)
        slot = self.dn % self.RING
        self.dn += 1
        key = ("d", slot)
        if self.dtar[slot] > 0:
            self._wait(q, (key, self.dtar[slot]))
        self.dtar[slot] += 16
        self.E[q].dma_start(out=out, in_=in_, **kw).then_inc(self.sems[key], 16)
        self._post((key, self.dtar[slot]), reads, writes)

    def barrier(self):
        for e in self.E:
            for e2 in self.E:
                if e2 != e and self.cnt[e2] > 0:
                    self._wait(e, (e2, self.cnt[e2]))
            for s in range(self.RING):
                if self.dtar[s] > 0:
                    self._wait(e, (("d", s), self.dtar[s]))

    def scope(self):
        outer = self.es
        k = self

        class _S:
            def __enter__(s):
                k.es = ExitStack()
                return s

            def __exit__(s, *a):
                k.barrier()
                k.es.close()
                k.es = outer
                return False
        return _S()

    def finish(self, deps):
        for d in deps:
            self._wait("sp", d.dep.w)


class DR:
    def __init__(self):
        self.dep = Dep()


class Prog:
    def __init__(self, debug=(), nlayers=2, phases=("p0", "p1", "p2", "p3", "p4"), ntiles=NT):
        self.debug = tuple(debug)
        self.nlayers = nlayers
        self.phases = phases
        self.ntiles = ntiles
        self.p2_stop = 9
        nc = self.nc = bass.Bass("TRN2", target_bir_lowering=False)
        self.I = {}
        self.S = {}
        self.es = ExitStack()
        self.k = K(nc, self.es)

    def inp(self, name, shape):
        self.I[name] = self.nc.dram_tensor(name, list(shape), F32, kind="ExternalInput").ap()
        return self.I[name]

    def scr(self, name, shape, dt=F32):
        kind = "ExternalOutput" if name in self.debug else "Internal"
        self.S[name] = self.nc.dram_tensor(name, list(shape), dt, kind=kind).ap()
        return self.S[name]

    def declare(self):
        inp = self.inp
        inp("xs", [LS, D]); inp("cvec", [2, D]); inp("w_ada", [2, D, 6 * D]); inp("b_ada", [2, 6 * D])
        inp("norm_mix", [2, D]); inp("norm_ffn", [2, D]); inp("w_in", [2, D, IN_COLS]); inp("conv", [2, 5, 1536])
        inp("dn_a_log", [2, 8]); inp("dn_dt_bias", [2, 8]); inp("dn_norm", [2, 128])
        inp("gla_w_gate2", [2, 2, 16, 256]); inp("gla_b_gate", [2, 2, 256]); inp("gla_norm", [2, 128])
        if "p4" in self.phases:
            inp("w_out", [2, D, D]); inp("ffn_w_gu", [D, 2 * D_FF]); inp("ffn_w_down", [D_FF, D])
            if self.nlayers > 1:
                inp("moe_router", [D, NE]); inp("moe_w_gu", [NE, D, 2 * D_EXP]); inp("moe_w_down", [NE, D_EXP, D])
        inp("final_norm", [D])
        self.out = self.nc.dram_tensor("out", [NOWN, D], F32, kind="ExternalOutput").ap()
        self.out_dep = DR()
        self.scr("tm", [LS, NTM]); self.scr("qkvT", [1536, LS + 8], BF16)
        self.scr("o_dn", [2, LS, 512]); self.scr("o_gla", [2, LS, 512]); self.scr("x1", [LS, D])
        self.x1_dep = [DR() for _ in range(NT)]
        self.scr("sa_kq", [17, 128, 4096], BF16); self.scr("sa_vt", [17, 128, 2048], BF16); self.scr("sa_kv", [17, 4, 128, 1024], BF16)
        self.sa_dep = [[DR() for _ in range(6)] for _ in range(17)]
        self.odn_dep = [[DR() for _ in range(NT)] for _ in range(2)]
        self.ogla_dep = [[DR() for _ in range(NT)] for _ in range(2)]
        self.tm_dep = [DR() for _ in range(NT)]
        self.qkv_dep = [DR() for _ in range(NT)]
        self.qkv_pad_dep = DR()

    def dbg(self, name, tile, shape, dt=F32):
        if name not in self.debug or name in self.S:
            return
        if len(shape) == 2 and len(tile.t.shape) == 4:
            ap = self.nc.dram_tensor(name, list(shape), dt, kind="ExternalOutput").ap()
            self.S[name] = ap
            self.k.dma("sp", ap, tile[:].rearrange("p a b c -> p (a b c)"), reads=[tile], writes=[DR()])
            return
        ap = self.nc.dram_tensor(name, list(shape), dt, kind="ExternalOutput").ap()
        self.S[name] = ap
        src_ap = tile[:]
        if len(shape) == 2 and len(tile.t.shape) == 3:
            src_ap = tile[:].rearrange("p a b -> p (a b)")
        self.k.dma("sp", ap, src_ap, reads=[tile], writes=[DR()])

    def consts(self):
        k = self.k
        self.ident_f = k.sb([128, 128], F32, "ident_f")
        self.ident_b = k.sb([128, 128], BF16, "ident_b")
        self.ones_b = k.sb([128, 128], BF16, "ones_b")
        self.ones_f = k.sb([128, 128], F32, "ones_f")
        self.zeros_f = k.sb([128, 8], BF16, "zeros_b")
        idf, idb = self.ident_f, self.ident_b
        k.op("pool", lambda e: e.memset(idf[:], 1.0), writes=[idf])
        k.op("pool", lambda e: e.affine_select(out=idf[:], in_=idf[:], pattern=[[1, 128]], compare_op=ALU.is_equal,
                                                fill=0.0, base=0, channel_multiplier=-1), reads=[idf], writes=[idf])
        k.op("pool", lambda e: e.tensor_copy(out=idb[:], in_=idf[:]), reads=[idf], writes=[idb])
        k.op("pool", lambda e: e.memset(self.ones_b[:], 1.0), writes=[self.ones_b])
        k.op("pool", lambda e: e.memset(self.ones_f[:], 1.0), writes=[self.ones_f])
        k.op("pool", lambda e: e.memset(self.zeros_f[:], 0.0), writes=[self.zeros_f])
        self.ps = [k.psum([128, 512], F32, "ps%d" % i) for i in range(8)]
        self.psi = 0

    def bank(self):
        b = self.ps[self.psi % 8]
        self.psi += 1
        return b

    def rows_to_cols(self, rows, R, n, dst):
        k = self.k
        b = self.bank()
        for c in range(n):
            k.op("pe", lambda e, c=c: e.transpose(out=b[:, c * R:(c + 1) * R], in_=rows[0:R, c * 128:(c + 1) * 128],
                                                  identity=self.ident_f[0:R, 0:R]), reads=[rows, self.ident_f], writes=[b])
        k.op("dve", lambda e: e.tensor_copy(out=dst[:].rearrange("p n r -> p (n r)"), in_=b[:, 0:n * R]), reads=[b], writes=[dst])

    def p0_alloc(self):
        k = self.k
        self.crow = k.sb([2, D], F32, "crow")
        self.cT = k.sb([128, 8, 2], F32, "cT")
        self.wada = [k.sb([128, 8, 512], BF16, "wada%d" % i) for i in range(2)]
        self.modrow = k.sb([2, 6 * D], F32, "modrow")
        self.brow = k.sb([2, 6 * D], F32, "brow")
        self.nrow = k.sb([2, D], F32, "nrow")
        self.fnrow = k.sb([1, D], F32, "fnrow")

    def p0(self, l):
        k, I = self.k, self.I
        crow, cT, scT, modrow, brow, modT = self.crow, self.cT, self.scT, self.modrow, self.brow, self.modT
        if l == 0:
            k.dma("sp", crow[:], I["cvec"], writes=[crow])
            self.rows_to_cols(crow, 2, 8, cT)
            k.op("act", lambda e: e.activation(out=scT[:], in_=cT[:], func=AF.Silu), reads=[cT], writes=[scT])
            k.dma("sp", self.fnrow[:], I["final_norm"].rearrange("(o n) -> o n", o=1), writes=[self.fnrow])
            self.rows_to_cols(self.fnrow, 1, 8, self.fnT)
        for r in range(2):
            k.dma("sp", brow[r:r + 1, :], I["b_ada"][l:l + 1, :], writes=[brow])
        k.dma("sp", self.nrow[0:1, :], I["norm_mix"][l:l + 1, :], writes=[self.nrow])
        k.dma("sp", self.nrow[1:2, :], I["norm_ffn"][l:l + 1, :], writes=[self.nrow])
        self.rows_to_cols(self.nrow, 2, 8, self.nT)
        wsrc = I["w_ada"][l].rearrange("(k p) n -> p k n", p=128)
        for g in range(12):
            wt = self.wada[g % 2]
            k.dma("pool", wt[:], wsrc[:, :, g * 512:(g + 1) * 512], writes=[wt], max_dma_last_dim=2048)
            b = self.bank()
            for kk in range(8):
                k.op("pe", lambda e, kk=kk: e.matmul(b[0:2, :], lhsT=scT[:, kk, :], rhs=wt[:, kk, :], start=(kk == 0), stop=(kk == 7)),
                     reads=[scT, wt], writes=[b])
            k.op("dve", lambda e, g=g: e.tensor_tensor(out=modrow[:, g * 512:(g + 1) * 512], in0=b[0:2, :], in1=brow[:, g * 512:(g + 1) * 512], op=ALU.add),
                 reads=[b, brow], writes=[modrow])
        self.rows_to_cols(modrow, 2, 48, modT)
        for r in range(2):
            k.op("dve", lambda e, r=r: e.scalar_tensor_tensor(out=self.g1T[:, :, r], in0=modT[:, 8:16, r], scalar=1.0, in1=self.nT[:, :, 0], op0=ALU.add, op1=ALU.mult),
                 reads=[modT, self.nT], writes=[self.g1T])
            k.op("dve", lambda e, r=r: e.scalar_tensor_tensor(out=self.g2T[:, :, r], in0=modT[:, 32:40, r], scalar=1.0, in1=self.nT[:, :, 1], op0=ALU.add, op1=ALU.mult),
                 reads=[modT, self.nT], writes=[self.g2T])

    def p1_alloc(self):
        k = self.k
        self.w_fm = k.sb([128, 8, 1536], BF16, "w_fm")
        self.w_tm = k.sb([128, 8, NTM], BF16, "w_tm")
        self.xt = [k.sb([128, D], F32, "xt%d" % i) for i in range(2)]
        self.junk = k.sb([128, D], BF16, "junk")
        self.ss = [k.sb([128, 2], F32, "ss%d" % i) for i in range(2)]
        self.xn = [k.sb([128, D], BF16, "xn%d" % i) for i in range(2)]
        self.hT = [k.sb([128, 8, 512], BF16, "hT%d" % i) for i in range(2)]
        self.tmo = [k.sb([128, NTM], F32, "tmo%d" % i) for i in range(2)]
        self.qo = [k.sb([128, 512], BF16, "qo%d" % i) for i in range(3)]

    def p1(self, l, xsrc, xsrc_dep):
        k, I, S = self.k, self.I, self.S
        wsrc = I["w_in"][l].rearrange("(k p) n -> p k n", p=128)
        for kk in range(8):
            k.dma("pool", self.w_fm[:, kk, :], wsrc[:, kk, 0:1536], writes=[self.w_fm], max_dma_last_dim=2048)
            k.dma("pool", self.w_tm[:, kk, :], wsrc[:, kk, 1536:IN_COLS], writes=[self.w_tm], max_dma_last_dim=2048)
        if l == 0:
            for (c0, n) in ((0, 2), (258, 4), (LS + 6, 2)):
                for ch in range(12):
                    k.dma("sp", S["qkvT"][ch * 128:(ch + 1) * 128, c0:c0 + n], self.zeros_f[:, 0:n], reads=[self.zeros_f], writes=[self.qkv_pad_dep])
        blocks = [(0, 2)] + [(2 + 4 * i, 4) for i in range(16)]
        blocks = [b_ for b_ in blocks if b_[0] < self.ntiles]

        def cyc(ids):
            st = [0]

            def f():
                b = self.ps[ids[st[0] % len(ids)]]
                st[0] += 1
                return b
            return f
        bk_p, bk_t, bk_f = cyc([0, 1]), cyc([2, 3, 4]), cyc([5, 6, 7])

        def prep(bk, hT, t0, nt):
            r = 1 if t0 < 2 else 0
            for ti in range(nt):
                i = t0 + ti
                x_t, ss, xn = self.xt[i % 2], self.ss[i % 2], self.xn[i % 2]
                k.dma("sp", x_t[:], xsrc[i * 128:(i + 1) * 128, :], reads=[xsrc_dep[i]], writes=[x_t])
                k.op("act", lambda e: e.activation(out=self.junk[:], in_=x_t[:], func=AF.Square, accum_out=ss[:, 0:1]), reads=[x_t], writes=[self.junk, ss])
                yield
                k.op("act", lambda e: e.activation(out=ss[:, 1:2], in_=ss[:, 0:1], func=AF.Sqrt, scale=1.0 / D, bias=self.eps_t[:, 0:1]), reads=[ss, self.eps_t], writes=[ss])
                yield
                k.op("dve", lambda e: e.reciprocal(out=ss[:, 1:2], in_=ss[:, 1:2]), reads=[ss], writes=[ss])
                k.op("dve", lambda e: e.tensor_scalar(out=xn[:], in0=x_t[:], scalar1=ss[:, 1:2], scalar2=None, op0=ALU.mult), reads=[x_t, ss], writes=[xn])
                yield
                b = bk()
                bb = b.t[:, :].bitcast(BF16)
                for kk in range(8):
                    k.op("pe", lambda e, kk=kk: e.transpose(out=bb[:, kk * 128:(kk + 1) * 128], in_=xn[:, kk * 128:(kk + 1) * 128], identity=self.ident_b[:]),
                         reads=[xn, self.ident_b], writes=[b])
                yield
                for kk in range(8):
                    if kk % 2 == 0:
                        k.op("act", lambda e, kk=kk: e.activation(out=hT[:, kk, ti * 128:(ti + 1) * 128], in_=bb[:, kk * 128:(kk + 1) * 128], func=AF.Identity,
                                                                  scale=self.g1T[:, kk, r:r + 1], bias=self.modT[:, kk, r:r + 1]),
                             reads=[b, self.g1T, self.modT], writes=[hT])
                    else:
                        k.op("dve", lambda e, kk=kk: e.tensor_scalar(out=hT[:, kk, ti * 128:(ti + 1) * 128], in0=bb[:, kk * 128:(kk + 1) * 128],
                                                                     scalar1=self.g1T[:, kk, r:r + 1], scalar2=self.modT[:, kk, r:r + 1], op0=ALU.mult, op1=ALU.add),
                             reads=[b, self.g1T, self.modT], writes=[hT])
                    if kk % 2:
                        yield

        def tmproj(bk, hT, t0, nt):
            ev = 0
            for ti in range(nt):
                i = t0 + ti
                tmo = self.tmo[i % 2]
                for cg in range(5):
                    n = min(512, NTM - cg * 512)
                    b2 = bk()
                    for kk in range(8):
                        k.op("pe", lambda e, kk=kk: e.matmul(b2[:, 0:n], lhsT=hT[:, kk, ti * 128:(ti + 1) * 128], rhs=self.w_tm[:, kk, cg * 512:cg * 512 + n],
                                                             start=(kk == 0), stop=(kk == 7)), reads=[hT, self.w_tm], writes=[b2])
                    yield
                    ev += 1
                    if ev % 2:
                        k.op("act", lambda e: e.copy(out=tmo[:, cg * 512:cg * 512 + n], in_=b2[:, 0:n]), reads=[b2], writes=[tmo])
                    else:
                        k.op("dve", lambda e: e.tensor_copy(out=tmo[:, cg * 512:cg * 512 + n], in_=b2[:, 0:n]), reads=[b2], writes=[tmo])
                k.dma("pool", S["tm"][i * 128:(i + 1) * 128, :], tmo[:], reads=[tmo], writes=[self.tm_dep[i]])
                yield

        def fmproj(bk, hT, t0, nt):
            T = nt * 128
            col0 = t0 * 128 + (2 if t0 < 2 else 6)
            for ch in range(12):
                b3 = bk()
                for kk in range(8):
                    k.op("pe", lambda e, kk=kk: e.matmul(b3[:, 0:T], lhsT=self.w_fm[:, kk, ch * 128:(ch + 1) * 128], rhs=hT[:, kk, 0:T], start=(kk == 0), stop=(kk == 7)),
                         reads=[hT, self.w_fm], writes=[b3])
                yield
                qo = self.qo[ch % 3]
                if ch % 2:
                    k.op("act", lambda e: e.copy(out=qo[:, 0:T], in_=b3[:, 0:T]), reads=[b3], writes=[qo])
                else:
                    k.op("dve", lambda e: e.tensor_copy(out=qo[:, 0:T], in_=b3[:, 0:T]), reads=[b3], writes=[qo])
                k.dma("pool", S["qkvT"][ch * 128:(ch + 1) * 128, col0:col0 + T], qo[:, 0:T], reads=[qo], writes=[self.qkv_dep[t0 + j] for j in range(nt)])
                yield

        self.interleave([prep(bk_p, self.hT[0], *blocks[0])])
        for bi, (t0, nt) in enumerate(blocks):
            hT = self.hT[bi % 2]
            gens = [tmproj(bk_t, hT, t0, nt), fmproj(bk_f, hT, t0, nt)]
            if bi + 1 < len(blocks):
                gens.append(prep(bk_p, self.hT[(bi + 1) % 2], *blocks[bi + 1]))
            self.interleave(gens)

    def dir_masks(self, d):
        k = self.k
        mI = k.sb([128, 4, 128], BF16, "maskI")
        mS = k.sb([128, 4, 128], BF16, "maskS")
        tri = k.sb([128, 128], F32, "tri")
        sgn = 1 if d == 0 else -1
        for (m, cmp) in ((mI, ALU.is_ge), (mS, ALU.is_gt)):
            k.op("pool", lambda e: e.memset(m[:], 1.0), writes=[m])
            k.op("pool", lambda e: e.affine_select(out=m[:], in_=m[:], pattern=[[0, 4], [sgn, 128]], compare_op=cmp, fill=0.0,
                                                    base=0, channel_multiplier=-sgn), reads=[m], writes=[m])
        k.op("pool", lambda e: e.tensor_copy(out=tri[:], in_=mI[:, 0, :]), reads=[mI], writes=[tri])
        return mI, mS, tri

    def bcast_row(self, src_row_ap, n, dst):
        self.k.dma("sp", dst[:, 0:n], src_row_ap.broadcast_to([128, n]), writes=[dst])

    @staticmethod
    def interleave(gens):
        gens = list(gens)
        while gens:
            nxt = []
            for g in gens:
                try:
                    next(g)
                    nxt.append(g)
                except StopIteration:
                    pass
            gens = nxt

    def p2(self, l):
        k, I, S = self.k, self.I, self.S
        sb = k.sb
        flat = lambda t: t[:].rearrange("p a b -> p (a b)")
        convrow = sb([5, 1536], F32, "convrow")
        cw = sb([128, 12, 5], F32, "cw")
        k.dma("sp", convrow[:], I["conv"][l], writes=[convrow])
        self.rows_to_cols(convrow, 5, 12, cw)
        dtb = sb([128, 8], F32, "dtb")
        nA = sb([128, 8], F32, "nA")
        self.bcast_row(I["dn_dt_bias"][l:l + 1, :], 8, dtb)
        self.bcast_row(I["dn_a_log"][l:l + 1, :], 8, nA)
        k.op("act", lambda e: e.activation(out=nA[:], in_=nA[:], func=AF.Exp), reads=[nA], writes=[nA])
        k.op("dve", lambda e: e.tensor_scalar(out=nA[:], in0=nA[:], scalar1=-1.0, scalar2=None, op0=ALU.mult), reads=[nA], writes=[nA])
        ident4b = sb([128, 4, 128], BF16, "ident4b")
        for h in range(4):
            k.op("pool", lambda e: e.tensor_copy(out=ident4b[:, h, :], in_=self.ident_f[:]), reads=[self.ident_f], writes=[ident4b])
        qeps = sb([128, 1], F32, "qeps")
        k.op("pool", lambda e: e.memset(qeps[:], 128.0 * EPS), writes=[qeps])
        bd32 = sb([128, 4, 128], BF16, "bd32"); m1 = sb([128, 4, 128], BF16, "m1"); m2 = sb([128, 4, 128], BF16, "m2")
        for (bs_, nb_, dst) in ((32, 4, bd32), (64, 2, m1)):
            E = sb([4, 128], F32, "Eblk%d" % bs_)
            k.op("pool", lambda e: e.memset(E[:], 1.0), writes=[E])
            k.op("pool", lambda e: e.affine_select(out=E[:], in_=E[:], pattern=[[1, 128]], compare_op=ALU.is_ge, fill=0.0, base=0, channel_multiplier=-bs_), reads=[E], writes=[E])
            k.op("pool", lambda e: e.affine_select(out=E[:], in_=E[:], pattern=[[-1, 128]], compare_op=ALU.is_ge, fill=0.0, base=bs_ - 1, channel_multiplier=bs_), reads=[E], writes=[E])
            bm = self.bank()
            k.op("pe", lambda e: e.matmul(bm[:, 0:128], lhsT=E[0:nb_, :], rhs=E[0:nb_, :], start=True, stop=True), reads=[E], writes=[bm])
            for h in range(4):
                k.op("dve", lambda e: e.tensor_copy(out=dst[:, h, :], in_=bm[:, 0:128]), reads=[bm], writes=[dst])
        k.op("dve", lambda e: e.tensor_scalar(out=m2[:], in0=m1[:], scalar1=-1.0, scalar2=1.0, op0=ALU.mult, op1=ALU.add), reads=[m1], writes=[m2])
        k.op("dve", lambda e: e.tensor_tensor(out=m1[:], in0=m1[:], in1=bd32[:], op=ALU.subtract), reads=[m1, bd32], writes=[m1])
        raw = sb([128, 4, 516], BF16, "raw")
        diag = [sb([128, 5, 128], BF16, "diag%d" % i) for i in range(2)]
        acc = [sb([128, 512], F32, "acc%d" % i) for i in range(2)]
        sil = [sb([128, 512], F32, "sil%d" % i) for i in range(2)]
        sq = [sb([128, 512], BF16, "sq0")] * 2
        rn = [sb([128, 512], F32, "rn0")] * 2
        KQT = [sb([128, 4, 2, 512], BF16, "KQT%d" % i) for i in range(2)]
        VT = [sb([128, 4, 512], BF16, "VT%d" % i) for i in range(2)]
        KVtm = [[sb([128, 8, 128], BF16, "KVtm%d_%d" % (j, i)) for i in range(4)] for j in range(2)]
        def mk(n, shape, dt, name):
            return [sb(shape, dt, "%s%d" % (name, i)) for i in range(n)]
        sc4 = sb([128, 4, 16], F32, "sc4")
        Gd = mk(2, [128, 4, 128], F32, "Gd")
        Dm = mk(2, [128, 4, 128], F32, "Dm")
        Eg = mk(2, [128, 4, 128], F32, "Eg")
        A = mk(2, [128, 4, 128], BF16, "A"); AT = mk(2, [128, 4, 128], BF16, "AT")
        Abd = mk(2, [128, 4, 128], BF16, "Abd"); ATbd = mk(2, [128, 4, 128], BF16, "ATbd")
        Xa = mk(2, [128, 4, 128], BF16, "Xa"); XTa = mk(2, [128, 4, 128], BF16, "XTa")
        YT = mk(2, [128, 4, 128], BF16, "YT")
        R0 = mk(2, [128, 4, 128], BF16, "R0"); R1 = mk(2, [128, 4, 128], BF16, "R1")
        B1T = mk(2, [128, 4, 128], BF16, "B1T"); B2T = mk(2, [128, 4, 128], BF16, "B2T")
        Keg = mk(2, [128, 4, 128], BF16, "Keg")
        def mk2(shape, dt, name):
            return [[sb(shape, dt, "%s%d_%d" % (name, j, i)) for i in range(4)] for j in range(2)]
        smb = [sb([128, 4, 32], F32, "smb%d" % j) for j in range(3)]
        attnT = mk2([128, 4, 128], BF16, "attnT")
        up = mk2([128, 4, 128], F32, "up")
        wT = mk2([128, 4, 128], BF16, "wT")
        kdec = mk2([128, 4, 128], BF16, "kdec")
        qdecT = mk2([128, 4, 128], BF16, "qdecT")
        vnew = [sb([128, 4, 128], BF16, "vnew%d" % i) for i in range(2)]
        osb = [sb([128, 4, 128], F32, "osb%d" % i) for i in range(2)]
        Sf = sb([128, 4, 128], F32, "Sf")
        Sb = sb([128, 4, 128], BF16, "Sb")

        def stage_a(bk, d, tri, si, bp, t0, nt):
            T = nt * 128
            col0 = t0 * 128 + (2 if t0 < 2 else 6)
            kq, vt = KQT[bp], VT[bp]
            smT = smb[si]
            v = lambda a, b_: smT[:, 0:nt, a:b_]
            bcs = lambda t_: t_[:, 4 * d:4 * d + 4].unsqueeze(1).to_broadcast([128, nt, 4])
            k.dma("sp", sc4[:, 0:nt, :], S["tm"][t0 * 128:(t0 + nt) * 128, 512:528].rearrange("(t p) n -> p t n", p=128),
                  reads=[self.tm_dep[i] for i in range(t0, t0 + nt)], writes=[sc4])
            k.op("act", lambda e: e.activation(out=v(0, 4), in_=sc4[:, 0:nt, 4 * d:4 * d + 4], func=AF.Exp, scale=-1.0), reads=[sc4], writes=[smT])
            k.op("pool", lambda e: e.tensor_tensor(out=v(4, 8), in0=sc4[:, 0:nt, 8 + 4 * d:12 + 4 * d], in1=bcs(dtb), op=ALU.add), reads=[sc4, dtb], writes=[smT])
            yield
            k.op("dve", lambda e: e.tensor_scalar(out=v(0, 4), in0=v(0, 4), scalar1=1.0, scalar2=None, op0=ALU.add), reads=[smT], writes=[smT])
            k.op("dve", lambda e: e.reciprocal(out=v(0, 4), in_=v(0, 4)), reads=[smT], writes=[smT])
            k.op("act", lambda e: e.activation(out=v(4, 8), in_=v(4, 8), func=AF.Exp), reads=[smT], writes=[smT])
            yield
            k.op("act", lambda e: e.activation(out=v(4, 8), in_=v(4, 8), func=AF.Ln, bias=1.0), reads=[smT], writes=[smT])
            yield
            k.op("pool", lambda e: e.tensor_tensor(out=v(4, 8), in0=v(4, 8), in1=bcs(nA), op=ALU.mult), reads=[smT, nA], writes=[smT])
            yield
            bs = bk()
            for ti in range(nt):
                k.op("pe", lambda e: e.matmul(bs[:, ti * 8:ti * 8 + 4], lhsT=tri[:], rhs=smT[:, ti, 4:8], start=True, stop=True), reads=[tri, smT], writes=[bs])
                k.op("pe", lambda e: e.matmul(bs[:, ti * 8 + 4:ti * 8 + 8], lhsT=self.ones_f[:], rhs=smT[:, ti, 4:8], start=True, stop=True), reads=[self.ones_f, smT], writes=[bs])
            yield
            k.op("dve", lambda e: e.tensor_copy(out=v(8, 16), in_=bs[:, 0:nt * 8].rearrange("p (t n) -> p t n", t=nt)), reads=[bs], writes=[smT])
            yield
            k.op("act", lambda e: e.activation(out=v(16, 20), in_=v(8, 12), func=AF.Exp), reads=[smT], writes=[smT])
            k.op("dve", lambda e: e.tensor_tensor(out=v(20, 24), in0=v(12, 16), in1=v(8, 12), op=ALU.subtract), reads=[smT], writes=[smT])
            yield
            k.op("act", lambda e: e.activation(out=v(20, 24), in_=v(20, 24), func=AF.Exp), reads=[smT], writes=[smT])
            k.op("act", lambda e: e.activation(out=v(24, 28), in_=v(12, 16), func=AF.Exp), reads=[smT], writes=[smT])
            yield
            k.op("dve", lambda e: e.tensor_tensor(out=v(20, 24), in0=v(20, 24), in1=v(0, 4), op=ALU.mult), reads=[smT], writes=[smT])
            yield
            blk = 0 if t0 < 2 else 1 + (t0 - 2) // 4
            if d == 1:
                k.dma("sp", kq[:].rearrange("p a b c -> p (a b c)"), S["sa_kq"][blk], reads=[self.sa_dep[blk][4]], writes=[kq])
                k.dma("sp", vt[:].rearrange("p a b -> p (a b)"), S["sa_vt"][blk], reads=[self.sa_dep[blk][5]], writes=[vt])
                yield
                for ti in range(nt):
                    k.dma("sp", KVtm[bp][ti][:].rearrange("p a b -> p (a b)"), S["sa_kv"][blk, ti], reads=[self.sa_dep[blk][ti]], writes=[KVtm[bp][ti]])
                    yield
                return
            for cg in range(3):
                k.dma("sp", raw[:, :, 0:T + 4], S["qkvT"][cg * 512:(cg + 1) * 512, col0 - 2:col0 + T + 2].rearrange("(c p) t -> p c t", p=128),
                      reads=[self.qkv_dep[i] for i in range(max(t0 - 1, 0), min(t0 + nt + 1, NT))] + [self.qkv_pad_dep], writes=[raw])
                for c4 in range(4):
                    ch = cg * 4 + c4
                    dg = diag[ch % 2]
                    t1, s_, q_, r_ = acc[ch % 2], sil[ch % 2], sq[0], rn[0]
                    for j in range(5):
                        k.op("dve", lambda e: e.tensor_scalar(out=dg[:, j, :], in0=self.ident_b[:], scalar1=cw[:, ch, j:j + 1], scalar2=None, op0=ALU.mult),
                             reads=[self.ident_b, cw], writes=[dg])
                    yield
                    bc_ = bk()
                    for j in range(5):
                        k.op("pe", lambda e: e.matmul(bc_[:, 0:T], lhsT=dg[:, j, :], rhs=raw[:, c4, j:j + T], start=(j == 0), stop=(j == 4)), reads=[dg, raw], writes=[bc_])
                    yield
                    k.op("act", lambda e: e.activation(out=t1[:, 0:T], in_=bc_[:, 0:T], func=AF.Exp, scale=-1.0), reads=[bc_], writes=[t1])
                    yield
                    k.op("act", lambda e: e.activation(out=t1[:, 0:T], in_=t1[:, 0:T], func=AF.Ln, bias=1.0), reads=[t1], writes=[t1])
                    yield
                    k.op("act", lambda e: e.activation(out=t1[:, 0:T], in_=t1[:, 0:T], func=AF.Exp, scale=-1.0), reads=[t1], writes=[t1])
                    yield
                    if ch >= 8:
                        k.op("dve", lambda e: e.tensor_tensor(out=vt[:, ch - 8, 0:T], in0=bc_[:, 0:T], in1=t1[:, 0:T], op=ALU.mult), reads=[bc_, t1], writes=[vt])
                        yield
                        continue
                    k.op("dve", lambda e: e.tensor_tensor(out=s_[:, 0:T], in0=bc_[:, 0:T], in1=t1[:, 0:T], op=ALU.mult), reads=[bc_, t1], writes=[s_])
                    yield
                    k.op("act", lambda e: e.activation(out=q_[:, 0:T], in_=s_[:, 0:T], func=AF.Square), reads=[s_], writes=[q_])
                    yield
                    b = bk()
                    k.op("pe", lambda e: e.matmul(b[:, 0:T], lhsT=self.ones_b[:], rhs=q_[:, 0:T], start=True, stop=True), reads=[self.ones_b, q_], writes=[b])
                    if ch < 4:
                        k.op("act", lambda e: e.activation(out=r_[:, 0:T], in_=b[:, 0:T], func=AF.Ln, scale=128.0, bias=qeps[:, 0:1]), reads=[b, qeps], writes=[r_])
                    else:
                        k.op("act", lambda e: e.activation(out=r_[:, 0:T], in_=b[:, 0:T], func=AF.Ln, scale=1.0, bias=self.eps_t[:, 0:1]), reads=[b, self.eps_t], writes=[r_])
                    yield
                    k.op("act", lambda e: e.activation(out=r_[:, 0:T], in_=r_[:, 0:T], func=AF.Exp, scale=-0.5), reads=[r_], writes=[r_])
                    yield
                    hh, which = (ch, 1) if ch < 4 else (ch - 4, 0)
                    k.op("dve", lambda e: e.tensor_tensor(out=kq[:, hh, which, 0:T], in0=s_[:, 0:T], in1=r_[:, 0:T], op=ALU.mult), reads=[s_, r_], writes=[kq])
                    yield
            for ti in range(nt):
                b = bk()
                bb = b.t[:, :].bitcast(BF16)
                for h in range(4):
                    k.op("pe", lambda e: e.transpose(out=bb[:, h * 128:(h + 1) * 128], in_=kq[:, h, 0, ti * 128:(ti + 1) * 128], identity=self.ident_b[:]),
                         reads=[kq, self.ident_b], writes=[b])
                    k.op("pe", lambda e: e.transpose(out=bb[:, (4 + h) * 128:(5 + h) * 128], in_=vt[:, h, ti * 128:(ti + 1) * 128], identity=self.ident_b[:]),
                         reads=[vt, self.ident_b], writes=[b])
                yield
                k.op("act", lambda e: e.copy(out=KVtm[bp][ti][:].rearrange("p a b -> p (a b)"), in_=bb[:, :]), reads=[b], writes=[KVtm[bp][ti]])
                k.dma("pool", S["sa_kv"][blk, ti], KVtm[bp][ti][:].rearrange("p a b -> p (a b)"), reads=[KVtm[bp][ti]], writes=[self.sa_dep[blk][ti]])
                yield
            k.dma("pool", S["sa_kq"][blk], kq[:].rearrange("p a b c -> p (a b c)"), reads=[kq], writes=[self.sa_dep[blk][4]])
            k.dma("pool", S["sa_vt"][blk], vt[:].rearrange("p a b -> p (a b)"), reads=[vt], writes=[self.sa_dep[blk][5]])
            yield

        def stage_b(bk, d, mI, mS, tri, si, bp, q, i, ti):
            kq, kv = KQT[bp], KVtm[bp][ti]
            tsl = slice(ti * 128, (ti + 1) * 128)
            smT = smb[si]
            sm_ = smT[:, ti, :]
            G = Gd[q]
            k.op("pool", lambda e: e.tensor_tensor(out=G[:], in0=mI[:], in1=sm_[:, 4:8].unsqueeze(2).to_broadcast([128, 4, 128]), op=ALU.mult),
                 reads=[mI, smT], writes=[G])
            yield
            bg = bk()
            k.op("pe", lambda e: e.matmul(bg[:, :], lhsT=self.ones_f[:], rhs=flat(G), start=True, stop=True), reads=[self.ones_f, G], writes=[bg])
            yield
            bg3 = bg[:, :].rearrange("p (a b) -> p a b", a=4)
            for h in range(4):
                k.op("dve", lambda e: e.tensor_scalar(out=Dm[q][:, h, :], in0=bg3[:, h, :], scalar1=sm_[:, 8 + h:9 + h], scalar2=0.0, op0=ALU.subtract, op1=ALU.min),
                     reads=[bg, smT], writes=[Dm[q]])
            yield
            k.op("act", lambda e: e.activation(out=flat(Eg[q]), in_=bg[:, :], func=AF.Exp), reads=[bg], writes=[Eg[q]])
            k.op("act", lambda e: e.activation(out=Dm[q][:], in_=Dm[q][:], func=AF.Exp), reads=[Dm[q]], writes=[Dm[q]])
            yield
            k.op("pool", lambda e: e.tensor_tensor(out=qdecT[bp][ti][:], in0=kq[:, :, 1, tsl], in1=Eg[q][:], op=ALU.mult), reads=[kq, Eg[q]], writes=[qdecT[bp][ti]])
            bet = sm_[:, 0:4].unsqueeze(2).to_broadcast([128, 4, 128])
            DmI, DmS = Gd[q], Eg[q]
            k.op("pool", lambda e: e.tensor_tensor(out=DmI[:], in0=Dm[q][:], in1=mI[:], op=ALU.mult), reads=[Dm[q], mI], writes=[DmI])
            yield
            k.op("pool", lambda e: e.tensor_tensor(out=DmS[:], in0=Dm[q][:], in1=mS[:], op=ALU.mult), reads=[Dm[q], mS], writes=[DmS])
            k.op("pool", lambda e: e.tensor_tensor(out=DmI[:], in0=DmI[:], in1=bet, op=ALU.mult), reads=[DmI, smT], writes=[DmI])
            k.op("pool", lambda e: e.tensor_tensor(out=DmS[:], in0=DmS[:], in1=bet, op=ALU.mult), reads=[DmS, smT], writes=[DmS])
            K3 = kv[:, 0:4, :]
            k.op("pool", lambda e: e.tensor_tensor(out=Keg[q][:], in0=K3, in1=sm_[:, 16:20].unsqueeze(2).to_broadcast([128, 4, 128]), op=ALU.mult), reads=[kv, smT], writes=[Keg[q]])
            k.op("pool", lambda e: e.tensor_tensor(out=kdec[bp][ti][:], in0=K3, in1=sm_[:, 20:24].unsqueeze(2).to_broadcast([128, 4, 128]), op=ALU.mult), reads=[kv, smT], writes=[kdec[bp][ti]])
            bkk, bkq = bk(), bk()
            for h in range(4):
                k.op("pe", lambda e: e.matmul(bkk[:, h * 128:(h + 1) * 128], lhsT=kq[:, h, 0, tsl], rhs=kq[:, h, 0, tsl], start=True, stop=True), reads=[kq], writes=[bkk])
                k.op("pe", lambda e: e.matmul(bkq[:, h * 128:(h + 1) * 128], lhsT=kq[:, h, 0, tsl], rhs=kq[:, h, 1, tsl], start=True, stop=True), reads=[kq], writes=[bkq])
            yield
            k.op("dve", lambda e: e.tensor_tensor(out=flat(A[q]), in0=bkk[:, :], in1=flat(DmS), op=ALU.mult), reads=[bkk, DmS], writes=[A[q]])
            k.op("dve", lambda e: e.tensor_tensor(out=flat(attnT[bp][ti]), in0=bkq[:, :], in1=flat(DmI), op=ALU.mult), reads=[bkq, DmI], writes=[attnT[bp][ti]])
            yield
            bt = bk()
            btb = bt.t[:, :].bitcast(BF16)
            for h in range(4):
                k.op("pe", lambda e: e.transpose(out=btb[:, h * 128:(h + 1) * 128], in_=A[q][:, h, :], identity=self.ident_b[:]), reads=[A[q], self.ident_b], writes=[bt])
            k.op("pool", lambda e: e.tensor_tensor(out=Abd[q][:], in0=A[q][:], in1=bd32[:], op=ALU.mult), reads=[A[q], bd32], writes=[Abd[q]])
            yield
            k.op("act", lambda e: e.copy(out=flat(AT[q]), in_=btb[:, 0:512]), reads=[bt], writes=[AT[q]])
            k.op("pool", lambda e: e.tensor_tensor(out=R0[q][:], in0=ident4b[:], in1=Abd[q][:], op=ALU.subtract), reads=[ident4b, Abd[q]], writes=[R0[q]])
            yield
            k.op("pool", lambda e: e.tensor_tensor(out=ATbd[q][:], in0=AT[q][:], in1=bd32[:], op=ALU.mult), reads=[AT[q], bd32], writes=[ATbd[q]])
            k.op("pool", lambda e: e.tensor_tensor(out=B1T[q][:], in0=AT[q][:], in1=m1[:], op=ALU.mult), reads=[AT[q], m1], writes=[B1T[q]])
            k.op("pool", lambda e: e.tensor_tensor(out=B2T[q][:], in0=AT[q][:], in1=m2[:], op=ALU.mult), reads=[AT[q], m2], writes=[B2T[q]])
            yield
            Xc, XTc, Rc = Abd[q], ATbd[q], R0[q]
            xbufs = [(Xa[q], XTa[q]), (A[q], AT[q])]
            rbufs = [R1[q], R0[q]]
            for lev in range(4):
                lastl = lev == 3
                Xn, XTn = xbufs[lev % 2]
                Rn = rbufs[lev % 2]
                b1 = bk()
                for h in range(4):
                    k.op("pe", lambda e: e.matmul(b1[:, h * 128:(h + 1) * 128], lhsT=Xc[:, h, :], rhs=XTc[:, h, :], start=True, stop=True), reads=[Xc, XTc], writes=[b1])
                if not lastl:
                    b2 = bk()
                    for h in range(4):
                        k.op("pe", lambda e: e.matmul(b2[:, h * 128:(h + 1) * 128], lhsT=XTc[:, h, :], rhs=Xc[:, h, :], start=True, stop=True), reads=[Xc, XTc], writes=[b2])
                yield
                k.op("dve", lambda e: e.tensor_tensor(out=flat(YT[q]), in0=b1[:, :], in1=flat(ident4b), op=ALU.add), reads=[b1, ident4b], writes=[YT[q]])
                if not lastl:
                    k.op("act", lambda e: e.copy(out=flat(XTn), in_=b1[:, :]), reads=[b1], writes=[XTn])
                    k.op("act", lambda e: e.copy(out=flat(Xn), in_=b2[:, :]), reads=[b2], writes=[Xn])
                yield
                b3 = bk()
                for h in range(4):
                    k.op("pe", lambda e: e.matmul(b3[:, h * 128:(h + 1) * 128], lhsT=YT[q][:, h, :], rhs=Rc[:, h, :], start=True, stop=True), reads=[YT[q], Rc], writes=[b3])
                yield
                k.op("dve", lambda e: e.tensor_copy(out=flat(Rn), in_=b3[:, :]), reads=[b3], writes=[Rn])
                yield
                Xc, XTc, Rc = Xn, XTn, Rn
            TTb, Pb = Xa[q], XTa[q]
            for mi, BT_ in enumerate((B1T[q], B2T[q])):
                Tn = rbufs[mi % 2]
                btt = bk()
                bttb = btt.t[:, :].bitcast(BF16)
                for h in range(4):
                    k.op("pe", lambda e: e.transpose(out=bttb[:, h * 128:(h + 1) * 128], in_=Rc[:, h, :], identity=self.ident_b[:]), reads=[Rc, self.ident_b], writes=[btt])
                bp_ = bk()
                for h in range(4):
                    k.op("pe", lambda e: e.matmul(bp_[:, h * 128:(h + 1) * 128], lhsT=BT_[:, h, :], rhs=Rc[:, h, :], start=True, stop=True), reads=[BT_, Rc], writes=[bp_])
                yield
                k.op("act", lambda e: e.copy(out=flat(TTb), in_=bttb[:, 0:512]), reads=[btt], writes=[TTb])
                k.op("dve", lambda e: e.tensor_copy(out=flat(Pb), in_=bp_[:, :]), reads=[bp_], writes=[Pb])
                yield
                bq = bk()
                for h in range(4):
                    k.op("pe", lambda e: e.matmul(bq[:, h * 128:(h + 1) * 128], lhsT=TTb[:, h, :], rhs=Pb[:, h, :], start=True, stop=True), reads=[TTb, Pb], writes=[bq])
                yield
                k.op("dve", lambda e: e.tensor_tensor(out=flat(Tn), in0=flat(Rc), in1=bq[:, :], op=ALU.subtract), reads=[Rc, bq], writes=[Tn])
                yield
                Rc = Tn
            T2T = Rc
            bu, bw = bk(), bk()
            for h in range(4):
                k.op("pe", lambda e: e.matmul(bu[:, h * 128:(h + 1) * 128], lhsT=T2T[:, h, :], rhs=kv[:, 4 + h, :], start=True, stop=True), reads=[T2T, kv], writes=[bu])
                k.op("pe", lambda e: e.matmul(bw[:, h * 128:(h + 1) * 128], lhsT=Keg[q][:, h, :], rhs=T2T[:, h, :], start=True, stop=True), reads=[T2T, Keg[q]], writes=[bw])
            yield
            k.op("act", lambda e: e.copy(out=flat(up[bp][ti]), in_=bu[:, :]), reads=[bu], writes=[up[bp][ti]])
            k.op("dve", lambda e: e.tensor_copy(out=flat(wT[bp][ti]), in_=bw[:, :]), reads=[bw], writes=[wT[bp][ti]])
            yield

        def scan(bk, d, si, bp, tiles):
            for n_, (i, ti) in enumerate(tiles):
                p = n_ % 2
                smT = smb[si]
                sm_ = smT[:, ti, :]
                bws = bk()
                for h in range(4):
                    k.op("pe", lambda e: e.matmul(bws[:, h * 128:(h + 1) * 128], lhsT=wT[bp][ti][:, h, :], rhs=Sb[:, h, :], start=True, stop=True), reads=[wT[bp][ti], Sb], writes=[bws])
                yield
                k.op("dve", lambda e: e.tensor_tensor(out=flat(vnew[p]), in0=flat(up[bp][ti]), in1=bws[:, :], op=ALU.subtract), reads=[up[bp][ti], bws], writes=[vnew[p]])
                yield
                bo, bkv = bk(), bk()
                for h in range(4):
                    k.op("pe", lambda e: e.matmul(bkv[:, h * 128:(h + 1) * 128], lhsT=kdec[bp][ti][:, h, :], rhs=vnew[p][:, h, :], start=True, stop=True), reads=[kdec[bp][ti], vnew[p]], writes=[bkv])
                for h in range(4):
                    k.op("pe", lambda e: e.matmul(bo[:, h * 128:(h + 1) * 128], lhsT=qdecT[bp][ti][:, h, :], rhs=Sb[:, h, :], start=True, stop=False), reads=[qdecT[bp][ti], Sb], writes=[bo])
                    k.op("pe", lambda e: e.matmul(bo[:, h * 128:(h + 1) * 128], lhsT=attnT[bp][ti][:, h, :], rhs=vnew[p][:, h, :], start=False, stop=True), reads=[attnT[bp][ti], vnew[p]], writes=[bo])
                yield
                bkv3 = bkv[:, :].rearrange("p (a b) -> p a b", a=4)
                for h in range(4):
                    k.op("dve", lambda e: e.scalar_tensor_tensor(out=Sf[:, h, :], in0=Sf[:, h, :], scalar=sm_[:, 24 + h:25 + h], in1=bkv3[:, h, :], op0=ALU.mult, op1=ALU.add),
                         reads=[Sf, smT, bkv], writes=[Sf])
                k.op("act", lambda e: e.copy(out=flat(osb[p]), in_=bo[:, :]), reads=[bo], writes=[osb[p]])
                yield
                k.op("act", lambda e: e.copy(out=Sb[:], in_=Sf[:]), reads=[Sf], writes=[Sb])
                k.dma("act", S["o_dn"][d, i * 128:(i + 1) * 128, :], flat(osb[p]), reads=[osb[p]], writes=[self.odn_dep[d][i]])
                yield

        def cyc(ids):
            st = [0]

            def f():
                b = self.ps[ids[st[0] % len(ids)]]
                st[0] += 1
                return b
            return f
        bk_b = [cyc([0, 1]), cyc([2, 3])]
        bk_a, bk_s = cyc([6, 7]), cyc([4, 5])
        blocks_f = [(0, 2)] + [(2 + 4 * i, 4) for i in range(16)]
        blocks_f = [b_ for b_ in blocks_f if b_[0] < self.ntiles]
        gbi = 0
        for d in range(2):
            mI, mS, tri = self.dir_masks(d)
            k.op("pool", lambda e: e.memset(Sf[:], 0.0), writes=[Sf])
            k.op("pool", lambda e: e.memset(Sb[:], 0.0), writes=[Sb])
            blocks = blocks_f if d == 0 else [blocks_f[0]] + blocks_f[:0:-1]
            nb = len(blocks)
            self.interleave([stage_a(bk_a, d, tri, gbi % 3, gbi % 2, *blocks[0])])
            prev = None
            for bi, (t0, nt) in enumerate(blocks):
                bp = (gbi + bi) % 2
                tis = list(range(nt)) if d == 0 else list(range(nt - 1, -1, -1))
                for pair in range(0, nt, 2):
                    gens = [stage_b(bk_b[q], d, mI, mS, tri, (gbi + bi) % 3, bp, q, t0 + ti, ti) for q, ti in enumerate(tis[pair:pair + 2])]
                    if pair == 0 and bi + 1 < nb:
                        gens.append(stage_a(bk_a, d, tri, (gbi + bi + 1) % 3, (gbi + bi + 1) % 2, *blocks[bi + 1]))
                    if prev is not None and (pair == 2 or nt == 2):
                        gens.append(scan(bk_s, d, *prev))
                    self.interleave(gens)
                prev = ((gbi + bi) % 3, bp, [(t0 + ti, ti) for ti in tis])
            self.interleave([scan(bk_s, d, *prev)])
            gbi += nb

    def p3(self, l):
        k, I, S = self.k, self.I, self.S
        sb = k.sb

        def two(shape, dt, name):
            return [sb(shape, dt, name + "%d" % i) for i in range(4)]
        qkv = two([128, 1024], F32, "gqkv")
        glr = two([128, 16], F32, "glr")
        lhsTg = two([64, 128], F32, "lhsTg")
        W2 = sb([64, 256], F32, "W2")
        ex = two([128, 256], F32, "gex")
        la = two([128, 256], F32, "la")
        bcs = two([128, 256], F32, "bcs")
        ebc = two([128, 256], F32, "ebc")
        enbc = two([128, 256], F32, "enbc")
        ekd = two([128, 256], F32, "ekd")
        qs = two([128, 256], BF16, "gqs")
        ks = two([128, 256], BF16, "gks")
        kd = two([128, 256], BF16, "gkd")
        vb = two([128, 4, 128], BF16, "gvb")
        qT = two([128, 4, 128], BF16, "gqTm")
        kT = two([128, 2, 128], BF16, "gkT")
        att = two([128, 4, 128], BF16, "gatt")
        osb = two([128, 512], F32, "gosb")
        etc_ = two([128, 4], F32, "getc")
        Sf = sb([128, 2, 128], F32, "gSf")
        Sb = sb([128, 2, 128], BF16, "gSb")
        for t_ in qT:
            k.op("pool", lambda e: e.memset(t_[:], 0.0), writes=[t_])
        for t_ in lhsTg:
            k.op("pool", lambda e: e.memset(t_[:], 0.0), writes=[t_])
            k.op("pool", lambda e: e.memset(t_[32:33, :], 1.0), writes=[t_])
        tm_lat = S["tm"][LC:LS, :].rearrange("(r w) n -> w r n", w=64)
        alltm = self.tm_dep
        for d in range(2):
            mI, mS, tri = self.dir_masks(d)
            k.op("pool", lambda e: e.memset(W2[:], 0.0), writes=[W2])
            k.dma("sp", W2[0:16, :], I["gla_w_gate2"][l, d], writes=[W2])
            k.dma("sp", W2[32:33, :], I["gla_b_gate"][l, d:d + 1, :], writes=[W2])
            k.op("pool", lambda e: e.memset(Sf[:], 0.0), writes=[Sf])
            k.op("pool", lambda e: e.memset(Sb[:], 0.0), writes=[Sb])
            order = [("c", 0), ("c", 1)] + [("g", j) for j in range(64)]
            if d == 1:
                order = [("c", 1), ("c", 0)] + [("g", j) for j in range(63, -1, -1)]
            if self.ntiles < NT:
                order = order[:self.ntiles]
            def cyc(ids):
                st = [0]

                def f():
                    b = self.ps[ids[st[0] % len(ids)]]
                    st[0] += 1
                    return b
                return f
            bk_preA, bk_preB, bk_post = cyc([0, 1]), cyc([2, 3]), cyc([4, 5])

            def pre(bk, p, kind, j):
                if kind == "c":
                    rows = S["tm"][j * 128:(j + 1) * 128, :]
                else:
                    rows = tm_lat[j]
                k.dma("sp", qkv[p][:], rows[:, 528:1552], reads=alltm, writes=[qkv[p]])
                k.dma("sp", glr[p][:], rows[:, 2064 + 16 * d:2080 + 16 * d], reads=alltm, writes=[glr[p]])
                b0 = bk()
                k.op("pe", lambda e: e.transpose(out=b0[0:16, 0:128], in_=glr[p][:, :], identity=self.ident_f[:]), reads=[glr[p], self.ident_f], writes=[b0])
                k.op("act", lambda e: e.copy(out=lhsTg[p][0:16, :], in_=b0[0:16, 0:128]), reads=[b0], writes=[lhsTg[p]])
                yield
                bgt = bk()
                k.op("pe", lambda e: e.matmul(bgt[:, 0:256], lhsT=lhsTg[p][:, :], rhs=W2[:, :], start=True, stop=True), reads=[lhsTg[p], W2], writes=[bgt])
                k.op("act", lambda e: e.activation(out=ex[p][:], in_=bgt[:, 0:256], func=AF.Exp, scale=-1.0), reads=[bgt], writes=[ex[p]])
                yield
                k.op("act", lambda e: e.activation(out=ex[p][:], in_=ex[p][:], func=AF.Ln, bias=1.0), reads=[ex[p]], writes=[ex[p]])
                yield
                k.op("dve", lambda e: e.tensor_scalar(out=la[p][:], in0=ex[p][:], scalar1=-1.0 / 16.0, scalar2=None, op0=ALU.mult), reads=[ex[p]], writes=[la[p]])
                yield
                bbc, bto = bk(), bk()
                k.op("pe", lambda e: e.matmul(bbc[:, 0:256], lhsT=tri[:], rhs=la[p][:], start=True, stop=True), reads=[tri, la[p]], writes=[bbc])
                k.op("pe", lambda e: e.matmul(bto[:, 0:256], lhsT=self.ones_f[:], rhs=la[p][:], start=True, stop=True), reads=[self.ones_f, la[p]], writes=[bto])
                for hp in range(2):
                    k.op("pe", lambda e: e.matmul(bto[:, 256 + 2 * hp:258 + 2 * hp], lhsT=la[p][:, hp * 128:(hp + 1) * 128], rhs=self.ones_f[:, 0:2], start=True, stop=True),
                         reads=[la[p], self.ones_f], writes=[bto])
                k.op("act", lambda e: e.activation(out=ebc[p][:], in_=bbc[:, 0:256], func=AF.Exp), reads=[bbc], writes=[ebc[p]])
                yield
                k.op("act", lambda e: e.activation(out=enbc[p][:], in_=bbc[:, 0:256], func=AF.Exp, scale=-1.0), reads=[bbc], writes=[enbc[p]])
                yield
                k.op("act", lambda e: e.copy(out=bcs[p][:], in_=bbc[:, 0:256]), reads=[bbc], writes=[bcs[p]])
                yield
                k.op("dve", lambda e: e.tensor_tensor(out=ekd[p][:], in0=bto[:, 0:256], in1=bcs[p][:], op=ALU.subtract), reads=[bto, bcs[p]], writes=[ekd[p]])
                yield
                k.op("act", lambda e: e.activation(out=ekd[p][:], in_=ekd[p][:], func=AF.Exp), reads=[ekd[p]], writes=[ekd[p]])
                yield
                k.op("act", lambda e: e.activation(out=etc_[p][:], in_=bto[:, 256:260], func=AF.Exp), reads=[bto], writes=[etc_[p]])
                yield
                k.op("dve", lambda e: e.scalar_tensor_tensor(out=qs[p][:], in0=qkv[p][:, 0:256], scalar=0.125, in1=ebc[p][:], op0=ALU.mult, op1=ALU.mult), reads=[qkv[p], ebc[p]], writes=[qs[p]])
                yield
                k.op("dve", lambda e: e.tensor_tensor(out=ks[p][:], in0=qkv[p][:, 256:512], in1=enbc[p][:], op=ALU.mult), reads=[qkv[p], enbc[p]], writes=[ks[p]])
                yield
                k.op("pool", lambda e: e.tensor_tensor(out=kd[p][:], in0=qkv[p][:, 256:512], in1=ekd[p][:], op=ALU.mult), reads=[qkv[p], ekd[p]], writes=[kd[p]])
                yield
                k.op("pool", lambda e: e.tensor_copy(out=vb[p][:].rearrange("p a b -> p (a b)"), in_=qkv[p][:, 512:1024]), reads=[qkv[p]], writes=[vb[p]])
                yield
                btr = bk()
                btrb = btr.t[:, :].bitcast(BF16)
                for hp in range(2):
                    k.op("pe", lambda e: e.transpose(out=btrb[:, hp * 128:(hp + 1) * 128], in_=qs[p][:, hp * 128:(hp + 1) * 128], identity=self.ident_b[:]), reads=[qs[p], self.ident_b], writes=[btr])
                    k.op("pe", lambda e: e.transpose(out=btrb[:, (2 + hp) * 128:(3 + hp) * 128], in_=ks[p][:, hp * 128:(hp + 1) * 128], identity=self.ident_b[:]), reads=[ks[p], self.ident_b], writes=[btr])
                for h in range(4):
                    hb, hp = h % 2, h // 2
                    k.op("act", lambda e: e.copy(out=qT[p][hb * 64:(hb + 1) * 64, h, :], in_=btrb[hb * 64:(hb + 1) * 64, hp * 128:(hp + 1) * 128]), reads=[btr], writes=[qT[p]])
                k.op("act", lambda e: e.copy(out=kT[p][:].rearrange("p a b -> p (a b)"), in_=btrb[:, 256:512]), reads=[btr], writes=[kT[p]])
                yield
                bat = bk()
                for h in range(4):
                    hb, hp = h % 2, h // 2
                    ps_ = slice(hb * 64, (hb + 1) * 64)
                    k.op("pe", lambda e: e.matmul(bat[:, h * 128:(h + 1) * 128], lhsT=kT[p][:, hp, :], rhs=qT[p][:, h, :], start=True, stop=True), reads=[kT[p], qT[p]], writes=[bat])
                k.op("dve", lambda e: e.tensor_tensor(out=att[p][:].rearrange("p a b -> p (a b)"), in0=bat[:, :], in1=mI[:].rearrange("p a b -> p (a b)"), op=ALU.mult), reads=[bat, mI], writes=[att[p]])
                yield
                yield

            def post(bk, p, kind, j):
                if kind == "c":
                    orows = S["o_gla"][d, j * 128:(j + 1) * 128, :]
                    odeps = [self.ogla_dep[d][j]]
                else:
                    orows = S["o_gla"][d, LC:LS, :].rearrange("(r w) n -> w r n", w=64)[j]
                    odeps = self.ogla_dep[d][2:]
                bo, bkv = bk(), bk()
                for h in range(4):
                    hb, hp = h % 2, h // 2
                    ps_ = slice(hb * 64, (hb + 1) * 64)
                    k.op("pe", lambda e: e.matmul(bo[:, h * 128:(h + 1) * 128], lhsT=qT[p][:, h, :], rhs=Sb[:, hp, :], start=True, stop=False), reads=[qT[p], Sb], writes=[bo])
                    k.op("pe", lambda e: e.matmul(bo[:, h * 128:(h + 1) * 128], lhsT=att[p][:, h, :], rhs=vb[p][:, h, :], start=False, stop=True), reads=[att[p], vb[p]], writes=[bo])
                for h in range(4):
                    hb, hp = h % 2, h // 2
                    k.op("pe", lambda e: e.matmul(bkv[:, h * 128:(h + 1) * 128], lhsT=kd[p][:, hp * 128:(hp + 1) * 128], rhs=vb[p][:, h, :], start=True, stop=True),
                         reads=[kd[p], vb[p]], writes=[bkv])
                k.op("act", lambda e: e.copy(out=osb[p][:], in_=bo[:, :]), reads=[bo], writes=[osb[p]])
                yield
                k.dma("act", orows, osb[p][:], reads=[osb[p]], writes=odeps)
                for h in range(4):
                    hb, hp = h % 2, h // 2
                    ps_ = slice(hb * 64, (hb + 1) * 64)
                    k.op("dve", lambda e: e.scalar_tensor_tensor(out=Sf[ps_, hp, :], in0=Sf[ps_, hp, :], scalar=etc_[p][ps_, 2 * hp:2 * hp + 1], in1=bkv[ps_, h * 128:(h + 1) * 128], op0=ALU.mult, op1=ALU.add),
                         reads=[Sf, etc_[p], bkv], writes=[Sf])
                k.op("act", lambda e: e.copy(out=Sb[:], in_=Sf[:]), reads=[Sf], writes=[Sb])
                yield


                yield

            def posts(items):
                for (p_, kind_, j_) in items:
                    yield from post(bk_post, p_, kind_, j_)

            n_ = len(order)
            first = [pre(bk_preA, 0, *order[0])]
            if n_ > 1:
                first.append(pre(bk_preB, 1, *order[1]))
            self.interleave(first)
            for R in range(0, n_, 2):
                gens = [posts([(s % 4,) + tuple(order[s]) for s in (R, R + 1) if s < n_])]
                if R + 2 < n_:
                    gens.append(pre(bk_preA, (R + 2) % 4, *order[R + 2]))
                if R + 3 < n_:
                    gens.append(pre(bk_preB, (R + 3) % 4, *order[R + 3]))
                self.interleave(gens)

    def p4(self, l):
        k, I, S = self.k, self.I, self.S
        sb = k.sb
        last = l == self.nlayers - 1
        moe = (l % 2 == 1)
        T = 1024
        NH = T // 512
        gnb = sb([128, 8, 128], F32, "gnb")
        for h in range(8):
            src_ = I["dn_norm"] if h < 4 else I["gla_norm"]
            k.dma("sp", gnb[:, h, :], src_[l:l + 1, :].broadcast_to([128, 128]), writes=[gnb])
        xT = sb([128, 8, T], F32, "xT")
        mT = sb([128, 8, T], BF16, "mT")
        sq2 = mT
        h2T = sb([128, 8, T], BF16, "h2T")
        rs2 = sb([128, T], F32, "rs2")
        tmpf = [sb([128, T], F32, "tmpf%d" % i) for i in range(2)]
        F = D_EXP if moe else D_FF
        NFC = F // 128
        actT = sb([128, NFC, T], BF16, "actT")
        ot = [sb([128, 512], F32, "ot%d" % i) for i in range(4)]
        zt = sb([128, 1024], F32, "zt")
        xt = sb([128, D], F32, "xt4")
        xo = xt
        osum = sb([128, 8, 128], F32, "osum")
        ssh = sb([128, 16], F32, "ssh")
        mb = sb([128, D], BF16, "mb")
        wb = [sb([128, 8, 512], BF16, "wb%d" % i) for i in range(3)]
        wd = [sb([128, 4, 512], BF16, "wd%d" % i) for i in range(2)]
        sg = [ot[0], ot[1]]
        if moe:
            rt = sb([128, 8, NE], F32, "rt")
            k.dma("sp", rt[:], I["moe_router"].rearrange("(k p) n -> p k n", p=128), writes=[rt])
            lgT = sb([8, T], F32, "lgT")
            lg = sb([128, 8, NE], F32, "lg")
            l2 = sb([128, 8, NE], F32, "l2")
            mx = sb([128, 16], F32, "mx")
            cbT = sb([8, T], F32, "cbT")
            CBe = [zt, xt]
            selE = sb([8, NE, 128], F32, "selE")
            for e_ in range(NE):
                k.op("pool", lambda e: e.tensor_copy(out=selE[0:8, e_, :], in_=self.ident_f[0:8, e_:e_ + 1].to_broadcast([8, 128])), reads=[self.ident_f], writes=[selE])
        ntb = T // 128
        if last:
            blocks = [(2 + ntb * i, ntb) for i in range(NOWN // T)]
        else:
            blocks = [(0, 2)] + [(2 + ntb * i, ntb) for i in range(LL // T)]
        wi = [0]

        def wload(src_ap, ncols):
            t_ = wb[wi[0] % 3]
            wi[0] += 1
            k.dma("pool", t_[:, :, 0:ncols], src_ap, writes=[t_], max_dma_last_dim=2048)
            return t_
        wout_src = I["w_out"][l].rearrange("(k p) n -> p k n", p=128)
        for (t0, nt) in blocks:
            if t0 >= self.ntiles:
                break
            Tb = nt * 128
            halves = [(h0, min(512, Tb - h0)) for h0 in range(0, Tb, 512)]
            r = 1 if t0 < 2 else 0
            for ti in range(nt):
                i = t0 + ti
                rows = slice(i * 128, (i + 1) * 128)
                tsl = slice(ti * 128, (ti + 1) * 128)
                k.dma("sp", ot[0][:], S["o_dn"][0, rows, :], reads=[self.odn_dep[0][i]], writes=[ot[0]])
                k.dma("sp", ot[1][:], S["o_dn"][1, rows, :], reads=[self.odn_dep[1][i]], writes=[ot[1]])
                k.dma("sp", ot[2][:], S["o_gla"][0, rows, :], reads=[self.ogla_dep[0][i]], writes=[ot[2]])
                k.dma("sp", ot[3][:], S["o_gla"][1, rows, :], reads=[self.ogla_dep[1][i]], writes=[ot[3]])
                k.dma("sp", zt[:, 0:512], S["tm"][rows, 0:512], reads=[self.tm_dep[i]], writes=[zt])
                k.dma("sp", zt[:, 512:1024], S["tm"][rows, 1552:2064], reads=[self.tm_dep[i]], writes=[zt])
                k.dma("sp", xt[:], self.xcur[rows, :], reads=[self.xcur_dep[i]], writes=[xt])
                osf = osum[:].rearrange("p a b -> p (a b)")
                k.op("dve", lambda e: e.tensor_tensor(out=osf[:, 0:512], in0=ot[0][:], in1=ot[1][:], op=ALU.add), reads=[ot[0], ot[1]], writes=[osum])
                k.op("dve", lambda e: e.tensor_tensor(out=osf[:, 512:1024], in0=ot[2][:], in1=ot[3][:], op=ALU.add), reads=[ot[2], ot[3]], writes=[osum])
                for hh in range(2):
                    k.op("act", lambda e: e.activation(out=ot[2 * hh][:], in_=osf[:, hh * 512:(hh + 1) * 512], func=AF.Square), reads=[osum], writes=[ot[2 * hh]])
                    k.op("dve", lambda e: e.tensor_reduce(out=ssh[:, 4 * hh:4 * hh + 4], in_=ot[2 * hh][:].rearrange("p (a b) -> p a b", a=4), axis=AX.X, op=ALU.add), reads=[ot[2 * hh]], writes=[ssh])
                k.op("act", lambda e: e.activation(out=ssh[:, 8:16], in_=ssh[:, 0:8], func=AF.Sqrt, scale=1.0 / 128.0, bias=self.eps_t[:, 0:1]), reads=[ssh, self.eps_t], writes=[ssh])
                k.op("dve", lambda e: e.reciprocal(out=ssh[:, 8:16], in_=ssh[:, 8:16]), reads=[ssh], writes=[ssh])
                k.op("act", lambda e: e.activation(out=zt[:], in_=zt[:], func=AF.Silu), reads=[zt], writes=[zt])
                for h in range(8):
                    k.op("dve", lambda e: e.scalar_tensor_tensor(out=osum[:, h, :], in0=osum[:, h, :], scalar=ssh[:, 8 + h:9 + h], in1=gnb[:, h, :], op0=ALU.mult, op1=ALU.mult),
                         reads=[osum, ssh, gnb], writes=[osum])
                k.op("dve", lambda e: e.tensor_tensor(out=mb[:], in0=osf, in1=zt[:], op=ALU.mult), reads=[osum, zt], writes=[mb])
                b = self.bank()
                bb = b.t[:, :].bitcast(BF16)
                for kk in range(8):
                    k.op("pe", lambda e: e.transpose(out=bb[:, kk * 128:(kk + 1) * 128], in_=mb[:, kk * 128:(kk + 1) * 128], identity=self.ident_b[:]), reads=[mb, self.ident_b], writes=[b])
                k.op("act", lambda e: e.copy(out=mT[:, :, tsl], in_=bb[:, :].rearrange("p (a b) -> p a b", a=8)), reads=[b], writes=[mT])
                for half in range(2):
                    b2 = self.bank()
                    for q in range(4):
                        kk = half * 4 + q
                        k.op("pe", lambda e: e.transpose(out=b2[:, q * 128:(q + 1) * 128], in_=xt[:, kk * 128:(kk + 1) * 128], identity=self.ident_f[:]), reads=[xt, self.ident_f], writes=[b2])
                    k.op("dve", lambda e: e.tensor_copy(out=xT[:, half * 4:half * 4 + 4, tsl], in_=b2[:, :].rearrange("p (a b) -> p a b", a=4)), reads=[b2], writes=[xT])
            for og in range(2):
                wt = wload(wout_src[:, :, og * 512:(og + 1) * 512], 512)
                for q in range(4):
                    oc = og * 4 + q
                    for (h0, hw) in halves:
                        b = self.bank()
                        for kk in range(8):
                            k.op("pe", lambda e: e.matmul(b[:, 0:hw], lhsT=wt[:, kk, q * 128:(q + 1) * 128], rhs=mT[:, kk, h0:h0 + hw], start=(kk == 0), stop=(kk == 7)), reads=[wt, mT], writes=[b])
                        k.op("dve", lambda e: e.scalar_tensor_tensor(out=xT[:, oc, h0:h0 + hw], in0=b[:, 0:hw], scalar=self.modT[:, 16 + oc, r:r + 1], in1=xT[:, oc, h0:h0 + hw], op0=ALU.mult, op1=ALU.add),
                             reads=[b, self.modT, xT], writes=[xT])

            def rms_bcast(dst):
                for kk in range(8):
                    k.op("act", lambda e: e.activation(out=sq2[:, kk, 0:Tb], in_=xT[:, kk, 0:Tb], func=AF.Square), reads=[xT], writes=[sq2])
                for (h0, hw) in halves:
                    bs_ = self.bank()
                    for kk in range(8):
                        k.op("pe", lambda e: e.matmul(bs_[:, 0:hw], lhsT=self.ones_b[:], rhs=sq2[:, kk, h0:h0 + hw], start=(kk == 0), stop=(kk == 7)), reads=[self.ones_b, sq2], writes=[bs_])
                    k.op("act", lambda e: e.activation(out=dst[:, h0:h0 + hw], in_=bs_[:, 0:hw], func=AF.Sqrt, scale=1.0 / D, bias=self.eps_t[:, 0:1]), reads=[bs_, self.eps_t], writes=[dst])
                k.op("dve", lambda e: e.reciprocal(out=dst[:, 0:Tb], in_=dst[:, 0:Tb]), reads=[dst], writes=[dst])
            rms_bcast(rs2)
            if moe:
                bl = [self.bank() for _ in halves]
            for kk in range(8):
                tf = tmpf[kk % 2]
                k.op("dve", lambda e: e.tensor_tensor(out=tf[:, 0:Tb], in0=xT[:, kk, 0:Tb], in1=rs2[:, 0:Tb], op=ALU.mult), reads=[xT, rs2], writes=[tf])
                k.op("act", lambda e: e.activation(out=h2T[:, kk, 0:Tb], in_=tf[:, 0:Tb], func=AF.Identity, scale=self.g2T[:, kk, r:r + 1], bias=self.modT[:, 24 + kk, r:r + 1]),
                     reads=[tf, self.g2T, self.modT], writes=[h2T])
                if moe:
                    k.op("dve", lambda e: e.tensor_scalar(out=tf[:, 0:Tb], in0=tf[:, 0:Tb], scalar1=self.g2T[:, kk, r:r + 1], scalar2=self.modT[:, 24 + kk, r:r + 1], op0=ALU.mult, op1=ALU.add),
                         reads=[tf, self.g2T, self.modT], writes=[tf])
                    for hi, (h0, hw) in enumerate(halves):
                        k.op("pe", lambda e: e.matmul(bl[hi][0:8, 0:hw], lhsT=rt[:, kk, :], rhs=tf[:, h0:h0 + hw], start=(kk == 0), stop=(kk == 7)), reads=[rt, tf], writes=[bl[hi]])
            experts = [None]
            if moe:
                experts = list(range(NE))
                for hi, (h0, hw) in enumerate(halves):
                    k.op("act", lambda e: e.copy(out=lgT[:, h0:h0 + hw], in_=bl[hi][0:8, 0:hw]), reads=[bl[hi]], writes=[lgT])
                bl2 = self.bank()
                for ti in range(nt):
                    k.op("pe", lambda e: e.transpose(out=bl2[:, ti * 8:(ti + 1) * 8], in_=lgT[0:8, ti * 128:(ti + 1) * 128], identity=self.ident_f[0:8, 0:8]), reads=[lgT, self.ident_f], writes=[bl2])
                k.op("dve", lambda e: e.tensor_copy(out=lg[:].rearrange("p a b -> p (a b)"), in_=bl2[:, 0:64]), reads=[bl2], writes=[lg])
                bc3 = lambda ap: ap.unsqueeze(2).to_broadcast([128, 8, NE])
                k.op("dve", lambda e: e.tensor_reduce(out=mx[:, 0:8], in_=lg[:], axis=AX.X, op=ALU.max), reads=[lg], writes=[mx])
                k.op("pool", lambda e: e.tensor_tensor(out=l2[:], in0=lg[:], in1=bc3(mx[:, 0:8]), op=ALU.subtract), reads=[lg, mx], writes=[l2])
                k.op("dve", lambda e: e.tensor_scalar(out=lg[:], in0=l2[:], scalar1=0.0, scalar2=-1e30, op0=ALU.is_equal, op1=ALU.mult), reads=[l2], writes=[lg])
                k.op("dve", lambda e: e.tensor_tensor(out=lg[:], in0=lg[:], in1=l2[:], op=ALU.add), reads=[lg, l2], writes=[lg])
                k.op("dve", lambda e: e.tensor_reduce(out=mx[:, 8:16], in_=lg[:], axis=AX.X, op=ALU.max), reads=[lg], writes=[mx])
                k.op("pool", lambda e: e.tensor_tensor(out=lg[:], in0=l2[:], in1=bc3(mx[:, 8:16]), op=ALU.subtract), reads=[l2, mx], writes=[lg])
                k.op("dve", lambda e: e.tensor_scalar(out=lg[:], in0=lg[:], scalar1=0.0, scalar2=None, op0=ALU.is_ge), reads=[lg], writes=[lg])
                k.op("act", lambda e: e.activation(out=l2[:], in_=l2[:], func=AF.Exp), reads=[l2], writes=[l2])
                k.op("dve", lambda e: e.tensor_tensor(out=l2[:], in0=l2[:], in1=lg[:], op=ALU.mult), reads=[l2, lg], writes=[l2])
                k.op("dve", lambda e: e.tensor_reduce(out=mx[:, 0:8], in_=l2[:], axis=AX.X, op=ALU.add), reads=[l2], writes=[mx])
                k.op("dve", lambda e: e.reciprocal(out=mx[:, 0:8], in_=mx[:, 0:8]), reads=[mx], writes=[mx])
                k.op("pool", lambda e: e.tensor_tensor(out=l2[:], in0=l2[:], in1=bc3(mx[:, 0:8]), op=ALU.mult), reads=[l2, mx], writes=[l2])
                for hi, (h0, hw) in enumerate(halves):
                    bl3 = self.bank()
                    for t4 in range(hw // 128):
                        ti = h0 // 128 + t4
                        k.op("pe", lambda e: e.transpose(out=bl3[0:8, t4 * 128:(t4 + 1) * 128], in_=l2[:, ti, :], identity=self.ident_f[:]), reads=[l2, self.ident_f], writes=[bl3])
                    k.op("act", lambda e: e.copy(out=cbT[:, h0:h0 + hw], in_=bl3[0:8, 0:hw]), reads=[bl3], writes=[cbT])
            for ex_ in experts:
                if moe:
                    wgu_src = I["moe_w_gu"][ex_].rearrange("(k p) n -> p k n", p=128)
                    wdn_src = I["moe_w_down"][ex_].rearrange("(f p) n -> p f n", p=128)
                    cbe = CBe[ex_ % 2]
                    for (h0, hw) in halves:
                        bcb = self.bank()
                        k.op("pe", lambda e: e.matmul(bcb[:, 0:hw], lhsT=selE[0:8, ex_, :], rhs=cbT[0:8, h0:h0 + hw], start=True, stop=True), reads=[selE, cbT], writes=[bcb])
                        k.op("act", lambda e: e.copy(out=cbe[:, h0:h0 + hw], in_=bcb[:, 0:hw]), reads=[bcb], writes=[cbe])
                else:
                    wgu_src = I["ffn_w_gu"].rearrange("(k p) n -> p k n", p=128)
                    wdn_src = I["ffn_w_down"].rearrange("(f p) n -> p f n", p=128)
                si = 0
                for g0 in range(0, F, 512):
                    gw = min(512, F - g0)
                    wgt = wload(wgu_src[:, :, g0:g0 + gw], gw)
                    wut = wload(wgu_src[:, :, F + g0:F + g0 + gw], gw)
                    for c in range(gw // 128):
                        fc = g0 // 128 + c
                        for (h0, hw) in halves:
                            bg_, bu_ = self.bank(), self.bank()
                            for kk in range(8):
                                k.op("pe", lambda e: e.matmul(bg_[:, 0:hw], lhsT=wgt[:, kk, c * 128:(c + 1) * 128], rhs=h2T[:, kk, h0:h0 + hw], start=(kk == 0), stop=(kk == 7)), reads=[wgt, h2T], writes=[bg_])
                            for kk in range(8):
                                k.op("pe", lambda e: e.matmul(bu_[:, 0:hw], lhsT=wut[:, kk, c * 128:(c + 1) * 128], rhs=h2T[:, kk, h0:h0 + hw], start=(kk == 0), stop=(kk == 7)), reads=[wut, h2T], writes=[bu_])
                            s_ = sg[si % 2]
                            si += 1
                            k.op("act", lambda e: e.activation(out=s_[:, 0:hw], in_=bg_[:, 0:hw], func=AF.Silu), reads=[bg_], writes=[s_])
                            if moe:
                                k.op("dve", lambda e: e.tensor_tensor(out=s_[:, 0:hw], in0=s_[:, 0:hw], in1=cbe[:, h0:h0 + hw], op=ALU.mult), reads=[s_, cbe], writes=[s_])
                            k.op("dve", lambda e: e.tensor_tensor(out=actT[:, fc, h0:h0 + hw], in0=bu_[:, 0:hw], in1=s_[:, 0:hw], op=ALU.mult), reads=[bu_, s_], writes=[actT])
                nf = F // 128
                for ocg in range(2):
                    banks = [[self.bank() for _ in range(4)] for _ in halves]
                    for f0 in range(0, nf, 4):
                        wdt = wd[wi[0] % 2]
                        wi[0] += 1
                        n4 = min(4, nf - f0)
                        k.dma("pool", wdt[:, 0:n4, 0:512], wdn_src[:, f0:f0 + n4, ocg * 512:(ocg + 1) * 512], writes=[wdt], max_dma_last_dim=2048)
                        for hi, (h0, hw) in enumerate(halves):
                            for q in range(4):
                                for fi in range(n4):
                                    f = f0 + fi
                                    k.op("pe", lambda e: e.matmul(banks[hi][q][:, 0:hw], lhsT=wdt[:, fi, q * 128:(q + 1) * 128], rhs=actT[:, f, h0:h0 + hw], start=(f == 0), stop=(f == nf - 1)),
                                         reads=[wdt, actT], writes=[banks[hi][q]])
                    for hi, (h0, hw) in enumerate(halves):
                        for q in range(4):
                            oc = ocg * 4 + q
                            k.op("dve", lambda e: e.scalar_tensor_tensor(out=xT[:, oc, h0:h0 + hw], in0=banks[hi][q][:, 0:hw], scalar=self.modT[:, 40 + oc, r:r + 1], in1=xT[:, oc, h0:h0 + hw], op0=ALU.mult, op1=ALU.add),
                                 reads=[banks[hi][q], self.modT, xT], writes=[xT])
            if last:
                rms_bcast(rs2)
                for kk in range(8):
                    k.op("dve", lambda e: e.scalar_tensor_tensor(out=xT[:, kk, 0:Tb], in0=xT[:, kk, 0:Tb], scalar=self.fnT[:, kk, 0:1], in1=rs2[:, 0:Tb], op0=ALU.mult, op1=ALU.mult),
                         reads=[xT, self.fnT, rs2], writes=[xT])
            for ti in range(nt):
                i = t0 + ti
                tsl = slice(ti * 128, (ti + 1) * 128)
                for half in range(2):
                    b2 = self.bank()
                    for q in range(4):
                        kk = half * 4 + q
                        k.op("pe", lambda e: e.transpose(out=b2[:, q * 128:(q + 1) * 128], in_=xT[:, kk, tsl], identity=self.ident_f[:]), reads=[xT, self.ident_f], writes=[b2])
                    k.op("act", lambda e: e.copy(out=xo[:, half * 512:(half + 1) * 512], in_=b2[:, :]), reads=[b2], writes=[xo])
                if last:
                    k.dma("sp", self.out[(i - 2) * 128:(i - 1) * 128, :], xo[:], reads=[xo], writes=[self.out_dep])
                else:
                    k.dma("sp", S["x1"][i * 128:(i + 1) * 128, :], xo[:], reads=[xo], writes=[self.x1_dep[i]])

    def build(self):
        self.declare()
        k = self.k
        self.consts()
        self.eps_t = k.sb([128, 1], F32, "eps_t")
        k.op("pool", lambda e: e.memset(self.eps_t[:], EPS), writes=[self.eps_t])
        self.modT = k.sb([128, 48, 2], F32, "modT")
        self.nT = k.sb([128, 8, 2], F32, "nT")
        self.g1T = k.sb([128, 8, 2], F32, "g1T")
        self.g2T = k.sb([128, 8, 2], F32, "g2T")
        self.fnT = k.sb([128, 8, 1], F32, "fnT")
        self.scT = k.sb([128, 8, 2], BF16, "scT")
        self.xs_dep = [DR() for _ in range(NT)]
        self.xcur, self.xcur_dep = self.I["xs"], self.xs_dep
        for l in range(self.nlayers):
            if "p0" in self.phases:
                with k.scope():
                    self.p0_alloc()
                    self.p0(l)
            if "p1" in self.phases:
                with k.scope():
                    self.p1_alloc()
                    self.p1(l, self.xcur, self.xcur_dep)
            if "p2" in self.phases:
                with k.scope():
                    self.p2(l)
            if "p3" in self.phases:
                with k.scope():
                    self.p3(l)
            if "p4" in self.phases:
                with k.scope():
                    self.p4(l)
                self.xcur, self.xcur_dep = self.S["x1"], self.x1_dep
        k.barrier()
        self.es.close()
        return self.nc


def _core_inputs(inp, core):
    b, j = core // 2, core % 2
    f = (lambda a: a[::-1]) if j else (lambda a: a)
    d = {}
    d["xs"] = np.ascontiguousarray(np.concatenate([f(inp["ctx"][b]), f(inp["x"][b])], axis=0))
    d["cvec"] = np.ascontiguousarray(np.stack([inp["c"][b], inp["c_ctx"]], axis=0))
    w_in = inp["w_in"]
    if j:
        w_in = w_in.copy()
        for base, n in ((2048, 4), (2056, 4), (3600, 16)):
            a = w_in[:, :, base:base + n].copy()
            w_in[:, :, base:base + n] = w_in[:, :, base + n:base + 2 * n]
            w_in[:, :, base + n:base + 2 * n] = a
    d["w_in"] = np.ascontiguousarray(w_in)
    sw = (lambda a: a[:, ::-1]) if j else (lambda a: a)
    d["conv"] = np.ascontiguousarray(sw(inp["conv_qkv"]))
    d["dn_a_log"] = np.ascontiguousarray(sw(inp["dn_a_log"]).reshape(2, 8))
    d["dn_dt_bias"] = np.ascontiguousarray(sw(inp["dn_dt_bias"]).reshape(2, 8))
    d["gla_w_gate2"] = np.ascontiguousarray(sw(inp["gla_w_gate2"]))
    d["gla_b_gate"] = np.ascontiguousarray(sw(inp["gla_b_gate"]))
    for n in ("w_ada", "b_ada", "norm_mix", "norm_ffn", "dn_norm", "gla_norm", "w_out", "final_norm"):
        d[n] = np.ascontiguousarray(inp[n])
    d["ffn_w_gu"] = np.ascontiguousarray(inp["ffn_w_gu"][0])
    d["ffn_w_down"] = np.ascontiguousarray(inp["ffn_w_down"][0])
    d["moe_router"] = np.ascontiguousarray(inp["moe_router"][0])
    d["moe_w_gu"] = np.ascontiguousarray(inp["moe_w_gu"][0])
    d["moe_w_down"] = np.ascontiguousarray(inp["moe_w_down"][0])
    return d


def kernel(**inputs):
    inp = {k_: np.asarray(v, dtype=np.float32) for k_, v in inputs.items()}
    prog = Prog()
    nc = prog.build()
    in_maps = []
    for core in range(8):
        ci = _core_inputs(inp, core)
        in_maps.append({n: ci[n] for n in prog.I})
    res = run_bass_kernel_spmd(nc, in_maps, core_ids=list(range(8)))
    out = np.empty((4, LL, D), np.float32)
    for core in range(8):
        b, j = core // 2, core % 2
        y = np.asarray(res.results[core]["out"], dtype=np.float32)
        if j == 0:
            out[b, 0:NOWN] = y
        else:
            out[b, LL - NOWN:LL] = y[::-1]
    return out
```

```python
import numpy as np
from contextlib import ExitStack
import concourse.bass as bass
import concourse.mybir as mybir
from concourse.bass_utils import run_bass_kernel_spmd

F32 = mybir.dt.float32
BF16 = mybir.dt.bfloat16
AF = mybir.ActivationFunctionType
ALU = mybir.AluOpType
AX = mybir.AxisListType

D = 1024
LC = 256
LL = 8192
LS = LC + LL
NT = LS // 128
IN_COLS = 3632
NTM = IN_COLS - 1536
EPS = 1e-6
D_FF = 2816
D_EXP = 3584
NE = 8
NOWN = 4096


class Dep:
    __slots__ = ("w", "r")

    def __init__(self):
        self.w = None
        self.r = {}


class Tile:
    excl = False

    def __init__(self, t):
        self.t = t
        self.dep = Dep()

    def __getitem__(self, idx):
        return self.t[idx]


class K:
    RING = 64

    def __init__(self, nc, es):
        self.nc = nc
        self.es = es
        self.E = {"pe": nc.tensor, "dve": nc.vector, "act": nc.scalar, "pool": nc.gpsimd, "sp": nc.sync}
        self.sems = {}
        for e in self.E:
            self.sems[e] = es.enter_context(nc.semaphore("sem_" + e))
        for i in range(self.RING):
            self.sems[("d", i)] = es.enter_context(nc.semaphore("dsem%d" % i))
        self.cnt = {e: 0 for e in self.E}
        self.seen = {e: {} for e in self.E}
        self.dn = 0
        self.qn = {}
        self.dtar = [0] * self.RING
        self.nt = 0

    def sb(self, shape, dt, name=None):
        self.nt += 1
        return Tile(self.es.enter_context(self.nc.sbuf_tensor("%s_%d" % (name or "t", self.nt), list(shape), dt)))

    def psum(self, shape, dt, name=None):
        self.nt += 1
        t = Tile(self.es.enter_context(self.nc.psum_tensor(name or ("p%d" % self.nt), list(shape), dt)))
        t.excl = True
        return t

    def _wait(self, eng, tok):
        if tok is None:
            return
        key, val = tok
        if key == eng and eng == "pe":
            return
        if self.seen[eng].get(key, 0) >= val:
            return
        self.E[eng].wait_ge(self.sems[key], val)
        self.seen[eng][key] = val

    def _pre(self, eng, reads, writes):
        for d in reads:
            self._wait(eng, d.dep.w)
        for d in writes:
            self._wait(eng, d.dep.w)
            for tok in d.dep.r.values():
                self._wait(eng, tok)

    def _post(self, tok, reads, writes):
        for d in reads:
            d.dep.r[tok[0]] = tok
        for d in writes:
            d.dep.w = tok
            d.dep.r = {}

    def op(self, eng, fn, reads=(), writes=()):
        ex = [d for d in reads if getattr(d, "excl", False)]
        if ex:
            writes = list(writes) + ex
            reads = [d for d in reads if not getattr(d, "excl", False)]
        self._pre(eng, reads, writes)
        ins = fn(self.E[eng])
        ins.then_inc(self.sems[eng], 1)
        self.cnt[eng] += 1
        self._post((eng, self.cnt[eng]), reads, writes)

    QRING = {"sp": (0, 32), "pool": (32, 24), "act": (56, 8)}

    def dma(self, q, out, in_, reads=(), writes=(), **kw):
        self._pre(q, reads, writes)
        base, n = self.QRING[q]
        cnt = self.qn.get(q, 0)
        self.qn[q] = cnt + 1
        slot = base + cnt % n
        self.dn += 1
        key = ("d", slot)
        if self.dtar[slot] > 0:
            self._wait(q, (key, self.dtar[slot]))
        self.dtar[slot] += 16
        self.E[q].dma_start(out=out, in_=in_, **kw).then_inc(self.sems[key], 16)
        self._post((key, self.dtar[slot]), reads, writes)

    def barrier(self):
        for e in self.E:
            for e2 in self.E:
                if e2 != e and self.cnt[e2] > 0:
                    self._wait(e, (e2, self.cnt[e2]))
            for s in range(self.RING):
                if self.dtar[s] > 0:
                    self._wait(e, (("d", s), self.dtar[s]))

    def scope(self):
        outer = self.es
        k = self

        class _S:
            def __enter__(s):
                k.es = ExitStack()
                return s

            def __exit__(s, *a):
                k.barrier()
                k.es.close()
                k.es = outer
                return False
        return _S()

    def finish(self, deps):
        for d in deps:
            self._wait("sp", d.dep.w)


class DR:
    def __init__(self):
        self.dep = Dep()


class Prog:
    def __init__(self, debug=(), nlayers=2, phases=("p0", "p1", "p2", "p3", "p4"), ntiles=NT):
        self.debug = tuple(debug)
        self.nlayers = nlayers
        self.phases = phases
        self.ntiles = ntiles
        self.p2_stop = 9
        nc = self.nc = bass.Bass("TRN2", target_bir_lowering=False)
        self.I = {}
        self.S = {}
        self.es = ExitStack()
        self.k = K(nc, self.es)

    def inp(self, name, shape):
        self.I[name] = self.nc.dram_tensor(name, list(shape), F32, kind="ExternalInput").ap()
        return self.I[name]

    def scr(self, name, shape, dt=F32):
        kind = "ExternalOutput" if name in self.debug else "Internal"
        self.S[name] = self.nc.dram_tensor(name, list(shape), dt, kind=kind).ap()
        return self.S[name]

    def declare(self):
        inp = self.inp
        inp("xs", [LS, D]); inp("cvec", [2, D]); inp("w_ada", [2, D, 6 * D]); inp("b_ada", [2, 6 * D])
        inp("norm_mix", [2, D]); inp("norm_ffn", [2, D]); inp("w_in", [2, D, IN_COLS]); inp("conv", [2, 5, 1536])
        inp("dn_a_log", [2, 8]); inp("dn_dt_bias", [2, 8]); inp("dn_norm", [2, 128])
        inp("gla_w_gate2", [2, 2, 16, 256]); inp("gla_b_gate", [2, 2, 256]); inp("gla_norm", [2, 128])
        if "p4" in self.phases:
            inp("w_out", [2, D, D]); inp("ffn_w_gu", [D, 2 * D_FF]); inp("ffn_w_down", [D_FF, D])
            if self.nlayers > 1:
                inp("moe_router", [D, NE]); inp("moe_w_gu", [NE, D, 2 * D_EXP]); inp("moe_w_down", [NE, D_EXP, D])
        inp("final_norm", [D])
        self.out = self.nc.dram_tensor("out", [NOWN, D], F32, kind="ExternalOutput").ap()
        self.out_dep = DR()
        self.scr("tm", [LS, NTM]); self.scr("qkvT", [1536, LS + 8], BF16)
        self.scr("o_dn", [2, LS, 512]); self.scr("o_gla", [2, LS, 512]); self.scr("x1", [LS, D])
        self.x1_dep = [DR() for _ in range(NT)]
        self.scr("sa_kq", [17, 128, 4096], BF16); self.scr("sa_vt", [17, 128, 2048], BF16); self.scr("sa_kv", [17, 4, 128, 1024], BF16)
        self.sa_dep = [[DR() for _ in range(7)] for _ in range(17)]
        self.odn_dep = [[DR() for _ in range(NT)] for _ in range(2)]
        self.ogla_dep = [[DR() for _ in range(NT)] for _ in range(2)]
        self.tm_dep = [DR() for _ in range(NT)]
        self.qkv_dep = [DR() for _ in range(NT)]
        self.qkv_pad_dep = DR()
        self.qkvc_dep = [[DR() for _ in range(12)] for _ in range(17)]

    def dbg(self, name, tile, shape, dt=F32):
        if name not in self.debug or name in self.S:
            return
        if len(shape) == 2 and len(tile.t.shape) == 4:
            ap = self.nc.dram_tensor(name, list(shape), dt, kind="ExternalOutput").ap()
            self.S[name] = ap
            self.k.dma("sp", ap, tile[:].rearrange("p a b c -> p (a b c)"), reads=[tile], writes=[DR()])
            return
        ap = self.nc.dram_tensor(name, list(shape), dt, kind="ExternalOutput").ap()
        self.S[name] = ap
        src_ap = tile[:]
        if len(shape) == 2 and len(tile.t.shape) == 3:
            src_ap = tile[:].rearrange("p a b -> p (a b)")
        self.k.dma("sp", ap, src_ap, reads=[tile], writes=[DR()])

    def consts(self):
        k = self.k
        self.ident_f = k.sb([128, 128], F32, "ident_f")
        self.ident_b = k.sb([128, 128], BF16, "ident_b")
        self.ones_b = k.sb([128, 128], BF16, "ones_b")
        self.ones_f = k.sb([128, 128], F32, "ones_f")
        self.zeros_f = k.sb([128, 8], BF16, "zeros_b")
        idf, idb = self.ident_f, self.ident_b
        k.op("pool", lambda e: e.memset(idf[:], 1.0), writes=[idf])
        k.op("pool", lambda e: e.affine_select(out=idf[:], in_=idf[:], pattern=[[1, 128]], compare_op=ALU.is_equal,
                                                fill=0.0, base=0, channel_multiplier=-1), reads=[idf], writes=[idf])
        k.op("pool", lambda e: e.tensor_copy(out=idb[:], in_=idf[:]), reads=[idf], writes=[idb])
        k.op("pool", lambda e: e.memset(self.ones_b[:], 1.0), writes=[self.ones_b])
        k.op("pool", lambda e: e.memset(self.ones_f[:], 1.0), writes=[self.ones_f])
        k.op("pool", lambda e: e.memset(self.zeros_f[:], 0.0), writes=[self.zeros_f])
        self.ps = [k.psum([128, 512], F32, "ps%d" % i) for i in range(8)]
        self.psi = 0

    def bank(self):
        b = self.ps[self.psi % 8]
        self.psi += 1
        return b

    def rows_to_cols(self, rows, R, n, dst):
        k = self.k
        b = self.bank()
        for c in range(n):
            k.op("pe", lambda e, c=c: e.transpose(out=b[:, c * R:(c + 1) * R], in_=rows[0:R, c * 128:(c + 1) * 128],
                                                  identity=self.ident_f[0:R, 0:R]), reads=[rows, self.ident_f], writes=[b])
        k.op("dve", lambda e: e.tensor_copy(out=dst[:].rearrange("p n r -> p (n r)"), in_=b[:, 0:n * R]), reads=[b], writes=[dst])

    def p0_alloc(self):
        k = self.k
        self.crow = k.sb([2, D], F32, "crow")
        self.cT = k.sb([128, 8, 2], F32, "cT")
        self.wada = [k.sb([128, 8, 512], BF16, "wada%d" % i) for i in range(2)]
        self.modrow = k.sb([2, 6 * D], F32, "modrow")
        self.brow = k.sb([2, 6 * D], F32, "brow")
        self.nrow = k.sb([2, D], F32, "nrow")
        self.fnrow = k.sb([1, D], F32, "fnrow")

    def p0(self, l):
        k, I = self.k, self.I
        crow, cT, scT, modrow, brow, modT = self.crow, self.cT, self.scT, self.modrow, self.brow, self.modT
        if l == 0:
            k.dma("sp", crow[:], I["cvec"], writes=[crow])
            self.rows_to_cols(crow, 2, 8, cT)
            k.op("act", lambda e: e.activation(out=scT[:], in_=cT[:], func=AF.Silu), reads=[cT], writes=[scT])
            k.dma("sp", self.fnrow[:], I["final_norm"].rearrange("(o n) -> o n", o=1), writes=[self.fnrow])
            self.rows_to_cols(self.fnrow, 1, 8, self.fnT)
        for r in range(2):
            k.dma("sp", brow[r:r + 1, :], I["b_ada"][l:l + 1, :], writes=[brow])
        k.dma("sp", self.nrow[0:1, :], I["norm_mix"][l:l + 1, :], writes=[self.nrow])
        k.dma("sp", self.nrow[1:2, :], I["norm_ffn"][l:l + 1, :], writes=[self.nrow])
        self.rows_to_cols(self.nrow, 2, 8, self.nT)
        wsrc = I["w_ada"][l].rearrange("(k p) n -> p k n", p=128)
        for g in range(12):
            wt = self.wada[g % 2]
            k.dma("pool", wt[:], wsrc[:, :, g * 512:(g + 1) * 512], writes=[wt], max_dma_last_dim=2048)
            b = self.bank()
            for kk in range(8):
                k.op("pe", lambda e, kk=kk: e.matmul(b[0:2, :], lhsT=scT[:, kk, :], rhs=wt[:, kk, :], start=(kk == 0), stop=(kk == 7)),
                     reads=[scT, wt], writes=[b])
            k.op("dve", lambda e, g=g: e.tensor_tensor(out=modrow[:, g * 512:(g + 1) * 512], in0=b[0:2, :], in1=brow[:, g * 512:(g + 1) * 512], op=ALU.add),
                 reads=[b, brow], writes=[modrow])
        self.rows_to_cols(modrow, 2, 48, modT)
        for r in range(2):
            k.op("dve", lambda e, r=r: e.scalar_tensor_tensor(out=self.g1T[:, :, r], in0=modT[:, 8:16, r], scalar=1.0, in1=self.nT[:, :, 0], op0=ALU.add, op1=ALU.mult),
                 reads=[modT, self.nT], writes=[self.g1T])
            k.op("dve", lambda e, r=r: e.scalar_tensor_tensor(out=self.g2T[:, :, r], in0=modT[:, 32:40, r], scalar=1.0, in1=self.nT[:, :, 1], op0=ALU.add, op1=ALU.mult),
                 reads=[modT, self.nT], writes=[self.g2T])

    def p1_alloc(self):
        k = self.k
        self.w_fm = k.sb([128, 8, 1536], BF16, "w_fm")
        self.w_tm = k.sb([128, 8, NTM], BF16, "w_tm")
        self.xt = [k.sb([128, D], F32, "xt%d" % i) for i in range(2)]
        self.junk = k.sb([128, D], BF16, "junk")
        self.ss = [k.sb([128, 2], F32, "ss%d" % i) for i in range(2)]
        self.xn = [k.sb([128, D], BF16, "xn%d" % i) for i in range(2)]
        self.hT = [k.sb([128, 8, 512], BF16, "hT%d" % i) for i in range(2)]
        self.tmo = [k.sb([128, NTM], F32, "tmo%d" % i) for i in range(2)]
        self.qo = [k.sb([128, 512], BF16, "qo%d" % i) for i in range(3)]
        self.sa_raw = [k.sb([128, 4, 516], BF16, "sa_raw%d" % i) for i in range(3)]
        self.sa_diag = [[k.sb([128, 5, 128], BF16, "sa_diag%d_%d" % (s, i)) for i in range(2)] for s in range(3)]
        self.sa_t1 = [[k.sb([128, 512], F32, "sa_t1_%d_%d" % (s, i)) for i in range(2)] for s in range(3)]
        self.sa_sil = [[k.sb([128, 512], F32, "sa_sil%d_%d" % (s, i)) for i in range(2)] for s in range(2)]
        self.sa_sq = [k.sb([128, 512], BF16, "sa_sq%d" % s) for s in range(2)]
        self.sa_rn = [k.sb([128, 512], F32, "sa_rn%d" % s) for s in range(2)]
        self.sa_q = k.sb([128, 4, 512], BF16, "sa_q")
        self.sa_k = k.sb([128, 4, 512], BF16, "sa_k")
        self.sa_vt = k.sb([128, 4, 512], BF16, "sa_vt")
        self.sa_kv = [k.sb([128, 8, 128], BF16, "sa_kv%d" % i) for i in range(2)]
        self.sa_convrow = k.sb([5, 1536], F32, "sa_convrow")
        self.sa_cw = k.sb([128, 12, 5], F32, "sa_cw")
        self.sa_qeps = k.sb([128, 1], F32, "sa_qeps")

    def p1(self, l, xsrc, xsrc_dep):
        k, I, S = self.k, self.I, self.S
        wsrc = I["w_in"][l].rearrange("(k p) n -> p k n", p=128)
        for kk in range(8):
            k.dma("pool", self.w_fm[:, kk, :], wsrc[:, kk, 0:1536], writes=[self.w_fm], max_dma_last_dim=2048)
            k.dma("pool", self.w_tm[:, kk, :], wsrc[:, kk, 1536:IN_COLS], writes=[self.w_tm], max_dma_last_dim=2048)
        if l == 0:
            for (c0, n) in ((0, 2), (258, 4), (LS + 6, 2)):
                for ch in range(12):
                    k.dma("sp", S["qkvT"][ch * 128:(ch + 1) * 128, c0:c0 + n], self.zeros_f[:, 0:n], reads=[self.zeros_f], writes=[self.qkv_pad_dep])
        blocks = [(0, 2)] + [(2 + 4 * i, 4) for i in range(16)]
        blocks = [b_ for b_ in blocks if b_[0] < self.ntiles]

        def cyc(ids):
            st = [0]

            def f():
                b = self.ps[ids[st[0] % len(ids)]]
                st[0] += 1
                return b
            return f
        bk_p, bk_t, bk_f = cyc([0]), cyc([1, 2]), cyc([3, 4])
        bk_sa = [cyc([5]), cyc([6]), cyc([7])]
        k.dma("sp", self.sa_convrow[:], I["conv"][l], writes=[self.sa_convrow])
        self.rows_to_cols(self.sa_convrow, 5, 12, self.sa_cw)
        k.op("pool", lambda e: e.memset(self.sa_qeps[:], 128.0 * EPS), writes=[self.sa_qeps])
        nblk = len(blocks)

        def stagea(bk, cg, t0, nt):
            T = nt * 128
            col0 = t0 * 128 + (2 if t0 < 2 else 6)
            blk = 0 if t0 < 2 else 1 + (t0 - 2) // 4
            raw, cw, qeps = self.sa_raw[cg], self.sa_cw, self.sa_qeps
            deps = [self.qkvc_dep[b_][cg * 4 + c_] for b_ in range(max(blk - 1, 0), min(blk + 2, nblk)) for c_ in range(4)] + [self.qkv_pad_dep]
            k.dma("sp", raw[:, :, 0:T + 4], S["qkvT"][cg * 512:(cg + 1) * 512, col0 - 2:col0 + T + 2].rearrange("(c p) t -> p c t", p=128),
                  reads=deps, writes=[raw])
            for c4 in range(4):
                ch = cg * 4 + c4
                dg = self.sa_diag[cg][c4 % 2]
                t1 = self.sa_t1[cg][c4 % 2]
                for j in range(5):
                    k.op("dve", lambda e: e.tensor_scalar(out=dg[:, j, :], in0=self.ident_b[:], scalar1=cw[:, ch, j:j + 1], scalar2=None, op0=ALU.mult),
                         reads=[self.ident_b, cw], writes=[dg])
                yield
                bc_ = bk()
                for j in range(5):
                    k.op("pe", lambda e: e.matmul(bc_[:, 0:T], lhsT=dg[:, j, :], rhs=raw[:, c4, j:j + T], start=(j == 0), stop=(j == 4)), reads=[dg, raw], writes=[bc_])
                yield
                k.op("act", lambda e: e.activation(out=t1[:, 0:T], in_=bc_[:, 0:T], func=AF.Exp, scale=-1.0), reads=[bc_], writes=[t1])
                yield
                k.op("act", lambda e: e.activation(out=t1[:, 0:T], in_=t1[:, 0:T], func=AF.Ln, bias=1.0), reads=[t1], writes=[t1])
                yield
                k.op("act", lambda e: e.activation(out=t1[:, 0:T], in_=t1[:, 0:T], func=AF.Exp, scale=-1.0), reads=[t1], writes=[t1])
                yield
                if cg == 2:
                    k.op("dve", lambda e: e.tensor_tensor(out=self.sa_vt[:, c4, 0:T], in0=bc_[:, 0:T], in1=t1[:, 0:T], op=ALU.mult), reads=[bc_, t1], writes=[self.sa_vt])
                    yield
                    continue
                s_, q_, r_ = self.sa_sil[cg][c4 % 2], self.sa_sq[cg], self.sa_rn[cg]
                k.op("dve", lambda e: e.tensor_tensor(out=s_[:, 0:T], in0=bc_[:, 0:T], in1=t1[:, 0:T], op=ALU.mult), reads=[bc_, t1], writes=[s_])
                yield
                k.op("act", lambda e: e.activation(out=q_[:, 0:T], in_=s_[:, 0:T], func=AF.Square), reads=[s_], writes=[q_])
                yield
                b = bk()
                k.op("pe", lambda e: e.matmul(b[:, 0:T], lhsT=self.ones_b[:], rhs=q_[:, 0:T], start=True, stop=True), reads=[self.ones_b, q_], writes=[b])
                if cg == 0:
                    k.op("act", lambda e: e.activation(out=r_[:, 0:T], in_=b[:, 0:T], func=AF.Ln, scale=128.0, bias=qeps[:, 0:1]), reads=[b, qeps], writes=[r_])
                else:
                    k.op("act", lambda e: e.activation(out=r_[:, 0:T], in_=b[:, 0:T], func=AF.Ln, scale=1.0, bias=self.eps_t[:, 0:1]), reads=[b, self.eps_t], writes=[r_])
                yield
                k.op("act", lambda e: e.activation(out=r_[:, 0:T], in_=r_[:, 0:T], func=AF.Exp, scale=-0.5), reads=[r_], writes=[r_])
                yield
                dst = self.sa_q if cg == 0 else self.sa_k
                k.op("dve", lambda e: e.tensor_tensor(out=dst[:, c4, 0:T], in0=s_[:, 0:T], in1=r_[:, 0:T], op=ALU.mult), reads=[s_, r_], writes=[dst])
                yield

        def stagea_fin(bk, t0, nt):
            blk = 0 if t0 < 2 else 1 + (t0 - 2) // 4
            kqd = S["sa_kq"][blk].rearrange("p (a b c) -> p a b c", a=4, b=2)
            k.dma("pool", kqd[:, :, 0, :], self.sa_k[:], reads=[self.sa_k], writes=[self.sa_dep[blk][4]])
            k.dma("pool", kqd[:, :, 1, :], self.sa_q[:], reads=[self.sa_q], writes=[self.sa_dep[blk][6]])
            k.dma("pool", S["sa_vt"][blk], self.sa_vt[:].rearrange("p a b -> p (a b)"), reads=[self.sa_vt], writes=[self.sa_dep[blk][5]])
            for ti in range(nt):
                kvt = self.sa_kv[ti % 2]
                b = bk()
                bb = b.t[:, :].bitcast(BF16)
                for h in range(4):
                    k.op("pe", lambda e: e.transpose(out=bb[:, h * 128:(h + 1) * 128], in_=self.sa_k[:, h, ti * 128:(ti + 1) * 128], identity=self.ident_b[:]),
                         reads=[self.sa_k, self.ident_b], writes=[b])
                    k.op("pe", lambda e: e.transpose(out=bb[:, (4 + h) * 128:(5 + h) * 128], in_=self.sa_vt[:, h, ti * 128:(ti + 1) * 128], identity=self.ident_b[:]),
                         reads=[self.sa_vt, self.ident_b], writes=[b])
                k.op("act", lambda e: e.copy(out=kvt[:].rearrange("p a b -> p (a b)"), in_=bb[:, :]), reads=[b], writes=[kvt])
                k.dma("pool", S["sa_kv"][blk, ti], kvt[:].rearrange("p a b -> p (a b)"), reads=[kvt], writes=[self.sa_dep[blk][ti]])
                yield

        def prep(bk, hT, t0, nt):
            r = 1 if t0 < 2 else 0
            for ti in range(nt):
                i = t0 + ti
                x_t, ss, xn = self.xt[i % 2], self.ss[i % 2], self.xn[i % 2]
                k.dma("sp", x_t[:], xsrc[i * 128:(i + 1) * 128, :], reads=[xsrc_dep[i]], writes=[x_t])
                k.op("act", lambda e: e.activation(out=self.junk[:], in_=x_t[:], func=AF.Square, accum_out=ss[:, 0:1]), reads=[x_t], writes=[self.junk, ss])
                yield
                k.op("act", lambda e: e.activation(out=ss[:, 1:2], in_=ss[:, 0:1], func=AF.Sqrt, scale=1.0 / D, bias=self.eps_t[:, 0:1]), reads=[ss, self.eps_t], writes=[ss])
                yield
                k.op("dve", lambda e: e.reciprocal(out=ss[:, 1:2], in_=ss[:, 1:2]), reads=[ss], writes=[ss])
                k.op("dve", lambda e: e.tensor_scalar(out=xn[:], in0=x_t[:], scalar1=ss[:, 1:2], scalar2=None, op0=ALU.mult), reads=[x_t, ss], writes=[xn])
                yield
                b = bk()
                bb = b.t[:, :].bitcast(BF16)
                for kk in range(8):
                    k.op("pe", lambda e, kk=kk: e.transpose(out=bb[:, kk * 128:(kk + 1) * 128], in_=xn[:, kk * 128:(kk + 1) * 128], identity=self.ident_b[:]),
                         reads=[xn, self.ident_b], writes=[b])
                yield
                for kk in range(8):
                    if kk % 2 == 0:
                        k.op("act", lambda e, kk=kk: e.activation(out=hT[:, kk, ti * 128:(ti + 1) * 128], in_=bb[:, kk * 128:(kk + 1) * 128], func=AF.Identity,
                                                                  scale=self.g1T[:, kk, r:r + 1], bias=self.modT[:, kk, r:r + 1]),
                             reads=[b, self.g1T, self.modT], writes=[hT])
                    else:
                        k.op("dve", lambda e, kk=kk: e.tensor_scalar(out=hT[:, kk, ti * 128:(ti + 1) * 128], in0=bb[:, kk * 128:(kk + 1) * 128],
                                                                     scalar1=self.g1T[:, kk, r:r + 1], scalar2=self.modT[:, kk, r:r + 1], op0=ALU.mult, op1=ALU.add),
                             reads=[b, self.g1T, self.modT], writes=[hT])
                    if kk % 2:
                        yield

        def tmproj(bk, hT, t0, nt):
            ev = 0
            for ti in range(nt):
                i = t0 + ti
                tmo = self.tmo[i % 2]
                for cg in range(5):
                    n = min(512, NTM - cg * 512)
                    b2 = bk()
                    for kk in range(8):
                        k.op("pe", lambda e, kk=kk: e.matmul(b2[:, 0:n], lhsT=hT[:, kk, ti * 128:(ti + 1) * 128], rhs=self.w_tm[:, kk, cg * 512:cg * 512 + n],
                                                             start=(kk == 0), stop=(kk == 7)), reads=[hT, self.w_tm], writes=[b2])
                    yield
                    ev += 1
                    if ev % 2:
                        k.op("act", lambda e: e.copy(out=tmo[:, cg * 512:cg * 512 + n], in_=b2[:, 0:n]), reads=[b2], writes=[tmo])
                    else:
                        k.op("dve", lambda e: e.tensor_copy(out=tmo[:, cg * 512:cg * 512 + n], in_=b2[:, 0:n]), reads=[b2], writes=[tmo])
                k.dma("pool", S["tm"][i * 128:(i + 1) * 128, :], tmo[:], reads=[tmo], writes=[self.tm_dep[i]])
                yield

        def fmproj(bk, hT, t0, nt):
            T = nt * 128
            col0 = t0 * 128 + (2 if t0 < 2 else 6)
            for ch in range(12):
                b3 = bk()
                for kk in range(8):
                    k.op("pe", lambda e, kk=kk: e.matmul(b3[:, 0:T], lhsT=self.w_fm[:, kk, ch * 128:(ch + 1) * 128], rhs=hT[:, kk, 0:T], start=(kk == 0), stop=(kk == 7)),
                         reads=[hT, self.w_fm], writes=[b3])
                yield
                qo = self.qo[ch % 3]
                if ch % 2:
                    k.op("act", lambda e: e.copy(out=qo[:, 0:T], in_=b3[:, 0:T]), reads=[b3], writes=[qo])
                else:
                    k.op("dve", lambda e: e.tensor_copy(out=qo[:, 0:T], in_=b3[:, 0:T]), reads=[b3], writes=[qo])
                k.dma("pool", S["qkvT"][ch * 128:(ch + 1) * 128, col0:col0 + T], qo[:, 0:T], reads=[qo], writes=[self.qkv_dep[t0 + j] for j in range(nt)] + [self.qkvc_dep[0 if t0 < 2 else 1 + (t0 - 2) // 4][ch]])
                yield

        self.interleave([prep(bk_p, self.hT[0], *blocks[0])])
        for bi, (t0, nt) in enumerate(blocks):
            hT = self.hT[bi % 2]
            gens = [tmproj(bk_t, hT, t0, nt), fmproj(bk_f, hT, t0, nt)]
            if bi + 1 < len(blocks):
                gens.append(prep(bk_p, self.hT[(bi + 1) % 2], *blocks[bi + 1]))
            if bi >= 2:
                gens += [stagea(bk_sa[cg], cg, *blocks[bi - 2]) for cg in range(3)]
            self.interleave(gens)
            if bi >= 2:
                self.interleave([stagea_fin(bk_p, *blocks[bi - 2])])
        for bi in range(max(nblk - 2, 0), nblk):
            self.interleave([stagea(bk_sa[cg], cg, *blocks[bi]) for cg in range(3)])
            self.interleave([stagea_fin(bk_p, *blocks[bi])])

    def dir_masks(self, d):
        k = self.k
        mI = k.sb([128, 4, 128], BF16, "maskI")
        mS = k.sb([128, 4, 128], BF16, "maskS")
        tri = k.sb([128, 128], F32, "tri")
        sgn = 1 if d == 0 else -1
        for (m, cmp) in ((mI, ALU.is_ge), (mS, ALU.is_gt)):
            k.op("pool", lambda e: e.memset(m[:], 1.0), writes=[m])
            k.op("pool", lambda e: e.affine_select(out=m[:], in_=m[:], pattern=[[0, 4], [sgn, 128]], compare_op=cmp, fill=0.0,
                                                    base=0, channel_multiplier=-sgn), reads=[m], writes=[m])
        k.op("pool", lambda e: e.tensor_copy(out=tri[:], in_=mI[:, 0, :]), reads=[mI], writes=[tri])
        return mI, mS, tri

    def bcast_row(self, src_row_ap, n, dst):
        self.k.dma("sp", dst[:, 0:n], src_row_ap.broadcast_to([128, n]), writes=[dst])

    @staticmethod
    def interleave(gens):
        gens = list(gens)
        while gens:
            nxt = []
            for g in gens:
                try:
                    next(g)
                    nxt.append(g)
                except StopIteration:
                    pass
            gens = nxt

    def p2(self, l):
        k, I, S = self.k, self.I, self.S
        sb = k.sb
        flat = lambda t: t[:].rearrange("p a b -> p (a b)")
        dtb = sb([128, 8], F32, "dtb")
        nA = sb([128, 8], F32, "nA")
        self.bcast_row(I["dn_dt_bias"][l:l + 1, :], 8, dtb)
        self.bcast_row(I["dn_a_log"][l:l + 1, :], 8, nA)
        k.op("act", lambda e: e.activation(out=nA[:], in_=nA[:], func=AF.Exp), reads=[nA], writes=[nA])
        k.op("dve", lambda e: e.tensor_scalar(out=nA[:], in0=nA[:], scalar1=-1.0, scalar2=None, op0=ALU.mult), reads=[nA], writes=[nA])
        ident4b = sb([128, 4, 128], BF16, "ident4b")
        for h in range(4):
            k.op("pool", lambda e: e.tensor_copy(out=ident4b[:, h, :], in_=self.ident_f[:]), reads=[self.ident_f], writes=[ident4b])
        bd32 = sb([128, 4, 128], BF16, "bd32"); m1 = sb([128, 4, 128], BF16, "m1"); m2 = sb([128, 4, 128], BF16, "m2")
        for (bs_, nb_, dst) in ((32, 4, bd32), (64, 2, m1)):
            E = sb([4, 128], F32, "Eblk%d" % bs_)
            k.op("pool", lambda e: e.memset(E[:], 1.0), writes=[E])
            k.op("pool", lambda e: e.affine_select(out=E[:], in_=E[:], pattern=[[1, 128]], compare_op=ALU.is_ge, fill=0.0, base=0, channel_multiplier=-bs_), reads=[E], writes=[E])
            k.op("pool", lambda e: e.affine_select(out=E[:], in_=E[:], pattern=[[-1, 128]], compare_op=ALU.is_ge, fill=0.0, base=bs_ - 1, channel_multiplier=bs_), reads=[E], writes=[E])
            bm = self.bank()
            k.op("pe", lambda e: e.matmul(bm[:, 0:128], lhsT=E[0:nb_, :], rhs=E[0:nb_, :], start=True, stop=True), reads=[E], writes=[bm])
            for h in range(4):
                k.op("dve", lambda e: e.tensor_copy(out=dst[:, h, :], in_=bm[:, 0:128]), reads=[bm], writes=[dst])
        k.op("dve", lambda e: e.tensor_scalar(out=m2[:], in0=m1[:], scalar1=-1.0, scalar2=1.0, op0=ALU.mult, op1=ALU.add), reads=[m1], writes=[m2])
        k.op("dve", lambda e: e.tensor_tensor(out=m1[:], in0=m1[:], in1=bd32[:], op=ALU.subtract), reads=[m1, bd32], writes=[m1])
        KQT = [sb([128, 4, 2, 512], BF16, "KQT%d" % i) for i in range(2)]
        VT = [sb([128, 4, 512], BF16, "VT%d" % i) for i in range(2)]
        KVtm = [[sb([128, 8, 128], BF16, "KVtm%d_%d" % (j, i)) for i in range(4)] for j in range(2)]
        def mk(n, shape, dt, name):
            return [sb(shape, dt, "%s%d" % (name, i)) for i in range(n)]
        sc4 = sb([128, 4, 16], F32, "sc4")
        Gd = mk(2, [128, 4, 128], F32, "Gd")
        Dm = mk(2, [128, 4, 128], F32, "Dm")
        Eg = mk(2, [128, 4, 128], F32, "Eg")
        A = mk(2, [128, 4, 128], BF16, "A"); AT = mk(2, [128, 4, 128], BF16, "AT")
        Abd = mk(2, [128, 4, 128], BF16, "Abd"); ATbd = mk(2, [128, 4, 128], BF16, "ATbd")
        Xa = mk(2, [128, 4, 128], BF16, "Xa"); XTa = mk(2, [128, 4, 128], BF16, "XTa")
        YT = mk(2, [128, 4, 128], BF16, "YT")
        R0 = mk(2, [128, 4, 128], BF16, "R0"); R1 = mk(2, [128, 4, 128], BF16, "R1")
        B1T = mk(2, [128, 4, 128], BF16, "B1T"); B2T = mk(2, [128, 4, 128], BF16, "B2T")
        Keg = mk(2, [128, 4, 128], BF16, "Keg")
        def mk2(shape, dt, name):
            return [[sb(shape, dt, "%s%d_%d" % (name, j, i)) for i in range(4)] for j in range(2)]
        smb = [sb([128, 4, 32], F32, "smb%d" % j) for j in range(3)]
        attnT = mk2([128, 4, 128], BF16, "attnT")
        up = mk2([128, 4, 128], F32, "up")
        wT = mk2([128, 4, 128], BF16, "wT")
        kdec = mk2([128, 4, 128], BF16, "kdec")
        qdecT = mk2([128, 4, 128], BF16, "qdecT")
        vnew = [sb([128, 4, 128], BF16, "vnew%d" % i) for i in range(2)]
        osb = [sb([128, 4, 128], F32, "osb%d" % i) for i in range(2)]
        Sf = sb([128, 4, 128], F32, "Sf")
        Sb = sb([128, 4, 128], BF16, "Sb")

        def stage_a(bk, d, tri, si, bp, t0, nt):
            T = nt * 128
            col0 = t0 * 128 + (2 if t0 < 2 else 6)
            kq, vt = KQT[bp], VT[bp]
            smT = smb[si]
            v = lambda a, b_: smT[:, 0:nt, a:b_]
            bcs = lambda t_: t_[:, 4 * d:4 * d + 4].unsqueeze(1).to_broadcast([128, nt, 4])
            k.dma("sp", sc4[:, 0:nt, :], S["tm"][t0 * 128:(t0 + nt) * 128, 512:528].rearrange("(t p) n -> p t n", p=128),
                  reads=[self.tm_dep[i] for i in range(t0, t0 + nt)], writes=[sc4])
            k.op("act", lambda e: e.activation(out=v(0, 4), in_=sc4[:, 0:nt, 4 * d:4 * d + 4], func=AF.Exp, scale=-1.0), reads=[sc4], writes=[smT])
            k.op("pool", lambda e: e.tensor_tensor(out=v(4, 8), in0=sc4[:, 0:nt, 8 + 4 * d:12 + 4 * d], in1=bcs(dtb), op=ALU.add), reads=[sc4, dtb], writes=[smT])
            yield
            k.op("dve", lambda e: e.tensor_scalar(out=v(0, 4), in0=v(0, 4), scalar1=1.0, scalar2=None, op0=ALU.add), reads=[smT], writes=[smT])
            k.op("dve", lambda e: e.reciprocal(out=v(0, 4), in_=v(0, 4)), reads=[smT], writes=[smT])
            k.op("act", lambda e: e.activation(out=v(4, 8), in_=v(4, 8), func=AF.Exp), reads=[smT], writes=[smT])
            yield
            k.op("act", lambda e: e.activation(out=v(4, 8), in_=v(4, 8), func=AF.Ln, bias=1.0), reads=[smT], writes=[smT])
            yield
            k.op("pool", lambda e: e.tensor_tensor(out=v(4, 8), in0=v(4, 8), in1=bcs(nA), op=ALU.mult), reads=[smT, nA], writes=[smT])
            yield
            bs = bk()
            for ti in range(nt):
                k.op("pe", lambda e: e.matmul(bs[:, ti * 8:ti * 8 + 4], lhsT=tri[:], rhs=smT[:, ti, 4:8], start=True, stop=True), reads=[tri, smT], writes=[bs])
                k.op("pe", lambda e: e.matmul(bs[:, ti * 8 + 4:ti * 8 + 8], lhsT=self.ones_f[:], rhs=smT[:, ti, 4:8], start=True, stop=True), reads=[self.ones_f, smT], writes=[bs])
            yield
            k.op("dve", lambda e: e.tensor_copy(out=v(8, 16), in_=bs[:, 0:nt * 8].rearrange("p (t n) -> p t n", t=nt)), reads=[bs], writes=[smT])
            yield
            k.op("act", lambda e: e.activation(out=v(16, 20), in_=v(8, 12), func=AF.Exp), reads=[smT], writes=[smT])
            k.op("dve", lambda e: e.tensor_tensor(out=v(20, 24), in0=v(12, 16), in1=v(8, 12), op=ALU.subtract), reads=[smT], writes=[smT])
            yield
            k.op("act", lambda e: e.activation(out=v(20, 24), in_=v(20, 24), func=AF.Exp), reads=[smT], writes=[smT])
            k.op("act", lambda e: e.activation(out=v(24, 28), in_=v(12, 16), func=AF.Exp), reads=[smT], writes=[smT])
            yield
            k.op("dve", lambda e: e.tensor_tensor(out=v(20, 24), in0=v(20, 24), in1=v(0, 4), op=ALU.mult), reads=[smT], writes=[smT])
            yield
            blk = 0 if t0 < 2 else 1 + (t0 - 2) // 4
            if True:
                k.dma("sp", kq[:].rearrange("p a b c -> p (a b c)"), S["sa_kq"][blk], reads=[self.sa_dep[blk][4], self.sa_dep[blk][6]], writes=[kq])
                k.dma("sp", vt[:].rearrange("p a b -> p (a b)"), S["sa_vt"][blk], reads=[self.sa_dep[blk][5]], writes=[vt])
                yield
                for ti in range(nt):
                    k.dma("sp", KVtm[bp][ti][:].rearrange("p a b -> p (a b)"), S["sa_kv"][blk, ti], reads=[self.sa_dep[blk][ti]], writes=[KVtm[bp][ti]])
                    yield
                return

        def stage_b(bk, d, mI, mS, tri, si, bp, q, i, ti):
            kq, kv = KQT[bp], KVtm[bp][ti]
            tsl = slice(ti * 128, (ti + 1) * 128)
            smT = smb[si]
            sm_ = smT[:, ti, :]
            G = Gd[q]
            k.op("pool", lambda e: e.tensor_tensor(out=G[:], in0=mI[:], in1=sm_[:, 4:8].unsqueeze(2).to_broadcast([128, 4, 128]), op=ALU.mult),
                 reads=[mI, smT], writes=[G])
            yield
            bg = bk()
            k.op("pe", lambda e: e.matmul(bg[:, :], lhsT=self.ones_f[:], rhs=flat(G), start=True, stop=True), reads=[self.ones_f, G], writes=[bg])
            yield
            bg3 = bg[:, :].rearrange("p (a b) -> p a b", a=4)
            for h in range(4):
                k.op("dve", lambda e: e.tensor_scalar(out=Dm[q][:, h, :], in0=bg3[:, h, :], scalar1=sm_[:, 8 + h:9 + h], scalar2=0.0, op0=ALU.subtract, op1=ALU.min),
                     reads=[bg, smT], writes=[Dm[q]])
            yield
            k.op("act", lambda e: e.activation(out=flat(Eg[q]), in_=bg[:, :], func=AF.Exp), reads=[bg], writes=[Eg[q]])
            k.op("act", lambda e: e.activation(out=Dm[q][:], in_=Dm[q][:], func=AF.Exp), reads=[Dm[q]], writes=[Dm[q]])
            yield
            k.op("pool", lambda e: e.tensor_tensor(out=qdecT[bp][ti][:], in0=kq[:, :, 1, tsl], in1=Eg[q][:], op=ALU.mult), reads=[kq, Eg[q]], writes=[qdecT[bp][ti]])
            bet = sm_[:, 0:4].unsqueeze(2).to_broadcast([128, 4, 128])
            DmI, DmS = Gd[q], Eg[q]
            k.op("pool", lambda e: e.tensor_tensor(out=DmI[:], in0=Dm[q][:], in1=mI[:], op=ALU.mult), reads=[Dm[q], mI], writes=[DmI])
            yield
            k.op("pool", lambda e: e.tensor_tensor(out=DmS[:], in0=Dm[q][:], in1=mS[:], op=ALU.mult), reads=[Dm[q], mS], writes=[DmS])
            k.op("pool", lambda e: e.tensor_tensor(out=DmI[:], in0=DmI[:], in1=bet, op=ALU.mult), reads=[DmI, smT], writes=[DmI])
            k.op("pool", lambda e: e.tensor_tensor(out=DmS[:], in0=DmS[:], in1=bet, op=ALU.mult), reads=[DmS, smT], writes=[DmS])
            K3 = kv[:, 0:4, :]
            k.op("pool", lambda e: e.tensor_tensor(out=Keg[q][:], in0=K3, in1=sm_[:, 16:20].unsqueeze(2).to_broadcast([128, 4, 128]), op=ALU.mult), reads=[kv, smT], writes=[Keg[q]])
            k.op("pool", lambda e: e.tensor_tensor(out=kdec[bp][ti][:], in0=K3, in1=sm_[:, 20:24].unsqueeze(2).to_broadcast([128, 4, 128]), op=ALU.mult), reads=[kv, smT], writes=[kdec[bp][ti]])
            bkk, bkq = bk(), bk()
            for h in range(4):
                k.op("pe", lambda e: e.matmul(bkk[:, h * 128:(h + 1) * 128], lhsT=kq[:, h, 0, tsl], rhs=kq[:, h, 0, tsl], start=True, stop=True), reads=[kq], writes=[bkk])
                k.op("pe", lambda e: e.matmul(bkq[:, h * 128:(h + 1) * 128], lhsT=kq[:, h, 0, tsl], rhs=kq[:, h, 1, tsl], start=True, stop=True), reads=[kq], writes=[bkq])
            yield
            k.op("dve", lambda e: e.tensor_tensor(out=flat(A[q]), in0=bkk[:, :], in1=flat(DmS), op=ALU.mult), reads=[bkk, DmS], writes=[A[q]])
            k.op("dve", lambda e: e.tensor_tensor(out=flat(attnT[bp][ti]), in0=bkq[:, :], in1=flat(DmI), op=ALU.mult), reads=[bkq, DmI], writes=[attnT[bp][ti]])
            yield
            bt = bk()
            btb = bt.t[:, :].bitcast(BF16)
            for h in range(4):
                k.op("pe", lambda e: e.transpose(out=btb[:, h * 128:(h + 1) * 128], in_=A[q][:, h, :], identity=self.ident_b[:]), reads=[A[q], self.ident_b], writes=[bt])
            k.op("pool", lambda e: e.tensor_tensor(out=Abd[q][:], in0=A[q][:], in1=bd32[:], op=ALU.mult), reads=[A[q], bd32], writes=[Abd[q]])
            yield
            k.op("act", lambda e: e.copy(out=flat(AT[q]), in_=btb[:, 0:512]), reads=[bt], writes=[AT[q]])
            k.op("pool", lambda e: e.tensor_tensor(out=R0[q][:], in0=ident4b[:], in1=Abd[q][:], op=ALU.subtract), reads=[ident4b, Abd[q]], writes=[R0[q]])
            yield
            k.op("pool", lambda e: e.tensor_tensor(out=ATbd[q][:], in0=AT[q][:], in1=bd32[:], op=ALU.mult), reads=[AT[q], bd32], writes=[ATbd[q]])
            k.op("pool", lambda e: e.tensor_tensor(out=B1T[q][:], in0=AT[q][:], in1=m1[:], op=ALU.mult), reads=[AT[q], m1], writes=[B1T[q]])
            k.op("pool", lambda e: e.tensor_tensor(out=B2T[q][:], in0=AT[q][:], in1=m2[:], op=ALU.mult), reads=[AT[q], m2], writes=[B2T[q]])
            yield
            Xc, XTc, Rc = Abd[q], ATbd[q], R0[q]
            xbufs = [(Xa[q], XTa[q]), (A[q], AT[q])]
            rbufs = [R1[q], R0[q]]
            for lev in range(4):
                lastl = lev == 3
                Xn, XTn = xbufs[lev % 2]
                Rn = rbufs[lev % 2]
                b1 = bk()
                for h in range(4):
                    k.op("pe", lambda e: e.matmul(b1[:, h * 128:(h + 1) * 128], lhsT=Xc[:, h, :], rhs=XTc[:, h, :], start=True, stop=True), reads=[Xc, XTc], writes=[b1])
                if not lastl:
                    b2 = bk()
                    for h in range(4):
                        k.op("pe", lambda e: e.matmul(b2[:, h * 128:(h + 1) * 128], lhsT=XTc[:, h, :], rhs=Xc[:, h, :], start=True, stop=True), reads=[Xc, XTc], writes=[b2])
                yield
                k.op("dve", lambda e: e.tensor_tensor(out=flat(YT[q]), in0=b1[:, :], in1=flat(ident4b), op=ALU.add), reads=[b1, ident4b], writes=[YT[q]])
                if not lastl:
                    k.op("act", lambda e: e.copy(out=flat(XTn), in_=b1[:, :]), reads=[b1], writes=[XTn])
                    k.op("act", lambda e: e.copy(out=flat(Xn), in_=b2[:, :]), reads=[b2], writes=[Xn])
                yield
                b3 = bk()
                for h in range(4):
                    k.op("pe", lambda e: e.matmul(b3[:, h * 128:(h + 1) * 128], lhsT=YT[q][:, h, :], rhs=Rc[:, h, :], start=True, stop=True), reads=[YT[q], Rc], writes=[b3])
                yield
                k.op("dve", lambda e: e.tensor_copy(out=flat(Rn), in_=b3[:, :]), reads=[b3], writes=[Rn])
                yield
                Xc, XTc, Rc = Xn, XTn, Rn
            TTb, Pb = Xa[q], XTa[q]
            for mi, BT_ in enumerate((B1T[q], B2T[q])):
                Tn = rbufs[mi % 2]
                btt = bk()
                bttb = btt.t[:, :].bitcast(BF16)
                for h in range(4):
                    k.op("pe", lambda e: e.transpose(out=bttb[:, h * 128:(h + 1) * 128], in_=Rc[:, h, :], identity=self.ident_b[:]), reads=[Rc, self.ident_b], writes=[btt])
                bp_ = bk()
                for h in range(4):
                    k.op("pe", lambda e: e.matmul(bp_[:, h * 128:(h + 1) * 128], lhsT=BT_[:, h, :], rhs=Rc[:, h, :], start=True, stop=True), reads=[BT_, Rc], writes=[bp_])
                yield
                k.op("act", lambda e: e.copy(out=flat(TTb), in_=bttb[:, 0:512]), reads=[btt], writes=[TTb])
                k.op("dve", lambda e: e.tensor_copy(out=flat(Pb), in_=bp_[:, :]), reads=[bp_], writes=[Pb])
                yield
                bq = bk()
                for h in range(4):
                    k.op("pe", lambda e: e.matmul(bq[:, h * 128:(h + 1) * 128], lhsT=TTb[:, h, :], rhs=Pb[:, h, :], start=True, stop=True), reads=[TTb, Pb], writes=[bq])
                yield
                k.op("dve", lambda e: e.tensor_tensor(out=flat(Tn), in0=flat(Rc), in1=bq[:, :], op=ALU.subtract), reads=[Rc, bq], writes=[Tn])
                yield
                Rc = Tn
            T2T = Rc
            bu, bw = bk(), bk()
            for h in range(4):
                k.op("pe", lambda e: e.matmul(bu[:, h * 128:(h + 1) * 128], lhsT=T2T[:, h, :], rhs=kv[:, 4 + h, :], start=True, stop=True), reads=[T2T, kv], writes=[bu])
                k.op("pe", lambda e: e.matmul(bw[:, h * 128:(h + 1) * 128], lhsT=Keg[q][:, h, :], rhs=T2T[:, h, :], start=True, stop=True), reads=[T2T, Keg[q]], writes=[bw])
            yield
            k.op("act", lambda e: e.copy(out=flat(up[bp][ti]), in_=bu[:, :]), reads=[bu], writes=[up[bp][ti]])
            k.op("dve", lambda e: e.tensor_copy(out=flat(wT[bp][ti]), in_=bw[:, :]), reads=[bw], writes=[wT[bp][ti]])
            yield

        def scan(bk, d, si, bp, tiles):
            for n_, (i, ti) in enumerate(tiles):
                p = n_ % 2
                smT = smb[si]
                sm_ = smT[:, ti, :]
                bws = bk()
                for h in range(4):
                    k.op("pe", lambda e: e.matmul(bws[:, h * 128:(h + 1) * 128], lhsT=wT[bp][ti][:, h, :], rhs=Sb[:, h, :], start=True, stop=True), reads=[wT[bp][ti], Sb], writes=[bws])
                yield
                k.op("dve", lambda e: e.tensor_tensor(out=flat(vnew[p]), in0=flat(up[bp][ti]), in1=bws[:, :], op=ALU.subtract), reads=[up[bp][ti], bws], writes=[vnew[p]])
                yield
                bo, bkv = bk(), bk()
                for h in range(4):
                    k.op("pe", lambda e: e.matmul(bkv[:, h * 128:(h + 1) * 128], lhsT=kdec[bp][ti][:, h, :], rhs=vnew[p][:, h, :], start=True, stop=True), reads=[kdec[bp][ti], vnew[p]], writes=[bkv])
                for h in range(4):
                    k.op("pe", lambda e: e.matmul(bo[:, h * 128:(h + 1) * 128], lhsT=qdecT[bp][ti][:, h, :], rhs=Sb[:, h, :], start=True, stop=False), reads=[qdecT[bp][ti], Sb], writes=[bo])
                    k.op("pe", lambda e: e.matmul(bo[:, h * 128:(h + 1) * 128], lhsT=attnT[bp][ti][:, h, :], rhs=vnew[p][:, h, :], start=False, stop=True), reads=[attnT[bp][ti], vnew[p]], writes=[bo])
                yield
                bkv3 = bkv[:, :].rearrange("p (a b) -> p a b", a=4)
                for h in range(4):
                    k.op("dve", lambda e: e.scalar_tensor_tensor(out=Sf[:, h, :], in0=Sf[:, h, :], scalar=sm_[:, 24 + h:25 + h], in1=bkv3[:, h, :], op0=ALU.mult, op1=ALU.add),
                         reads=[Sf, smT, bkv], writes=[Sf])
                k.op("act", lambda e: e.copy(out=flat(osb[p]), in_=bo[:, :]), reads=[bo], writes=[osb[p]])
                yield
                k.op("act", lambda e: e.copy(out=Sb[:], in_=Sf[:]), reads=[Sf], writes=[Sb])
                k.dma("act", S["o_dn"][d, i * 128:(i + 1) * 128, :], flat(osb[p]), reads=[osb[p]], writes=[self.odn_dep[d][i]])
                yield

        def cyc(ids):
            st = [0]

            def f():
                b = self.ps[ids[st[0] % len(ids)]]
                st[0] += 1
                return b
            return f
        bk_b = [cyc([0, 1]), cyc([2, 3])]
        bk_a, bk_s = cyc([6, 7]), cyc([4, 5])
        blocks_f = [(0, 2)] + [(2 + 4 * i, 4) for i in range(16)]
        blocks_f = [b_ for b_ in blocks_f if b_[0] < self.ntiles]
        gbi = 0
        for d in range(2):
            mI, mS, tri = self.dir_masks(d)
            k.op("pool", lambda e: e.memset(Sf[:], 0.0), writes=[Sf])
            k.op("pool", lambda e: e.memset(Sb[:], 0.0), writes=[Sb])
            blocks = blocks_f if d == 0 else [blocks_f[0]] + blocks_f[:0:-1]
            nb = len(blocks)
            self.interleave([stage_a(bk_a, d, tri, gbi % 3, gbi % 2, *blocks[0])])
            prev = None
            for bi, (t0, nt) in enumerate(blocks):
                bp = (gbi + bi) % 2
                tis = list(range(nt)) if d == 0 else list(range(nt - 1, -1, -1))
                for pair in range(0, nt, 2):
                    gens = [stage_b(bk_b[q], d, mI, mS, tri, (gbi + bi) % 3, bp, q, t0 + ti, ti) for q, ti in enumerate(tis[pair:pair + 2])]
                    if pair == 0 and bi + 1 < nb:
                        gens.append(stage_a(bk_a, d, tri, (gbi + bi + 1) % 3, (gbi + bi + 1) % 2, *blocks[bi + 1]))
                    if prev is not None and (pair == 2 or nt == 2):
                        gens.append(scan(bk_s, d, *prev))
                    self.interleave(gens)
                prev = ((gbi + bi) % 3, bp, [(t0 + ti, ti) for ti in tis])
            self.interleave([scan(bk_s, d, *prev)])
            gbi += nb

    def p3(self, l):
        k, I, S = self.k, self.I, self.S
        sb = k.sb

        def two(shape, dt, name):
            return [sb(shape, dt, name + "%d" % i) for i in range(4)]
        qkv = two([128, 1024], F32, "gqkv")
        glr = two([128, 16], F32, "glr")
        lhsTg = two([64, 128], F32, "lhsTg")
        W2 = sb([64, 256], F32, "W2")
        ex = two([128, 256], F32, "gex")
        la = two([128, 256], F32, "la")
        bcs = two([128, 256], F32, "bcs")
        ebc = two([128, 256], F32, "ebc")
        enbc = two([128, 256], F32, "enbc")
        ekd = two([128, 256], F32, "ekd")
        qs = two([128, 256], BF16, "gqs")
        ks = two([128, 256], BF16, "gks")
        kd = two([128, 256], BF16, "gkd")
        vb = two([128, 4, 128], BF16, "gvb")
        qT = two([128, 4, 128], BF16, "gqTm")
        kT = two([128, 2, 128], BF16, "gkT")
        att = two([128, 4, 128], BF16, "gatt")
        osb = two([128, 512], F32, "gosb")
        etc_ = two([128, 4], F32, "getc")
        Sf = sb([128, 2, 128], F32, "gSf")
        Sb = sb([128, 2, 128], BF16, "gSb")
        for t_ in qT:
            k.op("pool", lambda e: e.memset(t_[:], 0.0), writes=[t_])
        for t_ in lhsTg:
            k.op("pool", lambda e: e.memset(t_[:], 0.0), writes=[t_])
            k.op("pool", lambda e: e.memset(t_[32:33, :], 1.0), writes=[t_])
        tm_lat = S["tm"][LC:LS, :].rearrange("(r w) n -> w r n", w=64)
        alltm = self.tm_dep
        for d in range(2):
            mI, mS, tri = self.dir_masks(d)
            k.op("pool", lambda e: e.memset(W2[:], 0.0), writes=[W2])
            k.dma("sp", W2[0:16, :], I["gla_w_gate2"][l, d], writes=[W2])
            k.dma("sp", W2[32:33, :], I["gla_b_gate"][l, d:d + 1, :], writes=[W2])
            k.op("pool", lambda e: e.memset(Sf[:], 0.0), writes=[Sf])
            k.op("pool", lambda e: e.memset(Sb[:], 0.0), writes=[Sb])
            order = [("c", 0), ("c", 1)] + [("g", j) for j in range(64)]
            if d == 1:
                order = [("c", 1), ("c", 0)] + [("g", j) for j in range(63, -1, -1)]
            if self.ntiles < NT:
                order = order[:self.ntiles]
            def cyc(ids):
                st = [0]

                def f():
                    b = self.ps[ids[st[0] % len(ids)]]
                    st[0] += 1
                    return b
                return f
            bk_preA, bk_preB, bk_post = cyc([0, 1]), cyc([2, 3]), cyc([4, 5])

            def pre(bk, p, kind, j):
                if kind == "c":
                    rows = S["tm"][j * 128:(j + 1) * 128, :]
                else:
                    rows = tm_lat[j]
                k.dma("sp", qkv[p][:], rows[:, 528:1552], reads=alltm, writes=[qkv[p]])
                k.dma("sp", glr[p][:], rows[:, 2064 + 16 * d:2080 + 16 * d], reads=alltm, writes=[glr[p]])
                b0 = bk()
                k.op("pe", lambda e: e.transpose(out=b0[0:16, 0:128], in_=glr[p][:, :], identity=self.ident_f[:]), reads=[glr[p], self.ident_f], writes=[b0])
                k.op("act", lambda e: e.copy(out=lhsTg[p][0:16, :], in_=b0[0:16, 0:128]), reads=[b0], writes=[lhsTg[p]])
                yield
                bgt = bk()
                k.op("pe", lambda e: e.matmul(bgt[:, 0:256], lhsT=lhsTg[p][:, :], rhs=W2[:, :], start=True, stop=True), reads=[lhsTg[p], W2], writes=[bgt])
                k.op("act", lambda e: e.activation(out=ex[p][:], in_=bgt[:, 0:256], func=AF.Exp, scale=-1.0), reads=[bgt], writes=[ex[p]])
                yield
                k.op("act", lambda e: e.activation(out=ex[p][:], in_=ex[p][:], func=AF.Ln, bias=1.0), reads=[ex[p]], writes=[ex[p]])
                yield
                k.op("dve", lambda e: e.tensor_scalar(out=la[p][:], in0=ex[p][:], scalar1=-1.0 / 16.0, scalar2=None, op0=ALU.mult), reads=[ex[p]], writes=[la[p]])
                yield
                bbc, bto = bk(), bk()
                k.op("pe", lambda e: e.matmul(bbc[:, 0:256], lhsT=tri[:], rhs=la[p][:], start=True, stop=True), reads=[tri, la[p]], writes=[bbc])
                k.op("pe", lambda e: e.matmul(bto[:, 0:256], lhsT=self.ones_f[:], rhs=la[p][:], start=True, stop=True), reads=[self.ones_f, la[p]], writes=[bto])
                for hp in range(2):
                    k.op("pe", lambda e: e.matmul(bto[:, 256 + 2 * hp:258 + 2 * hp], lhsT=la[p][:, hp * 128:(hp + 1) * 128], rhs=self.ones_f[:, 0:2], start=True, stop=True),
                         reads=[la[p], self.ones_f], writes=[bto])
                k.op("act", lambda e: e.activation(out=ebc[p][:], in_=bbc[:, 0:256], func=AF.Exp), reads=[bbc], writes=[ebc[p]])
                yield
                k.op("act", lambda e: e.activation(out=enbc[p][:], in_=bbc[:, 0:256], func=AF.Exp, scale=-1.0), reads=[bbc], writes=[enbc[p]])
                yield
                k.op("act", lambda e: e.copy(out=bcs[p][:], in_=bbc[:, 0:256]), reads=[bbc], writes=[bcs[p]])
                yield
                k.op("dve", lambda e: e.tensor_tensor(out=ekd[p][:], in0=bto[:, 0:256], in1=bcs[p][:], op=ALU.subtract), reads=[bto, bcs[p]], writes=[ekd[p]])
                yield
                k.op("act", lambda e: e.activation(out=ekd[p][:], in_=ekd[p][:], func=AF.Exp), reads=[ekd[p]], writes=[ekd[p]])
                yield
                k.op("act", lambda e: e.activation(out=etc_[p][:], in_=bto[:, 256:260], func=AF.Exp), reads=[bto], writes=[etc_[p]])
                yield
                k.op("dve", lambda e: e.scalar_tensor_tensor(out=qs[p][:], in0=qkv[p][:, 0:256], scalar=0.125, in1=ebc[p][:], op0=ALU.mult, op1=ALU.mult), reads=[qkv[p], ebc[p]], writes=[qs[p]])
                yield
                k.op("dve", lambda e: e.tensor_tensor(out=ks[p][:], in0=qkv[p][:, 256:512], in1=enbc[p][:], op=ALU.mult), reads=[qkv[p], enbc[p]], writes=[ks[p]])
                yield
                k.op("pool", lambda e: e.tensor_tensor(out=kd[p][:], in0=qkv[p][:, 256:512], in1=ekd[p][:], op=ALU.mult), reads=[qkv[p], ekd[p]], writes=[kd[p]])
                yield
                k.op("pool", lambda e: e.tensor_copy(out=vb[p][:].rearrange("p a b -> p (a b)"), in_=qkv[p][:, 512:1024]), reads=[qkv[p]], writes=[vb[p]])
                yield
                btr = bk()
                btrb = btr.t[:, :].bitcast(BF16)
                for hp in range(2):
                    k.op("pe", lambda e: e.transpose(out=btrb[:, hp * 128:(hp + 1) * 128], in_=qs[p][:, hp * 128:(hp + 1) * 128], identity=self.ident_b[:]), reads=[qs[p], self.ident_b], writes=[btr])
                    k.op("pe", lambda e: e.transpose(out=btrb[:, (2 + hp) * 128:(3 + hp) * 128], in_=ks[p][:, hp * 128:(hp + 1) * 128], identity=self.ident_b[:]), reads=[ks[p], self.ident_b], writes=[btr])
                for h in range(4):
                    hb, hp = h % 2, h // 2
                    k.op("act", lambda e: e.copy(out=qT[p][hb * 64:(hb + 1) * 64, h, :], in_=btrb[hb * 64:(hb + 1) * 64, hp * 128:(hp + 1) * 128]), reads=[btr], writes=[qT[p]])
                k.op("act", lambda e: e.copy(out=kT[p][:].rearrange("p a b -> p (a b)"), in_=btrb[:, 256:512]), reads=[btr], writes=[kT[p]])
                yield
                bat = bk()
                for h in range(4):
                    hb, hp = h % 2, h // 2
                    ps_ = slice(hb * 64, (hb + 1) * 64)
                    k.op("pe", lambda e: e.matmul(bat[:, h * 128:(h + 1) * 128], lhsT=kT[p][:, hp, :], rhs=qT[p][:, h, :], start=True, stop=True), reads=[kT[p], qT[p]], writes=[bat])
                k.op("dve", lambda e: e.tensor_tensor(out=att[p][:].rearrange("p a b -> p (a b)"), in0=bat[:, :], in1=mI[:].rearrange("p a b -> p (a b)"), op=ALU.mult), reads=[bat, mI], writes=[att[p]])
                yield
                yield

            def post(bk, p, kind, j):
                if kind == "c":
                    orows = S["o_gla"][d, j * 128:(j + 1) * 128, :]
                    odeps = [self.ogla_dep[d][j]]
                else:
                    orows = S["o_gla"][d, LC:LS, :].rearrange("(r w) n -> w r n", w=64)[j]
                    odeps = self.ogla_dep[d][2:]
                bo, bkv = bk(), bk()
                for h in range(4):
                    hb, hp = h % 2, h // 2
                    ps_ = slice(hb * 64, (hb + 1) * 64)
                    k.op("pe", lambda e: e.matmul(bo[:, h * 128:(h + 1) * 128], lhsT=qT[p][:, h, :], rhs=Sb[:, hp, :], start=True, stop=False), reads=[qT[p], Sb], writes=[bo])
                    k.op("pe", lambda e: e.matmul(bo[:, h * 128:(h + 1) * 128], lhsT=att[p][:, h, :], rhs=vb[p][:, h, :], start=False, stop=True), reads=[att[p], vb[p]], writes=[bo])
                for h in range(4):
                    hb, hp = h % 2, h // 2
                    k.op("pe", lambda e: e.matmul(bkv[:, h * 128:(h + 1) * 128], lhsT=kd[p][:, hp * 128:(hp + 1) * 128], rhs=vb[p][:, h, :], start=True, stop=True),
                         reads=[kd[p], vb[p]], writes=[bkv])
                k.op("act", lambda e: e.copy(out=osb[p][:], in_=bo[:, :]), reads=[bo], writes=[osb[p]])
                yield
                k.dma("act", orows, osb[p][:], reads=[osb[p]], writes=odeps)
                for h in range(4):
                    hb, hp = h % 2, h // 2
                    ps_ = slice(hb * 64, (hb + 1) * 64)
                    k.op("dve", lambda e: e.scalar_tensor_tensor(out=Sf[ps_, hp, :], in0=Sf[ps_, hp, :], scalar=etc_[p][ps_, 2 * hp:2 * hp + 1], in1=bkv[ps_, h * 128:(h + 1) * 128], op0=ALU.mult, op1=ALU.add),
                         reads=[Sf, etc_[p], bkv], writes=[Sf])
                k.op("act", lambda e: e.copy(out=Sb[:], in_=Sf[:]), reads=[Sf], writes=[Sb])
                yield


                yield

            def posts(items):
                for (p_, kind_, j_) in items:
                    yield from post(bk_post, p_, kind_, j_)

            n_ = len(order)
            first = [pre(bk_preA, 0, *order[0])]
            if n_ > 1:
                first.append(pre(bk_preB, 1, *order[1]))
            self.interleave(first)
            for R in range(0, n_, 2):
                gens = [posts([(s % 4,) + tuple(order[s]) for s in (R, R + 1) if s < n_])]
                if R + 2 < n_:
                    gens.append(pre(bk_preA, (R + 2) % 4, *order[R + 2]))
                if R + 3 < n_:
                    gens.append(pre(bk_preB, (R + 3) % 4, *order[R + 3]))
                self.interleave(gens)

    def p4(self, l):
        k, I, S = self.k, self.I, self.S
        sb = k.sb
        last = l == self.nlayers - 1
        moe = (l % 2 == 1)
        T = 1024
        NH = T // 512
        gnb = sb([128, 8, 128], F32, "gnb")
        for h in range(8):
            src_ = I["dn_norm"] if h < 4 else I["gla_norm"]
            k.dma("sp", gnb[:, h, :], src_[l:l + 1, :].broadcast_to([128, 128]), writes=[gnb])
        xT = sb([128, 8, T], F32, "xT")
        mT = sb([128, 8, T], BF16, "mT")
        sq2 = mT
        h2T = sb([128, 8, T], BF16, "h2T")
        rs2 = sb([128, T], F32, "rs2")
        tmpf = [sb([128, T], F32, "tmpf%d" % i) for i in range(2)]
        F = D_EXP if moe else D_FF
        NFC = F // 128
        actT = sb([128, NFC, T], BF16, "actT")
        ot = [sb([128, 512], F32, "ot%d" % i) for i in range(4)]
        zt = sb([128, 1024], F32, "zt")
        xt = sb([128, D], F32, "xt4")
        xo = xt
        osum = sb([128, 8, 128], F32, "osum")
        ssh = sb([128, 16], F32, "ssh")
        mb = sb([128, D], BF16, "mb")
        wb = [sb([128, 8, 512], BF16, "wb%d" % i) for i in range(3)]
        wd = [sb([128, 4, 512], BF16, "wd%d" % i) for i in range(2)]
        sg = [ot[0], ot[1]]
        if moe:
            rt = sb([128, 8, NE], F32, "rt")
            k.dma("sp", rt[:], I["moe_router"].rearrange("(k p) n -> p k n", p=128), writes=[rt])
            lgT = sb([8, T], F32, "lgT")
            lg = sb([128, 8, NE], F32, "lg")
            l2 = sb([128, 8, NE], F32, "l2")
            mx = sb([128, 16], F32, "mx")
            cbT = sb([8, T], F32, "cbT")
            CBe = [zt, xt]
            selE = sb([8, NE, 128], F32, "selE")
            for e_ in range(NE):
                k.op("pool", lambda e: e.tensor_copy(out=selE[0:8, e_, :], in_=self.ident_f[0:8, e_:e_ + 1].to_broadcast([8, 128])), reads=[self.ident_f], writes=[selE])
        ntb = T // 128
        if last:
            blocks = [(2 + ntb * i, ntb) for i in range(NOWN // T)]
        else:
            blocks = [(0, 2)] + [(2 + ntb * i, ntb) for i in range(LL // T)]
        wi = [0]

        def wload(src_ap, ncols):
            t_ = wb[wi[0] % 3]
            wi[0] += 1
            k.dma("pool", t_[:, :, 0:ncols], src_ap, writes=[t_], max_dma_last_dim=2048)
            return t_
        wout_src = I["w_out"][l].rearrange("(k p) n -> p k n", p=128)
        for (t0, nt) in blocks:
            if t0 >= self.ntiles:
                break
            Tb = nt * 128
            halves = [(h0, min(512, Tb - h0)) for h0 in range(0, Tb, 512)]
            r = 1 if t0 < 2 else 0
            for ti in range(nt):
                i = t0 + ti
                rows = slice(i * 128, (i + 1) * 128)
                tsl = slice(ti * 128, (ti + 1) * 128)
                k.dma("sp", ot[0][:], S["o_dn"][0, rows, :], reads=[self.odn_dep[0][i]], writes=[ot[0]])
                k.dma("sp", ot[1][:], S["o_dn"][1, rows, :], reads=[self.odn_dep[1][i]], writes=[ot[1]])
                k.dma("sp", ot[2][:], S["o_gla"][0, rows, :], reads=[self.ogla_dep[0][i]], writes=[ot[2]])
                k.dma("sp", ot[3][:], S["o_gla"][1, rows, :], reads=[self.ogla_dep[1][i]], writes=[ot[3]])
                k.dma("sp", zt[:, 0:512], S["tm"][rows, 0:512], reads=[self.tm_dep[i]], writes=[zt])
                k.dma("sp", zt[:, 512:1024], S["tm"][rows, 1552:2064], reads=[self.tm_dep[i]], writes=[zt])
                k.dma("sp", xt[:], self.xcur[rows, :], reads=[self.xcur_dep[i]], writes=[xt])
                osf = osum[:].rearrange("p a b -> p (a b)")
                k.op("dve", lambda e: e.tensor_tensor(out=osf[:, 0:512], in0=ot[0][:], in1=ot[1][:], op=ALU.add), reads=[ot[0], ot[1]], writes=[osum])
                k.op("dve", lambda e: e.tensor_tensor(out=osf[:, 512:1024], in0=ot[2][:], in1=ot[3][:], op=ALU.add), reads=[ot[2], ot[3]], writes=[osum])
                for hh in range(2):
                    k.op("act", lambda e: e.activation(out=ot[2 * hh][:], in_=osf[:, hh * 512:(hh + 1) * 512], func=AF.Square), reads=[osum], writes=[ot[2 * hh]])
                    k.op("dve", lambda e: e.tensor_reduce(out=ssh[:, 4 * hh:4 * hh + 4], in_=ot[2 * hh][:].rearrange("p (a b) -> p a b", a=4), axis=AX.X, op=ALU.add), reads=[ot[2 * hh]], writes=[ssh])
                k.op("act", lambda e: e.activation(out=ssh[:, 8:16], in_=ssh[:, 0:8], func=AF.Sqrt, scale=1.0 / 128.0, bias=self.eps_t[:, 0:1]), reads=[ssh, self.eps_t], writes=[ssh])
                k.op("dve", lambda e: e.reciprocal(out=ssh[:, 8:16], in_=ssh[:, 8:16]), reads=[ssh], writes=[ssh])
                k.op("act", lambda e: e.activation(out=zt[:], in_=zt[:], func=AF.Silu), reads=[zt], writes=[zt])
                for h in range(8):
                    k.op("dve", lambda e: e.scalar_tensor_tensor(out=osum[:, h, :], in0=osum[:, h, :], scalar=ssh[:, 8 + h:9 + h], in1=gnb[:, h, :], op0=ALU.mult, op1=ALU.mult),
                         reads=[osum, ssh, gnb], writes=[osum])
                k.op("dve", lambda e: e.tensor_tensor(out=mb[:], in0=osf, in1=zt[:], op=ALU.mult), reads=[osum, zt], writes=[mb])
                b = self.bank()
                bb = b.t[:, :].bitcast(BF16)
                for kk in range(8):
                    k.op("pe", lambda e: e.transpose(out=bb[:, kk * 128:(kk + 1) * 128], in_=mb[:, kk * 128:(kk + 1) * 128], identity=self.ident_b[:]), reads=[mb, self.ident_b], writes=[b])
                k.op("act", lambda e: e.copy(out=mT[:, :, tsl], in_=bb[:, :].rearrange("p (a b) -> p a b", a=8)), reads=[b], writes=[mT])
                for half in range(2):
                    b2 = self.bank()
                    for q in range(4):
                        kk = half * 4 + q
                        k.op("pe", lambda e: e.transpose(out=b2[:, q * 128:(q + 1) * 128], in_=xt[:, kk * 128:(kk + 1) * 128], identity=self.ident_f[:]), reads=[xt, self.ident_f], writes=[b2])
                    k.op("dve", lambda e: e.tensor_copy(out=xT[:, half * 4:half * 4 + 4, tsl], in_=b2[:, :].rearrange("p (a b) -> p a b", a=4)), reads=[b2], writes=[xT])
            for og in range(2):
                wt = wload(wout_src[:, :, og * 512:(og + 1) * 512], 512)
                for q in range(4):
                    oc = og * 4 + q
                    for (h0, hw) in halves:
                        b = self.bank()
                        for kk in range(8):
                            k.op("pe", lambda e: e.matmul(b[:, 0:hw], lhsT=wt[:, kk, q * 128:(q + 1) * 128], rhs=mT[:, kk, h0:h0 + hw], start=(kk == 0), stop=(kk == 7)), reads=[wt, mT], writes=[b])
                        k.op("dve", lambda e: e.scalar_tensor_tensor(out=xT[:, oc, h0:h0 + hw], in0=b[:, 0:hw], scalar=self.modT[:, 16 + oc, r:r + 1], in1=xT[:, oc, h0:h0 + hw], op0=ALU.mult, op1=ALU.add),
                             reads=[b, self.modT, xT], writes=[xT])

            def rms_bcast(dst):
                for kk in range(8):
                    k.op("act", lambda e: e.activation(out=sq2[:, kk, 0:Tb], in_=xT[:, kk, 0:Tb], func=AF.Square), reads=[xT], writes=[sq2])
                for (h0, hw) in halves:
                    bs_ = self.bank()
                    for kk in range(8):
                        k.op("pe", lambda e: e.matmul(bs_[:, 0:hw], lhsT=self.ones_b[:], rhs=sq2[:, kk, h0:h0 + hw], start=(kk == 0), stop=(kk == 7)), reads=[self.ones_b, sq2], writes=[bs_])
                    k.op("act", lambda e: e.activation(out=dst[:, h0:h0 + hw], in_=bs_[:, 0:hw], func=AF.Sqrt, scale=1.0 / D, bias=self.eps_t[:, 0:1]), reads=[bs_, self.eps_t], writes=[dst])
                k.op("dve", lambda e: e.reciprocal(out=dst[:, 0:Tb], in_=dst[:, 0:Tb]), reads=[dst], writes=[dst])
            rms_bcast(rs2)
            if moe:
                bl = [self.bank() for _ in halves]
            for kk in range(8):
                tf = tmpf[kk % 2]
                k.op("dve", lambda e: e.tensor_tensor(out=tf[:, 0:Tb], in0=xT[:, kk, 0:Tb], in1=rs2[:, 0:Tb], op=ALU.mult), reads=[xT, rs2], writes=[tf])
                k.op("act", lambda e: e.activation(out=h2T[:, kk, 0:Tb], in_=tf[:, 0:Tb], func=AF.Identity, scale=self.g2T[:, kk, r:r + 1], bias=self.modT[:, 24 + kk, r:r + 1]),
                     reads=[tf, self.g2T, self.modT], writes=[h2T])
                if moe:
                    k.op("dve", lambda e: e.tensor_scalar(out=tf[:, 0:Tb], in0=tf[:, 0:Tb], scalar1=self.g2T[:, kk, r:r + 1], scalar2=self.modT[:, 24 + kk, r:r + 1], op0=ALU.mult, op1=ALU.add),
                         reads=[tf, self.g2T, self.modT], writes=[tf])
                    for hi, (h0, hw) in enumerate(halves):
                        k.op("pe", lambda e: e.matmul(bl[hi][0:8, 0:hw], lhsT=rt[:, kk, :], rhs=tf[:, h0:h0 + hw], start=(kk == 0), stop=(kk == 7)), reads=[rt, tf], writes=[bl[hi]])
            experts = [None]
            if moe:
                experts = list(range(NE))
                for hi, (h0, hw) in enumerate(halves):
                    k.op("act", lambda e: e.copy(out=lgT[:, h0:h0 + hw], in_=bl[hi][0:8, 0:hw]), reads=[bl[hi]], writes=[lgT])
                bl2 = self.bank()
                for ti in range(nt):
                    k.op("pe", lambda e: e.transpose(out=bl2[:, ti * 8:(ti + 1) * 8], in_=lgT[0:8, ti * 128:(ti + 1) * 128], identity=self.ident_f[0:8, 0:8]), reads=[lgT, self.ident_f], writes=[bl2])
                k.op("dve", lambda e: e.tensor_copy(out=lg[:].rearrange("p a b -> p (a b)"), in_=bl2[:, 0:64]), reads=[bl2], writes=[lg])
                bc3 = lambda ap: ap.unsqueeze(2).to_broadcast([128, 8, NE])
                k.op("dve", lambda e: e.tensor_reduce(out=mx[:, 0:8], in_=lg[:], axis=AX.X, op=ALU.max), reads=[lg], writes=[mx])
                k.op("pool", lambda e: e.tensor_tensor(out=l2[:], in0=lg[:], in1=bc3(mx[:, 0:8]), op=ALU.subtract), reads=[lg, mx], writes=[l2])
                k.op("dve", lambda e: e.tensor_scalar(out=lg[:], in0=l2[:], scalar1=0.0, scalar2=-1e30, op0=ALU.is_equal, op1=ALU.mult), reads=[l2], writes=[lg])
                k.op("dve", lambda e: e.tensor_tensor(out=lg[:], in0=lg[:], in1=l2[:], op=ALU.add), reads=[lg, l2], writes=[lg])
                k.op("dve", lambda e: e.tensor_reduce(out=mx[:, 8:16], in_=lg[:], axis=AX.X, op=ALU.max), reads=[lg], writes=[mx])
                k.op("pool", lambda e: e.tensor_tensor(out=lg[:], in0=l2[:], in1=bc3(mx[:, 8:16]), op=ALU.subtract), reads=[l2, mx], writes=[lg])
                k.op("dve", lambda e: e.tensor_scalar(out=lg[:], in0=lg[:], scalar1=0.0, scalar2=None, op0=ALU.is_ge), reads=[lg], writes=[lg])
                k.op("act", lambda e: e.activation(out=l2[:], in_=l2[:], func=AF.Exp), reads=[l2], writes=[l2])
                k.op("dve", lambda e: e.tensor_tensor(out=l2[:], in0=l2[:], in1=lg[:], op=ALU.mult), reads=[l2, lg], writes=[l2])
                k.op("dve", lambda e: e.tensor_reduce(out=mx[:, 0:8], in_=l2[:], axis=AX.X, op=ALU.add), reads=[l2], writes=[mx])
                k.op("dve", lambda e: e.reciprocal(out=mx[:, 0:8], in_=mx[:, 0:8]), reads=[mx], writes=[mx])
                k.op("pool", lambda e: e.tensor_tensor(out=l2[:], in0=l2[:], in1=bc3(mx[:, 0:8]), op=ALU.mult), reads=[l2, mx], writes=[l2])
                for hi, (h0, hw) in enumerate(halves):
                    bl3 = self.bank()
                    for t4 in range(hw // 128):
                        ti = h0 // 128 + t4
                        k.op("pe", lambda e: e.transpose(out=bl3[0:8, t4 * 128:(t4 + 1) * 128], in_=l2[:, ti, :], identity=self.ident_f[:]), reads=[l2, self.ident_f], writes=[bl3])
                    k.op("act", lambda e: e.copy(out=cbT[:, h0:h0 + hw], in_=bl3[0:8, 0:hw]), reads=[bl3], writes=[cbT])
            for ex_ in experts:
                if moe:
                    wgu_src = I["moe_w_gu"][ex_].rearrange("(k p) n -> p k n", p=128)
                    wdn_src = I["moe_w_down"][ex_].rearrange("(f p) n -> p f n", p=128)
                    cbe = CBe[ex_ % 2]
                    for (h0, hw) in halves:
                        bcb = self.bank()
                        k.op("pe", lambda e: e.matmul(bcb[:, 0:hw], lhsT=selE[0:8, ex_, :], rhs=cbT[0:8, h0:h0 + hw], start=True, stop=True), reads=[selE, cbT], writes=[bcb])
                        k.op("act", lambda e: e.copy(out=cbe[:, h0:h0 + hw], in_=bcb[:, 0:hw]), reads=[bcb], writes=[cbe])
                else:
                    wgu_src = I["ffn_w_gu"].rearrange("(k p) n -> p k n", p=128)
                    wdn_src = I["ffn_w_down"].rearrange("(f p) n -> p f n", p=128)
                si = 0
                for g0 in range(0, F, 512):
                    gw = min(512, F - g0)
                    wgt = wload(wgu_src[:, :, g0:g0 + gw], gw)
                    wut = wload(wgu_src[:, :, F + g0:F + g0 + gw], gw)
                    for c in range(gw // 128):
                        fc = g0 // 128 + c
                        for (h0, hw) in halves:
                            bg_, bu_ = self.bank(), self.bank()
                            for kk in range(8):
                                k.op("pe", lambda e: e.matmul(bg_[:, 0:hw], lhsT=wgt[:, kk, c * 128:(c + 1) * 128], rhs=h2T[:, kk, h0:h0 + hw], start=(kk == 0), stop=(kk == 7)), reads=[wgt, h2T], writes=[bg_])
                            for kk in range(8):
                                k.op("pe", lambda e: e.matmul(bu_[:, 0:hw], lhsT=wut[:, kk, c * 128:(c + 1) * 128], rhs=h2T[:, kk, h0:h0 + hw], start=(kk == 0), stop=(kk == 7)), reads=[wut, h2T], writes=[bu_])
                            s_ = sg[si % 2]
                            si += 1
                            k.op("act", lambda e: e.activation(out=s_[:, 0:hw], in_=bg_[:, 0:hw], func=AF.Silu), reads=[bg_], writes=[s_])
                            if moe:
                                k.op("dve", lambda e: e.tensor_tensor(out=s_[:, 0:hw], in0=s_[:, 0:hw], in1=cbe[:, h0:h0 + hw], op=ALU.mult), reads=[s_, cbe], writes=[s_])
                            k.op("dve", lambda e: e.tensor_tensor(out=actT[:, fc, h0:h0 + hw], in0=bu_[:, 0:hw], in1=s_[:, 0:hw], op=ALU.mult), reads=[bu_, s_], writes=[actT])
                nf = F // 128
                for ocg in range(2):
                    banks = [[self.bank() for _ in range(4)] for _ in halves]
                    for f0 in range(0, nf, 4):
                        wdt = wd[wi[0] % 2]
                        wi[0] += 1
                        n4 = min(4, nf - f0)
                        k.dma("pool", wdt[:, 0:n4, 0:512], wdn_src[:, f0:f0 + n4, ocg * 512:(ocg + 1) * 512], writes=[wdt], max_dma_last_dim=2048)
                        for hi, (h0, hw) in enumerate(halves):
                            for q in range(4):
                                for fi in range(n4):
                                    f = f0 + fi
                                    k.op("pe", lambda e: e.matmul(banks[hi][q][:, 0:hw], lhsT=wdt[:, fi, q * 128:(q + 1) * 128], rhs=actT[:, f, h0:h0 + hw], start=(f == 0), stop=(f == nf - 1)),
                                         reads=[wdt, actT], writes=[banks[hi][q]])
                    for hi, (h0, hw) in enumerate(halves):
                        for q in range(4):
                            oc = ocg * 4 + q
                            k.op("dve", lambda e: e.scalar_tensor_tensor(out=xT[:, oc, h0:h0 + hw], in0=banks[hi][q][:, 0:hw], scalar=self.modT[:, 40 + oc, r:r + 1], in1=xT[:, oc, h0:h0 + hw], op0=ALU.mult, op1=ALU.add),
                                 reads=[banks[hi][q], self.modT, xT], writes=[xT])
            if last:
                rms_bcast(rs2)
                for kk in range(8):
                    k.op("dve", lambda e: e.scalar_tensor_tensor(out=xT[:, kk, 0:Tb], in0=xT[:, kk, 0:Tb], scalar=self.fnT[:, kk, 0:1], in1=rs2[:, 0:Tb], op0=ALU.mult, op1=ALU.mult),
                         reads=[xT, self.fnT, rs2], writes=[xT])
            for ti in range(nt):
                i = t0 + ti
                tsl = slice(ti * 128, (ti + 1) * 128)
                for half in range(2):
                    b2 = self.bank()
                    for q in range(4):
                        kk = half * 4 + q
                        k.op("pe", lambda e: e.transpose(out=b2[:, q * 128:(q + 1) * 128], in_=xT[:, kk, tsl], identity=self.ident_f[:]), reads=[xT, self.ident_f], writes=[b2])
                    k.op("act", lambda e: e.copy(out=xo[:, half * 512:(half + 1) * 512], in_=b2[:, :]), reads=[b2], writes=[xo])
                if last:
                    k.dma("sp", self.out[(i - 2) * 128:(i - 1) * 128, :], xo[:], reads=[xo], writes=[self.out_dep])
                else:
                    k.dma("sp", S["x1"][i * 128:(i + 1) * 128, :], xo[:], reads=[xo], writes=[self.x1_dep[i]])

    def build(self):
        self.declare()
        k = self.k
        self.consts()
        self.eps_t = k.sb([128, 1], F32, "eps_t")
        k.op("pool", lambda e: e.memset(self.eps_t[:], EPS), writes=[self.eps_t])
        self.modT = k.sb([128, 48, 2], F32, "modT")
        self.nT = k.sb([128, 8, 2], F32, "nT")
        self.g1T = k.sb([128, 8, 2], F32, "g1T")
        self.g2T = k.sb([128, 8, 2], F32, "g2T")
        self.fnT = k.sb([128, 8, 1], F32, "fnT")
        self.scT = k.sb([128, 8, 2], BF16, "scT")
        self.xs_dep = [DR() for _ in range(NT)]
        self.xcur, self.xcur_dep = self.I["xs"], self.xs_dep
        for l in range(self.nlayers):
            if "p0" in self.phases:
                with k.scope():
                    self.p0_alloc()
                    self.p0(l)
            if "p1" in self.phases:
                with k.scope():
                    self.p1_alloc()
                    self.p1(l, self.xcur, self.xcur_dep)
            if "p2" in self.phases:
                with k.scope():
                    self.p2(l)
            if "p3" in self.phases:
                with k.scope():
                    self.p3(l)
            if "p4" in self.phases:
                with k.scope():
                    self.p4(l)
                self.xcur, self.xcur_dep = self.S["x1"], self.x1_dep
        k.barrier()
        self.es.close()
        return self.nc


def _core_inputs(inp, core):
    b, j = core // 2, core % 2
    f = (lambda a: a[::-1]) if j else (lambda a: a)
    d = {}
    d["xs"] = np.ascontiguousarray(np.concatenate([f(inp["ctx"][b]), f(inp["x"][b])], axis=0))
    d["cvec"] = np.ascontiguousarray(np.stack([inp["c"][b], inp["c_ctx"]], axis=0))
    w_in = inp["w_in"]
    if j:
        w_in = w_in.copy()
        for base, n in ((2048, 4), (2056, 4), (3600, 16)):
            a = w_in[:, :, base:base + n].copy()
            w_in[:, :, base:base + n] = w_in[:, :, base + n:base + 2 * n]
            w_in[:, :, base + n:base + 2 * n] = a
    d["w_in"] = np.ascontiguousarray(w_in)
    sw = (lambda a: a[:, ::-1]) if j else (lambda a: a)
    d["conv"] = np.ascontiguousarray(sw(inp["conv_qkv"]))
    d["dn_a_log"] = np.ascontiguousarray(sw(inp["dn_a_log"]).reshape(2, 8))
    d["dn_dt_bias"] = np.ascontiguousarray(sw(inp["dn_dt_bias"]).reshape(2, 8))
    d["gla_w_gate2"] = np.ascontiguousarray(sw(inp["gla_w_gate2"]))
    d["gla_b_gate"] = np.ascontiguousarray(sw(inp["gla_b_gate"]))
    for n in ("w_ada", "b_ada", "norm_mix", "norm_ffn", "dn_norm", "gla_norm", "w_out", "final_norm"):
        d[n] = np.ascontiguousarray(inp[n])
    d["ffn_w_gu"] = np.ascontiguousarray(inp["ffn_w_gu"][0])
    d["ffn_w_down"] = np.ascontiguousarray(inp["ffn_w_down"][0])
    d["moe_router"] = np.ascontiguousarray(inp["moe_router"][0])
    d["moe_w_gu"] = np.ascontiguousarray(inp["moe_w_gu"][0])
    d["moe_w_down"] = np.ascontiguousarray(inp["moe_w_down"][0])
    return d


def kernel(**inputs):
    inp = {k_: np.asarray(v, dtype=np.float32) for k_, v in inputs.items()}
    prog = Prog()
    nc = prog.build()
    in_maps = []
    for core in range(8):
        ci = _core_inputs(inp, core)
        in_maps.append({n: ci[n] for n in prog.I})
    res = run_bass_kernel_spmd(nc, in_maps, core_ids=list(range(8)))
    out = np.empty((4, LL, D), np.float32)
    for core in range(8):
        b, j = core // 2, core % 2
        y = np.asarray(res.results[core]["out"], dtype=np.float32)
        if j == 0:
            out[b, 0:NOWN] = y
        else:
            out[b, LL - NOWN:LL] = y[::-1]
    return out
```

```python
import numpy as np
from contextlib import ExitStack
import concourse.bass as bass
import concourse.mybir as mybir
from concourse.bass_utils import run_bass_kernel_spmd

F32 = mybir.dt.float32
BF16 = mybir.dt.bfloat16
AF = mybir.ActivationFunctionType
ALU = mybir.AluOpType
AX = mybir.AxisListType

D = 1024
LC = 256
LL = 8192
LS = LC + LL
NT = LS // 128
IN_COLS = 3632
NTM = IN_COLS - 1536
EPS = 1e-6
D_FF = 2816
D_EXP = 3584
NE = 8
NOWN = 4096


class Dep:
    __slots__ = ("w", "r")

    def __init__(self):
        self.w = None
        self.r = {}


class Tile:
    excl = False

    def __init__(self, t):
        self.t = t
        self.dep = Dep()

    def __getitem__(self, idx):
        return self.t[idx]


class K:
    RING = 64

    def __init__(self, nc, es):
        self.nc = nc
        self.es = es
        self.E = {"pe": nc.tensor, "dve": nc.vector, "act": nc.scalar, "pool": nc.gpsimd, "sp": nc.sync}
        self.sems = {}
        for e in self.E:
            self.sems[e] = es.enter_context(nc.semaphore("sem_" + e))
        for i in range(self.RING):
            self.sems[("d", i)] = es.enter_context(nc.semaphore("dsem%d" % i))
        self.cnt = {e: 0 for e in self.E}
        self.seen = {e: {} for e in self.E}
        self.dn = 0
        self.qn = {}
        self.dtar = [0] * self.RING
        self.nt = 0

    def sb(self, shape, dt, name=None):
        self.nt += 1
        return Tile(self.es.enter_context(self.nc.sbuf_tensor("%s_%d" % (name or "t", self.nt), list(shape), dt)))

    def psum(self, shape, dt, name=None):
        self.nt += 1
        t = Tile(self.es.enter_context(self.nc.psum_tensor(name or ("p%d" % self.nt), list(shape), dt)))
        t.excl = True
        return t

    def _wait(self, eng, tok):
        if tok is None:
            return
        key, val = tok
        if key == eng and eng == "pe":
            return
        if self.seen[eng].get(key, 0) >= val:
            return
        self.E[eng].wait_ge(self.sems[key], val)
        self.seen[eng][key] = val

    def _pre(self, eng, reads, writes):
        for d in reads:
            self._wait(eng, d.dep.w)
        for d in writes:
            self._wait(eng, d.dep.w)
            for tok in d.dep.r.values():
                self._wait(eng, tok)

    def _post(self, tok, reads, writes):
        for d in reads:
            d.dep.r[tok[0]] = tok
        for d in writes:
            d.dep.w = tok
            d.dep.r = {}

    def op(self, eng, fn, reads=(), writes=()):
        ex = [d for d in reads if getattr(d, "excl", False)]
        if ex:
            writes = list(writes) + ex
            reads = [d for d in reads if not getattr(d, "excl", False)]
        self._pre(eng, reads, writes)
        ins = fn(self.E[eng])
        ins.then_inc(self.sems[eng], 1)
        self.cnt[eng] += 1
        self._post((eng, self.cnt[eng]), reads, writes)

    QRING = {"sp": (0, 32), "pool": (32, 24), "act": (56, 8)}

    def dma(self, q, out, in_, reads=(), writes=(), **kw):
        self._pre(q, reads, writes)
        base, n = self.QRING[q]
        cnt = self.qn.get(q, 0)
        self.qn[q] = cnt + 1
        slot = base + cnt % n
        self.dn += 1
        key = ("d", slot)
        if self.dtar[slot] > 0:
            self._wait(q, (key, self.dtar[slot]))
        self.dtar[slot] += 16
        self.E[q].dma_start(out=out, in_=in_, **kw).then_inc(self.sems[key], 16)
        self._post((key, self.dtar[slot]), reads, writes)

    def barrier(self):
        for e in self.E:
            for e2 in self.E:
                if e2 != e and self.cnt[e2] > 0:
                    self._wait(e, (e2, self.cnt[e2]))
            for s in range(self.RING):
                if self.dtar[s] > 0:
                    self._wait(e, (("d", s), self.dtar[s]))

    def scope(self):
        outer = self.es
        k = self

        class _S:
            def __enter__(s):
                k.es = ExitStack()
                return s

            def __exit__(s, *a):
                k.barrier()
                k.es.close()
                k.es = outer
                return False
        return _S()

    def finish(self, deps):
        for d in deps:
            self._wait("sp", d.dep.w)


class DR:
    def __init__(self):
        self.dep = Dep()


class Prog:
    def __init__(self, debug=(), nlayers=2, phases=("p0", "p1", "p2", "p3", "p4"), ntiles=NT):
        self.debug = tuple(debug)
        self.nlayers = nlayers
        self.phases = phases
        self.ntiles = ntiles
        self.p2_stop = 9
        nc = self.nc = bass.Bass("TRN2", target_bir_lowering=False)
        self.I = {}
        self.S = {}
        self.es = ExitStack()
        self.k = K(nc, self.es)

    def inp(self, name, shape):
        self.I[name] = self.nc.dram_tensor(name, list(shape), F32, kind="ExternalInput").ap()
        return self.I[name]

    def scr(self, name, shape, dt=F32):
        kind = "ExternalOutput" if name in self.debug else "Internal"
        self.S[name] = self.nc.dram_tensor(name, list(shape), dt, kind=kind).ap()
        return self.S[name]

    def declare(self):
        inp = self.inp
        inp("xs", [LS, D]); inp("cvec", [2, D]); inp("w_ada", [2, D, 6 * D]); inp("b_ada", [2, 6 * D])
        inp("norm_mix", [2, D]); inp("norm_ffn", [2, D]); inp("w_in", [2, D, IN_COLS]); inp("conv", [2, 5, 1536])
        inp("dn_a_log", [2, 8]); inp("dn_dt_bias", [2, 8]); inp("dn_norm", [2, 128])
        inp("gla_w_gate2", [2, 2, 16, 256]); inp("gla_b_gate", [2, 2, 256]); inp("gla_norm", [2, 128])
        if "p4" in self.phases:
            inp("w_out", [2, D, D]); inp("ffn_w_gu", [D, 2 * D_FF]); inp("ffn_w_down", [D_FF, D])
            if self.nlayers > 1:
                inp("moe_router", [D, NE]); inp("moe_w_gu", [NE, D, 2 * D_EXP]); inp("moe_w_down", [NE, D_EXP, D])
        inp("final_norm", [D])
        self.out = self.nc.dram_tensor("out", [NOWN, D], F32, kind="ExternalOutput").ap()
        self.out_dep = DR()
        self.scr("tm", [LS, NTM]); self.scr("qkvT", [1536, LS + 8], BF16)
        self.scr("o_dn", [2, LS, 512]); self.scr("o_gla", [2, LS, 512]); self.scr("x1", [LS, D])
        self.x1_dep = [DR() for _ in range(NT)]
        self.scr("sa_kq", [17, 128, 4096], BF16); self.scr("sa_vt", [17, 128, 2048], BF16); self.scr("sa_kv", [17, 4, 128, 1024], BF16)
        self.sa_dep = [[DR() for _ in range(7)] for _ in range(17)]
        self.odn_dep = [[DR() for _ in range(NT)] for _ in range(2)]
        self.ogla_dep = [[DR() for _ in range(NT)] for _ in range(2)]
        self.tm_dep = [DR() for _ in range(NT)]
        self.qkv_dep = [DR() for _ in range(NT)]
        self.qkv_pad_dep = DR()
        self.qkvc_dep = [[DR() for _ in range(12)] for _ in range(17)]

    def dbg(self, name, tile, shape, dt=F32):
        if name not in self.debug or name in self.S:
            return
        if len(shape) == 2 and len(tile.t.shape) == 4:
            ap = self.nc.dram_tensor(name, list(shape), dt, kind="ExternalOutput").ap()
            self.S[name] = ap
            self.k.dma("sp", ap, tile[:].rearrange("p a b c -> p (a b c)"), reads=[tile], writes=[DR()])
            return
        ap = self.nc.dram_tensor(name, list(shape), dt, kind="ExternalOutput").ap()
        self.S[name] = ap
        src_ap = tile[:]
        if len(shape) == 2 and len(tile.t.shape) == 3:
            src_ap = tile[:].rearrange("p a b -> p (a b)")
        self.k.dma("sp", ap, src_ap, reads=[tile], writes=[DR()])

    def consts(self):
        k = self.k
        self.ident_f = k.sb([128, 128], F32, "ident_f")
        self.ident_b = k.sb([128, 128], BF16, "ident_b")
        self.ones_b = k.sb([128, 128], BF16, "ones_b")
        self.ones_f = k.sb([128, 128], F32, "ones_f")
        self.zeros_f = k.sb([128, 8], BF16, "zeros_b")
        idf, idb = self.ident_f, self.ident_b
        k.op("pool", lambda e: e.memset(idf[:], 1.0), writes=[idf])
        k.op("pool", lambda e: e.affine_select(out=idf[:], in_=idf[:], pattern=[[1, 128]], compare_op=ALU.is_equal,
                                                fill=0.0, base=0, channel_multiplier=-1), reads=[idf], writes=[idf])
        k.op("pool", lambda e: e.tensor_copy(out=idb[:], in_=idf[:]), reads=[idf], writes=[idb])
        k.op("pool", lambda e: e.memset(self.ones_b[:], 1.0), writes=[self.ones_b])
        k.op("pool", lambda e: e.memset(self.ones_f[:], 1.0), writes=[self.ones_f])
        k.op("pool", lambda e: e.memset(self.zeros_f[:], 0.0), writes=[self.zeros_f])
        self.ps = [k.psum([128, 512], F32, "ps%d" % i) for i in range(8)]
        self.psi = 0

    def bank(self):
        b = self.ps[self.psi % 8]
        self.psi += 1
        return b

    def rows_to_cols(self, rows, R, n, dst):
        k = self.k
        b = self.bank()
        for c in range(n):
            k.op("pe", lambda e, c=c: e.transpose(out=b[:, c * R:(c + 1) * R], in_=rows[0:R, c * 128:(c + 1) * 128],
                                                  identity=self.ident_f[0:R, 0:R]), reads=[rows, self.ident_f], writes=[b])
        k.op("dve", lambda e: e.tensor_copy(out=dst[:].rearrange("p n r -> p (n r)"), in_=b[:, 0:n * R]), reads=[b], writes=[dst])

    def p0_alloc(self):
        k = self.k
        self.crow = k.sb([2, D], F32, "crow")
        self.cT = k.sb([128, 8, 2], F32, "cT")
        self.wada = [k.sb([128, 8, 512], BF16, "wada%d" % i) for i in range(2)]
        self.modrow = k.sb([2, 6 * D], F32, "modrow")
        self.brow = k.sb([2, 6 * D], F32, "brow")
        self.nrow = k.sb([2, D], F32, "nrow")
        self.fnrow = k.sb([1, D], F32, "fnrow")

    def p0(self, l):
        k, I = self.k, self.I
        crow, cT, scT, modrow, brow, modT = self.crow, self.cT, self.scT, self.modrow, self.brow, self.modT
        if l == 0:
            k.dma("sp", crow[:], I["cvec"], writes=[crow])
            self.rows_to_cols(crow, 2, 8, cT)
            k.op("act", lambda e: e.activation(out=scT[:], in_=cT[:], func=AF.Silu), reads=[cT], writes=[scT])
            k.dma("sp", self.fnrow[:], I["final_norm"].rearrange("(o n) -> o n", o=1), writes=[self.fnrow])
            self.rows_to_cols(self.fnrow, 1, 8, self.fnT)
        for r in range(2):
            k.dma("sp", brow[r:r + 1, :], I["b_ada"][l:l + 1, :], writes=[brow])
        k.dma("sp", self.nrow[0:1, :], I["norm_mix"][l:l + 1, :], writes=[self.nrow])
        k.dma("sp", self.nrow[1:2, :], I["norm_ffn"][l:l + 1, :], writes=[self.nrow])
        self.rows_to_cols(self.nrow, 2, 8, self.nT)
        wsrc = I["w_ada"][l].rearrange("(k p) n -> p k n", p=128)
        for g in range(12):
            wt = self.wada[g % 2]
            k.dma("pool", wt[:], wsrc[:, :, g * 512:(g + 1) * 512], writes=[wt], max_dma_last_dim=2048)
            b = self.bank()
            for kk in range(8):
                k.op("pe", lambda e, kk=kk: e.matmul(b[0:2, :], lhsT=scT[:, kk, :], rhs=wt[:, kk, :], start=(kk == 0), stop=(kk == 7)),
                     reads=[scT, wt], writes=[b])
            k.op("dve", lambda e, g=g: e.tensor_tensor(out=modrow[:, g * 512:(g + 1) * 512], in0=b[0:2, :], in1=brow[:, g * 512:(g + 1) * 512], op=ALU.add),
                 reads=[b, brow], writes=[modrow])
        self.rows_to_cols(modrow, 2, 48, modT)
        for r in range(2):
            k.op("dve", lambda e, r=r: e.scalar_tensor_tensor(out=self.g1T[:, :, r], in0=modT[:, 8:16, r], scalar=1.0, in1=self.nT[:, :, 0], op0=ALU.add, op1=ALU.mult),
                 reads=[modT, self.nT], writes=[self.g1T])
            k.op("dve", lambda e, r=r: e.scalar_tensor_tensor(out=self.g2T[:, :, r], in0=modT[:, 32:40, r], scalar=1.0, in1=self.nT[:, :, 1], op0=ALU.add, op1=ALU.mult),
                 reads=[modT, self.nT], writes=[self.g2T])

    def p1_alloc(self):
        k = self.k
        self.w_fm = k.sb([128, 8, 1536], BF16, "w_fm")
        self.w_tm = k.sb([128, 8, NTM], BF16, "w_tm")
        self.xt = [k.sb([128, D], F32, "xt%d" % i) for i in range(2)]
        self.junk = k.sb([128, D], BF16, "junk")
        self.ss = [k.sb([128, 2], F32, "ss%d" % i) for i in range(2)]
        self.xn = [k.sb([128, D], BF16, "xn%d" % i) for i in range(2)]
        self.hT = [k.sb([128, 8, 512], BF16, "hT%d" % i) for i in range(2)]
        self.tmo = [k.sb([128, NTM], F32, "tmo%d" % i) for i in range(2)]
        self.qo = [k.sb([128, 512], BF16, "qo%d" % i) for i in range(3)]
        self.sa_raw = [k.sb([128, 4, 516], BF16, "sa_raw%d" % i) for i in range(3)]
        self.sa_diag = [[k.sb([128, 5, 128], BF16, "sa_diag%d_%d" % (s, i)) for i in range(2)] for s in range(3)]
        self.sa_t1 = [[k.sb([128, 512], F32, "sa_t1_%d_%d" % (s, i)) for i in range(2)] for s in range(3)]
        self.sa_sil = [[k.sb([128, 512], F32, "sa_sil%d_%d" % (s, i)) for i in range(2)] for s in range(2)]
        self.sa_sq = [k.sb([128, 512], BF16, "sa_sq%d" % s) for s in range(2)]
        self.sa_rn = [k.sb([128, 512], F32, "sa_rn%d" % s) for s in range(2)]
        self.sa_q = k.sb([128, 4, 512], BF16, "sa_q")
        self.sa_k = k.sb([128, 4, 512], BF16, "sa_k")
        self.sa_vt = k.sb([128, 4, 512], BF16, "sa_vt")
        self.sa_kv = [k.sb([128, 8, 128], BF16, "sa_kv%d" % i) for i in range(2)]
        self.sa_convrow = k.sb([5, 1536], F32, "sa_convrow")
        self.sa_cw = k.sb([128, 12, 5], F32, "sa_cw")
        self.sa_qeps = k.sb([128, 1], F32, "sa_qeps")

    def p1(self, l, xsrc, xsrc_dep):
        k, I, S = self.k, self.I, self.S
        wsrc = I["w_in"][l].rearrange("(k p) n -> p k n", p=128)
        for kk in range(8):
            k.dma("pool", self.w_fm[:, kk, :], wsrc[:, kk, 0:1536], writes=[self.w_fm], max_dma_last_dim=2048)
            k.dma("pool", self.w_tm[:, kk, :], wsrc[:, kk, 1536:IN_COLS], writes=[self.w_tm], max_dma_last_dim=2048)
        if l == 0:
            for (c0, n) in ((0, 2), (258, 4), (LS + 6, 2)):
                for ch in range(12):
                    k.dma("sp", S["qkvT"][ch * 128:(ch + 1) * 128, c0:c0 + n], self.zeros_f[:, 0:n], reads=[self.zeros_f], writes=[self.qkv_pad_dep])
        blocks = [(0, 2)] + [(2 + 4 * i, 4) for i in range(16)]
        blocks = [b_ for b_ in blocks if b_[0] < self.ntiles]

        def cyc(ids):
            st = [0]

            def f():
                b = self.ps[ids[st[0] % len(ids)]]
                st[0] += 1
                return b
            return f
        bk_p, bk_t, bk_f = cyc([0]), cyc([1, 2]), cyc([3, 4])
        bk_sa = [cyc([5]), cyc([6]), cyc([7])]
        k.dma("sp", self.sa_convrow[:], I["conv"][l], writes=[self.sa_convrow])
        self.rows_to_cols(self.sa_convrow, 5, 12, self.sa_cw)
        k.op("pool", lambda e: e.memset(self.sa_qeps[:], 128.0 * EPS), writes=[self.sa_qeps])
        for t_ in (self.sa_q, self.sa_k, self.sa_vt):
            k.op("pool", lambda e, t_=t_: e.memset(t_[:], 0.0), writes=[t_])
        nblk = len(blocks)

        def stagea(bk, cg, t0, nt):
            T = nt * 128
            col0 = t0 * 128 + (2 if t0 < 2 else 6)
            blk = 0 if t0 < 2 else 1 + (t0 - 2) // 4
            raw, cw, qeps = self.sa_raw[cg], self.sa_cw, self.sa_qeps
            deps = [self.qkvc_dep[b_][cg * 4 + c_] for b_ in range(max(blk - 1, 0), min(blk + 2, nblk)) for c_ in range(4)] + [self.qkv_pad_dep]
            k.dma("sp", raw[:, :, 0:T + 4], S["qkvT"][cg * 512:(cg + 1) * 512, col0 - 2:col0 + T + 2].rearrange("(c p) t -> p c t", p=128),
                  reads=deps, writes=[raw])
            for c4 in range(4):
                ch = cg * 4 + c4
                dg = self.sa_diag[cg][c4 % 2]
                t1 = self.sa_t1[cg][c4 % 2]
                for j in range(5):
                    k.op("dve", lambda e: e.tensor_scalar(out=dg[:, j, :], in0=self.ident_b[:], scalar1=cw[:, ch, j:j + 1], scalar2=None, op0=ALU.mult),
                         reads=[self.ident_b, cw], writes=[dg])
                yield
                bc_ = bk()
                for j in range(5):
                    k.op("pe", lambda e: e.matmul(bc_[:, 0:T], lhsT=dg[:, j, :], rhs=raw[:, c4, j:j + T], start=(j == 0), stop=(j == 4)), reads=[dg, raw], writes=[bc_])
                yield
                k.op("act", lambda e: e.activation(out=t1[:, 0:T], in_=bc_[:, 0:T], func=AF.Exp, scale=-1.0), reads=[bc_], writes=[t1])
                yield
                k.op("act", lambda e: e.activation(out=t1[:, 0:T], in_=t1[:, 0:T], func=AF.Ln, bias=1.0), reads=[t1], writes=[t1])
                yield
                k.op("act", lambda e: e.activation(out=t1[:, 0:T], in_=t1[:, 0:T], func=AF.Exp, scale=-1.0), reads=[t1], writes=[t1])
                yield
                if cg == 2:
                    k.op("dve", lambda e: e.tensor_tensor(out=self.sa_vt[:, c4, 0:T], in0=bc_[:, 0:T], in1=t1[:, 0:T], op=ALU.mult), reads=[bc_, t1], writes=[self.sa_vt])
                    yield
                    continue
                s_, q_, r_ = self.sa_sil[cg][c4 % 2], self.sa_sq[cg], self.sa_rn[cg]
                k.op("dve", lambda e: e.tensor_tensor(out=s_[:, 0:T], in0=bc_[:, 0:T], in1=t1[:, 0:T], op=ALU.mult), reads=[bc_, t1], writes=[s_])
                yield
                k.op("act", lambda e: e.activation(out=q_[:, 0:T], in_=s_[:, 0:T], func=AF.Square), reads=[s_], writes=[q_])
                yield
                b = bk()
                k.op("pe", lambda e: e.matmul(b[:, 0:T], lhsT=self.ones_b[:], rhs=q_[:, 0:T], start=True, stop=True), reads=[self.ones_b, q_], writes=[b])
                if cg == 0:
                    k.op("act", lambda e: e.activation(out=r_[:, 0:T], in_=b[:, 0:T], func=AF.Ln, scale=128.0, bias=qeps[:, 0:1]), reads=[b, qeps], writes=[r_])
                else:
                    k.op("act", lambda e: e.activation(out=r_[:, 0:T], in_=b[:, 0:T], func=AF.Ln, scale=1.0, bias=self.eps_t[:, 0:1]), reads=[b, self.eps_t], writes=[r_])
                yield
                k.op("act", lambda e: e.activation(out=r_[:, 0:T], in_=r_[:, 0:T], func=AF.Exp, scale=-0.5), reads=[r_], writes=[r_])
                yield
                dst = self.sa_q if cg == 0 else self.sa_k
                k.op("dve", lambda e: e.tensor_tensor(out=dst[:, c4, 0:T], in0=s_[:, 0:T], in1=r_[:, 0:T], op=ALU.mult), reads=[s_, r_], writes=[dst])
                yield

        def stagea_fin(bk, t0, nt):
            blk = 0 if t0 < 2 else 1 + (t0 - 2) // 4
            kqd = S["sa_kq"][blk].rearrange("p (a b c) -> p a b c", a=4, b=2)
            k.dma("pool", kqd[:, :, 0, :], self.sa_k[:], reads=[self.sa_k], writes=[self.sa_dep[blk][4]])
            k.dma("pool", kqd[:, :, 1, :], self.sa_q[:], reads=[self.sa_q], writes=[self.sa_dep[blk][6]])
            k.dma("pool", S["sa_vt"][blk], self.sa_vt[:].rearrange("p a b -> p (a b)"), reads=[self.sa_vt], writes=[self.sa_dep[blk][5]])
            for ti in range(nt):
                kvt = self.sa_kv[ti % 2]
                b = bk()
                bb = b.t[:, :].bitcast(BF16)
                for h in range(4):
                    k.op("pe", lambda e: e.transpose(out=bb[:, h * 128:(h + 1) * 128], in_=self.sa_k[:, h, ti * 128:(ti + 1) * 128], identity=self.ident_b[:]),
                         reads=[self.sa_k, self.ident_b], writes=[b])
                    k.op("pe", lambda e: e.transpose(out=bb[:, (4 + h) * 128:(5 + h) * 128], in_=self.sa_vt[:, h, ti * 128:(ti + 1) * 128], identity=self.ident_b[:]),
                         reads=[self.sa_vt, self.ident_b], writes=[b])
                k.op("act", lambda e: e.copy(out=kvt[:].rearrange("p a b -> p (a b)"), in_=bb[:, :]), reads=[b], writes=[kvt])
                k.dma("pool", S["sa_kv"][blk, ti], kvt[:].rearrange("p a b -> p (a b)"), reads=[kvt], writes=[self.sa_dep[blk][ti]])
                yield

        def prep(bk, hT, t0, nt):
            r = 1 if t0 < 2 else 0
            for ti in range(nt):
                i = t0 + ti
                x_t, ss, xn = self.xt[i % 2], self.ss[i % 2], self.xn[i % 2]
                k.dma("sp", x_t[:], xsrc[i * 128:(i + 1) * 128, :], reads=[xsrc_dep[i]], writes=[x_t])
                k.op("act", lambda e: e.activation(out=self.junk[:], in_=x_t[:], func=AF.Square, accum_out=ss[:, 0:1]), reads=[x_t], writes=[self.junk, ss])
                yield
                k.op("act", lambda e: e.activation(out=ss[:, 1:2], in_=ss[:, 0:1], func=AF.Sqrt, scale=1.0 / D, bias=self.eps_t[:, 0:1]), reads=[ss, self.eps_t], writes=[ss])
                yield
                k.op("dve", lambda e: e.reciprocal(out=ss[:, 1:2], in_=ss[:, 1:2]), reads=[ss], writes=[ss])
                k.op("dve", lambda e: e.tensor_scalar(out=xn[:], in0=x_t[:], scalar1=ss[:, 1:2], scalar2=None, op0=ALU.mult), reads=[x_t, ss], writes=[xn])
                yield
                b = bk()
                bb = b.t[:, :].bitcast(BF16)
                for kk in range(8):
                    k.op("pe", lambda e, kk=kk: e.transpose(out=bb[:, kk * 128:(kk + 1) * 128], in_=xn[:, kk * 128:(kk + 1) * 128], identity=self.ident_b[:]),
                         reads=[xn, self.ident_b], writes=[b])
                yield
                for kk in range(8):
                    if kk % 2 == 0:
                        k.op("act", lambda e, kk=kk: e.activation(out=hT[:, kk, ti * 128:(ti + 1) * 128], in_=bb[:, kk * 128:(kk + 1) * 128], func=AF.Identity,
                                                                  scale=self.g1T[:, kk, r:r + 1], bias=self.modT[:, kk, r:r + 1]),
                             reads=[b, self.g1T, self.modT], writes=[hT])
                    else:
                        k.op("dve", lambda e, kk=kk: e.tensor_scalar(out=hT[:, kk, ti * 128:(ti + 1) * 128], in0=bb[:, kk * 128:(kk + 1) * 128],
                                                                     scalar1=self.g1T[:, kk, r:r + 1], scalar2=self.modT[:, kk, r:r + 1], op0=ALU.mult, op1=ALU.add),
                             reads=[b, self.g1T, self.modT], writes=[hT])
                    if kk % 2:
                        yield

        def tmproj(bk, hT, t0, nt):
            ev = 0
            for ti in range(nt):
                i = t0 + ti
                tmo = self.tmo[i % 2]
                for cg in range(5):
                    n = min(512, NTM - cg * 512)
                    b2 = bk()
                    for kk in range(8):
                        k.op("pe", lambda e, kk=kk: e.matmul(b2[:, 0:n], lhsT=hT[:, kk, ti * 128:(ti + 1) * 128], rhs=self.w_tm[:, kk, cg * 512:cg * 512 + n],
                                                             start=(kk == 0), stop=(kk == 7)), reads=[hT, self.w_tm], writes=[b2])
                    yield
                    ev += 1
                    if ev % 2:
                        k.op("act", lambda e: e.copy(out=tmo[:, cg * 512:cg * 512 + n], in_=b2[:, 0:n]), reads=[b2], writes=[tmo])
                    else:
                        k.op("dve", lambda e: e.tensor_copy(out=tmo[:, cg * 512:cg * 512 + n], in_=b2[:, 0:n]), reads=[b2], writes=[tmo])
                k.dma("pool", S["tm"][i * 128:(i + 1) * 128, :], tmo[:], reads=[tmo], writes=[self.tm_dep[i]])
                yield

        def fmproj(bk, hT, t0, nt):
            T = nt * 128
            col0 = t0 * 128 + (2 if t0 < 2 else 6)
            for ch in range(12):
                b3 = bk()
                for kk in range(8):
                    k.op("pe", lambda e, kk=kk: e.matmul(b3[:, 0:T], lhsT=self.w_fm[:, kk, ch * 128:(ch + 1) * 128], rhs=hT[:, kk, 0:T], start=(kk == 0), stop=(kk == 7)),
                         reads=[hT, self.w_fm], writes=[b3])
                yield
                qo = self.qo[ch % 3]
                if ch % 2:
                    k.op("act", lambda e: e.copy(out=qo[:, 0:T], in_=b3[:, 0:T]), reads=[b3], writes=[qo])
                else:
                    k.op("dve", lambda e: e.tensor_copy(out=qo[:, 0:T], in_=b3[:, 0:T]), reads=[b3], writes=[qo])
                k.dma("pool", S["qkvT"][ch * 128:(ch + 1) * 128, col0:col0 + T], qo[:, 0:T], reads=[qo], writes=[self.qkv_dep[t0 + j] for j in range(nt)] + [self.qkvc_dep[0 if t0 < 2 else 1 + (t0 - 2) // 4][ch]])
                yield

        self.interleave([prep(bk_p, self.hT[0], *blocks[0])])
        for bi, (t0, nt) in enumerate(blocks):
            hT = self.hT[bi % 2]
            gens = [tmproj(bk_t, hT, t0, nt), fmproj(bk_f, hT, t0, nt)]
            if bi + 1 < len(blocks):
                gens.append(prep(bk_p, self.hT[(bi + 1) % 2], *blocks[bi + 1]))
            if bi >= 2:
                gens += [stagea(bk_sa[cg], cg, *blocks[bi - 2]) for cg in range(3)]
            self.interleave(gens)
            if bi >= 2:
                self.interleave([stagea_fin(bk_p, *blocks[bi - 2])])
        for bi in range(max(nblk - 2, 0), nblk):
            self.interleave([stagea(bk_sa[cg], cg, *blocks[bi]) for cg in range(3)])
            self.interleave([stagea_fin(bk_p, *blocks[bi])])

    def dir_masks(self, d):
        k = self.k
        mI = k.sb([128, 4, 128], BF16, "maskI")
        mS = k.sb([128, 4, 128], BF16, "maskS")
        tri = k.sb([128, 128], F32, "tri")
        sgn = 1 if d == 0 else -1
        for (m, cmp) in ((mI, ALU.is_ge), (mS, ALU.is_gt)):
            k.op("pool", lambda e: e.memset(m[:], 1.0), writes=[m])
            k.op("pool", lambda e: e.affine_select(out=m[:], in_=m[:], pattern=[[0, 4], [sgn, 128]], compare_op=cmp, fill=0.0,
                                                    base=0, channel_multiplier=-sgn), reads=[m], writes=[m])
        k.op("pool", lambda e: e.tensor_copy(out=tri[:], in_=mI[:, 0, :]), reads=[mI], writes=[tri])
        return mI, mS, tri

    def bcast_row(self, src_row_ap, n, dst):
        self.k.dma("sp", dst[:, 0:n], src_row_ap.broadcast_to([128, n]), writes=[dst])

    @staticmethod
    def interleave(gens):
        gens = list(gens)
        while gens:
            nxt = []
            for g in gens:
                try:
                    next(g)
                    nxt.append(g)
                except StopIteration:
                    pass
            gens = nxt

    def p2(self, l):
        k, I, S = self.k, self.I, self.S
        sb = k.sb
        flat = lambda t: t[:].rearrange("p a b -> p (a b)")
        dtb = sb([128, 8], F32, "dtb")
        nA = sb([128, 8], F32, "nA")
        self.bcast_row(I["dn_dt_bias"][l:l + 1, :], 8, dtb)
        self.bcast_row(I["dn_a_log"][l:l + 1, :], 8, nA)
        k.op("act", lambda e: e.activation(out=nA[:], in_=nA[:], func=AF.Exp), reads=[nA], writes=[nA])
        k.op("dve", lambda e: e.tensor_scalar(out=nA[:], in0=nA[:], scalar1=-1.0, scalar2=None, op0=ALU.mult), reads=[nA], writes=[nA])
        ident4b = sb([128, 4, 128], BF16, "ident4b")
        for h in range(4):
            k.op("pool", lambda e: e.tensor_copy(out=ident4b[:, h, :], in_=self.ident_f[:]), reads=[self.ident_f], writes=[ident4b])
        bd32 = sb([128, 4, 128], BF16, "bd32"); m1 = sb([128, 4, 128], BF16, "m1"); m2 = sb([128, 4, 128], BF16, "m2")
        for (bs_, nb_, dst) in ((32, 4, bd32), (64, 2, m1)):
            E = sb([4, 128], F32, "Eblk%d" % bs_)
            k.op("pool", lambda e: e.memset(E[:], 1.0), writes=[E])
            k.op("pool", lambda e: e.affine_select(out=E[:], in_=E[:], pattern=[[1, 128]], compare_op=ALU.is_ge, fill=0.0, base=0, channel_multiplier=-bs_), reads=[E], writes=[E])
            k.op("pool", lambda e: e.affine_select(out=E[:], in_=E[:], pattern=[[-1, 128]], compare_op=ALU.is_ge, fill=0.0, base=bs_ - 1, channel_multiplier=bs_), reads=[E], writes=[E])
            bm = self.bank()
            k.op("pe", lambda e: e.matmul(bm[:, 0:128], lhsT=E[0:nb_, :], rhs=E[0:nb_, :], start=True, stop=True), reads=[E], writes=[bm])
            for h in range(4):
                k.op("dve", lambda e: e.tensor_copy(out=dst[:, h, :], in_=bm[:, 0:128]), reads=[bm], writes=[dst])
        k.op("dve", lambda e: e.tensor_scalar(out=m2[:], in0=m1[:], scalar1=-1.0, scalar2=1.0, op0=ALU.mult, op1=ALU.add), reads=[m1], writes=[m2])
        k.op("dve", lambda e: e.tensor_tensor(out=m1[:], in0=m1[:], in1=bd32[:], op=ALU.subtract), reads=[m1, bd32], writes=[m1])
        KQT = [sb([128, 4, 2, 512], BF16, "KQT%d" % i) for i in range(2)]
        VT = [sb([128, 4, 512], BF16, "VT%d" % i) for i in range(2)]
        KVtm = [[sb([128, 8, 128], BF16, "KVtm%d_%d" % (j, i)) for i in range(4)] for j in range(2)]
        def mk(n, shape, dt, name):
            return [sb(shape, dt, "%s%d" % (name, i)) for i in range(n)]
        sc4 = sb([128, 4, 16], F32, "sc4")
        Gd = mk(2, [128, 4, 128], F32, "Gd")
        Dm = mk(2, [128, 4, 128], F32, "Dm")
        Eg = mk(2, [128, 4, 128], F32, "Eg")
        A = mk(2, [128, 4, 128], BF16, "A"); AT = mk(2, [128, 4, 128], BF16, "AT")
        Abd = mk(2, [128, 4, 128], BF16, "Abd"); ATbd = mk(2, [128, 4, 128], BF16, "ATbd")
        Xa = mk(2, [128, 4, 128], BF16, "Xa"); XTa = mk(2, [128, 4, 128], BF16, "XTa")
        YT = mk(2, [128, 4, 128], BF16, "YT")
        R0 = mk(2, [128, 4, 128], BF16, "R0"); R1 = mk(2, [128, 4, 128], BF16, "R1")
        B1T = mk(2, [128, 4, 128], BF16, "B1T"); B2T = mk(2, [128, 4, 128], BF16, "B2T")
        Keg = mk(2, [128, 4, 128], BF16, "Keg")
        def mk2(shape, dt, name):
            return [[sb(shape, dt, "%s%d_%d" % (name, j, i)) for i in range(4)] for j in range(2)]
        smb = [sb([128, 4, 32], F32, "smb%d" % j) for j in range(3)]
        attnT = mk2([128, 4, 128], BF16, "attnT")
        up = mk2([128, 4, 128], F32, "up")
        wT = mk2([128, 4, 128], BF16, "wT")
        kdec = mk2([128, 4, 128], BF16, "kdec")
        qdecT = mk2([128, 4, 128], BF16, "qdecT")
        vnew = [sb([128, 4, 128], BF16, "vnew%d" % i) for i in range(2)]
        osb = [sb([128, 4, 128], F32, "osb%d" % i) for i in range(2)]
        Sf = sb([128, 4, 128], F32, "Sf")
        Sb = sb([128, 4, 128], BF16, "Sb")

        def stage_a(bk, d, tri, si, bp, t0, nt):
            T = nt * 128
            col0 = t0 * 128 + (2 if t0 < 2 else 6)
            kq, vt = KQT[bp], VT[bp]
            smT = smb[si]
            v = lambda a, b_: smT[:, 0:nt, a:b_]
            bcs = lambda t_: t_[:, 4 * d:4 * d + 4].unsqueeze(1).to_broadcast([128, nt, 4])
            k.dma("sp", sc4[:, 0:nt, :], S["tm"][t0 * 128:(t0 + nt) * 128, 512:528].rearrange("(t p) n -> p t n", p=128),
                  reads=[self.tm_dep[i] for i in range(t0, t0 + nt)], writes=[sc4])
            k.op("act", lambda e: e.activation(out=v(0, 4), in_=sc4[:, 0:nt, 4 * d:4 * d + 4], func=AF.Exp, scale=-1.0), reads=[sc4], writes=[smT])
            k.op("pool", lambda e: e.tensor_tensor(out=v(4, 8), in0=sc4[:, 0:nt, 8 + 4 * d:12 + 4 * d], in1=bcs(dtb), op=ALU.add), reads=[sc4, dtb], writes=[smT])
            yield
            k.op("dve", lambda e: e.tensor_scalar(out=v(0, 4), in0=v(0, 4), scalar1=1.0, scalar2=None, op0=ALU.add), reads=[smT], writes=[smT])
            k.op("dve", lambda e: e.reciprocal(out=v(0, 4), in_=v(0, 4)), reads=[smT], writes=[smT])
            k.op("act", lambda e: e.activation(out=v(4, 8), in_=v(4, 8), func=AF.Exp), reads=[smT], writes=[smT])
            yield
            k.op("act", lambda e: e.activation(out=v(4, 8), in_=v(4, 8), func=AF.Ln, bias=1.0), reads=[smT], writes=[smT])
            yield
            k.op("pool", lambda e: e.tensor_tensor(out=v(4, 8), in0=v(4, 8), in1=bcs(nA), op=ALU.mult), reads=[smT, nA], writes=[smT])
            yield
            bs = bk()
            for ti in range(nt):
                k.op("pe", lambda e: e.matmul(bs[:, ti * 8:ti * 8 + 4], lhsT=tri[:], rhs=smT[:, ti, 4:8], start=True, stop=True), reads=[tri, smT], writes=[bs])
                k.op("pe", lambda e: e.matmul(bs[:, ti * 8 + 4:ti * 8 + 8], lhsT=self.ones_f[:], rhs=smT[:, ti, 4:8], start=True, stop=True), reads=[self.ones_f, smT], writes=[bs])
            yield
            k.op("dve", lambda e: e.tensor_copy(out=v(8, 16), in_=bs[:, 0:nt * 8].rearrange("p (t n) -> p t n", t=nt)), reads=[bs], writes=[smT])
            yield
            k.op("act", lambda e: e.activation(out=v(16, 20), in_=v(8, 12), func=AF.Exp), reads=[smT], writes=[smT])
            k.op("dve", lambda e: e.tensor_tensor(out=v(20, 24), in0=v(12, 16), in1=v(8, 12), op=ALU.subtract), reads=[smT], writes=[smT])
            yield
            k.op("act", lambda e: e.activation(out=v(20, 24), in_=v(20, 24), func=AF.Exp), reads=[smT], writes=[smT])
            k.op("act", lambda e: e.activation(out=v(24, 28), in_=v(12, 16), func=AF.Exp), reads=[smT], writes=[smT])
            yield
            k.op("dve", lambda e: e.tensor_tensor(out=v(20, 24), in0=v(20, 24), in1=v(0, 4), op=ALU.mult), reads=[smT], writes=[smT])
            yield
            blk = 0 if t0 < 2 else 1 + (t0 - 2) // 4
            if True:
                k.dma("sp", kq[:].rearrange("p a b c -> p (a b c)"), S["sa_kq"][blk], reads=[self.sa_dep[blk][4], self.sa_dep[blk][6]], writes=[kq])
                k.dma("sp", vt[:].rearrange("p a b -> p (a b)"), S["sa_vt"][blk], reads=[self.sa_dep[blk][5]], writes=[vt])
                yield
                for ti in range(nt):
                    k.dma("sp", KVtm[bp][ti][:].rearrange("p a b -> p (a b)"), S["sa_kv"][blk, ti], reads=[self.sa_dep[blk][ti]], writes=[KVtm[bp][ti]])
                    yield
                return

        def stage_b(bk, d, mI, mS, tri, si, bp, q, i, ti):
            kq, kv = KQT[bp], KVtm[bp][ti]
            tsl = slice(ti * 128, (ti + 1) * 128)
            smT = smb[si]
            sm_ = smT[:, ti, :]
            G = Gd[q]
            k.op("pool", lambda e: e.tensor_tensor(out=G[:], in0=mI[:], in1=sm_[:, 4:8].unsqueeze(2).to_broadcast([128, 4, 128]), op=ALU.mult),
                 reads=[mI, smT], writes=[G])
            yield
            bg = bk()
            k.op("pe", lambda e: e.matmul(bg[:, :], lhsT=self.ones_f[:], rhs=flat(G), start=True, stop=True), reads=[self.ones_f, G], writes=[bg])
            yield
            bg3 = bg[:, :].rearrange("p (a b) -> p a b", a=4)
            for h in range(4):
                k.op("dve", lambda e: e.tensor_scalar(out=Dm[q][:, h, :], in0=bg3[:, h, :], scalar1=sm_[:, 8 + h:9 + h], scalar2=0.0, op0=ALU.subtract, op1=ALU.min),
                     reads=[bg, smT], writes=[Dm[q]])
            yield
            k.op("act", lambda e: e.activation(out=flat(Eg[q]), in_=bg[:, :], func=AF.Exp), reads=[bg], writes=[Eg[q]])
            k.op("act", lambda e: e.activation(out=Dm[q][:], in_=Dm[q][:], func=AF.Exp), reads=[Dm[q]], writes=[Dm[q]])
            yield
            k.op("pool", lambda e: e.tensor_tensor(out=qdecT[bp][ti][:], in0=kq[:, :, 1, tsl], in1=Eg[q][:], op=ALU.mult), reads=[kq, Eg[q]], writes=[qdecT[bp][ti]])
            bet = sm_[:, 0:4].unsqueeze(2).to_broadcast([128, 4, 128])
            DmI, DmS = Gd[q], Eg[q]
            k.op("pool", lambda e: e.tensor_tensor(out=DmI[:], in0=Dm[q][:], in1=mI[:], op=ALU.mult), reads=[Dm[q], mI], writes=[DmI])
            yield
            k.op("pool", lambda e: e.tensor_tensor(out=DmS[:], in0=Dm[q][:], in1=mS[:], op=ALU.mult), reads=[Dm[q], mS], writes=[DmS])
            k.op("pool", lambda e: e.tensor_tensor(out=DmI[:], in0=DmI[:], in1=bet, op=ALU.mult), reads=[DmI, smT], writes=[DmI])
            k.op("pool", lambda e: e.tensor_tensor(out=DmS[:], in0=DmS[:], in1=bet, op=ALU.mult), reads=[DmS, smT], writes=[DmS])
            K3 = kv[:, 0:4, :]
            k.op("pool", lambda e: e.tensor_tensor(out=Keg[q][:], in0=K3, in1=sm_[:, 16:20].unsqueeze(2).to_broadcast([128, 4, 128]), op=ALU.mult), reads=[kv, smT], writes=[Keg[q]])
            k.op("pool", lambda e: e.tensor_tensor(out=kdec[bp][ti][:], in0=K3, in1=sm_[:, 20:24].unsqueeze(2).to_broadcast([128, 4, 128]), op=ALU.mult), reads=[kv, smT], writes=[kdec[bp][ti]])
            bkk, bkq = bk(), bk()
            for h in range(4):
                k.op("pe", lambda e: e.matmul(bkk[:, h * 128:(h + 1) * 128], lhsT=kq[:, h, 0, tsl], rhs=kq[:, h, 0, tsl], start=True, stop=True), reads=[kq], writes=[bkk])
                k.op("pe", lambda e: e.matmul(bkq[:, h * 128:(h + 1) * 128], lhsT=kq[:, h, 0, tsl], rhs=kq[:, h, 1, tsl], start=True, stop=True), reads=[kq], writes=[bkq])
            yield
            k.op("dve", lambda e: e.tensor_tensor(out=flat(A[q]), in0=bkk[:, :], in1=flat(DmS), op=ALU.mult), reads=[bkk, DmS], writes=[A[q]])
            k.op("dve", lambda e: e.tensor_tensor(out=flat(attnT[bp][ti]), in0=bkq[:, :], in1=flat(DmI), op=ALU.mult), reads=[bkq, DmI], writes=[attnT[bp][ti]])
            yield
            bt = bk()
            btb = bt.t[:, :].bitcast(BF16)
            for h in range(4):
                k.op("pe", lambda e: e.transpose(out=btb[:, h * 128:(h + 1) * 128], in_=A[q][:, h, :], identity=self.ident_b[:]), reads=[A[q], self.ident_b], writes=[bt])
            k.op("pool", lambda e: e.tensor_tensor(out=Abd[q][:], in0=A[q][:], in1=bd32[:], op=ALU.mult), reads=[A[q], bd32], writes=[Abd[q]])
            yield
            k.op("act", lambda e: e.copy(out=flat(AT[q]), in_=btb[:, 0:512]), reads=[bt], writes=[AT[q]])
            k.op("pool", lambda e: e.tensor_tensor(out=R0[q][:], in0=ident4b[:], in1=Abd[q][:], op=ALU.subtract), reads=[ident4b, Abd[q]], writes=[R0[q]])
            yield
            k.op("pool", lambda e: e.tensor_tensor(out=ATbd[q][:], in0=AT[q][:], in1=bd32[:], op=ALU.mult), reads=[AT[q], bd32], writes=[ATbd[q]])
            k.op("pool", lambda e: e.tensor_tensor(out=B1T[q][:], in0=AT[q][:], in1=m1[:], op=ALU.mult), reads=[AT[q], m1], writes=[B1T[q]])
            k.op("pool", lambda e: e.tensor_tensor(out=B2T[q][:], in0=AT[q][:], in1=m2[:], op=ALU.mult), reads=[AT[q], m2], writes=[B2T[q]])
            yield
            Xc, XTc, Rc = Abd[q], ATbd[q], R0[q]
            xbufs = [(Xa[q], XTa[q]), (A[q], AT[q])]
            rbufs = [R1[q], R0[q]]
            for lev in range(4):
                lastl = lev == 3
                Xn, XTn = xbufs[lev % 2]
                Rn = rbufs[lev % 2]
                b1 = bk()
                for h in range(4):
                    k.op("pe", lambda e: e.matmul(b1[:, h * 128:(h + 1) * 128], lhsT=Xc[:, h, :], rhs=XTc[:, h, :], start=True, stop=True), reads=[Xc, XTc], writes=[b1])
                if not lastl:
                    b2 = bk()
                    for h in range(4):
                        k.op("pe", lambda e: e.matmul(b2[:, h * 128:(h + 1) * 128], lhsT=XTc[:, h, :], rhs=Xc[:, h, :], start=True, stop=True), reads=[Xc, XTc], writes=[b2])
                yield
                k.op("dve", lambda e: e.tensor_tensor(out=flat(YT[q]), in0=b1[:, :], in1=flat(ident4b), op=ALU.add), reads=[b1, ident4b], writes=[YT[q]])
                if not lastl:
                    k.op("act", lambda e: e.copy(out=flat(XTn), in_=b1[:, :]), reads=[b1], writes=[XTn])
                    k.op("act", lambda e: e.copy(out=flat(Xn), in_=b2[:, :]), reads=[b2], writes=[Xn])
                yield
                b3 = bk()
                for h in range(4):
                    k.op("pe", lambda e: e.matmul(b3[:, h * 128:(h + 1) * 128], lhsT=YT[q][:, h, :], rhs=Rc[:, h, :], start=True, stop=True), reads=[YT[q], Rc], writes=[b3])
                yield
                k.op("dve", lambda e: e.tensor_copy(out=flat(Rn), in_=b3[:, :]), reads=[b3], writes=[Rn])
                yield
                Xc, XTc, Rc = Xn, XTn, Rn
            TTb, Pb = Xa[q], XTa[q]
            for mi, BT_ in enumerate((B1T[q], B2T[q])):
                Tn = rbufs[mi % 2]
                btt = bk()
                bttb = btt.t[:, :].bitcast(BF16)
                for h in range(4):
                    k.op("pe", lambda e: e.transpose(out=bttb[:, h * 128:(h + 1) * 128], in_=Rc[:, h, :], identity=self.ident_b[:]), reads=[Rc, self.ident_b], writes=[btt])
                bp_ = bk()
                for h in range(4):
                    k.op("pe", lambda e: e.matmul(bp_[:, h * 128:(h + 1) * 128], lhsT=BT_[:, h, :], rhs=Rc[:, h, :], start=True, stop=True), reads=[BT_, Rc], writes=[bp_])
                yield
                k.op("act", lambda e: e.copy(out=flat(TTb), in_=bttb[:, 0:512]), reads=[btt], writes=[TTb])
                k.op("dve", lambda e: e.tensor_copy(out=flat(Pb), in_=bp_[:, :]), reads=[bp_], writes=[Pb])
                yield
                bq = bk()
                for h in range(4):
                    k.op("pe", lambda e: e.matmul(bq[:, h * 128:(h + 1) * 128], lhsT=TTb[:, h, :], rhs=Pb[:, h, :], start=True, stop=True), reads=[TTb, Pb], writes=[bq])
                yield
                k.op("dve", lambda e: e.tensor_tensor(out=flat(Tn), in0=flat(Rc), in1=bq[:, :], op=ALU.subtract), reads=[Rc, bq], writes=[Tn])
                yield
                Rc = Tn
            T2T = Rc
            bu, bw = bk(), bk()
            for h in range(4):
                k.op("pe", lambda e: e.matmul(bu[:, h * 128:(h + 1) * 128], lhsT=T2T[:, h, :], rhs=kv[:, 4 + h, :], start=True, stop=True), reads=[T2T, kv], writes=[bu])
                k.op("pe", lambda e: e.matmul(bw[:, h * 128:(h + 1) * 128], lhsT=Keg[q][:, h, :], rhs=T2T[:, h, :], start=True, stop=True), reads=[T2T, Keg[q]], writes=[bw])
            yield
            k.op("act", lambda e: e.copy(out=flat(up[bp][ti]), in_=bu[:, :]), reads=[bu], writes=[up[bp][ti]])
            k.op("dve", lambda e: e.tensor_copy(out=flat(wT[bp][ti]), in_=bw[:, :]), reads=[bw], writes=[wT[bp][ti]])
            yield

        def scan(bk, d, si, bp, tiles):
            for n_, (i, ti) in enumerate(tiles):
                p = n_ % 2
                smT = smb[si]
                sm_ = smT[:, ti, :]
                bws = bk()
                for h in range(4):
                    k.op("pe", lambda e: e.matmul(bws[:, h * 128:(h + 1) * 128], lhsT=wT[bp][ti][:, h, :], rhs=Sb[:, h, :], start=True, stop=True), reads=[wT[bp][ti], Sb], writes=[bws])
                yield
                k.op("dve", lambda e: e.tensor_tensor(out=flat(vnew[p]), in0=flat(up[bp][ti]), in1=bws[:, :], op=ALU.subtract), reads=[up[bp][ti], bws], writes=[vnew[p]])
                yield
                bo, bkv = bk(), bk()
                for h in range(4):
                    k.op("pe", lambda e: e.matmul(bkv[:, h * 128:(h + 1) * 128], lhsT=kdec[bp][ti][:, h, :], rhs=vnew[p][:, h, :], start=True, stop=True), reads=[kdec[bp][ti], vnew[p]], writes=[bkv])
                for h in range(4):
                    k.op("pe", lambda e: e.matmul(bo[:, h * 128:(h + 1) * 128], lhsT=qdecT[bp][ti][:, h, :], rhs=Sb[:, h, :], start=True, stop=False), reads=[qdecT[bp][ti], Sb], writes=[bo])
                    k.op("pe", lambda e: e.matmul(bo[:, h * 128:(h + 1) * 128], lhsT=attnT[bp][ti][:, h, :], rhs=vnew[p][:, h, :], start=False, stop=True), reads=[attnT[bp][ti], vnew[p]], writes=[bo])
                yield
                bkv3 = bkv[:, :].rearrange("p (a b) -> p a b", a=4)
                for h in range(4):
                    k.op("dve", lambda e: e.scalar_tensor_tensor(out=Sf[:, h, :], in0=Sf[:, h, :], scalar=sm_[:, 24 + h:25 + h], in1=bkv3[:, h, :], op0=ALU.mult, op1=ALU.add),
                         reads=[Sf, smT, bkv], writes=[Sf])
                k.op("act", lambda e: e.copy(out=flat(osb[p]), in_=bo[:, :]), reads=[bo], writes=[osb[p]])
                yield
                k.op("act", lambda e: e.copy(out=Sb[:], in_=Sf[:]), reads=[Sf], writes=[Sb])
                k.dma("act", S["o_dn"][d, i * 128:(i + 1) * 128, :], flat(osb[p]), reads=[osb[p]], writes=[self.odn_dep[d][i]])
                yield

        def cyc(ids):
            st = [0]

            def f():
                b = self.ps[ids[st[0] % len(ids)]]
                st[0] += 1
                return b
            return f
        bk_b = [cyc([0, 1]), cyc([2, 3])]
        bk_a, bk_s = cyc([6, 7]), cyc([4, 5])
        blocks_f = [(0, 2)] + [(2 + 4 * i, 4) for i in range(16)]
        blocks_f = [b_ for b_ in blocks_f if b_[0] < self.ntiles]
        gbi = 0
        for d in range(2):
            mI, mS, tri = self.dir_masks(d)
            k.op("pool", lambda e: e.memset(Sf[:], 0.0), writes=[Sf])
            k.op("pool", lambda e: e.memset(Sb[:], 0.0), writes=[Sb])
            blocks = blocks_f if d == 0 else [blocks_f[0]] + blocks_f[:0:-1]
            nb = len(blocks)
            self.interleave([stage_a(bk_a, d, tri, gbi % 3, gbi % 2, *blocks[0])])
            prev = None
            for bi, (t0, nt) in enumerate(blocks):
                bp = (gbi + bi) % 2
                tis = list(range(nt)) if d == 0 else list(range(nt - 1, -1, -1))
                for pair in range(0, nt, 2):
                    gens = [stage_b(bk_b[q], d, mI, mS, tri, (gbi + bi) % 3, bp, q, t0 + ti, ti) for q, ti in enumerate(tis[pair:pair + 2])]
                    if pair == 0 and bi + 1 < nb:
                        gens.append(stage_a(bk_a, d, tri, (gbi + bi + 1) % 3, (gbi + bi + 1) % 2, *blocks[bi + 1]))
                    if prev is not None and (pair == 2 or nt == 2):
                        gens.append(scan(bk_s, d, *prev))
                    self.interleave(gens)
                prev = ((gbi + bi) % 3, bp, [(t0 + ti, ti) for ti in tis])
            self.interleave([scan(bk_s, d, *prev)])
            gbi += nb

    def p3(self, l):
        k, I, S = self.k, self.I, self.S
        sb = k.sb

        def two(shape, dt, name):
            return [sb(shape, dt, name + "%d" % i) for i in range(4)]
        qkv = two([128, 1024], F32, "gqkv")
        glr = two([128, 16], F32, "glr")
        lhsTg = two([64, 128], F32, "lhsTg")
        W2 = sb([64, 256], F32, "W2")
        ex = two([128, 256], F32, "gex")
        la = two([128, 256], F32, "la")
        bcs = two([128, 256], F32, "bcs")
        ebc = two([128, 256], F32, "ebc")
        enbc = two([128, 256], F32, "enbc")
        ekd = two([128, 256], F32, "ekd")
        qs = two([128, 256], BF16, "gqs")
        ks = two([128, 256], BF16, "gks")
        kd = two([128, 256], BF16, "gkd")
        vb = two([128, 4, 128], BF16, "gvb")
        qT = two([128, 4, 128], BF16, "gqTm")
        kT = two([128, 2, 128], BF16, "gkT")
        att = two([128, 4, 128], BF16, "gatt")
        osb = two([128, 512], F32, "gosb")
        etc_ = two([128, 4], F32, "getc")
        Sf = sb([128, 2, 128], F32, "gSf")
        Sb = sb([128, 2, 128], BF16, "gSb")
        for t_ in qT:
            k.op("pool", lambda e: e.memset(t_[:], 0.0), writes=[t_])
        for t_ in lhsTg:
            k.op("pool", lambda e: e.memset(t_[:], 0.0), writes=[t_])
            k.op("pool", lambda e: e.memset(t_[32:33, :], 1.0), writes=[t_])
        tm_lat = S["tm"][LC:LS, :].rearrange("(r w) n -> w r n", w=64)
        alltm = self.tm_dep
        for d in range(2):
            mI, mS, tri = self.dir_masks(d)
            k.op("pool", lambda e: e.memset(W2[:], 0.0), writes=[W2])
            k.dma("sp", W2[0:16, :], I["gla_w_gate2"][l, d], writes=[W2])
            k.dma("sp", W2[32:33, :], I["gla_b_gate"][l, d:d + 1, :], writes=[W2])
            k.op("pool", lambda e: e.memset(Sf[:], 0.0), writes=[Sf])
            k.op("pool", lambda e: e.memset(Sb[:], 0.0), writes=[Sb])
            order = [("c", 0), ("c", 1)] + [("g", j) for j in range(64)]
            if d == 1:
                order = [("c", 1), ("c", 0)] + [("g", j) for j in range(63, -1, -1)]
            if self.ntiles < NT:
                order = order[:self.ntiles]
            def cyc(ids):
                st = [0]

                def f():
                    b = self.ps[ids[st[0] % len(ids)]]
                    st[0] += 1
                    return b
                return f
            bk_preA, bk_preB, bk_post = cyc([0, 1]), cyc([2, 3]), cyc([4, 5])

            def pre(bk, p, kind, j):
                if kind == "c":
                    rows = S["tm"][j * 128:(j + 1) * 128, :]
                else:
                    rows = tm_lat[j]
                k.dma("sp", qkv[p][:], rows[:, 528:1552], reads=alltm, writes=[qkv[p]])
                k.dma("sp", glr[p][:], rows[:, 2064 + 16 * d:2080 + 16 * d], reads=alltm, writes=[glr[p]])
                b0 = bk()
                k.op("pe", lambda e: e.transpose(out=b0[0:16, 0:128], in_=glr[p][:, :], identity=self.ident_f[:]), reads=[glr[p], self.ident_f], writes=[b0])
                k.op("act", lambda e: e.copy(out=lhsTg[p][0:16, :], in_=b0[0:16, 0:128]), reads=[b0], writes=[lhsTg[p]])
                yield
                bgt = bk()
                k.op("pe", lambda e: e.matmul(bgt[:, 0:256], lhsT=lhsTg[p][:, :], rhs=W2[:, :], start=True, stop=True), reads=[lhsTg[p], W2], writes=[bgt])
                k.op("act", lambda e: e.activation(out=ex[p][:], in_=bgt[:, 0:256], func=AF.Exp, scale=-1.0), reads=[bgt], writes=[ex[p]])
                yield
                k.op("act", lambda e: e.activation(out=ex[p][:], in_=ex[p][:], func=AF.Ln, bias=1.0), reads=[ex[p]], writes=[ex[p]])
                yield
                k.op("dve", lambda e: e.tensor_scalar(out=la[p][:], in0=ex[p][:], scalar1=-1.0 / 16.0, scalar2=None, op0=ALU.mult), reads=[ex[p]], writes=[la[p]])
                yield
                bbc, bto = bk(), bk()
                k.op("pe", lambda e: e.matmul(bbc[:, 0:256], lhsT=tri[:], rhs=la[p][:], start=True, stop=True), reads=[tri, la[p]], writes=[bbc])
                k.op("pe", lambda e: e.matmul(bto[:, 0:256], lhsT=self.ones_f[:], rhs=la[p][:], start=True, stop=True), reads=[self.ones_f, la[p]], writes=[bto])
                for hp in range(2):
                    k.op("pe", lambda e: e.matmul(bto[:, 256 + 2 * hp:258 + 2 * hp], lhsT=la[p][:, hp * 128:(hp + 1) * 128], rhs=self.ones_f[:, 0:2], start=True, stop=True),
                         reads=[la[p], self.ones_f], writes=[bto])
                k.op("act", lambda e: e.activation(out=ebc[p][:], in_=bbc[:, 0:256], func=AF.Exp), reads=[bbc], writes=[ebc[p]])
                yield
                k.op("act", lambda e: e.activation(out=enbc[p][:], in_=bbc[:, 0:256], func=AF.Exp, scale=-1.0), reads=[bbc], writes=[enbc[p]])
                yield
                k.op("act", lambda e: e.copy(out=bcs[p][:], in_=bbc[:, 0:256]), reads=[bbc], writes=[bcs[p]])
                yield
                k.op("dve", lambda e: e.tensor_tensor(out=ekd[p][:], in0=bto[:, 0:256], in1=bcs[p][:], op=ALU.subtract), reads=[bto, bcs[p]], writes=[ekd[p]])
                yield
                k.op("act", lambda e: e.activation(out=ekd[p][:], in_=ekd[p][:], func=AF.Exp), reads=[ekd[p]], writes=[ekd[p]])
                yield
                k.op("act", lambda e: e.activation(out=etc_[p][:], in_=bto[:, 256:260], func=AF.Exp), reads=[bto], writes=[etc_[p]])
                yield
                k.op("dve", lambda e: e.scalar_tensor_tensor(out=qs[p][:], in0=qkv[p][:, 0:256], scalar=0.125, in1=ebc[p][:], op0=ALU.mult, op1=ALU.mult), reads=[qkv[p], ebc[p]], writes=[qs[p]])
                yield
                k.op("dve", lambda e: e.tensor_tensor(out=ks[p][:], in0=qkv[p][:, 256:512], in1=enbc[p][:], op=ALU.mult), reads=[qkv[p], enbc[p]], writes=[ks[p]])
                yield
                k.op("pool", lambda e: e.tensor_tensor(out=kd[p][:], in0=qkv[p][:, 256:512], in1=ekd[p][:], op=ALU.mult), reads=[qkv[p], ekd[p]], writes=[kd[p]])
                yield
                k.op("pool", lambda e: e.tensor_copy(out=vb[p][:].rearrange("p a b -> p (a b)"), in_=qkv[p][:, 512:1024]), reads=[qkv[p]], writes=[vb[p]])
                yield
                btr = bk()
                btrb = btr.t[:, :].bitcast(BF16)
                for hp in range(2):
                    k.op("pe", lambda e: e.transpose(out=btrb[:, hp * 128:(hp + 1) * 128], in_=qs[p][:, hp * 128:(hp + 1) * 128], identity=self.ident_b[:]), reads=[qs[p], self.ident_b], writes=[btr])
                    k.op("pe", lambda e: e.transpose(out=btrb[:, (2 + hp) * 128:(3 + hp) * 128], in_=ks[p][:, hp * 128:(hp + 1) * 128], identity=self.ident_b[:]), reads=[ks[p], self.ident_b], writes=[btr])
                for h in range(4):
                    hb, hp = h % 2, h // 2
                    k.op("act", lambda e: e.copy(out=qT[p][hb * 64:(hb + 1) * 64, h, :], in_=btrb[hb * 64:(hb + 1) * 64, hp * 128:(hp + 1) * 128]), reads=[btr], writes=[qT[p]])
                k.op("act", lambda e: e.copy(out=kT[p][:].rearrange("p a b -> p (a b)"), in_=btrb[:, 256:512]), reads=[btr], writes=[kT[p]])
                yield
                bat = bk()
                for h in range(4):
                    hb, hp = h % 2, h // 2
                    ps_ = slice(hb * 64, (hb + 1) * 64)
                    k.op("pe", lambda e: e.matmul(bat[:, h * 128:(h + 1) * 128], lhsT=kT[p][:, hp, :], rhs=qT[p][:, h, :], start=True, stop=True), reads=[kT[p], qT[p]], writes=[bat])
                k.op("dve", lambda e: e.tensor_tensor(out=att[p][:].rearrange("p a b -> p (a b)"), in0=bat[:, :], in1=mI[:].rearrange("p a b -> p (a b)"), op=ALU.mult), reads=[bat, mI], writes=[att[p]])
                yield
                yield

            def post(bk, p, kind, j):
                if kind == "c":
                    orows = S["o_gla"][d, j * 128:(j + 1) * 128, :]
                    odeps = [self.ogla_dep[d][j]]
                else:
                    orows = S["o_gla"][d, LC:LS, :].rearrange("(r w) n -> w r n", w=64)[j]
                    odeps = self.ogla_dep[d][2:]
                bo, bkv = bk(), bk()
                for h in range(4):
                    hb, hp = h % 2, h // 2
                    ps_ = slice(hb * 64, (hb + 1) * 64)
                    k.op("pe", lambda e: e.matmul(bo[:, h * 128:(h + 1) * 128], lhsT=qT[p][:, h, :], rhs=Sb[:, hp, :], start=True, stop=False), reads=[qT[p], Sb], writes=[bo])
                    k.op("pe", lambda e: e.matmul(bo[:, h * 128:(h + 1) * 128], lhsT=att[p][:, h, :], rhs=vb[p][:, h, :], start=False, stop=True), reads=[att[p], vb[p]], writes=[bo])
                for h in range(4):
                    hb, hp = h % 2, h // 2
                    k.op("pe", lambda e: e.matmul(bkv[:, h * 128:(h + 1) * 128], lhsT=kd[p][:, hp * 128:(hp + 1) * 128], rhs=vb[p][:, h, :], start=True, stop=True),
                         reads=[kd[p], vb[p]], writes=[bkv])
                k.op("act", lambda e: e.copy(out=osb[p][:], in_=bo[:, :]), reads=[bo], writes=[osb[p]])
                yield
                k.dma("act", orows, osb[p][:], reads=[osb[p]], writes=odeps)
                for h in range(4):
                    hb, hp = h % 2, h // 2
                    ps_ = slice(hb * 64, (hb + 1) * 64)
                    k.op("dve", lambda e: e.scalar_tensor_tensor(out=Sf[ps_, hp, :], in0=Sf[ps_, hp, :], scalar=etc_[p][ps_, 2 * hp:2 * hp + 1], in1=bkv[ps_, h * 128:(h + 1) * 128], op0=ALU.mult, op1=ALU.add),
                         reads=[Sf, etc_[p], bkv], writes=[Sf])
                k.op("act", lambda e: e.copy(out=Sb[:], in_=Sf[:]), reads=[Sf], writes=[Sb])
                yield


                yield

            def posts(items):
                for (p_, kind_, j_) in items:
                    yield from post(bk_post, p_, kind_, j_)

            n_ = len(order)
            first = [pre(bk_preA, 0, *order[0])]
            if n_ > 1:
                first.append(pre(bk_preB, 1, *order[1]))
            self.interleave(first)
            for R in range(0, n_, 2):
                gens = [posts([(s % 4,) + tuple(order[s]) for s in (R, R + 1) if s < n_])]
                if R + 2 < n_:
                    gens.append(pre(bk_preA, (R + 2) % 4, *order[R + 2]))
                if R + 3 < n_:
                    gens.append(pre(bk_preB, (R + 3) % 4, *order[R + 3]))
                self.interleave(gens)

    def p4(self, l):
        k, I, S = self.k, self.I, self.S
        sb = k.sb
        last = l == self.nlayers - 1
        moe = (l % 2 == 1)
        T = 1024
        NH = T // 512
        gnb = sb([128, 8, 128], F32, "gnb")
        for h in range(8):
            src_ = I["dn_norm"] if h < 4 else I["gla_norm"]
            k.dma("sp", gnb[:, h, :], src_[l:l + 1, :].broadcast_to([128, 128]), writes=[gnb])
        xT = sb([128, 8, T], F32, "xT")
        mT = sb([128, 8, T], BF16, "mT")
        sq2 = mT
        h2T = sb([128, 8, T], BF16, "h2T")
        rs2 = sb([128, T], F32, "rs2")
        tmpf = [sb([128, T], F32, "tmpf%d" % i) for i in range(2)]
        F = D_EXP if moe else D_FF
        NFC = F // 128
        actT = sb([128, NFC, T], BF16, "actT")
        ot = [sb([128, 512], F32, "ot%d" % i) for i in range(4)]
        zt = sb([128, 1024], F32, "zt")
        xt = sb([128, D], F32, "xt4")
        xo = xt
        osum = sb([128, 8, 128], F32, "osum")
        ssh = sb([128, 16], F32, "ssh")
        mb = sb([128, D], BF16, "mb")
        wb = [sb([128, 8, 512], BF16, "wb%d" % i) for i in range(3)]
        wd = [sb([128, 4, 512], BF16, "wd%d" % i) for i in range(2)]
        sg = [ot[0], ot[1]]
        if moe:
            rt = sb([128, 8, NE], F32, "rt")
            k.dma("sp", rt[:], I["moe_router"].rearrange("(k p) n -> p k n", p=128), writes=[rt])
            lgT = sb([8, T], F32, "lgT")
            lg = sb([128, 8, NE], F32, "lg")
            l2 = sb([128, 8, NE], F32, "l2")
            mx = sb([128, 16], F32, "mx")
            cbT = sb([8, T], F32, "cbT")
            CBe = [zt, xt]
            selE = sb([8, NE, 128], F32, "selE")
            for e_ in range(NE):
                k.op("pool", lambda e: e.tensor_copy(out=selE[0:8, e_, :], in_=self.ident_f[0:8, e_:e_ + 1].to_broadcast([8, 128])), reads=[self.ident_f], writes=[selE])
        ntb = T // 128
        if last:
            blocks = [(2 + ntb * i, ntb) for i in range(NOWN // T)]
        else:
            blocks = [(0, 2)] + [(2 + ntb * i, ntb) for i in range(LL // T)]
        wi = [0]

        def wload(src_ap, ncols):
            t_ = wb[wi[0] % 3]
            wi[0] += 1
            k.dma("pool", t_[:, :, 0:ncols], src_ap, writes=[t_], max_dma_last_dim=2048)
            return t_
        wout_src = I["w_out"][l].rearrange("(k p) n -> p k n", p=128)
        for (t0, nt) in blocks:
            if t0 >= self.ntiles:
                break
            Tb = nt * 128
            halves = [(h0, min(512, Tb - h0)) for h0 in range(0, Tb, 512)]
            r = 1 if t0 < 2 else 0
            for ti in range(nt):
                i = t0 + ti
                rows = slice(i * 128, (i + 1) * 128)
                tsl = slice(ti * 128, (ti + 1) * 128)
                k.dma("sp", ot[0][:], S["o_dn"][0, rows, :], reads=[self.odn_dep[0][i]], writes=[ot[0]])
                k.dma("sp", ot[1][:], S["o_dn"][1, rows, :], reads=[self.odn_dep[1][i]], writes=[ot[1]])
                k.dma("sp", ot[2][:], S["o_gla"][0, rows, :], reads=[self.ogla_dep[0][i]], writes=[ot[2]])
                k.dma("sp", ot[3][:], S["o_gla"][1, rows, :], reads=[self.ogla_dep[1][i]], writes=[ot[3]])
                k.dma("sp", zt[:, 0:512], S["tm"][rows, 0:512], reads=[self.tm_dep[i]], writes=[zt])
                k.dma("sp", zt[:, 512:1024], S["tm"][rows, 1552:2064], reads=[self.tm_dep[i]], writes=[zt])
                k.dma("sp", xt[:], self.xcur[rows, :], reads=[self.xcur_dep[i]], writes=[xt])
                osf = osum[:].rearrange("p a b -> p (a b)")
                k.op("dve", lambda e: e.tensor_tensor(out=osf[:, 0:512], in0=ot[0][:], in1=ot[1][:], op=ALU.add), reads=[ot[0], ot[1]], writes=[osum])
                k.op("dve", lambda e: e.tensor_tensor(out=osf[:, 512:1024], in0=ot[2][:], in1=ot[3][:], op=ALU.add), reads=[ot[2], ot[3]], writes=[osum])
                for hh in range(2):
                    k.op("act", lambda e: e.activation(out=ot[2 * hh][:], in_=osf[:, hh * 512:(hh + 1) * 512], func=AF.Square), reads=[osum], writes=[ot[2 * hh]])
                    k.op("dve", lambda e: e.tensor_reduce(out=ssh[:, 4 * hh:4 * hh + 4], in_=ot[2 * hh][:].rearrange("p (a b) -> p a b", a=4), axis=AX.X, op=ALU.add), reads=[ot[2 * hh]], writes=[ssh])
                k.op("act", lambda e: e.activation(out=ssh[:, 8:16], in_=ssh[:, 0:8], func=AF.Sqrt, scale=1.0 / 128.0, bias=self.eps_t[:, 0:1]), reads=[ssh, self.eps_t], writes=[ssh])
                k.op("dve", lambda e: e.reciprocal(out=ssh[:, 8:16], in_=ssh[:, 8:16]), reads=[ssh], writes=[ssh])
                k.op("act", lambda e: e.activation(out=zt[:], in_=zt[:], func=AF.Silu), reads=[zt], writes=[zt])
                for h in range(8):
                    k.op("dve", lambda e: e.scalar_tensor_tensor(out=osum[:, h, :], in0=osum[:, h, :], scalar=ssh[:, 8 + h:9 + h], in1=gnb[:, h, :], op0=ALU.mult, op1=ALU.mult),
                         reads=[osum, ssh, gnb], writes=[osum])
                k.op("dve", lambda e: e.tensor_tensor(out=mb[:], in0=osf, in1=zt[:], op=ALU.mult), reads=[osum, zt], writes=[mb])
                b = self.bank()
                bb = b.t[:, :].bitcast(BF16)
                for kk in range(8):
                    k.op("pe", lambda e: e.transpose(out=bb[:, kk * 128:(kk + 1) * 128], in_=mb[:, kk * 128:(kk + 1) * 128], identity=self.ident_b[:]), reads=[mb, self.ident_b], writes=[b])
                k.op("act", lambda e: e.copy(out=mT[:, :, tsl], in_=bb[:, :].rearrange("p (a b) -> p a b", a=8)), reads=[b], writes=[mT])
                for half in range(2):
                    b2 = self.bank()
                    for q in range(4):
                        kk = half * 4 + q
                        k.op("pe", lambda e: e.transpose(out=b2[:, q * 128:(q + 1) * 128], in_=xt[:, kk * 128:(kk + 1) * 128], identity=self.ident_f[:]), reads=[xt, self.ident_f], writes=[b2])
                    k.op("dve", lambda e: e.tensor_copy(out=xT[:, half * 4:half * 4 + 4, tsl], in_=b2[:, :].rearrange("p (a b) -> p a b", a=4)), reads=[b2], writes=[xT])
            for og in range(2):
                wt = wload(wout_src[:, :, og * 512:(og + 1) * 512], 512)
                for q in range(4):
                    oc = og * 4 + q
                    for (h0, hw) in halves:
                        b = self.bank()
                        for kk in range(8):
                            k.op("pe", lambda e: e.matmul(b[:, 0:hw], lhsT=wt[:, kk, q * 128:(q + 1) * 128], rhs=mT[:, kk, h0:h0 + hw], start=(kk == 0), stop=(kk == 7)), reads=[wt, mT], writes=[b])
                        k.op("dve", lambda e: e.scalar_tensor_tensor(out=xT[:, oc, h0:h0 + hw], in0=b[:, 0:hw], scalar=self.modT[:, 16 + oc, r:r + 1], in1=xT[:, oc, h0:h0 + hw], op0=ALU.mult, op1=ALU.add),
                             reads=[b, self.modT, xT], writes=[xT])

            def rms_bcast(dst):
                for kk in range(8):
                    k.op("act", lambda e: e.activation(out=sq2[:, kk, 0:Tb], in_=xT[:, kk, 0:Tb], func=AF.Square), reads=[xT], writes=[sq2])
                for (h0, hw) in halves:
                    bs_ = self.bank()
                    for kk in range(8):
                        k.op("pe", lambda e: e.matmul(bs_[:, 0:hw], lhsT=self.ones_b[:], rhs=sq2[:, kk, h0:h0 + hw], start=(kk == 0), stop=(kk == 7)), reads=[self.ones_b, sq2], writes=[bs_])
                    k.op("act", lambda e: e.activation(out=dst[:, h0:h0 + hw], in_=bs_[:, 0:hw], func=AF.Sqrt, scale=1.0 / D, bias=self.eps_t[:, 0:1]), reads=[bs_, self.eps_t], writes=[dst])
                k.op("dve", lambda e: e.reciprocal(out=dst[:, 0:Tb], in_=dst[:, 0:Tb]), reads=[dst], writes=[dst])
            rms_bcast(rs2)
            if moe:
                bl = [self.bank() for _ in halves]
            for kk in range(8):
                tf = tmpf[kk % 2]
                k.op("dve", lambda e: e.tensor_tensor(out=tf[:, 0:Tb], in0=xT[:, kk, 0:Tb], in1=rs2[:, 0:Tb], op=ALU.mult), reads=[xT, rs2], writes=[tf])
                k.op("act", lambda e: e.activation(out=h2T[:, kk, 0:Tb], in_=tf[:, 0:Tb], func=AF.Identity, scale=self.g2T[:, kk, r:r + 1], bias=self.modT[:, 24 + kk, r:r + 1]),
                     reads=[tf, self.g2T, self.modT], writes=[h2T])
                if moe:
                    k.op("dve", lambda e: e.tensor_scalar(out=tf[:, 0:Tb], in0=tf[:, 0:Tb], scalar1=self.g2T[:, kk, r:r + 1], scalar2=self.modT[:, 24 + kk, r:r + 1], op0=ALU.mult, op1=ALU.add),
                         reads=[tf, self.g2T, self.modT], writes=[tf])
                    for hi, (h0, hw) in enumerate(halves):
                        k.op("pe", lambda e: e.matmul(bl[hi][0:8, 0:hw], lhsT=rt[:, kk, :], rhs=tf[:, h0:h0 + hw], start=(kk == 0), stop=(kk == 7)), reads=[rt, tf], writes=[bl[hi]])
            experts = [None]
            if moe:
                experts = list(range(NE))
                for hi, (h0, hw) in enumerate(halves):
                    k.op("act", lambda e: e.copy(out=lgT[:, h0:h0 + hw], in_=bl[hi][0:8, 0:hw]), reads=[bl[hi]], writes=[lgT])
                bl2 = self.bank()
                for ti in range(nt):
                    k.op("pe", lambda e: e.transpose(out=bl2[:, ti * 8:(ti + 1) * 8], in_=lgT[0:8, ti * 128:(ti + 1) * 128], identity=self.ident_f[0:8, 0:8]), reads=[lgT, self.ident_f], writes=[bl2])
                k.op("dve", lambda e: e.tensor_copy(out=lg[:].rearrange("p a b -> p (a b)"), in_=bl2[:, 0:64]), reads=[bl2], writes=[lg])
                bc3 = lambda ap: ap.unsqueeze(2).to_broadcast([128, 8, NE])
                k.op("dve", lambda e: e.tensor_reduce(out=mx[:, 0:8], in_=lg[:], axis=AX.X, op=ALU.max), reads=[lg], writes=[mx])
                k.op("pool", lambda e: e.tensor_tensor(out=l2[:], in0=lg[:], in1=bc3(mx[:, 0:8]), op=ALU.subtract), reads=[lg, mx], writes=[l2])
                k.op("dve", lambda e: e.tensor_scalar(out=lg[:], in0=l2[:], scalar1=0.0, scalar2=-1e30, op0=ALU.is_equal, op1=ALU.mult), reads=[l2], writes=[lg])
                k.op("dve", lambda e: e.tensor_tensor(out=lg[:], in0=lg[:], in1=l2[:], op=ALU.add), reads=[lg, l2], writes=[lg])
                k.op("dve", lambda e: e.tensor_reduce(out=mx[:, 8:16], in_=lg[:], axis=AX.X, op=ALU.max), reads=[lg], writes=[mx])
                k.op("pool", lambda e: e.tensor_tensor(out=lg[:], in0=l2[:], in1=bc3(mx[:, 8:16]), op=ALU.subtract), reads=[l2, mx], writes=[lg])
                k.op("dve", lambda e: e.tensor_scalar(out=lg[:], in0=lg[:], scalar1=0.0, scalar2=None, op0=ALU.is_ge), reads=[lg], writes=[lg])
                k.op("act", lambda e: e.activation(out=l2[:], in_=l2[:], func=AF.Exp), reads=[l2], writes=[l2])
                k.op("dve", lambda e: e.tensor_tensor(out=l2[:], in0=l2[:], in1=lg[:], op=ALU.mult), reads=[l2, lg], writes=[l2])
                k.op("dve", lambda e: e.tensor_reduce(out=mx[:, 0:8], in_=l2[:], axis=AX.X, op=ALU.add), reads=[l2], writes=[mx])
                k.op("dve", lambda e: e.reciprocal(out=mx[:, 0:8], in_=mx[:, 0:8]), reads=[mx], writes=[mx])
                k.op("pool", lambda e: e.tensor_tensor(out=l2[:], in0=l2[:], in1=bc3(mx[:, 0:8]), op=ALU.mult), reads=[l2, mx], writes=[l2])
                for hi, (h0, hw) in enumerate(halves):
                    bl3 = self.bank()
                    for t4 in range(hw // 128):
                        ti = h0 // 128 + t4
                        k.op("pe", lambda e: e.transpose(out=bl3[0:8, t4 * 128:(t4 + 1) * 128], in_=l2[:, ti, :], identity=self.ident_f[:]), reads=[l2, self.ident_f], writes=[bl3])
                    k.op("act", lambda e: e.copy(out=cbT[:, h0:h0 + hw], in_=bl3[0:8, 0:hw]), reads=[bl3], writes=[cbT])
            for ex_ in experts:
                if moe:
                    wgu_src = I["moe_w_gu"][ex_].rearrange("(k p) n -> p k n", p=128)
                    wdn_src = I["moe_w_down"][ex_].rearrange("(f p) n -> p f n", p=128)
                    cbe = CBe[ex_ % 2]
                    for (h0, hw) in halves:
                        bcb = self.bank()
                        k.op("pe", lambda e: e.matmul(bcb[:, 0:hw], lhsT=selE[0:8, ex_, :], rhs=cbT[0:8, h0:h0 + hw], start=True, stop=True), reads=[selE, cbT], writes=[bcb])
                        k.op("act", lambda e: e.copy(out=cbe[:, h0:h0 + hw], in_=bcb[:, 0:hw]), reads=[bcb], writes=[cbe])
                else:
                    wgu_src = I["ffn_w_gu"].rearrange("(k p) n -> p k n", p=128)
                    wdn_src = I["ffn_w_down"].rearrange("(f p) n -> p f n", p=128)
                si = 0
                for g0 in range(0, F, 512):
                    gw = min(512, F - g0)
                    wgt = wload(wgu_src[:, :, g0:g0 + gw], gw)
                    wut = wload(wgu_src[:, :, F + g0:F + g0 + gw], gw)
                    for c in range(gw // 128):
                        fc = g0 // 128 + c
                        for (h0, hw) in halves:
                            bg_, bu_ = self.bank(), self.bank()
                            for kk in range(8):
                                k.op("pe", lambda e: e.matmul(bg_[:, 0:hw], lhsT=wgt[:, kk, c * 128:(c + 1) * 128], rhs=h2T[:, kk, h0:h0 + hw], start=(kk == 0), stop=(kk == 7)), reads=[wgt, h2T], writes=[bg_])
                            for kk in range(8):
                                k.op("pe", lambda e: e.matmul(bu_[:, 0:hw], lhsT=wut[:, kk, c * 128:(c + 1) * 128], rhs=h2T[:, kk, h0:h0 + hw], start=(kk == 0), stop=(kk == 7)), reads=[wut, h2T], writes=[bu_])
                            s_ = sg[si % 2]
                            si += 1
                            k.op("act", lambda e: e.activation(out=s_[:, 0:hw], in_=bg_[:, 0:hw], func=AF.Silu), reads=[bg_], writes=[s_])
                            if moe:
                                k.op("dve", lambda e: e.tensor_tensor(out=s_[:, 0:hw], in0=s_[:, 0:hw], in1=cbe[:, h0:h0 + hw], op=ALU.mult), reads=[s_, cbe], writes=[s_])
                            k.op("dve", lambda e: e.tensor_tensor(out=actT[:, fc, h0:h0 + hw], in0=bu_[:, 0:hw], in1=s_[:, 0:hw], op=ALU.mult), reads=[bu_, s_], writes=[actT])
                nf = F // 128
                for ocg in range(2):
                    banks = [[self.bank() for _ in range(4)] for _ in halves]
                    for f0 in range(0, nf, 4):
                        wdt = wd[wi[0] % 2]
                        wi[0] += 1
                        n4 = min(4, nf - f0)
                        k.dma("pool", wdt[:, 0:n4, 0:512], wdn_src[:, f0:f0 + n4, ocg * 512:(ocg + 1) * 512], writes=[wdt], max_dma_last_dim=2048)
                        for hi, (h0, hw) in enumerate(halves):
                            for q in range(4):
                                for fi in range(n4):
                                    f = f0 + fi
                                    k.op("pe", lambda e: e.matmul(banks[hi][q][:, 0:hw], lhsT=wdt[:, fi, q * 128:(q + 1) * 128], rhs=actT[:, f, h0:h0 + hw], start=(f == 0), stop=(f == nf - 1)),
                                         reads=[wdt, actT], writes=[banks[hi][q]])
                    for hi, (h0, hw) in enumerate(halves):
                        for q in range(4):
                            oc = ocg * 4 + q
                            k.op("dve", lambda e: e.scalar_tensor_tensor(out=xT[:, oc, h0:h0 + hw], in0=banks[hi][q][:, 0:hw], scalar=self.modT[:, 40 + oc, r:r + 1], in1=xT[:, oc, h0:h0 + hw], op0=ALU.mult, op1=ALU.add),
                                 reads=[banks[hi][q], self.modT, xT], writes=[xT])
            if last:
                rms_bcast(rs2)
                for kk in range(8):
                    k.op("dve", lambda e: e.scalar_tensor_tensor(out=xT[:, kk, 0:Tb], in0=xT[:, kk, 0:Tb], scalar=self.fnT[:, kk, 0:1], in1=rs2[:, 0:Tb], op0=ALU.mult, op1=ALU.mult),
                         reads=[xT, self.fnT, rs2], writes=[xT])
            for ti in range(nt):
                i = t0 + ti
                tsl = slice(ti * 128, (ti + 1) * 128)
                for half in range(2):
                    b2 = self.bank()
                    for q in range(4):
                        kk = half * 4 + q
                        k.op("pe", lambda e: e.transpose(out=b2[:, q * 128:(q + 1) * 128], in_=xT[:, kk, tsl], identity=self.ident_f[:]), reads=[xT, self.ident_f], writes=[b2])
                    k.op("act", lambda e: e.copy(out=xo[:, half * 512:(half + 1) * 512], in_=b2[:, :]), reads=[b2], writes=[xo])
                if last:
                    k.dma("sp", self.out[(i - 2) * 128:(i - 1) * 128, :], xo[:], reads=[xo], writes=[self.out_dep])
                else:
                    k.dma("sp", S["x1"][i * 128:(i + 1) * 128, :], xo[:], reads=[xo], writes=[self.x1_dep[i]])

    def build(self):
        self.declare()
        k = self.k
        self.consts()
        self.eps_t = k.sb([128, 1], F32, "eps_t")
        k.op("pool", lambda e: e.memset(self.eps_t[:], EPS), writes=[self.eps_t])
        self.modT = k.sb([128, 48, 2], F32, "modT")
        self.nT = k.sb([128, 8, 2], F32, "nT")
        self.g1T = k.sb([128, 8, 2], F32, "g1T")
        self.g2T = k.sb([128, 8, 2], F32, "g2T")
        self.fnT = k.sb([128, 8, 1], F32, "fnT")
        self.scT = k.sb([128, 8, 2], BF16, "scT")
        self.xs_dep = [DR() for _ in range(NT)]
        self.xcur, self.xcur_dep = self.I["xs"], self.xs_dep
        for l in range(self.nlayers):
            if "p0" in self.phases:
                with k.scope():
                    self.p0_alloc()
                    self.p0(l)
            if "p1" in self.phases:
                with k.scope():
                    self.p1_alloc()
                    self.p1(l, self.xcur, self.xcur_dep)
            if "p2" in self.phases:
                with k.scope():
                    self.p2(l)
            if "p3" in self.phases:
                with k.scope():
                    self.p3(l)
            if "p4" in self.phases:
                with k.scope():
                    self.p4(l)
                self.xcur, self.xcur_dep = self.S["x1"], self.x1_dep
        k.barrier()
        self.es.close()
        return self.nc


def _core_inputs(inp, core):
    b, j = core // 2, core % 2
    f = (lambda a: a[::-1]) if j else (lambda a: a)
    d = {}
    d["xs"] = np.ascontiguousarray(np.concatenate([f(inp["ctx"][b]), f(inp["x"][b])], axis=0))
    d["cvec"] = np.ascontiguousarray(np.stack([inp["c"][b], inp["c_ctx"]], axis=0))
    w_in = inp["w_in"]
    if j:
        w_in = w_in.copy()
        for base, n in ((2048, 4), (2056, 4), (3600, 16)):
            a = w_in[:, :, base:base + n].copy()
            w_in[:, :, base:base + n] = w_in[:, :, base + n:base + 2 * n]
            w_in[:, :, base + n:base + 2 * n] = a
    d["w_in"] = np.ascontiguousarray(w_in)
    sw = (lambda a: a[:, ::-1]) if j else (lambda a: a)
    d["conv"] = np.ascontiguousarray(sw(inp["conv_qkv"]))
    d["dn_a_log"] = np.ascontiguousarray(sw(inp["dn_a_log"]).reshape(2, 8))
    d["dn_dt_bias"] = np.ascontiguousarray(sw(inp["dn_dt_bias"]).reshape(2, 8))
    d["gla_w_gate2"] = np.ascontiguousarray(sw(inp["gla_w_gate2"]))
    d["gla_b_gate"] = np.ascontiguousarray(sw(inp["gla_b_gate"]))
    for n in ("w_ada", "b_ada", "norm_mix", "norm_ffn", "dn_norm", "gla_norm", "w_out", "final_norm"):
        d[n] = np.ascontiguousarray(inp[n])
    d["ffn_w_gu"] = np.ascontiguousarray(inp["ffn_w_gu"][0])
    d["ffn_w_down"] = np.ascontiguousarray(inp["ffn_w_down"][0])
    d["moe_router"] = np.ascontiguousarray(inp["moe_router"][0])
    d["moe_w_gu"] = np.ascontiguousarray(inp["moe_w_gu"][0])
    d["moe_w_down"] = np.ascontiguousarray(inp["moe_w_down"][0])
    return d


def kernel(**inputs):
    inp = {k_: np.asarray(v, dtype=np.float32) for k_, v in inputs.items()}
    prog = Prog()
    nc = prog.build()
    in_maps = []
    for core in range(8):
        ci = _core_inputs(inp, core)
        in_maps.append({n: ci[n] for n in prog.I})
    res = run_bass_kernel_spmd(nc, in_maps, core_ids=list(range(8)))
    out = np.empty((4, LL, D), np.float32)
    for core in range(8):
        b, j = core // 2, core % 2
        y = np.asarray(res.results[core]["out"], dtype=np.float32)
        if j == 0:
            out[b, 0:NOWN] = y
        else:
            out[b, LL - NOWN:LL] = y[::-1]
    return out
```
